# Optimizing a Trainium2 kernel written in Bass

```python
import math
import jax, jax.numpy as jnp
from jax import lax
import numpy as np

D_MODEL = 1024
BATCH = 2
SEQ = 16384
DEPTH = 2

GRID_W = 64
CTX_LEN = 256
N_MIXERS = 2
N_HEADS = 16
HEAD_DIM = D_MODEL // N_HEADS
WIN_H = 8
WIN_W = 16
Q_BLOCK = 128
D_RNN = 1536
RNN_BLOCKS = 16
RNN_BW = D_RNN // RNN_BLOCKS
CONV_W = 4
CONV_LEFT = 2
LRU_C = 8.0
D_FF = 2816
N_EXPERTS = 8
TOP_K = 2
D_FF_EXPERT = 3584
EPS = 1e-6

kernel_name = "hybrid_na_rglru_moe_dit"


def rmsnorm(x, g):
    xf = x.astype(jnp.float32)
    y = xf * lax.rsqrt(jnp.mean(xf * xf, axis=-1, keepdims=True) + EPS)
    return (y * g.astype(jnp.float32)).astype(x.dtype)


def adaln(cvec, w_mod, b_mod):
    m = jax.nn.silu(cvec) @ w_mod + b_mod
    return jnp.split(m, 6, axis=-1)


def modulate(h, shift, scale):
    return h * (1.0 + scale) + shift


def swiglu(h, w1, w3, w2):
    return (jax.nn.silu(h @ w1) * (h @ w3)) @ w2


def moe_ffn(h, w_router, b_router, w1, w3, w2):
    logits = (h @ w_router).astype(jnp.float32) + b_router.astype(jnp.float32)
    top_v, top_i = lax.top_k(logits, TOP_K)
    gates = jax.nn.softmax(top_v, axis=-1)
    comb = jnp.sum(jax.nn.one_hot(top_i, N_EXPERTS, dtype=jnp.float32) * gates[..., None], axis=-2)
    comb = comb.astype(h.dtype)
    y = jnp.zeros_like(h)
    for e in range(N_EXPERTS):
        y = y + comb[..., e:e + 1] * swiglu(h, w1[e], w3[e], w2[e])
    return y


def na_tables(rows):
    kh = min(WIN_H, rows)
    s = rows * GRID_W
    t = jnp.arange(s, dtype=jnp.int32)
    r = t // GRID_W
    col = t % GRID_W
    rs = jnp.clip(r - kh // 2, 0, rows - kh)
    cs = jnp.clip(col - WIN_W // 2, 0, GRID_W - WIN_W)
    kr = rs[:, None, None] + jnp.arange(kh, dtype=jnp.int32)[None, :, None]
    kc = cs[:, None, None] + jnp.arange(WIN_W, dtype=jnp.int32)[None, None, :]
    idx = (kr * GRID_W + kc).reshape(s, kh * WIN_W)
    dr = kr - r[:, None, None] + (WIN_H - 1)
    dc = kc - col[:, None, None] + (WIN_W - 1)
    bidx = (dr * (2 * WIN_W - 1) + dc).reshape(s, kh * WIN_W)
    return idx, bidx


def na_mixer(h_lat, h_ctx, w_qkv, q_gain, k_gain, rpb, w_o, need_ctx):
    b, s, d = h_lat.shape
    rows = s // GRID_W
    scale = HEAD_DIM ** -0.5

    def qkv(h):
        q, k, v = jnp.split(h @ w_qkv, 3, axis=-1)
        shp = h.shape[:2] + (N_HEADS, HEAD_DIM)
        return rmsnorm(q.reshape(shp), q_gain), rmsnorm(k.reshape(shp), k_gain), v.reshape(shp)

    ql, kl, vl = qkv(h_lat)
    qc, kc, vc = qkv(h_ctx)
    idx, bidx = na_tables(rows)
    n_keys = idx.shape[1]
    bias_tab = rpb.reshape(N_HEADS, -1)
    nb = s // Q_BLOCK

    def block(args):
        q_b, idx_b, bidx_b = args
        k_g = kl[:, idx_b]
        v_g = vl[:, idx_b]
        s_loc = jnp.einsum('bqhd,bqkhd->bhqk', q_b, k_g) * scale + bias_tab[:, bidx_b][None]
        s_ctx = jnp.einsum('bqhd,bchd->bhqc', q_b, kc) * scale
        p = jax.nn.softmax(jnp.concatenate([s_loc, s_ctx], axis=-1).astype(jnp.float32), axis=-1)
        p = p.astype(q_b.dtype)
        return (jnp.einsum('bhqk,bqkhd->bqhd', p[..., :n_keys], v_g)
                + jnp.einsum('bhqc,bchd->bqhd', p[..., n_keys:], vc))

    q_blocks = ql.reshape(b, nb, Q_BLOCK, N_HEADS, HEAD_DIM).transpose(1, 0, 2, 3, 4)
    o = lax.map(block, (q_blocks, idx.reshape(nb, Q_BLOCK, n_keys), bidx.reshape(nb, Q_BLOCK, n_keys)))
    o = o.transpose(1, 0, 2, 3, 4).reshape(b, s, d)
    y_lat = o @ w_o
    y_ctx = None
    if need_ctx:
        sc = jnp.einsum('bqhd,bkhd->bhqk', qc, kc) * scale
        pc = jax.nn.softmax(sc.astype(jnp.float32), axis=-1).astype(qc.dtype)
        oc = jnp.einsum('bhqk,bkhd->bqhd', pc, vc)
        y_ctx = oc.reshape(h_ctx.shape[0], h_ctx.shape[1], d) @ w_o
    return y_lat, y_ctx


def dwconv(x, w, bias):
    y = lax.conv_general_dilated(x, w[:, None, :], window_strides=(1,),
                                 padding=[(CONV_LEFT, CONV_W - 1 - CONV_LEFT)],
                                 dimension_numbers=('NWC', 'WIO', 'NWC'),
                                 feature_group_count=x.shape[-1])
    return y + bias


def blockdiag(x, w, bias):
    xb = x.reshape(x.shape[:-1] + (RNN_BLOCKS, RNN_BW))
    return jnp.einsum('bnkc,kcd->bnkd', xb, w).reshape(x.shape) + bias


def lru_coeffs(xc, wa, ba, wx, bx, lam):
    xf = xc.astype(jnp.float32)
    r = jax.nn.sigmoid(blockdiag(xc, wa, ba).astype(jnp.float32))
    i = jax.nn.sigmoid(blockdiag(xc, wx, bx).astype(jnp.float32))
    log_a = -LRU_C * r * jax.nn.softplus(-lam.astype(jnp.float32))
    a = jnp.exp(log_a)
    mult = jnp.sqrt(-jnp.expm1(2.0 * log_a))
    return a, mult * (i * xf)


def linear_scan(a, u, h0, reverse):
    def comb(lft, rgt):
        return (lft[0] * rgt[0], rgt[0] * lft[1] + rgt[1])
    a_cum, b_cum = lax.associative_scan(comb, (a, u), reverse=reverse, axis=1)
    return a_cum * h0[:, None, :] + b_cum


def rglru_mixer(h_lat, h_ctx, w_in, conv_w, conv_b, gate_a_w, gate_a_b, gate_x_w, gate_x_b, lam,
                w_out, need_ctx):
    def branches(h):
        g, xr = jnp.split(h @ w_in, 2, axis=-1)
        return g, dwconv(xr, conv_w, conv_b)

    g_l, xc_l = branches(h_lat)
    g_c, xc_c = branches(h_ctx)
    zeros0 = jnp.zeros((h_ctx.shape[0], D_RNN), jnp.float32)
    hs_l = jnp.zeros(xc_l.shape, jnp.float32)
    hs_c = jnp.zeros(xc_c.shape, jnp.float32)
    for d, rev in enumerate((False, True)):
        a_c, u_c = lru_coeffs(xc_c, gate_a_w[d], gate_a_b[d], gate_x_w[d], gate_x_b[d], lam[d])
        h_c = linear_scan(a_c, u_c, zeros0, rev)
        h0 = h_c[:, 0] if rev else h_c[:, -1]
        a_l, u_l = lru_coeffs(xc_l, gate_a_w[d], gate_a_b[d], gate_x_w[d], gate_x_b[d], lam[d])
        hs_l = hs_l + linear_scan(a_l, u_l, h0, rev)
        hs_c = hs_c + h_c
    y_lat = (jax.nn.gelu(g_l) * hs_l.astype(g_l.dtype)) @ w_out
    y_ctx = None
    if need_ctx:
        y_ctx = (jax.nn.gelu(g_c) * hs_c.astype(g_c.dtype)) @ w_out
    return y_lat, y_ctx


def setup_inputs(seed: int = 0) -> dict:
    key = jax.random.key(seed)
    ks = list(jax.random.split(key, 40))
    nrm = lambda k, shp, s: jax.random.normal(k, shp, jnp.float32) * s
    D = D_MODEL
    inp = {}
    inp['x'] = nrm(ks[0], (BATCH, SEQ, D), 1.0)
    inp['c'] = nrm(ks[1], (BATCH, D), 1.0)
    inp['ctx'] = nrm(ks[2], (BATCH, CTX_LEN, D), 1.0)
    inp['c_ctx'] = nrm(ks[3], (D,), 1.0)
    inp['l0_w_mod'] = nrm(ks[4], (D, 6 * D), 0.5 * D ** -0.5)
    inp['l0_b_mod'] = nrm(ks[5], (6 * D,), 0.02)
    inp['l0_norm1'] = 1.0 + nrm(ks[6], (D,), 0.05)
    inp['l0_norm2'] = 1.0 + nrm(ks[7], (D,), 0.05)
    inp['l0_w_qkv'] = nrm(ks[8], (D, 3 * D), D ** -0.5)
    inp['l0_q_gain'] = 1.0 + nrm(ks[9], (HEAD_DIM,), 0.05)
    inp['l0_k_gain'] = 1.0 + nrm(ks[10], (HEAD_DIM,), 0.05)
    inp['l0_rpb'] = nrm(ks[11], (N_HEADS, 2 * WIN_H - 1, 2 * WIN_W - 1), 0.2)
    inp['l0_w_o'] = nrm(ks[12], (D, D), D ** -0.5)
    inp['l0_ffn_w1'] = nrm(ks[13], (D, D_FF), D ** -0.5)
    inp['l0_ffn_w3'] = nrm(ks[14], (D, D_FF), D ** -0.5)
    inp['l0_ffn_w2'] = nrm(ks[15], (D_FF, D), D_FF ** -0.5)
    inp['l1_w_mod'] = nrm(ks[16], (D, 6 * D), 0.5 * D ** -0.5)
    inp['l1_b_mod'] = nrm(ks[17], (6 * D,), 0.02)
    inp['l1_norm1'] = 1.0 + nrm(ks[18], (D,), 0.05)
    inp['l1_norm2'] = 1.0 + nrm(ks[19], (D,), 0.05)
    inp['l1_w_in'] = nrm(ks[20], (D, 2 * D_RNN), D ** -0.5)
    inp['l1_conv_w'] = nrm(ks[21], (CONV_W, D_RNN), CONV_W ** -0.5)
    inp['l1_conv_b'] = nrm(ks[22], (D_RNN,), 0.02)
    inp['l1_gate_a_w'] = nrm(ks[23], (2, RNN_BLOCKS, RNN_BW, RNN_BW), RNN_BW ** -0.5)
    inp['l1_gate_a_b'] = nrm(ks[24], (2, D_RNN), 0.02)
    inp['l1_gate_x_w'] = nrm(ks[25], (2, RNN_BLOCKS, RNN_BW, RNN_BW), RNN_BW ** -0.5)
    inp['l1_gate_x_b'] = nrm(ks[26], (2, D_RNN), 0.02)
    u = jax.random.uniform(ks[27], (2, D_RNN), jnp.float32, minval=0.9, maxval=0.999)
    s = u ** (1.0 / LRU_C)
    inp['l1_lam'] = jnp.log(s) - jnp.log1p(-s)
    inp['l1_w_out'] = nrm(ks[28], (D_RNN, D), D_RNN ** -0.5)
    inp['l1_router_w'] = nrm(ks[29], (D, N_EXPERTS), D ** -0.5)
    inp['l1_router_b'] = nrm(ks[30], (N_EXPERTS,), 0.01)
    inp['l1_moe_w1'] = nrm(ks[31], (N_EXPERTS, D, D_FF_EXPERT), D ** -0.5)
    inp['l1_moe_w3'] = nrm(ks[32], (N_EXPERTS, D, D_FF_EXPERT), D ** -0.5)
    inp['l1_moe_w2'] = nrm(ks[33], (N_EXPERTS, D_FF_EXPERT, D), D_FF_EXPERT ** -0.5)
    return inp


def reference(x, c, ctx, c_ctx,
              l0_w_mod, l0_b_mod, l0_norm1, l0_norm2, l0_w_qkv, l0_q_gain, l0_k_gain, l0_rpb, l0_w_o,
              l0_ffn_w1, l0_ffn_w3, l0_ffn_w2,
              l1_w_mod, l1_b_mod, l1_norm1, l1_norm2, l1_w_in, l1_conv_w, l1_conv_b,
              l1_gate_a_w, l1_gate_a_b, l1_gate_x_w, l1_gate_x_b, l1_lam, l1_w_out,
              l1_router_w, l1_router_b, l1_moe_w1, l1_moe_w3, l1_moe_w2):
    layers = [
        dict(w_mod=l0_w_mod, b_mod=l0_b_mod, norm1=l0_norm1, norm2=l0_norm2,
             mixer=dict(w_qkv=l0_w_qkv, q_gain=l0_q_gain, k_gain=l0_k_gain, rpb=l0_rpb, w_o=l0_w_o),
             ffn=dict(w1=l0_ffn_w1, w3=l0_ffn_w3, w2=l0_ffn_w2)),
        dict(w_mod=l1_w_mod, b_mod=l1_b_mod, norm1=l1_norm1, norm2=l1_norm2,
             mixer=dict(w_in=l1_w_in, conv_w=l1_conv_w, conv_b=l1_conv_b, gate_a_w=l1_gate_a_w,
                        gate_a_b=l1_gate_a_b, gate_x_w=l1_gate_x_w, gate_x_b=l1_gate_x_b,
                        lam=l1_lam, w_out=l1_w_out),
             ffn=dict(w_router=l1_router_w, b_router=l1_router_b, w1=l1_moe_w1, w3=l1_moe_w3,
                      w2=l1_moe_w2)),
    ]
    x_lat, x_ctx = x, ctx
    for i in range(DEPTH):
        p = layers[i]
        need_ctx = i < DEPTH - 1
        m_l = adaln(c[:, None, :], p['w_mod'], p['b_mod'])
        m_c = adaln(c_ctx[None, None, :], p['w_mod'], p['b_mod'])
        h_l = modulate(rmsnorm(x_lat, p['norm1']), m_l[0], m_l[1])
        h_c = modulate(rmsnorm(x_ctx, p['norm1']), m_c[0], m_c[1])
        if i % N_MIXERS == 0:
            y_l, y_c = na_mixer(h_l, h_c, need_ctx=need_ctx, **p['mixer'])
        else:
            y_l, y_c = rglru_mixer(h_l, h_c, need_ctx=need_ctx, **p['mixer'])
        x_lat = x_lat + m_l[2] * y_l
        h_l = modulate(rmsnorm(x_lat, p['norm2']), m_l[3], m_l[4])
        ffn = swiglu if i % 2 == 0 else moe_ffn
        f_args = p['ffn']
        if i % 2 == 0:
            f_l = swiglu(h_l, f_args['w1'], f_args['w3'], f_args['w2'])
        else:
            f_l = moe_ffn(h_l, f_args['w_router'], f_args['b_router'], f_args['w1'], f_args['w3'], f_args['w2'])
        x_lat = x_lat + m_l[5] * f_l
        if need_ctx:
            x_ctx = x_ctx + m_c[2] * y_c
            h_c = modulate(rmsnorm(x_ctx, p['norm2']), m_c[3], m_c[4])
            if i % 2 == 0:
                f_c = swiglu(h_c, f_args['w1'], f_args['w3'], f_args['w2'])
            else:
                f_c = moe_ffn(h_c, f_args['w_router'], f_args['b_router'], f_args['w1'], f_args['w3'], f_args['w2'])
            x_ctx = x_ctx + m_c[5] * f_c
    return x_lat
```

```python
import numpy as np
from contextlib import ExitStack
import concourse.bass as bass
import concourse.mybir as mybir
from concourse.bass_utils import run_bass_kernel_spmd

F32 = mybir.dt.float32
BF16 = mybir.dt.bfloat16
AF = mybir.ActivationFunctionType
ALU = mybir.AluOpType
AX = mybir.AxisListType

ENGS = ("pe", "act", "dve", "pool", "sp")


class Buf:
    __slots__ = ("name", "t", "last_w", "reads")

    def __init__(self, name, t=None):
        self.name = name
        self.t = t
        self.last_w = None
        self.reads = []

    def __getitem__(self, k):
        return self.t[k]


class Op:
    __slots__ = ("eng", "fn", "deps", "signal", "pos", "dma_key", "val", "is_dma", "inc")

    def __init__(self, eng, fn):
        self.eng = eng
        self.fn = fn
        self.deps = []
        self.signal = False
        self.pos = 0
        self.is_dma = False
        self.dma_key = None
        self.val = 0
        self.inc = 16


class Sched:
    ARENA_F32 = 53000

    def __init__(self, nc, same_engine_sync=True):
        self.nc = nc
        self.ops = {e: [] for e in ENGS}
        self.same = same_engine_sync
        self.waited = {e: {} for e in ENGS}
        self.dma_cnt = {}
        self.dma_keys = []
        self.slot_of = {}
        self.bar_pos = {}
        self.off = 0
        self.arena = None
        self.peak = 0
        self.psum = None
        self.ndram = 0

    def sb(self, name, shape, dtype, off=None):
        if self.arena is None:
            self.arena = self.nc.alloc_sbuf_tensor("arena", [128, self.ARENA_F32], F32)
        esz = 2 if dtype == BF16 else 4
        nel = int(np.prod(shape[1:]))
        nbytes = (nel * esz + 63) // 64 * 64
        if off is None:
            off = self.off
            self.off += nbytes
        assert off + nbytes <= self.ARENA_F32 * 4, (name, off, nbytes)
        self.peak = max(self.peak, off + nbytes)
        a = self.arena[0:shape[0], off // 4: off // 4 + nbytes // 4]
        if dtype != F32:
            a = a.bitcast(dtype)
        a = a[:, 0:nel]
        if len(shape) > 2:
            names = "abcdefg"[:len(shape) - 1]
            pat = "p (" + " ".join(names) + ") -> p " + " ".join(names)
            a = a.rearrange(pat, **{nm: shape[1 + i] for i, nm in enumerate(names[:-1])})
        return Buf(name, a)

    def ps(self, name, col, ncols, dtype=F32, parts=128):
        if self.psum is None:
            self.psum = self.nc.alloc_psum_tensor("psum_all", [128, 4096], F32).ap()
        a = self.psum[0:parts, col:col + ncols]
        if dtype != F32:
            a = a.bitcast(dtype)
        return Buf(name, a)

    def dram(self, name, shape, dtype, kind="Internal"):
        return Buf(name, self.nc.dram_tensor(name, list(shape), dtype, kind=kind).ap())

    def _need(self, op, prod):
        if prod is None or prod is op:
            return
        e = op.eng
        w = self.waited[e]
        if prod.is_dma:
            k = ("d", prod.dma_key)
            if w.get(k, 0) >= prod.val:
                return
            w[k] = prod.val
            op.deps.append(prod)
            return
        if prod.eng == e and (not self.same or e == "pe"):
            return
        if w.get(prod.eng, -1) >= prod.pos:
            return
        w[prod.eng] = prod.pos
        prod.signal = True
        op.deps.append(prod)

    def add(self, eng, fn, reads=(), writes=(), dma_buf=None, inc=16):
        op = Op(eng, fn)
        op.inc = inc
        op.pos = len(self.ops[eng])
        if dma_buf is not None:
            op.is_dma = True
            bid = id(dma_buf)
            if bid not in self.slot_of:
                slot = len(self.slot_of)
                self.slot_of[bid] = slot
                if slot >= len(self.dma_keys):
                    self.dma_keys.append(slot)
                    self.dma_cnt[slot] = 0
            key = self.slot_of[bid]
            self.dma_cnt[key] += inc
            op.dma_key = key
            op.val = self.dma_cnt[key]
        for b in reads:
            self._need(op, b.last_w)
        for b in writes:
            self._need(op, b.last_w)
            for r in b.reads:
                self._need(op, r)
        for b in reads:
            b.reads.append(op)
        for b in writes:
            b.last_w = op
            b.reads = []
        self.ops[eng].append(op)
        return op

    def dma(self, eng, out, in_, sbuf, reads=(), writes=(), **kw):
        return self.add(eng, lambda e: e.dma_start(out=out, in_=in_, **kw),
                        reads=reads, writes=writes, dma_buf=sbuf)

    def barrier(self):
        lasts = []
        for e in ENGS:
            for op in reversed(self.ops[e]):
                if not op.is_dma and op.fn is not None:
                    lasts.append(op)
                    break
        last_dma = {}
        for e in ENGS:
            for op in self.ops[e][self.bar_pos.get(e, 0):]:
                if op.is_dma:
                    last_dma[op.dma_key] = op
        for e in ENGS:
            op = Op(e, None)
            op.pos = len(self.ops[e])
            for p in lasts:
                if p.eng != e:
                    self._need(op, p)
            for p in last_dma.values():
                self._need(op, p)
            self.ops[e].append(op)
            self.bar_pos[e] = len(self.ops[e])
        self.slot_of = {}

    def emit(self):
        nc = self.nc
        with ExitStack() as st:
            esem = {e: st.enter_context(nc.semaphore(f"s_{e}")) for e in ENGS}
            dsem = {k: st.enter_context(nc.semaphore(f"d_{i}")) for i, k in enumerate(self.dma_keys)}
            for e in ENGS:
                c = 0
                for op in self.ops[e]:
                    if op.is_dma:
                        continue
                    if op.signal:
                        c += 1
                        op.val = c
            block = st.enter_context(nc.Block())

            def run(ename, eng):
                for op in self.ops[ename]:
                    for p in op.deps:
                        if p.is_dma:
                            eng.wait_ge(dsem[p.dma_key], p.val)
                        else:
                            eng.wait_ge(esem[p.eng], p.val)
                    if op.fn is None:
                        continue
                    ins = op.fn(eng)
                    if op.is_dma:
                        ins.then_inc(dsem[op.dma_key], op.inc)
                    elif op.signal:
                        ins.then_inc(esem[ename], 1)

            @block.tensor
            def _(eng):
                run("pe", eng)

            @block.scalar
            def _(eng):
                run("act", eng)

            @block.vector
            def _(eng):
                run("dve", eng)

            @block.gpsimd
            def _(eng):
                run("pool", eng)

            @block.sync
            def _(eng):
                run("sp", eng)


D = 1024
KC = 8
NH = 16
HD = 64
NQT = 34
NKC = 38
NCH = 40
NT1 = 36
DFF = 2816
NFC = 22
DRNN = 1536
RCH = 16
RB = 96
NE = 8
DFE = 3584
EPS = 1e-6
NEG = -30000.0


def mm(S, ps, out, lhsT, rhs, start, stop, reads):
    S.add("pe", lambda e: e.matmul(out, lhsT=lhsT, rhs=rhs, start=start, stop=stop), reads=reads, writes=[ps])


def tr(S, ps, out, in_, ident, reads):
    S.add("pe", lambda e: e.transpose(out=out, in_=in_, identity=ident), reads=reads, writes=[ps])


def act(S, out, in_, func, reads, writes, bias=None, scale=None, accum_out=None):
    kw = {}
    if bias is not None:
        kw["bias"] = bias
    if scale is not None:
        kw["scale"] = scale
    if accum_out is not None:
        kw["accum_out"] = accum_out
    S.add("act", lambda e: e.activation(out=out, in_=in_, func=func, **kw), reads=reads, writes=writes)


def ts(S, eng, out, in0, s1, s2, op0, op1, reads, writes):
    if op1 is None:
        S.add(eng, lambda e: e.tensor_scalar(out=out, in0=in0, scalar1=s1, scalar2=None, op0=op0), reads=reads, writes=writes)
    else:
        S.add(eng, lambda e: e.tensor_scalar(out=out, in0=in0, scalar1=s1, scalar2=s2, op0=op0, op1=op1), reads=reads, writes=writes)


def stt(S, eng, out, in0, scalar, in1, op0, op1, reads, writes):
    S.add(eng, lambda e: e.scalar_tensor_tensor(out=out, in0=in0, scalar=scalar, in1=in1, op0=op0, op1=op1),
          reads=reads, writes=writes)


def tt(S, eng, out, in0, in1, op, reads, writes):
    S.add(eng, lambda e: e.tensor_tensor(out=out, in0=in0, in1=in1, op=op), reads=reads, writes=writes)


def cp(S, eng, out, in_, reads, writes):
    if eng == "act":
        S.add("act", lambda e: e.copy(out=out, in_=in_), reads=reads, writes=writes)
    else:
        S.add(eng, lambda e: e.tensor_copy(out=out, in_=in_), reads=reads, writes=writes)


def pp_view(vec_ap):
    return vec_ap.rearrange("(c p) -> p c", p=128)


def bc_view(row_ap, n):
    return bass.AP(row_ap.tensor, row_ap.offset, [[0, 128], [1, n]])


class Common:
    def __init__(self, S):
        self.identf = S.sb("identf", [128, 128], F32)
        self.identb = S.sb("identb", [128, 128], BF16)
        self.junk = S.sb("junk", [128, 1024], BF16)
        self.epsb = S.sb("epsb", [128, 1], F32)
        S.add("pool", lambda e: e.memset(self.epsb[:, :], EPS), writes=[self.epsb])
        for b, in (self.identf,), (self.identb,):
            S.add("pool", lambda e, b=b: e.memset(b[:], 1.0), writes=[b])
            S.add("pool", lambda e, b=b: e.affine_select(out=b[:], in_=b[:], pattern=[[-1, 128]], compare_op=ALU.is_equal,
                                                         fill=0.0, base=0, channel_multiplier=1), reads=[b], writes=[b])


def norm_tile(S, C, xt, ss, rstd, xs, pst, hT_dst, G, Sft, reads_extra=(), hT32_dst=None):
    hbuf, hfn = hT_dst
    act(S, C.junk[:, :], xt[:, :], AF.Square, reads=[xt], writes=[C.junk, ss], accum_out=ss[:, 0:1])
    ts(S, "dve", rstd[:, 0:1], ss[:, 0:1], 1.0 / D, EPS, ALU.mult, ALU.add, reads=[ss], writes=[rstd])
    act(S, rstd[:, 0:1], rstd[:, 0:1], AF.Sqrt, reads=[rstd], writes=[rstd])
    S.add("dve", lambda e: e.reciprocal(out=rstd[:, 0:1], in_=rstd[:, 0:1]), reads=[rstd], writes=[rstd])
    act(S, xs[:, :], xt[:, :], AF.Identity, reads=[xt, rstd], writes=[xs], scale=rstd[:, 0:1])
    for half in range(2):
        p = pst[half]
        for q in range(4):
            kc = half * 4 + q
            tr(S, p, p[:, 128 * q:128 * q + 128], xs[:, 128 * kc:128 * kc + 128], C.identf[:, :], reads=[xs, C.identf])
        for q in range(4):
            kc = half * 4 + q
            src = p[:, 128 * q:128 * q + 128]
            if hT32_dst is not None:
                b32, f32fn = hT32_dst
                if kc % 2 == 0:
                    ts(S, "dve", f32fn(kc), src, G[:, kc:kc + 1], Sft[:, kc:kc + 1], ALU.mult, ALU.add,
                       reads=[p, G, Sft], writes=[b32])
                else:
                    act(S, f32fn(kc), src, AF.Identity, reads=[p, G, Sft], writes=[b32],
                        bias=Sft[:, kc:kc + 1], scale=G[:, kc:kc + 1])
                cp(S, "pool", hfn(kc), f32fn(kc), reads=[b32], writes=[hbuf])
            elif kc % 2 == 0:
                ts(S, "dve", hfn(kc), src, G[:, kc:kc + 1], Sft[:, kc:kc + 1], ALU.mult, ALU.add,
                   reads=[p, G, Sft], writes=[hbuf])
            else:
                act(S, hfn(kc), src, AF.Identity, reads=[p, G, Sft], writes=[hbuf],
                    bias=Sft[:, kc:kc + 1], scale=G[:, kc:kc + 1])


def load_mod_pp(S, dst, col, mod_d, layer, stream, which, tmp_ok=True):
    src = pp_view(mod_d[layer, stream, which * D:(which + 1) * D])
    S.dma("sp", dst[:, col * 8:col * 8 + 8], src, dst, reads=[mod_d], writes=[dst], allow_slow_non_contiguous=True)


def phase0_adaln(S, C, I, mod_d):
    m0 = S.off
    cT = S.sb("cT", [128, 8, 2], F32)
    sc = S.sb("sc", [128, 8, 2], F32)
    rep = S.sb("rep", [128, 16, 128], F32)
    wblk = [S.sb(f"wblk{i}", [128, 8, 512], F32) for i in range(2)]
    bblk = [S.sb(f"bblk{i}", [128, 512], F32) for i in range(2)]
    res = [S.sb(f"res{i}", [128, 512], F32) for i in range(4)]
    pss = [S.ps(f"p0ps{i}", 512 * i, 512) for i in range(4)]
    for s in range(2):
        S.dma("sp", cT[:, :, s], pp_view(I["cv"][s, :]), cT, writes=[cT], allow_slow_non_contiguous=True)
    act(S, sc[:, :, :], cT[:, :, :], AF.Silu, reads=[cT], writes=[sc])
    for kc in range(8):
        for s in range(2):
            cp(S, "dve", rep[:, kc * 2 + s, :], sc[:, kc, s:s + 1].to_broadcast([128, 128]), reads=[sc], writes=[rep])
    it = 0
    for l in range(2):
        wm = I[f"l{l}_w_mod"]
        bm = I[f"l{l}_b_mod"]
        for j in range(12):
            wb = wblk[it % 2]
            bb = bblk[it % 2]
            S.dma("sp", wb[:, :, :], wm[:, 512 * j:512 * j + 512].rearrange("(kc p) n -> p kc n", p=128), wb, writes=[wb])
            S.dma("sp", bb[:, :], bc_view(bm[512 * j:512 * j + 512], 512), bb, writes=[bb])
            for s in range(2):
                p = pss[(it % 2) * 2 + s]
                r = res[(it % 2) * 2 + s]
                for kc in range(8):
                    mm(S, p, p[:, :], rep[:, kc * 2 + s, :], wb[:, kc, :], kc == 0, kc == 7, reads=[rep, wb])
                tt(S, "dve", r[:, :], p[:, :], bb[:, :], ALU.add, reads=[p, bb], writes=[r])
                S.dma("sp", mod_d[l, s:s + 1, 512 * j:512 * j + 512], r[0:1, :], r, reads=[r], writes=[mod_d])
            it += 1
    S.barrier()
    S.off = m0


def phase1_qkv(S, C, I, mod_d, QT, KT, V):
    m0 = S.off
    wq = S.sb("wqkv", [128, 8, 3072], BF16)
    S.dma("pool", wq[:, :, :], I["l0_w_qkv"].rearrange("(kc p) n -> p kc n", p=128), wq, writes=[wq])
    mods = S.sb("mods1", [128, 32], F32)
    tmpm = S.sb("tmpm1", [128, 24], F32)
    S.dma("sp", tmpm[:, 0:8], pp_view(I["l0_norm1"]), tmpm, writes=[tmpm], allow_slow_non_contiguous=True)
    for s in range(2):
        load_mod_pp(S, tmpm, 1 + s, mod_d, 0, s, 1)
        load_mod_pp(S, mods, 2 * s + 1, mod_d, 0, s, 0)
        stt(S, "dve", mods[:, 16 * s:16 * s + 8], tmpm[:, 8 + 8 * s:16 + 8 * s], 1.0, tmpm[:, 0:8], ALU.add, ALU.mult,
            reads=[tmpm], writes=[mods])
    Gs = [Buf("G", mods[:, 0:8]), Buf("Gc", mods[:, 16:24])]
    Ss = [Buf("S", mods[:, 8:16]), Buf("Sc", mods[:, 24:32])]
    gains = S.sb("gains", [128, 2], F32)
    for half in range(2):
        S.dma("sp", gains[64 * half:64 * half + 64, 0:1], I["l0_q_gain"].rearrange("(p o) -> p o", o=1), gains, writes=[gains])
        S.dma("sp", gains[64 * half:64 * half + 64, 1:2], I["l0_k_gain"].rearrange("(p o) -> p o", o=1), gains, writes=[gains])
    ts(S, "dve", gains[:, 0:1], gains[:, 0:1], HD ** -0.5, None, ALU.mult, None, reads=[gains], writes=[gains])
    bd = S.sb("bd", [128, 128], BF16)
    S.add("pool", lambda e: e.memset(bd[:, :], 0.0), writes=[bd])
    S.add("pool", lambda e: e.memset(bd[0:64, 0:64], 1.0 / 64), reads=[bd], writes=[bd])
    S.add("pool", lambda e: e.memset(bd[64:128, 64:128], 1.0 / 64), reads=[bd], writes=[bd])
    xin = [S.sb(f"xin{i}", [128, 1024], F32) for i in range(3)]
    xs = [S.sb(f"xs{i}", [128, 1024], F32) for i in range(2)]
    ss = [S.sb(f"ss{i}", [128, 1], F32) for i in range(2)]
    rstd = [S.sb(f"rstd{i}", [128, 1], F32) for i in range(2)]
    hT = [S.sb(f"hT{i}", [128, 8, 512], BF16) for i in range(2)]
    sq = [S.sb(f"sq{i}", [128, 512], BF16) for i in range(2)]
    rs = [S.sb(f"rs{i}", [128, 512], F32) for i in range(2)]
    qn = [S.sb(f"qn{i}", [128, 512], BF16) for i in range(3)]
    vt = [S.sb(f"vt{i}", [128, 1024], BF16) for i in range(2)]
    pT = [S.ps(f"pT{i}", 512 * i, 512) for i in range(2)]
    pQ = [S.ps(f"pQ{i}", 1024 + 512 * i, 512) for i in range(2)]
    pR = [S.ps(f"pR{i}", 2048 + 512 * i, 512) for i in range(2)]
    pV = [S.ps(f"pV{i}", 3072 + 512 * i, 512) for i in range(2)]
    nt = 0
    nqk = 0
    for blk in range(NCH // 4):
        hb = hT[blk % 2]
        for t in range(4):
            g = blk * 4 + t
            s = 0 if g >= 2 else 1
            xt = xin[nt % 3]
            S.dma("sp", xt[:, :], I["xk"][128 * g:128 * g + 128, :], xt, writes=[xt])
            norm_tile(S, C, xt, ss[nt % 2], rstd[nt % 2], xs[nt % 2], pT,
                      (hb, lambda kc, hb=hb, t=t: hb[:, kc, 128 * t:128 * t + 128]), Gs[s], Ss[s])
            nt += 1
        for which, dst_d in ((0, QT), (1, KT)):
            for hp in range(8):
                p = pQ[nqk % 2]
                pr = pR[nqk % 2]
                col = which * 1024 + 128 * hp
                for kc in range(8):
                    mm(S, p, p[:, :], wq[:, kc, col:col + 128], hb[:, kc, :], kc == 0, kc == 7, reads=[wq, hb])
                sqb = sq[nqk % 2]
                act(S, sqb[:, :], p[:, :], AF.Square, reads=[p], writes=[sqb])
                mm(S, pr, pr[:, :], bd[:, :], sqb[:, :], True, True, reads=[bd, sqb])
                rsb = rs[nqk % 2]
                act(S, rsb[:, :], pr[:, :], AF.Sqrt, reads=[pr, C.epsb], writes=[rsb], bias=C.epsb[:, 0:1])
                S.add("dve", lambda e, rsb=rsb: e.reciprocal(out=rsb[:, :], in_=rsb[:, :]), reads=[rsb], writes=[rsb])
                qb = qn[nqk % 3]
                stt(S, "dve", qb[:, :], p[:, :], gains[:, which:which + 1], rsb[:, :], ALU.mult, ALU.mult,
                    reads=[p, gains, rsb], writes=[qb])
                S.dma("sp", dst_d[hp, :, 512 * blk:512 * blk + 512], qb[:, :], qb, reads=[qb], writes=[dst_d])
                nqk += 1
        for t in range(4):
            g = blk * 4 + t
            vb = vt[g % 2]
            for cb in range(2):
                p = pV[cb]
                for kc in range(8):
                    mm(S, p, p[:, :], hb[:, kc, 128 * t:128 * t + 128], wq[:, kc, 2048 + 512 * cb:2048 + 512 * cb + 512],
                       kc == 0, kc == 7, reads=[hb, wq])
                cp(S, "act", vb[:, 512 * cb:512 * cb + 512], p[:, :], reads=[p], writes=[vb])
            S.dma("sp", V[128 * g:128 * g + 128, :], vb[:, :], vb, reads=[vb], writes=[V])
    S.barrier()
    S.off = m0


def phase2_attn(S, C, I, mod_d, QT, KT, V, x1a, h2T):
    m0 = S.off
    wo = S.sb("wo", [128, 8, 1024], BF16)
    S.dma("pool", wo[:, :, :], I["l0_w_o"].rearrange("(hp p) n -> p hp n", p=128), wo, writes=[wo])
    bgen = S.sb("bgen", [128, 16, 5, 128], BF16)
    bspec = S.sb("bspec", [128, 16, 5, 128], BF16)
    S.dma("pool", bgen[:, :, :, :], I["bt"][0], bgen, writes=[bgen])
    mods = S.sb("mods2", [128, 32], F32)
    tmpm = S.sb("tmpm2", [128, 24], F32)
    S.dma("sp", tmpm[:, 0:8], pp_view(I["l0_norm2"]), tmpm, writes=[tmpm], allow_slow_non_contiguous=True)
    g1b = []
    for s in range(2):
        load_mod_pp(S, tmpm, 1 + s, mod_d, 0, s, 4)
        load_mod_pp(S, mods, 2 * s + 1, mod_d, 0, s, 3)
        stt(S, "dve", mods[:, 16 * s:16 * s + 8], tmpm[:, 8 + 8 * s:16 + 8 * s], 1.0, tmpm[:, 0:8], ALU.add, ALU.mult,
            reads=[tmpm], writes=[mods])
        gb = S.sb(f"g1b{s}", [128, 1024], F32)
        S.dma("sp", gb[:, :], bc_view(mod_d[0, s, 2 * D:3 * D], D), gb, reads=[mod_d], writes=[gb])
        g1b.append(gb)
    Gs = [Buf("G2", mods[:, 0:8]), Buf("G2c", mods[:, 16:24])]
    Ss = [Buf("S2", mods[:, 8:16]), Buf("S2c", mods[:, 24:32])]
    RING = 6
    ktr = S.sb("ktr", [128, 8, RING, 128], BF16)
    vr = S.sb("vr", [128, RING, 16, 65], BF16)
    ktc = S.sb("ktc", [128, 8, 256], BF16)
    vc = S.sb("vc", [128, 2, 16, 65], BF16)
    kslots = [Buf(f"ks{i}", None) for i in range(RING)]
    vslots = [Buf(f"vs{i}", None) for i in range(RING)]
    S.add("pool", lambda e: e.memset(vr[:, :, :, 64:65], 1.0), writes=vslots)
    S.add("pool", lambda e: e.memset(vc[:, :, :, 64:65], 1.0), writes=[vc])
    S.dma("sp", ktc[:, :, :], KT[:, :, 0:256].rearrange("hp p t -> p hp t"), ktc, reads=[KT], writes=[ktc])
    for cc in range(2):
        S.dma("sp", vc[:, cc, :, 0:64], V[128 * cc:128 * cc + 128, :].rearrange("p (h d) -> p h d", h=16), vc,
              reads=[V], writes=[vc])
    qt = [S.sb(f"qt{i}", [128, 8, 128], BF16) for i in range(2)]
    xt_ = [S.sb(f"x2in{i}", [128, 1024], F32) for i in range(2)]
    tb = [S.sb(f"tb{i}", [128, 5, 128], F32) for i in range(2)]
    pt = [S.sb(f"pt{i}", [128, 7, 128], BF16) for i in range(2)]
    rec = [S.sb(f"rec{i}", [128, 1], F32) for i in range(4)]
    on = [S.sb(f"on{i}", [128, 16, 64], BF16) for i in range(2)]
    oT = [S.sb(f"oT{i}", [128, 8, 128], BF16) for i in range(2)]
    x1t = [S.sb(f"x1t{i}", [128, 1024], F32) for i in range(2)]
    xs = [S.sb(f"xs2{i}", [128, 1024], F32) for i in range(2)]
    ss = [S.sb(f"ss2{i}", [128, 1], F32) for i in range(2)]
    rstd = [S.sb(f"rstd2{i}", [128, 1], F32) for i in range(2)]
    h2 = [S.sb(f"h2t{i}", [128, 8, 128], BF16) for i in range(2)]
    pS = [S.ps(f"pS{i}", 1024 * i, 896) for i in range(2)]
    pO = [S.ps(f"pO{i}", 2048 + 128 * i, 65) for i in range(4)]
    pY = [S.ps(f"pY{i}", 2560 + 512 * i, 512) for i in range(2)]
    pOT = S.ps("pOT", 3584, 512, BF16)

    loaded = set()

    def ensure_chunk(g):
        if g in loaded:
            return
        loaded.add(g)
        sl = g % RING
        S.dma("sp", ktr[:, :, sl, :], KT[:, :, 128 * g:128 * g + 128].rearrange("hp p t -> p hp t"), kslots[sl],
              reads=[KT], writes=[kslots[sl]])
        S.dma("sp", vr[:, sl, :, 0:64], V[128 * g:128 * g + 128, :].rearrange("p (h d) -> p h d", h=16), vslots[sl],
              reads=[V], writes=[vslots[sl]])

    spec_lts = {1: 1, 2: 2, 31: 3, 32: 4}
    nh = 0
    for ti in range(NT1):
        is_ctx = ti < 2
        lt = ti - 2
        g = ti if is_ctx else lt + 4
        s = 1 if is_ctx else 0
        q = qt[ti % 2]
        S.dma("sp", q[:, :, :], QT[:, :, 128 * g:128 * g + 128].rearrange("hp p t -> p hp t"), q, reads=[QT], writes=[q])
        xt = xt_[ti % 2]
        S.dma("sp", xt[:, :], I["xk"][128 * g:128 * g + 128, :], xt, writes=[xt])
        nloc = 0 if is_ctx else 5
        if not is_ctx:
            for j in range(5):
                ensure_chunk(lt + 2 + j)
            if lt in spec_lts:
                S.dma("pool", bspec[:, :, :, :], I["bt"][spec_lts[lt]], bspec, writes=[bspec])
                bias = bspec
            else:
                bias = bgen
        onb = on[ti % 2]
        for h in range(NH):
            hp, half = h // 2, h % 2
            lo = 64 * half
            p = pS[nh % 2]
            ptb = pt[nh % 2]
            for j in range(nloc):
                sl = (lt + 2 + j) % RING
                mm(S, p, p[:, 128 * j:128 * j + 128], ktr[lo:lo + 64, hp, sl, :], q[lo:lo + 64, hp, :], True, False,
                   reads=[kslots[sl], q])
                mm(S, p, p[:, 128 * j:128 * j + 128], bias[:, h, j, :], C.identb[:, :], False, True,
                   reads=[bias, C.identb])
            for cc in range(2):
                jj = nloc + cc
                mm(S, p, p[:, 128 * jj:128 * jj + 128], ktc[lo:lo + 64, hp, 128 * cc:128 * cc + 128], q[lo:lo + 64, hp, :],
                   True, True, reads=[ktc, q])
            nn = nloc + 2
            act(S, ptb[:, 0:nn, :], p[:, 0:128 * nn].rearrange("p (j q) -> p j q", j=nn), AF.Exp, reads=[p], writes=[ptb])
            po = pO[nh % 4]
            n = nloc + 2
            for j in range(nloc):
                sl = (lt + 2 + j) % RING
                mm(S, po, po[:, :], ptb[:, j, :], vr[:, sl, h, :], j == 0, False, reads=[ptb, vslots[sl]])
            for cc in range(2):
                mm(S, po, po[:, :], ptb[:, nloc + cc, :], vc[:, cc, h, :], (nloc + cc) == 0, cc == 1, reads=[ptb, vc])
            r_ = rec[nh % 4]
            S.add("dve", lambda e, r_=r_, po=po: e.reciprocal(out=r_[:, 0:1], in_=po[:, 64:65]), reads=[po], writes=[r_])
            ts(S, "dve", onb[:, h, :], po[:, 0:64], r_[:, 0:1], None, ALU.mult, None, reads=[po, r_], writes=[onb])
            nh += 1
        otb = oT[ti % 2]
        for hp in range(8):
            tr(S, pOT, pOT[:, 128 * hp:128 * hp + 128], onb[:, 2 * hp:2 * hp + 2, :].rearrange("p a b -> p (a b)"),
               C.identb[:, :], reads=[onb, C.identb])
        cp(S, "act", otb[:, 0:4, :], pOT[:, 0:512].rearrange("p (a b) -> p a b", a=4), reads=[pOT], writes=[otb])
        cp(S, "dve", otb[:, 4:8, :], pOT[:, 512:1024].rearrange("p (a b) -> p a b", a=4), reads=[pOT], writes=[otb])
        x1 = x1t[ti % 2]
        for cb in range(2):
            py = pY[cb]
            for hp in range(8):
                mm(S, py, py[:, :], otb[:, hp, :], wo[:, hp, 512 * cb:512 * cb + 512], hp == 0, hp == 7, reads=[otb, wo])
            tt(S, "dve", x1[:, 512 * cb:512 * cb + 512], py[:, :], g1b[s][:, 512 * cb:512 * cb + 512], ALU.mult,
               reads=[py, g1b[s]], writes=[x1])
        tt(S, "pool", x1[:, :], x1[:, :], xt[:, :], ALU.add, reads=[x1, xt], writes=[x1])
        S.dma("sp", x1a[128 * ti:128 * ti + 128, :], x1[:, :], x1, reads=[x1], writes=[x1a])
        hb = h2[ti % 2]
        norm_tile(S, C, x1, ss[ti % 2], rstd[ti % 2], xs[ti % 2], pY,
                  (hb, lambda kc, hb=hb: hb[:, kc, :]), Gs[s], Ss[s])
        S.dma("sp", h2T[:, :, 128 * ti:128 * ti + 128].rearrange("kc p t -> p kc t"), hb[:, :, :], hb, reads=[hb], writes=[h2T])
    S.barrier()
    S.off = m0


def phase3_ffn(S, C, I, mod_d, x1a, h2T, x1):
    m0 = S.off
    w1 = S.sb("w1", [128, 8, DFF], BF16)
    w3 = S.sb("w3", [128, 8, DFF], BF16)
    w2 = S.sb("w2", [128, NFC, 1024], BF16)
    for kc in range(8):
        S.dma("pool", w1[:, kc, :], I["l0_ffn_w1"][128 * kc:128 * kc + 128, :], w1, writes=[w1])
        S.dma("pool", w3[:, kc, :], I["l0_ffn_w3"][128 * kc:128 * kc + 128, :], w3, writes=[w3])
    for fc in range(NFC):
        S.dma("pool", w2[:, fc, :], I["l0_ffn_w2"][128 * fc:128 * fc + 128, :], w2, writes=[w2])
    g2b = []
    for s in range(2):
        gb = S.sb(f"g2b{s}", [128, 1024], F32)
        S.dma("sp", gb[:, :], bc_view(mod_d[0, s, 5 * D:6 * D], D), gb, reads=[mod_d], writes=[gb])
        g2b.append(gb)
    hb_ = [S.sb(f"h3b{i}", [128, 8, 512], BF16) for i in range(2)]
    hid = S.sb("hid", [128, NFC, 512], BF16)
    sa = [S.sb(f"sa{i}", [128, 512], F32) for i in range(2)]
    xa = [S.sb(f"xa{i}", [128, 1024], F32) for i in range(2)]
    xo = [S.sb(f"xo{i}", [128, 1024], F32) for i in range(2)]
    pA = [S.ps(f"pA{i}", 512 * i, 512) for i in range(2)]
    pB = [S.ps(f"pB{i}", 1024 + 512 * i, 512) for i in range(2)]
    pY = [S.ps(f"pY3{i}", 2048 + 512 * i, 512) for i in range(4)]
    nf = 0
    nt = 0
    for blk in range(NT1 // 4):
        hb = hb_[blk % 2]
        S.dma("sp", hb[:, :, :], h2T[:, :, 512 * blk:512 * blk + 512].rearrange("kc p t -> p kc t"), hb, reads=[h2T], writes=[hb])
        for fc in range(NFC):
            pa, pb = pA[nf % 2], pB[nf % 2]
            for kc in range(8):
                mm(S, pa, pa[:, :], w1[:, kc, 128 * fc:128 * fc + 128], hb[:, kc, :], kc == 0, kc == 7, reads=[w1, hb])
            for kc in range(8):
                mm(S, pb, pb[:, :], w3[:, kc, 128 * fc:128 * fc + 128], hb[:, kc, :], kc == 0, kc == 7, reads=[w3, hb])
            sb_ = sa[nf % 2]
            act(S, sb_[:, :], pa[:, :], AF.Silu, reads=[pa], writes=[sb_])
            tt(S, "dve", hid[:, fc, :], sb_[:, :], pb[:, :], ALU.mult, reads=[sb_, pb], writes=[hid])
            nf += 1
        for t in range(4):
            ti = blk * 4 + t
            s = 1 if ti < 2 else 0
            xab = xa[nt % 2]
            S.dma("sp", xab[:, :], x1a[128 * ti:128 * ti + 128, :], xab, reads=[x1a], writes=[xab])
            xob = xo[nt % 2]
            for cb in range(2):
                py = pY[(nt % 2) * 2 + cb]
                for fc in range(NFC):
                    mm(S, py, py[:, :], hid[:, fc, 128 * t:128 * t + 128], w2[:, fc, 512 * cb:512 * cb + 512],
                       fc == 0, fc == NFC - 1, reads=[hid, w2])
                tt(S, "dve", xob[:, 512 * cb:512 * cb + 512], py[:, :], g2b[s][:, 512 * cb:512 * cb + 512], ALU.mult,
                   reads=[py, g2b[s]], writes=[xob])
            tt(S, "pool", xob[:, :], xob[:, :], xab[:, :], ALU.add, reads=[xob, xab], writes=[xob])
            S.dma("sp", x1[128 * ti:128 * ti + 128, :], xob[:, :], xob, reads=[xob], writes=[x1])
            nt += 1
    S.barrier()
    S.off = m0


class RnnConsts:
    pass


def rnn_setup(S, C, I, mod_d, pass2):
    R = RnnConsts()
    R.win_x = S.sb("win_x", [128, 8, DRNN], BF16)
    S.dma("pool", R.win_x[:, :, :], I["l1_w_in"][:, DRNN:2 * DRNN].rearrange("(kc p) n -> p kc n", p=128), R.win_x,
          writes=[R.win_x])
    if pass2:
        R.win_g = S.sb("win_g", [128, 8, DRNN], BF16)
        S.dma("pool", R.win_g[:, :, :], I["l1_w_in"][:, 0:DRNN].rearrange("(kc p) n -> p kc n", p=128), R.win_g,
              writes=[R.win_g])
        R.wout = S.sb("wout", [RB, RCH, D], BF16)
        S.dma("pool", R.wout[:, :, :], I["l1_w_out"].rearrange("(ch p) n -> p ch n", p=RB), R.wout, writes=[R.wout])
    R.ga = S.sb("ga", [RB, 2, RCH, RB], BF16)
    R.gx = S.sb("gx", [RB, 2, RCH, RB], BF16)
    for d in range(2):
        S.dma("pool", R.ga[:, d, :, :], I["l1_gate_a_w"][d].rearrange("k c o -> c k o"), R.ga, writes=[R.ga])
        S.dma("pool", R.gx[:, d, :, :], I["l1_gate_x_w"][d].rearrange("k c o -> c k o"), R.gx, writes=[R.gx])
    R.cw = S.sb("cw", [RB, 5, RCH], F32)
    for j in range(4):
        S.dma("sp", R.cw[:, j, :], I["l1_conv_w"][j].rearrange("(ch p) -> p ch", p=RB), R.cw, writes=[R.cw],
              allow_slow_non_contiguous=True)
    S.dma("sp", R.cw[:, 4, :], I["l1_conv_b"].rearrange("(ch p) -> p ch", p=RB), R.cw, writes=[R.cw],
          allow_slow_non_contiguous=True)
    R.gb = S.sb("gb", [RB, 4, RCH], F32)
    R.c1 = S.sb("c1", [RB, 2, RCH], F32)
    lam = S.sb("lamt", [RB, 2 * RCH], F32)
    for d in range(2):
        S.dma("sp", R.gb[:, d, :], I["l1_gate_a_b"][d].rearrange("(ch p) -> p ch", p=RB), R.gb, writes=[R.gb],
              allow_slow_non_contiguous=True)
        S.dma("sp", R.gb[:, 2 + d, :], I["l1_gate_x_b"][d].rearrange("(ch p) -> p ch", p=RB), R.gb, writes=[R.gb],
              allow_slow_non_contiguous=True)
        S.dma("sp", lam[:, RCH * d:RCH * d + RCH], I["l1_lam"][d].rearrange("(ch p) -> p ch", p=RB), lam, writes=[lam],
              allow_slow_non_contiguous=True)
    n = 2 * RCH
    t = S.sb("sp_t", [RB, n], F32)
    w = S.sb("sp_w", [RB, n], F32)
    w2 = S.sb("sp_w2", [RB, n], F32)
    pl = S.sb("sp_pl", [RB, n], F32)
    s2 = S.sb("sp_s2", [RB, n], F32)
    mk = S.sb("sp_mk", [RB, n], F32)
    act(S, t[:, :], lam[:, :], AF.Exp, reads=[lam], writes=[t], scale=-1.0)
    ts(S, "dve", w[:, :], t[:, :], 2.0, None, ALU.add, None, reads=[t], writes=[w])
    S.add("dve", lambda e: e.reciprocal(out=w[:, :], in_=w[:, :]), reads=[w], writes=[w])
    tt(S, "dve", w[:, :], w[:, :], t[:, :], ALU.mult, reads=[w, t], writes=[w])
    tt(S, "dve", w2[:, :], w[:, :], w[:, :], ALU.mult, reads=[w], writes=[w2])
    ts(S, "dve", pl[:, :], w2[:, :], 1.0 / 11, 1.0 / 9, ALU.mult, ALU.add, reads=[w2], writes=[pl])
    for cf in (1.0 / 7, 1.0 / 5, 1.0 / 3, 1.0):
        tt(S, "dve", pl[:, :], pl[:, :], w2[:, :], ALU.mult, reads=[pl, w2], writes=[pl])
        ts(S, "dve", pl[:, :], pl[:, :], cf, None, ALU.add, None, reads=[pl], writes=[pl])
    tt(S, "dve", pl[:, :], pl[:, :], w[:, :], ALU.mult, reads=[pl, w], writes=[pl])
    ts(S, "dve", s2[:, :], t[:, :], 1.0, None, ALU.add, None, reads=[t], writes=[s2])
    act(S, s2[:, :], s2[:, :], AF.Ln, reads=[s2], writes=[s2])
    ts(S, "dve", mk[:, :], t[:, :], 0.5, None, ALU.is_lt, None, reads=[t], writes=[mk])
    stt(S, "dve", pl[:, :], pl[:, :], 2.0, s2[:, :], ALU.mult, ALU.subtract, reads=[pl, s2], writes=[pl])
    tt(S, "dve", pl[:, :], pl[:, :], mk[:, :], ALU.mult, reads=[pl, mk], writes=[pl])
    tt(S, "dve", pl[:, :], pl[:, :], s2[:, :], ALU.add, reads=[pl, s2], writes=[pl])
    ts(S, "dve", R.c1[:, :, :].rearrange("p a b -> p (a b)"), pl[:, :], -8.0, None, ALU.mult, None, reads=[pl], writes=[R.c1])
    R.ones = S.sb("ones_r", [RB, 1], F32)
    S.add("pool", lambda e: e.memset(R.ones[:, :], 1.0 + 2.0 ** -23), writes=[R.ones])
    R.ngb = S.sb("ngb", [RB, 4, RCH], F32)
    ts(S, "dve", R.ngb[:, :, :], R.gb[:, :, :], -1.0, None, ALU.mult, None, reads=[R.gb], writes=[R.ngb])
    R.msk = S.sb("msk", [128, 2], F32)
    S.dma("sp", R.msk[:, :], I["masks"], R.msk, writes=[R.msk])
    R.X = [S.sb(f"X{i}", [RB, 515], F32) for i in range(2)]
    R.xc = [S.sb(f"xc{i}", [RB, 512], F32) for i in range(2)]
    R.xcb = [S.sb(f"xcb{i}", [RB, 512], BF16) for i in range(2)]
    R.r = [S.sb(f"r{i}", [RB, 512], F32) for i in range(2)]
    R.iu = [S.sb(f"iu{i}", [RB, 512], F32) for i in range(2)]
    R.a = [S.sb(f"a{i}", [RB, 512], F32) for i in range(2)]
    R.m = [S.sb(f"m{i}", [RB, 512], F32) for i in range(2)]
    R.hs = [S.sb(f"hs{i}", [RB, 512], F32) for i in range(2)]
    R.win = [S.sb(f"hwin{i}", [128, 8, 516], BF16) for i in range(2)]
    R.pb = [S.ps(f"rb{i}", 512 * i, 512) for i in range(8)]
    R.pxB = [S.ps(f"pxB{i}", 3584 + 4 * i, 3, parts=RB) for i in range(2)]
    return R


def rnn_front(S, R, hwin, L, ch, nchunk, is_ctx, mask_before, mask_after):
    X = R.X[nchunk % 2]
    xc = R.xc[nchunk % 2]
    xcb = R.xcb[nchunk % 2]
    px = R.pb[nchunk % 2]
    col = 96 * ch
    if is_ctx:
        for kc in range(8):
            mm(S, px, px[0:RB, 0:L], R.win_x[:, kc, col:col + RB], hwin[:, kc, 0:L], kc == 0, kc == 7, reads=[R.win_x, hwin])
        S.add("pool", lambda e: e.memset(X[:, 0:2], 0.0), writes=[X])
        S.add("pool", lambda e: e.memset(X[:, L + 2:L + 3], 0.0), writes=[X])
        cp(S, "act", X[:, 2:L + 2], px[0:RB, 0:L], reads=[px], writes=[X])
    else:
        pxb = R.pxB[nchunk % 2]
        for kc in range(8):
            mm(S, px, px[0:RB, 0:512], R.win_x[:, kc, col:col + RB], hwin[:, kc, 0:512], kc == 0, kc == 7, reads=[R.win_x, hwin])
        for kc in range(8):
            mm(S, pxb, pxb[:, 0:3], R.win_x[:, kc, col:col + RB], hwin[:, kc, 512:515], kc == 0, kc == 7, reads=[R.win_x, hwin])
        cp(S, "act", X[:, 0:512], px[0:RB, 0:512], reads=[px], writes=[X])
        cp(S, "dve", X[:, 512:515], pxb[:, 0:3], reads=[pxb], writes=[X])
        if mask_before:
            ts(S, "dve", X[:, 0:2], X[:, 0:2], R.msk[0:RB, 0:1], None, ALU.mult, None, reads=[X, R.msk], writes=[X])
        if mask_after:
            ts(S, "dve", X[:, 514:515], X[:, 514:515], R.msk[0:RB, 1:2], None, ALU.mult, None, reads=[X, R.msk], writes=[X])
    ts(S, "dve", xc[:, 0:L], X[:, 0:L], R.cw[:, 0, ch:ch + 1], R.cw[:, 4, ch:ch + 1], ALU.mult, ALU.add,
       reads=[X, R.cw], writes=[xc])
    for j in range(1, 4):
        stt(S, "dve", xc[:, 0:L], X[:, j:j + L], R.cw[:, j, ch:ch + 1], xc[:, 0:L], ALU.mult, ALU.add,
            reads=[X, R.cw, xc], writes=[xc])
    cp(S, "pool", xcb[:, 0:L], xc[:, 0:L], reads=[xc], writes=[xcb])


def rnn_back(S, C, R, L, ch, nchunk, init, sumr, hs_out, extra_sig=None):
    xc = R.xc[nchunk % 2]
    xcb = R.xcb[nchunk % 2]
    for d in range(2):
        pr = R.pb[2 + d]
        pi = R.pb[4 + d]
        mm(S, pr, pr[0:RB, 0:L], R.ga[:, d, ch, :], xcb[:, 0:L], True, True, reads=[R.ga, xcb])
        mm(S, pi, pi[0:RB, 0:L], R.gx[:, d, ch, :], xcb[:, 0:L], True, True, reads=[R.gx, xcb])
    for d in range(2):
        pr = R.pb[2 + d]
        pi = R.pb[4 + d]
        r, iu = R.r[d], R.iu[d]
        if sumr is not None:
            act(S, r[:, 0:L], pr[0:RB, 0:L], AF.Sigmoid, reads=[pr, R.gb], writes=[r, sumr[d][0]],
                bias=R.gb[:, d, ch:ch + 1], accum_out=sumr[d][1])
        else:
            act(S, r[:, 0:L], pr[0:RB, 0:L], AF.Sigmoid, reads=[pr, R.gb], writes=[r], bias=R.gb[:, d, ch:ch + 1])
        act(S, iu[:, 0:L], pi[0:RB, 0:L], AF.Sigmoid, reads=[pi, R.gb], writes=[iu], bias=R.gb[:, 2 + d, ch:ch + 1])
    if extra_sig is not None:
        extra_sig()
    for d in range(2):
        r, a = R.r[d], R.a[d]
        act(S, a[:, 0:L], r[:, 0:L], AF.Exp, reads=[r, R.c1], writes=[a], scale=R.c1[:, d, ch:ch + 1])
    for d in range(2):
        a, m = R.a[d], R.m[d]
        act(S, m[:, 0:L], a[:, 0:L], AF.Square, reads=[a], writes=[m])
    for d in range(2):
        m = R.m[d]
        act(S, m[:, 0:L], m[:, 0:L], AF.Ln, reads=[m, R.ones], writes=[m], bias=R.ones[:, 0:1], scale=-1.0)
    for d in range(2):
        m = R.m[d]
        act(S, m[:, 0:L], m[:, 0:L], AF.Exp, reads=[m], writes=[m], scale=0.5)
    for d in range(2):
        r, iu, a, m, hs = R.r[d], R.iu[d], R.a[d], R.m[d], hs_out[d]
        tt(S, "dve", iu[:, 0:L], iu[:, 0:L], xc[:, 0:L], ALU.mult, reads=[iu, xc], writes=[iu])
        tt(S, "dve", iu[:, 0:L], iu[:, 0:L], m[:, 0:L], ALU.mult, reads=[iu, m], writes=[iu])
        ini = init[d]
        ini_reads = [] if isinstance(ini, float) else [ini[0]]
        ini_ap = ini if isinstance(ini, float) else ini[1]
        if d == 0:
            S.add("dve", lambda e, hs=hs, a=a, iu=iu, ini_ap=ini_ap: e.tensor_tensor_scan(
                out=hs[:, 0:L], data0=a[:, 0:L], data1=iu[:, 0:L], initial=ini_ap, op0=ALU.mult, op1=ALU.add),
                reads=[a, iu] + ini_reads, writes=[hs])
        else:
            def rv(buf):
                ap = buf[:, 0:L]
                return bass.AP(ap.tensor, ap.offset + (L - 1), [list(ap.ap[0]), [-1, L]])
            S.add("dve", lambda e, hs=hs, a=a, iu=iu, ini_ap=ini_ap: e.tensor_tensor_scan(
                out=rv(hs), data0=rv(a), data1=rv(iu), initial=ini_ap, op0=ALU.mult, op1=ALU.add),
                reads=[a, iu] + ini_reads, writes=[hs])


def phase4a_h1(S, C, I, mod_d, x1, hT1):
    m0 = S.off
    mods = S.sb("mods4", [128, 32], F32)
    tmpm = S.sb("tmpm4", [128, 24], F32)
    S.dma("sp", tmpm[:, 0:8], pp_view(I["l1_norm1"]), tmpm, writes=[tmpm], allow_slow_non_contiguous=True)
    for s in range(2):
        load_mod_pp(S, tmpm, 1 + s, mod_d, 1, s, 1)
        load_mod_pp(S, mods, 2 * s + 1, mod_d, 1, s, 0)
        stt(S, "dve", mods[:, 16 * s:16 * s + 8], tmpm[:, 8 + 8 * s:16 + 8 * s], 1.0, tmpm[:, 0:8], ALU.add, ALU.mult,
            reads=[tmpm], writes=[mods])
    Gs = [Buf("G4", mods[:, 0:8]), Buf("G4c", mods[:, 16:24])]
    Ss = [Buf("S4", mods[:, 8:16]), Buf("S4c", mods[:, 24:32])]
    xin = [S.sb(f"x4in{i}", [128, 1024], F32) for i in range(3)]
    xs = [S.sb(f"xs4{i}", [128, 1024], F32) for i in range(2)]
    ss = [S.sb(f"ss4{i}", [128, 1], F32) for i in range(2)]
    rstd = [S.sb(f"rstd4{i}", [128, 1], F32) for i in range(2)]
    hb_ = [S.sb(f"h4{i}", [128, 8, 128], BF16) for i in range(2)]
    pT = [S.ps(f"pT4{i}", 512 * i, 512) for i in range(2)]
    for ti in range(NT1):
        s = 1 if ti < 2 else 0
        xt = xin[ti % 3]
        S.dma("sp", xt[:, :], x1[128 * ti:128 * ti + 128, :], xt, reads=[x1], writes=[xt])
        hb = hb_[ti % 2]
        norm_tile(S, C, xt, ss[ti % 2], rstd[ti % 2], xs[ti % 2], pT, (hb, lambda kc, hb=hb: hb[:, kc, :]), Gs[s], Ss[s])
        S.dma("sp", hT1[:, :, 128 * ti:128 * ti + 128].rearrange("kc p t -> p kc t"), hb[:, :, :], hb, reads=[hb], writes=[hT1])
    S.barrier()
    S.off = m0


def seg_info(seg):
    if seg == 0:
        return True, 256, 0, False, False
    b = seg - 1
    s = 256 + 128 + 512 * b
    return False, 512, s - 2, b == 0, b == 7


def load_win(S, R, hT1, seg, n):
    is_ctx, L, w0, _, _ = seg_info(seg)
    hw = R.win[n % 2]
    wl = 256 if is_ctx else 515
    S.dma("sp", hw[:, :, 0:wl], hT1[:, :, w0:w0 + wl].rearrange("kc p t -> p kc t"), hw, reads=[hT1], writes=[hw])
    return hw


def phase4b_pass1(S, C, I, mod_d, hT1, SAB):
    m0 = S.off
    R = rnn_setup(S, C, I, mod_d, pass2=False)
    sab = S.sb("sab", [RB, 2, 9, 2, RCH], F32)
    S.add("pool", lambda e: e.memset(sab[:, 0, :, :, :], 0.0), writes=[sab])
    work = []
    for seg in range(9):
        for ch in range(RCH):
            work.append((seg, ch))
    wins = {0: load_win(S, R, hT1, 0, 0)}

    def front(n):
        seg, ch = work[n]
        is_ctx, L, w0, mb, ma = seg_info(seg)
        if ch == 0 and seg + 1 < 9:
            wins[seg + 1] = load_win(S, R, hT1, seg + 1, seg + 1)
        rnn_front(S, R, wins[seg], L, ch, n, is_ctx, mb, ma)

    front(0)
    for n, (seg, ch) in enumerate(work):
        is_ctx, L, w0, mb, ma = seg_info(seg)
        if n + 1 < len(work):
            front(n + 1)
        sumr = [(sab, sab[:, 0, seg, d, ch:ch + 1]) for d in range(2)]
        rnn_back(S, C, R, L, ch, n, [0.0, 0.0], sumr, R.hs)
        cp(S, "pool", sab[:, 1, seg, 0, ch:ch + 1], R.hs[0][:, L - 1:L], reads=[R.hs[0]], writes=[sab])
        cp(S, "pool", sab[:, 1, seg, 1, ch:ch + 1], R.hs[1][:, 0:1], reads=[R.hs[1]], writes=[sab])
    for seg in range(9):
        tt(S, "dve", sab[:, 0, seg, :, :], sab[:, 0, seg, :, :], R.c1[:, :, :], ALU.mult, reads=[sab, R.c1], writes=[sab])
    act(S, sab[:, 0, :, :, :], sab[:, 0, :, :, :], AF.Exp, reads=[sab], writes=[sab])
    S.dma("sp", SAB[:, :], sab[:, :, :, :, :].rearrange("p a s d c -> p (a s d c)"), sab, reads=[sab], writes=[SAB])
    S.barrier()
    S.off = m0


def phase5_fold(S, C, I, SALL, SOWN, hin, NR=4):
    m0 = S.off
    sall = S.sb("sall", [RB, NR, 2 * 9 * 2 * RCH], F32)
    sown = S.sb("sown", [RB, 2, 9, 2, RCH], F32)
    sel = S.sb("sel", [RB, 2 * NR], F32)
    S.dma("sp", sall[:, :, :], SALL.t.rearrange("(j p) f -> p j f", p=RB), sall, reads=[SALL], writes=[sall])
    S.dma("sp", sown[:, :, :, :, :].rearrange("p a s d c -> p (a s d c)"), SOWN[:, :], sown, reads=[SOWN], writes=[sown])
    S.dma("sp", sel[:, :], I["sel"], sel, writes=[sel])
    sv = sall[:, :, :].rearrange("p j (a s d c) -> p j a s d c", a=2, s=9, d=2)
    h = S.sb("hfold", [RB, RCH], F32)
    ae = S.sb("aeff", [RB, 8, RCH], F32)
    be = S.sb("beff", [RB, 8, RCH], F32)
    for d in range(2):
        cp(S, "dve", h[:, :], sown[:, 1, 0, d, :], reads=[sown], writes=[h])
        order = range(NR) if d == 0 else range(NR - 1, -1, -1)
        for j in order:
            mj = sel[:, d * NR + j:d * NR + j + 1]
            ts(S, "dve", ae[:, :, :], sv[:, j, 0, 1:9, d, :], -1.0, None, ALU.add, None, reads=[sall], writes=[ae])
            ts(S, "dve", ae[:, :, :], ae[:, :, :], mj, None, ALU.mult, None, reads=[ae, sel], writes=[ae])
            ts(S, "dve", ae[:, :, :], ae[:, :, :], 1.0, None, ALU.add, None, reads=[ae], writes=[ae])
            ts(S, "dve", be[:, :, :], sv[:, j, 1, 1:9, d, :], mj, None, ALU.mult, None, reads=[sall, sel], writes=[be])
            border = range(8) if d == 0 else range(7, -1, -1)
            for b in border:
                tt(S, "dve", h[:, :], h[:, :], ae[:, b, :], ALU.mult, reads=[h, ae], writes=[h])
                tt(S, "dve", h[:, :], h[:, :], be[:, b, :], ALU.add, reads=[h, be], writes=[h])
        border = list(range(8)) if d == 0 else list(range(7, -1, -1))
        for n, b in enumerate(border):
            cp(S, "dve", hin[:, d, b, :], h[:, :], reads=[h], writes=[hin])
            if n < 7:
                tt(S, "dve", h[:, :], h[:, :], sown[:, 0, 1 + b, d, :], ALU.mult, reads=[h, sown], writes=[h])
                tt(S, "dve", h[:, :], h[:, :], sown[:, 1, 1 + b, d, :], ALU.add, reads=[h, sown], writes=[h])
    S.barrier()
    S.off = m0


def phase6_pass2(S, C, I, mod_d, x1, hT1, hin, x2, h2T1, comb, XS=None, maskall=None):
    R = rnn_setup(S, C, I, mod_d, pass2=True)
    mods = S.sb("mods6", [128, 16], F32)
    tmpm = S.sb("tmpm6", [128, 16], F32)
    S.dma("sp", tmpm[:, 0:8], pp_view(I["l1_norm2"]), tmpm, writes=[tmpm], allow_slow_non_contiguous=True)
    load_mod_pp(S, tmpm, 1, mod_d, 1, 0, 4)
    load_mod_pp(S, mods, 1, mod_d, 1, 0, 3)
    stt(S, "dve", mods[:, 0:8], tmpm[:, 8:16], 1.0, tmpm[:, 0:8], ALU.add, ALU.mult, reads=[tmpm], writes=[mods])
    G2 = Buf("G6", mods[:, 0:8])
    S2 = Buf("S6", mods[:, 8:16])
    g1b = S.sb("g1b6", [128, 1024], F32)
    S.dma("sp", g1b[:, :], bc_view(mod_d[1, 0, 2 * D:3 * D], D), g1b, reads=[mod_d], writes=[g1b])
    wr = S.sb("wr", [128, 8, NE], F32)
    S.dma("sp", wr[:, :, :], I["l1_router_w"].rearrange("(kc p) e -> p kc e", p=128), wr, writes=[wr])
    brb = S.sb("brb", [128, NE], F32)
    S.dma("sp", brb[:, :], bc_view(I["l1_router_b"], NE), brb, writes=[brb])
    yin = S.sb("yin", [RB, RCH, 512], BF16)
    gg = [S.sb(f"gg{i}", [RB, 512], F32) for i in range(2)]
    xt_ = [S.sb(f"x6in{i}", [128, 1024], F32) for i in range(1)] * 2
    ytmp = [S.sb(f"y6{i}", [128, 1024], F32) for i in range(2)]
    xs = [S.sb(f"xs6{i}", [128, 1024], F32) for i in range(1)] * 2
    ss = [S.sb(f"ss6{i}", [128, 1], F32) for i in range(2)]
    rstd = [S.sb(f"rstd6{i}", [128, 1], F32) for i in range(2)]
    h2 = [S.sb(f"h6{i}", [128, 8, 128], BF16) for i in range(2)]
    h32 = [S.sb(f"h32{i}", [128, 8, 128], F32) for i in range(1)] * 2
    rt = [S.sb(f"rt{i}", [128, 48], F32) for i in range(2)]
    xsb = [S.sb(f"xsb{i}", [128, 1024], BF16) for i in range(2)] if XS is not None else None
    pg = R.pb[6]
    plog = S.ps("plog", 3584 + 16, 8)
    ntile = 0
    xg = [S.sb(f"xg{i}", [RB, 512], F32) for i in range(2)]
    work = [(seg, ch) for seg in range(1, 9) for ch in range(RCH)]
    wins = {1: load_win(S, R, hT1, 1, 0)}

    def front(n):
        seg, ch = work[n]
        is_ctx, L, w0, mb, ma = seg_info(seg)
        if ch == 0 and seg + 1 < 9:
            wins[seg + 1] = load_win(S, R, hT1, seg + 1, seg)
        rnn_front(S, R, wins[seg], L, ch, n, False, mb, ma)
        hw = wins[seg]
        for kc in range(8):
            mm(S, pg, pg[0:RB, :], R.win_g[:, kc, 96 * ch:96 * ch + RB], hw[:, kc, 2:514], kc == 0, kc == 7, reads=[R.win_g, hw])
        x_ = xg[n % 2]
        cp(S, "act", x_[:, :], pg[0:RB, :], reads=[pg], writes=[x_])

    front(0)
    for n, (seg, ch) in enumerate(work):
        b = seg - 1
        if True:
            init = [(hin, hin[:, d, b, ch:ch + 1]) for d in range(2)]
            x_ = xg[n % 2]
            g_ = gg[n % 2]
            act(S, g_[:, :], x_[:, :], AF.Square, reads=[x_], writes=[g_])
            ts(S, "dve", g_[:, :], g_[:, :], 0.044715, 1.0, ALU.mult, ALU.add, reads=[g_], writes=[g_])
            tt(S, "dve", g_[:, :], g_[:, :], x_[:, :], ALU.mult, reads=[g_, x_], writes=[g_])
            if n + 1 < len(work):
                front(n + 1)

            def gsig(g_=g_):
                act(S, g_[:, :], g_[:, :], AF.Sigmoid, reads=[g_], writes=[g_], scale=1.5957691216057308)
            rnn_back(S, C, R, 512, ch, n, init, None, R.hs, extra_sig=gsig)
            tt(S, "pool", g_[:, :], g_[:, :], x_[:, :], ALU.mult, reads=[g_, x_], writes=[g_])
            tt(S, "pool", R.hs[0][:, :], R.hs[0][:, :], R.hs[1][:, :], ALU.add, reads=[R.hs[0], R.hs[1]], writes=[R.hs[0]])
            tt(S, "pool", yin[:, ch, :], g_[:, :], R.hs[0][:, :], ALU.mult, reads=[g_, R.hs[0]], writes=[yin])
        if ch != RCH - 1:
            continue
        for t in range(4):
            mt = 4 * b + t
            ti = 2 + 1 + mt
            xt = xt_[ntile % 2]
            S.dma("sp", xt[:, :], x1[128 * ti:128 * ti + 128, :], xt, reads=[x1], writes=[xt])
            yt = ytmp[ntile % 2]
            for cb in range(2):
                py = R.pb[cb]
                for ch in range(RCH):
                    mm(S, py, py[:, :], yin[:, ch, 128 * t:128 * t + 128], R.wout[:, ch, 512 * cb:512 * cb + 512],
                       ch == 0, ch == RCH - 1, reads=[yin, R.wout])
                tt(S, "dve", yt[:, 512 * cb:512 * cb + 512], py[:, :], g1b[:, 512 * cb:512 * cb + 512], ALU.mult,
                   reads=[py, g1b], writes=[yt])
            tt(S, "pool", yt[:, :], yt[:, :], xt[:, :], ALU.add, reads=[yt, xt], writes=[yt])
            S.dma("sp", x2[128 * mt:128 * mt + 128, :], yt[:, :], yt, reads=[yt], writes=[x2])
            hb = h2[ntile % 2]
            hf = h32[ntile % 2]
            norm_tile(S, C, yt, ss[ntile % 2], rstd[ntile % 2], xs[ntile % 2], [R.pb[0], R.pb[1]],
                      (hb, lambda kc, hb=hb: hb[:, kc, :]), G2, S2,
                      hT32_dst=(hf, lambda kc, hf=hf: hf[:, kc, :]))
            if XS is None:
                S.dma("sp", h2T1[:, :, 128 * mt:128 * mt + 128].rearrange("kc p t -> p kc t"), hb[:, :, :], hb, reads=[hb], writes=[h2T1])
            else:
                xb_ = xsb[ntile % 2]
                cp(S, "pool", xb_[:, :], xs[ntile % 2][:, :], reads=[xs[ntile % 2]], writes=[xb_])
                S.dma("sp", XS[128 * mt:128 * mt + 128, :], xb_[:, :], xb_, reads=[xb_], writes=[XS])
            for kc in range(8):
                mm(S, plog, plog[:, :], hf[:, kc, :], wr[:, kc, :], kc == 0, kc == 7, reads=[hf, wr])
            r_ = rt[ntile % 2]
            lg, m8, ex, em, den, nv1 = r_[:, 0:8], r_[:, 8:16], r_[:, 16:24], r_[:, 24:32], r_[:, 32:33], r_[:, 33:34]
            tt(S, "dve", lg, plog[:, :], brb[:, :], ALU.add, reads=[plog, brb], writes=[r_])
            S.add("dve", lambda e, m8=m8, lg=lg: e.max(out=m8, in_=lg), reads=[r_], writes=[r_])
            ts(S, "dve", nv1, r_[:, 8:9], -1.0, None, ALU.mult, None, reads=[r_], writes=[r_])
            act(S, ex, lg, AF.Exp, reads=[r_], writes=[r_], bias=nv1)
            ts(S, "dve", em, lg, r_[:, 9:10], None, ALU.is_ge, None, reads=[r_], writes=[r_])
            if maskall is not None:
                cp(S, "dve", maskall[:, mt, :], em, reads=[r_], writes=[maskall])
            tt(S, "dve", em, em, ex, ALU.mult, reads=[r_], writes=[r_])
            S.add("dve", lambda e, den=den, em=em: e.reduce_sum(out=den, in_=em, axis=AX.X), reads=[r_], writes=[r_])
            S.add("dve", lambda e, den=den: e.reciprocal(out=den, in_=den), reads=[r_], writes=[r_])
            ts(S, "dve", comb[:, mt, :], em, den, None, ALU.mult, None, reads=[r_], writes=[comb])
            ntile += 1
    S.barrier()


def phase7_moe(S, C, I, mod_d, x2, h2T1, comb, out_d):
    m0 = S.off
    m5b = S.sb("m5b", [128, 1024], F32)
    S.dma("sp", m5b[:, :], bc_view(mod_d[1, 0, 5 * D:6 * D], D), m5b, reads=[mod_d], writes=[m5b])
    hT = S.sb("hTg", [128, 8, 2048], BF16)
    acc = [S.sb(f"acc{i}", [128, 1024], F32) for i in range(16)]
    NWB = 2
    w1g = [S.sb(f"w1g{i}", [128, 8, 512], BF16) for i in range(NWB)]
    w3g = [S.sb(f"w3g{i}", [128, 8, 512], BF16) for i in range(NWB)]
    w2g = [S.sb(f"w2g{i}", [128, 4, 1024], BF16) for i in range(NWB)]
    hid = [S.sb(f"hidm{i}", [128, 4, 512], BF16) for i in range(2)]
    sa = [S.sb(f"sam{i}", [128, 512], F32) for i in range(2)]
    xin = [S.sb(f"x7in{i}", [128, 1024], F32) for i in range(2)]
    pA = [S.ps(f"pA7{i}", 512 * i, 512) for i in range(2)]
    pB = [S.ps(f"pB7{i}", 1024 + 512 * i, 512) for i in range(2)]
    pY = [S.ps(f"pY7{i}", 2048 + 512 * i, 512) for i in range(4)]
    nw = 0
    nf = 0
    nh = 0
    nt = 0

    def load_w(e, fg, n):
        S.dma("pool", w1g[n % NWB][:, :, :], I["l1_moe_w1"][e, :, 512 * fg:512 * fg + 512].rearrange("(kc p) n -> p kc n", p=128),
              w1g[n % NWB], writes=[w1g[n % NWB]])
        S.dma("pool", w3g[n % NWB][:, :, :], I["l1_moe_w3"][e, :, 512 * fg:512 * fg + 512].rearrange("(kc p) n -> p kc n", p=128),
              w3g[n % NWB], writes=[w3g[n % NWB]])
        S.dma("pool", w2g[n % NWB][:, :, :], I["l1_moe_w2"][e, 512 * fg:512 * fg + 512, :].rearrange("(fc p) n -> p fc n", p=128),
              w2g[n % NWB], writes=[w2g[n % NWB]])

    steps = [(G, e, fg) for G in range(2) for e in range(NE) for fg in range(7)]
    load_w(steps[0][1], steps[0][2], 0)
    for si, (G, e, fg) in enumerate(steps):
        if e == 0 and fg == 0:
            S.dma("sp", hT[:, :, :], h2T1[:, :, 2048 * G:2048 * G + 2048].rearrange("kc p t -> p kc t"), hT, reads=[h2T1], writes=[hT])
        if si + 1 < len(steps):
            load_w(steps[si + 1][1], steps[si + 1][2], si + 1)
        a1, a3, a2 = w1g[si % NWB], w3g[si % NWB], w2g[si % NWB]
        first = (e == 0 and fg == 0)
        for tb in range(4):
            hd = hid[nh % 2]
            for fc in range(4):
                pa, pb = pA[nf % 2], pB[nf % 2]
                for kc in range(8):
                    mm(S, pa, pa[:, :], a1[:, kc, 128 * fc:128 * fc + 128], hT[:, kc, 512 * tb:512 * tb + 512], kc == 0, kc == 7,
                       reads=[a1, hT])
                for kc in range(8):
                    mm(S, pb, pb[:, :], a3[:, kc, 128 * fc:128 * fc + 128], hT[:, kc, 512 * tb:512 * tb + 512], kc == 0, kc == 7,
                       reads=[a3, hT])
                sb_ = sa[nf % 2]
                act(S, sb_[:, :], pa[:, :], AF.Silu, reads=[pa], writes=[sb_])
                tt(S, "dve", hd[:, fc, :], sb_[:, :], pb[:, :], ALU.mult, reads=[sb_, pb], writes=[hd])
                nf += 1
            for t in range(4):
                tl = 4 * tb + t
                mt = 16 * G + tl
                ac = acc[tl]
                for cb in range(2):
                    py = pY[(nt % 2) * 2 + cb]
                    for fc in range(4):
                        mm(S, py, py[:, :], hd[:, fc, 128 * t:128 * t + 128], a2[:, fc, 512 * cb:512 * cb + 512], fc == 0, fc == 3,
                           reads=[hd, a2])
                    sl = slice(512 * cb, 512 * cb + 512)
                    if first:
                        ts(S, "dve", ac[:, sl], py[:, :], comb[:, mt, e:e + 1], None, ALU.mult, None, reads=[py, comb], writes=[ac])
                    else:
                        stt(S, "dve", ac[:, sl], py[:, :], comb[:, mt, e:e + 1], ac[:, sl], ALU.mult, ALU.add,
                            reads=[py, comb, ac], writes=[ac])
                nt += 1
            nh += 1
        if e == NE - 1 and fg == 6:
            for tl in range(16):
                mt = 16 * G + tl
                xt = xin[tl % 2]
                S.dma("sp", xt[:, :], x2[128 * mt:128 * mt + 128, :], xt, reads=[x2], writes=[xt])
                ac = acc[tl]
                tt(S, "pool", ac[:, :], ac[:, :], m5b[:, :], ALU.mult, reads=[ac, m5b], writes=[ac])
                tt(S, "pool", xt[:, :], xt[:, :], ac[:, :], ALU.add, reads=[xt, ac], writes=[xt])
                S.dma("sp", out_d[128 * mt:128 * mt + 128, :], xt[:, :], xt, reads=[xt], writes=[out_d])
    S.barrier()
    S.off = m0


I32 = mybir.dt.int32
NGRP = 16
GSZ = 1024


class Route:
    pass


def phase_route(S, C, I, comb, maskall):
    Rt = Route()
    Rt.slotA_i = S.sb("slotA_i", [128, 32], I32)
    Rt.slotB_i = S.sb("slotB_i", [128, 32], I32)
    Rt.gA = S.sb("gA", [128, 32], F32)
    Rt.gB = S.sb("gB", [128, 32], F32)
    Rt.Eg_i = S.sb("Eg_i", [128, NGRP], I32)
    Rt.tokA = S.sb("tokA", [128, 32, 8], I32)
    Rt.tokB = S.sb("tokB", [128, 32, 8], I32)
    m0 = S.off
    rc = S.sb("rc", [128, 184], F32)
    S.dma("sp", rc[:, :], I["rconst"], rc, writes=[rc])
    Lb = S.sb("Lb", [128, 128], BF16)
    ob = S.sb("ob", [128, 128], BF16)
    mb = S.sb("mb", [128, 256], BF16)
    cp(S, "dve", Lb[:, :], rc[:, 0:128], reads=[rc], writes=[Lb])
    S.add("pool", lambda e: e.memset(ob[:, :], 1.0), writes=[ob])
    cp(S, "dve", mb[:, :], maskall[:, :, :].rearrange("p t e -> p (t e)"), reads=[maskall], writes=[mb])
    p_r = S.ps("p_rin", 0, 256)
    p_c = S.ps("p_cnt", 512, 256)
    mm(S, p_r, p_r[:, :], Lb[:, :], mb[:, :], True, True, reads=[Lb, mb])
    mm(S, p_c, p_c[:, :], ob[:, :], mb[:, :], True, True, reads=[ob, mb])
    rin = S.sb("rin", [128, 32, NE], F32)
    cnt = S.sb("cnt", [128, 32, NE], F32)
    cp(S, "dve", rin[:, :, :].rearrange("p t e -> p (t e)"), p_r[:, :], reads=[p_r], writes=[rin])
    cp(S, "dve", cnt[:, :, :].rearrange("p t e -> p (t e)"), p_c[:, :], reads=[p_c], writes=[cnt])
    ones32 = S.sb("ones32", [128, 32], F32)
    S.add("pool", lambda e: e.memset(ones32[:, :], 1.0), writes=[ones32])
    inc = S.sb("inc", [128, NE, 32], F32)
    for e_ in range(NE):
        S.add("dve", lambda e, e_=e_: e.tensor_tensor_scan(out=inc[:, e_, :], data0=ones32[:, :], data1=cnt[:, :, e_],
                                                           initial=0.0, op0=ALU.mult, op1=ALU.add),
              reads=[ones32, cnt], writes=[inc])
    pre = S.sb("pre", [128, NE, 32], F32)
    tt(S, "dve", pre[:, :, :], inc[:, :, :], cnt[:, :, :].rearrange("p t e -> p e t"), ALU.subtract, reads=[inc, cnt], writes=[pre])
    sm = S.sb("route_sm", [128, 64], F32)
    n_e, G_, gend, gstart, sbase, tmp8 = (sm[:, 0:8], sm[:, 8:16], sm[:, 16:24], sm[:, 24:32], sm[:, 32:40], sm[:, 40:48])
    cp(S, "dve", n_e, inc[:, :, 31], reads=[inc], writes=[sm])
    ts(S, "dve", G_, n_e, 0.0, None, ALU.is_gt, None, reads=[sm], writes=[sm])
    for k in (1, 2, 3):
        ts(S, "dve", tmp8, n_e, float(GSZ * k), None, ALU.is_gt, None, reads=[sm], writes=[sm])
        tt(S, "dve", G_, G_, tmp8, ALU.add, reads=[sm], writes=[sm])
    S.add("dve", lambda e: e.tensor_tensor_scan(out=gend, data0=ones32[:, 0:8], data1=G_, initial=0.0, op0=ALU.mult, op1=ALU.add),
          reads=[sm, ones32], writes=[sm])
    tt(S, "dve", gstart, gend, G_, ALU.subtract, reads=[sm], writes=[sm])
    ts(S, "dve", sbase, gstart, float(GSZ), None, ALU.mult, None, reads=[sm], writes=[sm])
    v = S.sb("route_v", [128, 32, NE], F32)
    tt(S, "dve", v[:, :, :], rin[:, :, :], pre[:, :, :].rearrange("p e t -> p t e"), ALU.add, reads=[rin, pre], writes=[v])
    sb_b = bass.AP(sbase.tensor, sbase.offset, [list(sbase.ap[0]), [0, 32], [1, NE]])
    tt(S, "dve", v[:, :, :], v[:, :, :], sb_b, ALU.add, reads=[v, sm], writes=[v])
    stt(S, "dve", v[:, :, :], v[:, :, :], 1.0, maskall[:, :, :], ALU.add, ALU.mult, reads=[v, maskall], writes=[v])
    m8 = S.sb("route_m8", [128, 32, NE], F32)
    for t_ in range(32):
        S.add("dve", lambda e, t_=t_: e.max(out=m8[:, t_, :], in_=v[:, t_, :]), reads=[v], writes=[m8])
    sf = S.sb("route_sf", [128, 2, 32], F32)
    oh = S.sb("route_oh", [128, 32, NE], F32)
    for which, (sl_i, g_) in enumerate(((Rt.slotA_i, Rt.gA), (Rt.slotB_i, Rt.gB))):
        top = m8[:, :, which]
        ts(S, "dve", sf[:, which, :], top, -1.0, None, ALU.add, None, reads=[m8], writes=[sf])
        cp(S, "dve", sl_i[:, :], sf[:, which, :], reads=[sf], writes=[sl_i])
        top_b = bass.AP(top.tensor, top.offset, [list(top.ap[0]), list(top.ap[1]), [0, NE]])
        tt(S, "dve", oh[:, :, :], v[:, :, :], top_b, ALU.is_equal, reads=[v, m8], writes=[oh])
        tt(S, "dve", oh[:, :, :], oh[:, :, :], comb[:, :, :], ALU.mult, reads=[oh, comb], writes=[oh])
        S.add("dve", lambda e, g_=g_: e.reduce_sum(out=g_[:, :], in_=oh[:, :, :], axis=AX.X), reads=[oh], writes=[g_])
    Eg = S.sb("Eg_f", [128, NGRP], F32)
    ind = S.sb("route_ind", [128, 2, NGRP], F32)
    gio = rc[:, 128:128 + NGRP]
    S.add("pool", lambda e: e.memset(Eg[:, :], 0.0), writes=[Eg])
    for e_ in range(1, NE):
        ts(S, "dve", ind[:, 0, :], gio, gstart[:, e_:e_ + 1], None, ALU.is_ge, None, reads=[rc, sm], writes=[ind])
        ts(S, "dve", ind[:, 1, :], gio, gend[:, e_:e_ + 1], None, ALU.is_lt, None, reads=[rc, sm], writes=[ind])
        tt(S, "dve", ind[:, 0, :], ind[:, 0, :], ind[:, 1, :], ALU.mult, reads=[ind], writes=[ind])
        stt(S, "dve", Eg[:, :], ind[:, 0, :], float(e_), Eg[:, :], ALU.mult, ALU.add, reads=[ind, Eg], writes=[Eg])
    cp(S, "dve", Rt.Eg_i[:, :], Eg[:, :], reads=[Eg], writes=[Rt.Eg_i])
    tk = rc[:, 152:184]
    tk_b = bass.AP(tk.tensor, tk.offset, [list(tk.ap[0]), list(tk.ap[1]), [0, 8]])
    tkf = S.sb("tkf", [128, 32, 8], F32)
    cp(S, "dve", tkf[:, :, :], tk_b, reads=[rc], writes=[tkf])
    cp(S, "dve", Rt.tokA[:, :, :], tkf[:, :, :], reads=[tkf], writes=[Rt.tokA])
    ts(S, "dve", tkf[:, :, :], tkf[:, :, :], 4096.0, None, ALU.add, None, reads=[tkf], writes=[tkf])
    cp(S, "dve", Rt.tokB[:, :, :], tkf[:, :, :], reads=[tkf], writes=[Rt.tokB])
    S.barrier()
    S.off = m0
    return Rt


def phase_permute(S, C, I, Rt, XS, Hslot, Tslot):
    m0 = S.off
    zer = S.sb("zer", [128, 8192], BF16)
    S.add("pool", lambda e: e.memset(zer[:, :], 0.0), writes=[zer])
    for i in range(NGRP):
        S.dma("sp", Hslot[GSZ * i:GSZ * i + GSZ, :].rearrange("(a p) n -> p a n", p=128),
              zer[:, :].rearrange("p (a n) -> p a n", a=8), zer, reads=[zer], writes=[Hslot])
    dump = S.sb("dumpi", [128, 1024], I32)
    S.add("pool", lambda e: e.memset(dump[:, :], 8192), writes=[dump])
    S.dma("sp", Tslot.t.rearrange("(p a) o -> p (a o)", p=128), dump[:, :], dump, reads=[dump], writes=[Tslot])
    for mt in range(32):
        for sl, tk_ in ((Rt.slotA_i, Rt.tokA), (Rt.slotB_i, Rt.tokB)):
            S.add("pool", lambda e, sl=sl, tk_=tk_, mt=mt: e.indirect_dma_start(
                out=Tslot[:, :], out_offset=bass.IndirectOffsetOnAxis(ap=sl[:, mt:mt + 1], axis=0),
                in_=tk_[:, mt, :], in_offset=None, bounds_check=None),
                reads=[tk_, sl, Tslot], writes=[Tslot], dma_buf=tk_)
    xt = [S.sb(f"xperm{i}", [128, 1024], BF16) for i in range(3)]
    for mt in range(32):
        x_ = xt[mt % 3]
        S.dma("sp", x_[:, :], XS[128 * mt:128 * mt + 128, :], x_, reads=[XS], writes=[x_])
        for sl in (Rt.slotA_i, Rt.slotB_i):
            S.add("pool", lambda e, x_=x_, sl=sl, mt=mt: e.indirect_dma_start(
                out=Hslot[:, :], out_offset=bass.IndirectOffsetOnAxis(ap=sl[:, mt:mt + 1], axis=0),
                in_=x_[:, :], in_offset=None, bounds_check=None),
                reads=[x_, sl, Hslot], writes=[Hslot], dma_buf=x_)
    S.barrier()
    S.off = m0


def phase7s_moe(S, C, I, mod_d, Rt, Hslot, Tslot, Yab):
    m0 = S.off
    mods = S.sb("mods7", [128, 16], F32)
    tmpm = S.sb("tmpm7", [128, 16], F32)
    S.dma("sp", tmpm[:, 0:8], pp_view(I["l1_norm2"]), tmpm, writes=[tmpm], allow_slow_non_contiguous=True)
    load_mod_pp(S, tmpm, 1, mod_d, 1, 0, 4)
    load_mod_pp(S, mods, 1, mod_d, 1, 0, 3)
    stt(S, "dve", mods[:, 0:8], tmpm[:, 8:16], 1.0, tmpm[:, 0:8], ALU.add, ALU.mult, reads=[tmpm], writes=[mods])
    hT = [S.sb(f"hTs{i}", [128, 8, GSZ], BF16) for i in range(2)]
    acc = [S.sb(f"accs{i}", [128, 1024], F32) for i in range(8)]
    NWB = 3
    w1g = [S.sb(f"w1s{i}", [128, 8, 512], BF16) for i in range(NWB)]
    w3g = [S.sb(f"w3s{i}", [128, 8, 512], BF16) for i in range(NWB)]
    w2g = [S.sb(f"w2s{i}", [128, 4, 1024], BF16) for i in range(NWB)]
    hid = [S.sb(f"hids{i}", [128, 4, 512], BF16) for i in range(2)]
    sa = [S.sb(f"sas{i}", [128, 512], F32) for i in range(2)]
    st_ = [S.sb(f"slt{i}", [128, 1024], BF16) for i in range(4)]
    tix = [S.sb(f"tix{i}", [128, 8], I32) for i in range(4)]
    pA = [S.ps(f"pA8{i}", 512 * i, 512) for i in range(2)]
    pB = [S.ps(f"pB8{i}", 1024 + 512 * i, 512) for i in range(2)]
    pY = [S.ps(f"pY8{i}", 2048 + 512 * i, 512) for i in range(4)]
    w1t, w3t, w2t = I["l1_moe_w1"], I["l1_moe_w3"], I["l1_moe_w2"]

    nreg = [0]

    def dyn_load(dst, static_ap, estride, g):
        off0 = static_ap.offset
        pat = [list(x) for x in static_ap.ap]
        tens = static_ap.tensor

        nreg[0] += 1
        rname = f"er{nreg[0]}"

        def allregs(e):
            hs = []
            try:
                while True:
                    nreg[0] += 1
                    hs.append(e.alloc_register(f"gc{nreg[0]}"))
            except ValueError:
                pass
            for h in hs:
                e.free_register(h)
            return hs

        def fn(e):
            before = allregs(e)
            with e.register(rname) as er:
                e.reg_load(er, Rt.Eg_i[0:1, g:g + 1])
                e.reg_mul(er, er, estride)
                e.reg_add(er, er, off0)
                ins = e.dma_start(out=dst[:, :, :], in_=bass.AP(tens, er, pat))
            after = {h.regnum for h in allregs(e)}
            for h in before:
                if h.regnum not in after:
                    e.free_register(h)
            return ins
        S.add("pool", fn, reads=[Rt.Eg_i], writes=[dst], dma_buf=dst)

    def load_w(g, fg, n):
        dyn_load(w1g[n % NWB], w1t[0, :, 512 * fg:512 * fg + 512].rearrange("(kc p) n -> p kc n", p=128), D * DFE, g)
        dyn_load(w3g[n % NWB], w3t[0, :, 512 * fg:512 * fg + 512].rearrange("(kc p) n -> p kc n", p=128), D * DFE, g)
        dyn_load(w2g[n % NWB], w2t[0, 512 * fg:512 * fg + 512, :].rearrange("(fc p) n -> p fc n", p=128), DFE * D, g)

    nst = [0]
    ntr = [0]

    def prologue(g):
        hb = hT[g % 2]
        for t in range(8):
            x_ = st_[nst[0] % 4]
            nst[0] += 1
            S.dma("sp", x_[:, :], Hslot[GSZ * g + 128 * t:GSZ * g + 128 * t + 128, :], x_, reads=[Hslot], writes=[x_])
            p = pA[ntr[0] % 2]
            ntr[0] += 1
            pv = p.t.bitcast(BF16)
            for kc in range(8):
                tr(S, p, pv[:, 128 * kc:128 * kc + 128], x_[:, 128 * kc:128 * kc + 128], C.identb[:, :], reads=[x_, C.identb])
            for kc in range(8):
                src = pv[:, 128 * kc:128 * kc + 128]
                if kc % 2 == 0:
                    ts(S, "dve", hb[:, kc, 128 * t:128 * t + 128], src, mods[:, kc:kc + 1], mods[:, 8 + kc:9 + kc], ALU.mult, ALU.add,
                       reads=[p, mods], writes=[hb])
                else:
                    act(S, hb[:, kc, 128 * t:128 * t + 128], src, AF.Identity, reads=[p, mods], writes=[hb],
                        bias=mods[:, 8 + kc:9 + kc], scale=mods[:, kc:kc + 1])

    steps = [(g, fg) for g in range(NGRP) for fg in range(7)]
    load_w(0, 0, 0)
    load_w(0, 1, 1)
    prologue(0)
    nf = 0
    nh = 0
    nt = 0
    for si, (g, fg) in enumerate(steps):
        if si + 2 < len(steps):
            load_w(steps[si + 2][0], steps[si + 2][1], si + 2)
        if fg == 6 and g + 1 < NGRP:
            prologue(g + 1)
        hb = hT[g % 2]
        a1, a3, a2 = w1g[si % NWB], w3g[si % NWB], w2g[si % NWB]
        for tb in range(2):
            hd = hid[nh % 2]
            for fc in range(4):
                pa, pb = pA[nf % 2], pB[nf % 2]
                for kc in range(8):
                    mm(S, pa, pa[:, :], a1[:, kc, 128 * fc:128 * fc + 128], hb[:, kc, 512 * tb:512 * tb + 512], kc == 0, kc == 7,
                       reads=[a1, hb])
                for kc in range(8):
                    mm(S, pb, pb[:, :], a3[:, kc, 128 * fc:128 * fc + 128], hb[:, kc, 512 * tb:512 * tb + 512], kc == 0, kc == 7,
                       reads=[a3, hb])
                sb_ = sa[nf % 2]
                act(S, sb_[:, :], pa[:, :], AF.Silu, reads=[pa], writes=[sb_])
                tt(S, "dve", hd[:, fc, :], sb_[:, :], pb[:, :], ALU.mult, reads=[sb_, pb], writes=[hd])
                nf += 1
            for t in range(4):
                tl = 4 * tb + t
                ac = acc[tl]
                for cb in range(2):
                    py = pY[(nt % 2) * 2 + cb]
                    for fc in range(4):
                        mm(S, py, py[:, :], hd[:, fc, 128 * t:128 * t + 128], a2[:, fc, 512 * cb:512 * cb + 512], fc == 0, fc == 3,
                           reads=[hd, a2])
                    sl = slice(512 * cb, 512 * cb + 512)
                    if fg == 0:
                        cp(S, "dve", ac[:, sl], py[:, :], reads=[py], writes=[ac])
                    else:
                        tt(S, "dve", ac[:, sl], py[:, :], ac[:, sl], ALU.add, reads=[py, ac], writes=[ac])
                if fg == 6:
                    tx = tix[(8 * g + tl) % 4]
                    S.dma("sp", tx[:, :], Tslot[GSZ * g + 128 * tl:GSZ * g + 128 * tl + 128, :], tx, reads=[Tslot], writes=[tx])
                    S.add("pool", lambda e, ac=ac, tx=tx: e.indirect_dma_start(
                        out=Yab[:, :], out_offset=bass.IndirectOffsetOnAxis(ap=tx[:, 0:1], axis=0),
                        in_=ac[:, :], in_offset=None, bounds_check=None),
                        reads=[ac, tx, Yab], writes=[Yab], dma_buf=ac)
                nt += 1
            nh += 1
    S.barrier()
    S.off = m0


def phase8_combine(S, C, I, mod_d, Rt, Yab, x2, out_d):
    m0 = S.off
    m5b = S.sb("m5bs", [128, 1024], F32)
    S.dma("sp", m5b[:, :], bc_view(mod_d[1, 0, 5 * D:6 * D], D), m5b, reads=[mod_d], writes=[m5b])
    ya = [S.sb(f"ya{i}", [128, 1024], F32) for i in range(2)]
    yb = [S.sb(f"yb{i}", [128, 1024], F32) for i in range(2)]
    xi = [S.sb(f"xc8{i}", [128, 1024], F32) for i in range(2)]
    for mt in range(32):
        a_, b_, x_ = ya[mt % 2], yb[mt % 2], xi[mt % 2]
        S.dma("sp", x_[:, :], x2[128 * mt:128 * mt + 128, :], x_, reads=[x2], writes=[x_])
        S.dma("sp", a_[:, :], Yab[128 * mt:128 * mt + 128, :], a_, reads=[Yab], writes=[a_])
        S.dma("sp", b_[:, :], Yab[4096 + 128 * mt:4096 + 128 * mt + 128, :], b_, reads=[Yab], writes=[b_])
        ts(S, "dve", a_[:, :], a_[:, :], Rt.gA[:, mt:mt + 1], None, ALU.mult, None, reads=[a_, Rt.gA], writes=[a_])
        stt(S, "dve", a_[:, :], b_[:, :], Rt.gB[:, mt:mt + 1], a_[:, :], ALU.mult, ALU.add, reads=[b_, Rt.gB, a_], writes=[a_])
        tt(S, "dve", a_[:, :], a_[:, :], m5b[:, :], ALU.mult, reads=[a_, m5b], writes=[a_])
        tt(S, "pool", x_[:, :], x_[:, :], a_[:, :], ALU.add, reads=[x_, a_], writes=[x_])
        S.dma("sp", out_d[128 * mt:128 * mt + 128, :], x_[:, :], x_, reads=[x_], writes=[out_d])
    S.barrier()
    S.off = m0


def declare_inputs(nc, names_shapes):
    I = {}
    for name, shape in names_shapes:
        I[name] = nc.dram_tensor(name, list(shape), F32, kind="ExternalInput").ap()
    return I


A_INPUTS = [
    ("xk", (NCH * 128, D)), ("cv", (2, D)), ("bt", (5, 128, 16, 5, 128)),
    ("l0_w_mod", (D, 6 * D)), ("l0_b_mod", (6 * D,)), ("l0_norm1", (D,)), ("l0_norm2", (D,)),
    ("l0_w_qkv", (D, 3 * D)), ("l0_q_gain", (HD,)), ("l0_k_gain", (HD,)), ("l0_w_o", (D, D)),
    ("l0_ffn_w1", (D, DFF)), ("l0_ffn_w3", (D, DFF)), ("l0_ffn_w2", (DFF, D)),
    ("l1_w_mod", (D, 6 * D)), ("l1_b_mod", (6 * D,)),
]


A_INPUTS2 = [
    ("l1_norm1", (D,)), ("l1_w_in", (D, 2 * DRNN)), ("l1_conv_w", (4, DRNN)), ("l1_conv_b", (DRNN,)),
    ("l1_gate_a_w", (2, RCH, RB, RB)), ("l1_gate_a_b", (2, DRNN)), ("l1_gate_x_w", (2, RCH, RB, RB)),
    ("l1_gate_x_b", (2, DRNN)), ("l1_lam", (2, DRNN)), ("masks", (128, 2)),
]
B_INPUTS = A_INPUTS2 + [
    ("x1", (NT1 * 128, D)), ("mod_in", (2, 2, 6 * D)), ("sall", (4 * RB, 576)), ("sown", (RB, 576)),
    ("sel", (RB, 8)), ("l1_norm2", (D,)), ("l1_w_out", (DRNN, D)), ("l1_router_w", (D, NE)), ("l1_router_b", (NE,)),
    ("l1_moe_w1", (NE, D, DFE)), ("l1_moe_w3", (NE, D, DFE)), ("l1_moe_w2", (NE, DFE, D)),
]


def build_A(debug=False):
    nc = bass.Bass("TRN2", target_bir_lowering=False)
    I = declare_inputs(nc, A_INPUTS + A_INPUTS2)
    S = Sched(nc)
    C = Common(S)
    mod_d = S.dram("mod_d", [2, 2, 6 * D], F32, kind="ExternalOutput")
    QT = S.dram("QT", [8, 128, NCH * 128], BF16)
    KT = S.dram("KT", [8, 128, NCH * 128], BF16)
    V = S.dram("V", [NCH * 128, D], BF16)
    x1a = S.dram("x1a", [NT1 * 128, D], F32, kind="ExternalOutput" if debug else "Internal")
    h2T = S.dram("h2T", [8, 128, NT1 * 128], BF16)
    x1 = S.dram("x1", [NT1 * 128, D], F32, kind="ExternalOutput")
    hT1 = S.dram("hT1", [8, 128, NT1 * 128], BF16)
    SAB = S.dram("sab_out", [RB, 576], F32, kind="ExternalOutput")
    phase0_adaln(S, C, I, mod_d)
    phase1_qkv(S, C, I, mod_d, QT, KT, V)
    phase2_attn(S, C, I, mod_d, QT, KT, V, x1a, h2T)
    phase3_ffn(S, C, I, mod_d, x1a, h2T, x1)
    phase4a_h1(S, C, I, mod_d, x1, hT1)
    phase4b_pass1(S, C, I, mod_d, hT1, SAB)
    S.emit()
    return nc


def build_B(debug=False):
    nc = bass.Bass("TRN2", target_bir_lowering=False)
    I = declare_inputs(nc, B_INPUTS)
    S = Sched(nc)
    C = Common(S)
    mod_d = Buf("mod_in", I["mod_in"])
    x1 = Buf("x1", I["x1"])
    SALL = Buf("sall", I["sall"])
    SOWN = Buf("sown", I["sown"])
    hT1 = S.dram("hT1", [8, 128, NT1 * 128], BF16)
    x2 = S.dram("x2", [32 * 128, D], F32, kind="ExternalOutput" if debug else "Internal")
    h2T1 = S.dram("h2T1", [8, 128, 32 * 128], BF16)
    out_d = S.dram("out", [32 * 128, D], F32, kind="ExternalOutput")
    hin = S.sb("hin", [RB, 2, 8, RCH], F32)
    comb = S.sb("comb", [128, 32, NE], F32)
    phase4a_h1(S, C, I, mod_d, x1, hT1)
    phase5_fold(S, C, I, SALL, SOWN, hin)
    m = S.off
    phase6_pass2(S, C, I, mod_d, x1, hT1, hin, x2, h2T1, comb)
    S.off = m
    phase7_moe(S, C, I, mod_d, x2, h2T1, comb, out_d)
    S.emit()
    return nc


F_INPUTS = A_INPUTS + A_INPUTS2 + [
    ("sel", (RB, 8)), ("l1_norm2", (D,)), ("l1_w_out", (DRNN, D)), ("l1_router_w", (D, NE)), ("l1_router_b", (NE,)),
    ("l1_moe_w1", (NE, D, DFE)), ("l1_moe_w3", (NE, D, DFE)), ("l1_moe_w2", (NE, DFE, D)), ("rconst", (128, 184)),
]


SPARSE = True


def build_fused():
    nc = bass.Bass("TRN2", target_bir_lowering=False)
    I = declare_inputs(nc, F_INPUTS)
    S = Sched(nc)
    C = Common(S)
    mod_d = S.dram("mod_d", [2, 2, 6 * D], F32)
    QT = S.dram("QT", [8, 128, NCH * 128], BF16)
    KT = S.dram("KT", [8, 128, NCH * 128], BF16)
    V = S.dram("V", [NCH * 128, D], BF16)
    x1a = S.dram("x1a", [NT1 * 128, D], F32)
    h2T = S.dram("h2T", [8, 128, NT1 * 128], BF16)
    x1 = S.dram("x1", [NT1 * 128, D], F32)
    hT1 = S.dram("hT1", [8, 128, NT1 * 128], BF16)
    SAB = S.dram("sab_b", [RB, 576], F32)
    SALL = S.dram("sall_g", [4 * RB, 576], F32)
    x2 = S.dram("x2", [32 * 128, D], F32)
    h2T1 = S.dram("h2T1", [8, 128, 32 * 128], BF16)
    out_d = S.dram("out", [32 * 128, D], F32, kind="ExternalOutput")
    phase0_adaln(S, C, I, mod_d)
    phase1_qkv(S, C, I, mod_d, QT, KT, V)
    phase2_attn(S, C, I, mod_d, QT, KT, V, x1a, h2T)
    phase3_ffn(S, C, I, mod_d, x1a, h2T, x1)
    phase4a_h1(S, C, I, mod_d, x1, hT1)
    phase4b_pass1(S, C, I, mod_d, hT1, SAB)
    cc = Buf("cc")
    S.add("pool", lambda e: e.collective_compute("AllGather", ALU.bypass, replica_groups=[[0, 1, 2, 3], [4, 5, 6, 7]],
                                                  ins=[SAB.t.opt()], outs=[SALL.t.opt()]),
          reads=[SAB], writes=[SALL], dma_buf=cc, inc=1)
    hin = S.sb("hin", [RB, 2, 8, RCH], F32)
    comb = S.sb("comb", [128, 32, NE], F32)
    phase5_fold(S, C, I, SALL, SAB, hin)
    if not SPARSE:
        m = S.off
        phase6_pass2(S, C, I, mod_d, x1, hT1, hin, x2, h2T1, comb)
        S.off = m
        phase7_moe(S, C, I, mod_d, x2, h2T1, comb, out_d)
    else:
        maskall = S.sb("maskall", [128, 32, NE], F32)
        XS = S.dram("XS", [32 * 128, D], BF16)
        Hslot = S.dram("Hslot", [NGRP * GSZ, D], BF16)
        Tslot = S.dram("Tslot", [NGRP * GSZ, 8], I32)
        Yab = S.dram("Yab", [8192 + 128, D], F32)
        m = S.off
        phase6_pass2(S, C, I, mod_d, x1, hT1, hin, x2, h2T1, comb, XS=XS, maskall=maskall)
        S.off = m
        Rt = phase_route(S, C, I, comb, maskall)
        phase_permute(S, C, I, Rt, XS, Hslot, Tslot)
        phase7s_moe(S, C, I, mod_d, Rt, Hslot, Tslot, Yab)
        phase8_combine(S, C, I, mod_d, Rt, Yab, x2, out_d)
    S.emit()
    return nc


def make_bias_tables(rpb, k):
    T0 = 32 * k
    out = np.empty((5, 128, 16, 5, 128), np.float32)
    p = np.arange(128)

    def table(gt, kts):
        qr = 2 * gt + p // 64
        qc = p % 64
        rs_ = np.clip(qr - 4, 0, 248)
        cs_ = np.clip(qc - 8, 0, 48)
        tab = np.full((128, 16, 5, 128), NEG, np.float32)
        for j, kt in enumerate(kts):
            if kt < 0 or kt > 127:
                continue
            kr = (2 * kt + p // 64)[:, None]
            kcol = (p % 64)[:, None]
            inwin = (kr >= rs_[None]) & (kr < rs_[None] + 8) & (kcol >= cs_[None]) & (kcol < cs_[None] + 16)
            dr = np.clip(kr - qr[None] + 7, 0, 14)
            dc = np.clip(kcol - qc[None] + 15, 0, 30)
            vals = rpb[:, dr, dc]
            tab[:, :, j, :] = np.where(inwin[:, None, :], vals.transpose(1, 0, 2), NEG)
        return tab

    def kts_for(lt):
        gt = T0 - 1 + lt
        kts = [gt - 2 + j for j in range(5)]
        if gt == 0:
            kts[0] = 3
        if gt == 127:
            kts[4] = 124
        return gt, kts

    out[0] = table(10, [8, 9, 10, 11, 12])
    for i, lt in enumerate((1, 2, 31, 32)):
        gt, kts = kts_for(lt)
        if gt < 0 or gt > 127:
            out[1 + i] = out[0]
        else:
            out[1 + i] = table(gt, kts)
    return np.ascontiguousarray(out.transpose(0, 4, 2, 3, 1))


def make_xk(x, ctx, b, k):
    T0 = 32 * k
    xk = np.zeros((NCH * 128, D), np.float32)
    xk[0:256] = ctx[b]
    for j in range(NKC):
        gt = T0 - 3 + j
        if k == 0 and j == 1:
            gt = 3
        if k == 3 and j == 36:
            gt = 124
        if 0 <= gt < 128:
            xk[256 + 128 * j:256 + 128 * j + 128] = x[b, 128 * gt:128 * gt + 128]
    return xk


_CACHE = {}


def _make_rconst():
    rc = np.zeros((128, 184), np.float32)
    p = np.arange(128)
    rc[:, 0:128] = (p[:, None] < p[None, :]).astype(np.float32)
    rc[:, 128:144] = np.arange(16, dtype=np.float32)[None, :]
    rc[:, 144:152] = np.arange(8, dtype=np.float32)[None, :]
    rc[:, 152:184] = (np.arange(32)[None, :] * 128 + p[:, None]).astype(np.float32)
    return rc


RCONST = _make_rconst()


def kernel(**inputs):
    inp = {k: np.ascontiguousarray(np.asarray(v, dtype=np.float32)) for k, v in inputs.items()}
    if "F" not in _CACHE:
        _CACHE["F"] = build_fused()
    nc = _CACHE["F"]
    n = 8
    maps = []
    for i in range(n):
        b, k = i // 4, i % 4
        sel = np.zeros((RB, 8), np.float32)
        for j in range(4):
            if j < k:
                sel[:, j] = 1.0
            if j > k:
                sel[:, 4 + j] = 1.0
        m = {"xk": make_xk(inp["x"], inp["ctx"], b, k),
             "cv": np.stack([inp["c"][b], inp["c_ctx"]]).astype(np.float32),
             "bt": make_bias_tables(inp["l0_rpb"], k),
             "masks": np.tile(np.array([[0.0 if k == 0 else 1.0, 0.0 if k == 3 else 1.0]], np.float32), (128, 1)),
             "sel": sel, "rconst": RCONST}
        for name, _ in F_INPUTS:
            if name not in m:
                m[name] = inp[name]
        maps.append(m)
    res = run_bass_kernel_spmd(nc, maps, core_ids=list(range(n)))
    out = np.empty((2, 16384, D), np.float32)
    for i in range(n):
        b, k = i // 4, i % 4
        out[b, 4096 * k:4096 * k + 4096] = np.asarray(res.results[i]["out"])
    return out
```

```python
import numpy as np
from contextlib import ExitStack
import concourse.bass as bass
import concourse.mybir as mybir
from concourse.bass_utils import run_bass_kernel_spmd

F32 = mybir.dt.float32
BF16 = mybir.dt.bfloat16
AF = mybir.ActivationFunctionType
ALU = mybir.AluOpType
AX = mybir.AxisListType

ENGS = ("pe", "act", "dve", "pool", "sp")


class Buf:
    __slots__ = ("name", "t", "last_w", "reads")

    def __init__(self, name, t=None):
        self.name = name
        self.t = t
        self.last_w = None
        self.reads = []

    def __getitem__(self, k):
        return self.t[k]


class Op:
    __slots__ = ("eng", "fn", "deps", "signal", "pos", "dma_key", "val", "is_dma", "inc")

    def __init__(self, eng, fn):
        self.eng = eng
        self.fn = fn
        self.deps = []
        self.signal = False
        self.pos = 0
        self.is_dma = False
        self.dma_key = None
        self.val = 0
        self.inc = 16


class Sched:
    ARENA_F32 = 53000

    def __init__(self, nc, same_engine_sync=True):
        self.nc = nc
        self.ops = {e: [] for e in ENGS}
        self.same = same_engine_sync
        self.waited = {e: {} for e in ENGS}
        self.dma_cnt = {}
        self.dma_keys = []
        self.slot_of = {}
        self.bar_pos = {}
        self.off = 0
        self.arena = None
        self.peak = 0
        self.psum = None
        self.ndram = 0

    def sb(self, name, shape, dtype, off=None):
        if self.arena is None:
            self.arena = self.nc.alloc_sbuf_tensor("arena", [128, self.ARENA_F32], F32)
        esz = 2 if dtype == BF16 else 4
        nel = int(np.prod(shape[1:]))
        nbytes = (nel * esz + 63) // 64 * 64
        if off is None:
            off = self.off
            self.off += nbytes
        assert off + nbytes <= self.ARENA_F32 * 4, (name, off, nbytes)
        self.peak = max(self.peak, off + nbytes)
        a = self.arena[0:shape[0], off // 4: off // 4 + nbytes // 4]
        if dtype != F32:
            a = a.bitcast(dtype)
        a = a[:, 0:nel]
        if len(shape) > 2:
            names = "abcdefg"[:len(shape) - 1]
            pat = "p (" + " ".join(names) + ") -> p " + " ".join(names)
            a = a.rearrange(pat, **{nm: shape[1 + i] for i, nm in enumerate(names[:-1])})
        return Buf(name, a)

    def ps(self, name, col, ncols, dtype=F32, parts=128):
        if self.psum is None:
            self.psum = self.nc.alloc_psum_tensor("psum_all", [128, 4096], F32).ap()
        a = self.psum[0:parts, col:col + ncols]
        if dtype != F32:
            a = a.bitcast(dtype)
        return Buf(name, a)

    def dram(self, name, shape, dtype, kind="Internal"):
        return Buf(name, self.nc.dram_tensor(name, list(shape), dtype, kind=kind).ap())

    def _need(self, op, prod):
        if prod is None or prod is op:
            return
        e = op.eng
        w = self.waited[e]
        if prod.is_dma:
            k = ("d", prod.dma_key)
            if w.get(k, 0) >= prod.val:
                return
            w[k] = prod.val
            op.deps.append(prod)
            return
        if prod.eng == e and (not self.same or e == "pe"):
            return
        if w.get(prod.eng, -1) >= prod.pos:
            return
        w[prod.eng] = prod.pos
        prod.signal = True
        op.deps.append(prod)

    def add(self, eng, fn, reads=(), writes=(), dma_buf=None, inc=16):
        op = Op(eng, fn)
        op.inc = inc
        op.pos = len(self.ops[eng])
        if dma_buf is not None:
            op.is_dma = True
            bid = id(dma_buf)
            if bid not in self.slot_of:
                slot = len(self.slot_of)
                self.slot_of[bid] = slot
                if slot >= len(self.dma_keys):
                    self.dma_keys.append(slot)
                    self.dma_cnt[slot] = 0
            key = self.slot_of[bid]
            self.dma_cnt[key] += inc
            op.dma_key = key
            op.val = self.dma_cnt[key]
        for b in reads:
            self._need(op, b.last_w)
        for b in writes:
            self._need(op, b.last_w)
            for r in b.reads:
                self._need(op, r)
        for b in reads:
            b.reads.append(op)
        for b in writes:
            b.last_w = op
            b.reads = []
        self.ops[eng].append(op)
        return op

    def dma(self, eng, out, in_, sbuf, reads=(), writes=(), **kw):
        return self.add(eng, lambda e: e.dma_start(out=out, in_=in_, **kw),
                        reads=reads, writes=writes, dma_buf=sbuf)

    def barrier(self):
        lasts = []
        for e in ENGS:
            for op in reversed(self.ops[e]):
                if not op.is_dma and op.fn is not None:
                    lasts.append(op)
                    break
        last_dma = {}
        for e in ENGS:
            for op in self.ops[e][self.bar_pos.get(e, 0):]:
                if op.is_dma:
                    last_dma[op.dma_key] = op
        for e in ENGS:
            op = Op(e, None)
            op.pos = len(self.ops[e])
            for p in lasts:
                if p.eng != e:
                    self._need(op, p)
            for p in last_dma.values():
                self._need(op, p)
            self.ops[e].append(op)
            self.bar_pos[e] = len(self.ops[e])
        self.slot_of = {}

    def emit(self):
        nc = self.nc
        with ExitStack() as st:
            esem = {e: st.enter_context(nc.semaphore(f"s_{e}")) for e in ENGS}
            dsem = {k: st.enter_context(nc.semaphore(f"d_{i}")) for i, k in enumerate(self.dma_keys)}
            for e in ENGS:
                c = 0
                for op in self.ops[e]:
                    if op.is_dma:
                        continue
                    if op.signal:
                        c += 1
                        op.val = c
            block = st.enter_context(nc.Block())

            def run(ename, eng):
                for op in self.ops[ename]:
                    for p in op.deps:
                        if p.is_dma:
                            eng.wait_ge(dsem[p.dma_key], p.val)
                        else:
                            eng.wait_ge(esem[p.eng], p.val)
                    if op.fn is None:
                        continue
                    ins = op.fn(eng)
                    if op.is_dma:
                        ins.then_inc(dsem[op.dma_key], op.inc)
                    elif op.signal:
                        ins.then_inc(esem[ename], 1)

            @block.tensor
            def _(eng):
                run("pe", eng)

            @block.scalar
            def _(eng):
                run("act", eng)

            @block.vector
            def _(eng):
                run("dve", eng)

            @block.gpsimd
            def _(eng):
                run("pool", eng)

            @block.sync
            def _(eng):
                run("sp", eng)


D = 1024
KC = 8
NH = 16
HD = 64
NQT = 34
NKC = 38
NCH = 40
NT1 = 36
DFF = 2816
NFC = 22
DRNN = 1536
RCH = 16
RB = 96
NE = 8
DFE = 3584
EPS = 1e-6
NEG = -30000.0


def mm(S, ps, out, lhsT, rhs, start, stop, reads):
    S.add("pe", lambda e: e.matmul(out, lhsT=lhsT, rhs=rhs, start=start, stop=stop), reads=reads, writes=[ps])


def tr(S, ps, out, in_, ident, reads):
    S.add("pe", lambda e: e.transpose(out=out, in_=in_, identity=ident), reads=reads, writes=[ps])


def act(S, out, in_, func, reads, writes, bias=None, scale=None, accum_out=None):
    kw = {}
    if bias is not None:
        kw["bias"] = bias
    if scale is not None:
        kw["scale"] = scale
    if accum_out is not None:
        kw["accum_out"] = accum_out
    S.add("act", lambda e: e.activation(out=out, in_=in_, func=func, **kw), reads=reads, writes=writes)


def ts(S, eng, out, in0, s1, s2, op0, op1, reads, writes):
    if op1 is None:
        S.add(eng, lambda e: e.tensor_scalar(out=out, in0=in0, scalar1=s1, scalar2=None, op0=op0), reads=reads, writes=writes)
    else:
        S.add(eng, lambda e: e.tensor_scalar(out=out, in0=in0, scalar1=s1, scalar2=s2, op0=op0, op1=op1), reads=reads, writes=writes)


def stt(S, eng, out, in0, scalar, in1, op0, op1, reads, writes):
    S.add(eng, lambda e: e.scalar_tensor_tensor(out=out, in0=in0, scalar=scalar, in1=in1, op0=op0, op1=op1),
          reads=reads, writes=writes)


def tt(S, eng, out, in0, in1, op, reads, writes):
    S.add(eng, lambda e: e.tensor_tensor(out=out, in0=in0, in1=in1, op=op), reads=reads, writes=writes)


def cp(S, eng, out, in_, reads, writes):
    if eng == "act":
        S.add("act", lambda e: e.copy(out=out, in_=in_), reads=reads, writes=writes)
    else:
        S.add(eng, lambda e: e.tensor_copy(out=out, in_=in_), reads=reads, writes=writes)


def pp_view(vec_ap):
    return vec_ap.rearrange("(c p) -> p c", p=128)


def bc_view(row_ap, n):
    return bass.AP(row_ap.tensor, row_ap.offset, [[0, 128], [1, n]])


class Common:
    def __init__(self, S):
        self.identf = S.sb("identf", [128, 128], F32)
        self.identb = S.sb("identb", [128, 128], BF16)
        self.junk = S.sb("junk", [128, 1024], BF16)
        self.epsb = S.sb("epsb", [128, 1], F32)
        S.add("pool", lambda e: e.memset(self.epsb[:, :], EPS), writes=[self.epsb])
        for b, in (self.identf,), (self.identb,):
            S.add("pool", lambda e, b=b: e.memset(b[:], 1.0), writes=[b])
            S.add("pool", lambda e, b=b: e.affine_select(out=b[:], in_=b[:], pattern=[[-1, 128]], compare_op=ALU.is_equal,
                                                         fill=0.0, base=0, channel_multiplier=1), reads=[b], writes=[b])


def norm_tile(S, C, xt, ss, rstd, xs, pst, hT_dst, G, Sft, reads_extra=(), hT32_dst=None):
    hbuf, hfn = hT_dst
    act(S, C.junk[:, :], xt[:, :], AF.Square, reads=[xt], writes=[C.junk, ss], accum_out=ss[:, 0:1])
    ts(S, "dve", rstd[:, 0:1], ss[:, 0:1], 1.0 / D, EPS, ALU.mult, ALU.add, reads=[ss], writes=[rstd])
    act(S, rstd[:, 0:1], rstd[:, 0:1], AF.Sqrt, reads=[rstd], writes=[rstd])
    S.add("dve", lambda e: e.reciprocal(out=rstd[:, 0:1], in_=rstd[:, 0:1]), reads=[rstd], writes=[rstd])
    act(S, xs[:, :], xt[:, :], AF.Identity, reads=[xt, rstd], writes=[xs], scale=rstd[:, 0:1])
    for half in range(2):
        p = pst[half]
        for q in range(4):
            kc = half * 4 + q
            tr(S, p, p[:, 128 * q:128 * q + 128], xs[:, 128 * kc:128 * kc + 128], C.identf[:, :], reads=[xs, C.identf])
        for q in range(4):
            kc = half * 4 + q
            src = p[:, 128 * q:128 * q + 128]
            if hT32_dst is not None:
                b32, f32fn = hT32_dst
                if kc % 2 == 0:
                    ts(S, "dve", f32fn(kc), src, G[:, kc:kc + 1], Sft[:, kc:kc + 1], ALU.mult, ALU.add,
                       reads=[p, G, Sft], writes=[b32])
                else:
                    act(S, f32fn(kc), src, AF.Identity, reads=[p, G, Sft], writes=[b32],
                        bias=Sft[:, kc:kc + 1], scale=G[:, kc:kc + 1])
                cp(S, "pool", hfn(kc), f32fn(kc), reads=[b32], writes=[hbuf])
            elif kc % 2 == 0:
                ts(S, "dve", hfn(kc), src, G[:, kc:kc + 1], Sft[:, kc:kc + 1], ALU.mult, ALU.add,
                   reads=[p, G, Sft], writes=[hbuf])
            else:
                act(S, hfn(kc), src, AF.Identity, reads=[p, G, Sft], writes=[hbuf],
                    bias=Sft[:, kc:kc + 1], scale=G[:, kc:kc + 1])


def load_mod_pp(S, dst, col, mod_d, layer, stream, which, tmp_ok=True):
    src = pp_view(mod_d[layer, stream, which * D:(which + 1) * D])
    S.dma("sp", dst[:, col * 8:col * 8 + 8], src, dst, reads=[mod_d], writes=[dst], allow_slow_non_contiguous=True)


def phase0_adaln(S, C, I, mod_d):
    m0 = S.off
    cT = S.sb("cT", [128, 8, 2], F32)
    sc = S.sb("sc", [128, 8, 2], F32)
    rep = S.sb("rep", [128, 16, 128], F32)
    wblk = [S.sb(f"wblk{i}", [128, 8, 512], F32) for i in range(2)]
    bblk = [S.sb(f"bblk{i}", [128, 512], F32) for i in range(2)]
    res = [S.sb(f"res{i}", [128, 512], F32) for i in range(4)]
    pss = [S.ps(f"p0ps{i}", 512 * i, 512) for i in range(4)]
    for s in range(2):
        S.dma("sp", cT[:, :, s], pp_view(I["cv"][s, :]), cT, writes=[cT], allow_slow_non_contiguous=True)
    act(S, sc[:, :, :], cT[:, :, :], AF.Silu, reads=[cT], writes=[sc])
    for kc in range(8):
        for s in range(2):
            cp(S, "dve", rep[:, kc * 2 + s, :], sc[:, kc, s:s + 1].to_broadcast([128, 128]), reads=[sc], writes=[rep])
    it = 0
    for l in range(2):
        wm = I[f"l{l}_w_mod"]
        bm = I[f"l{l}_b_mod"]
        for j in range(12):
            wb = wblk[it % 2]
            bb = bblk[it % 2]
            S.dma("sp", wb[:, :, :], wm[:, 512 * j:512 * j + 512].rearrange("(kc p) n -> p kc n", p=128), wb, writes=[wb])
            S.dma("sp", bb[:, :], bc_view(bm[512 * j:512 * j + 512], 512), bb, writes=[bb])
            for s in range(2):
                p = pss[(it % 2) * 2 + s]
                r = res[(it % 2) * 2 + s]
                for kc in range(8):
                    mm(S, p, p[:, :], rep[:, kc * 2 + s, :], wb[:, kc, :], kc == 0, kc == 7, reads=[rep, wb])
                tt(S, "dve", r[:, :], p[:, :], bb[:, :], ALU.add, reads=[p, bb], writes=[r])
                S.dma("sp", mod_d[l, s:s + 1, 512 * j:512 * j + 512], r[0:1, :], r, reads=[r], writes=[mod_d])
            it += 1
    S.barrier()
    S.off = m0


def phase1_qkv(S, C, I, mod_d, QT, KT, V):
    m0 = S.off
    wq = S.sb("wqkv", [128, 8, 3072], BF16)
    S.dma("pool", wq[:, :, :], I["l0_w_qkv"].rearrange("(kc p) n -> p kc n", p=128), wq, writes=[wq])
    mods = S.sb("mods1", [128, 32], F32)
    tmpm = S.sb("tmpm1", [128, 24], F32)
    S.dma("sp", tmpm[:, 0:8], pp_view(I["l0_norm1"]), tmpm, writes=[tmpm], allow_slow_non_contiguous=True)
    for s in range(2):
        load_mod_pp(S, tmpm, 1 + s, mod_d, 0, s, 1)
        load_mod_pp(S, mods, 2 * s + 1, mod_d, 0, s, 0)
        stt(S, "dve", mods[:, 16 * s:16 * s + 8], tmpm[:, 8 + 8 * s:16 + 8 * s], 1.0, tmpm[:, 0:8], ALU.add, ALU.mult,
            reads=[tmpm], writes=[mods])
    Gs = [Buf("G", mods[:, 0:8]), Buf("Gc", mods[:, 16:24])]
    Ss = [Buf("S", mods[:, 8:16]), Buf("Sc", mods[:, 24:32])]
    gains = S.sb("gains", [128, 2], F32)
    for half in range(2):
        S.dma("sp", gains[64 * half:64 * half + 64, 0:1], I["l0_q_gain"].rearrange("(p o) -> p o", o=1), gains, writes=[gains])
        S.dma("sp", gains[64 * half:64 * half + 64, 1:2], I["l0_k_gain"].rearrange("(p o) -> p o", o=1), gains, writes=[gains])
    ts(S, "dve", gains[:, 0:1], gains[:, 0:1], HD ** -0.5, None, ALU.mult, None, reads=[gains], writes=[gains])
    bd = S.sb("bd", [128, 128], BF16)
    S.add("pool", lambda e: e.memset(bd[:, :], 0.0), writes=[bd])
    S.add("pool", lambda e: e.memset(bd[0:64, 0:64], 1.0 / 64), reads=[bd], writes=[bd])
    S.add("pool", lambda e: e.memset(bd[64:128, 64:128], 1.0 / 64), reads=[bd], writes=[bd])
    xin = [S.sb(f"xin{i}", [128, 1024], F32) for i in range(3)]
    xs = [S.sb(f"xs{i}", [128, 1024], F32) for i in range(2)]
    ss = [S.sb(f"ss{i}", [128, 1], F32) for i in range(2)]
    rstd = [S.sb(f"rstd{i}", [128, 1], F32) for i in range(2)]
    hT = [S.sb(f"hT{i}", [128, 8, 512], BF16) for i in range(2)]
    sq = [S.sb(f"sq{i}", [128, 512], BF16) for i in range(2)]
    rs = [S.sb(f"rs{i}", [128, 512], F32) for i in range(2)]
    qn = [S.sb(f"qn{i}", [128, 512], BF16) for i in range(3)]
    vt = [S.sb(f"vt{i}", [128, 1024], BF16) for i in range(2)]
    pT = [S.ps(f"pT{i}", 512 * i, 512) for i in range(2)]
    pQ = [S.ps(f"pQ{i}", 1024 + 512 * i, 512) for i in range(2)]
    pR = [S.ps(f"pR{i}", 2048 + 512 * i, 512) for i in range(2)]
    pV = [S.ps(f"pV{i}", 3072 + 512 * i, 512) for i in range(2)]
    nt = 0
    nqk = 0
    for blk in range(NCH // 4):
        hb = hT[blk % 2]
        for t in range(4):
            g = blk * 4 + t
            s = 0 if g >= 2 else 1
            xt = xin[nt % 3]
            S.dma("sp", xt[:, :], I["xk"][128 * g:128 * g + 128, :], xt, writes=[xt])
            norm_tile(S, C, xt, ss[nt % 2], rstd[nt % 2], xs[nt % 2], pT,
                      (hb, lambda kc, hb=hb, t=t: hb[:, kc, 128 * t:128 * t + 128]), Gs[s], Ss[s])
            nt += 1
        for which, dst_d in ((0, QT), (1, KT)):
            for hp in range(8):
                p = pQ[nqk % 2]
                pr = pR[nqk % 2]
                col = which * 1024 + 128 * hp
                for kc in range(8):
                    mm(S, p, p[:, :], wq[:, kc, col:col + 128], hb[:, kc, :], kc == 0, kc == 7, reads=[wq, hb])
                sqb = sq[nqk % 2]
                act(S, sqb[:, :], p[:, :], AF.Square, reads=[p], writes=[sqb])
                mm(S, pr, pr[:, :], bd[:, :], sqb[:, :], True, True, reads=[bd, sqb])
                rsb = rs[nqk % 2]
                act(S, rsb[:, :], pr[:, :], AF.Sqrt, reads=[pr, C.epsb], writes=[rsb], bias=C.epsb[:, 0:1])
                S.add("dve", lambda e, rsb=rsb: e.reciprocal(out=rsb[:, :], in_=rsb[:, :]), reads=[rsb], writes=[rsb])
                qb = qn[nqk % 3]
                stt(S, "dve", qb[:, :], p[:, :], gains[:, which:which + 1], rsb[:, :], ALU.mult, ALU.mult,
                    reads=[p, gains, rsb], writes=[qb])
                S.dma("sp", dst_d[hp, :, 512 * blk:512 * blk + 512], qb[:, :], qb, reads=[qb], writes=[dst_d])
                nqk += 1
        for t in range(4):
            g = blk * 4 + t
            vb = vt[g % 2]
            for cb in range(2):
                p = pV[cb]
                for kc in range(8):
                    mm(S, p, p[:, :], hb[:, kc, 128 * t:128 * t + 128], wq[:, kc, 2048 + 512 * cb:2048 + 512 * cb + 512],
                       kc == 0, kc == 7, reads=[hb, wq])
                cp(S, "act", vb[:, 512 * cb:512 * cb + 512], p[:, :], reads=[p], writes=[vb])
            S.dma("sp", V[128 * g:128 * g + 128, :], vb[:, :], vb, reads=[vb], writes=[V])
    S.barrier()
    S.off = m0


def phase2_attn(S, C, I, mod_d, QT, KT, V, x1a, h2T):
    m0 = S.off
    wo = S.sb("wo", [128, 8, 1024], BF16)
    S.dma("pool", wo[:, :, :], I["l0_w_o"].rearrange("(hp p) n -> p hp n", p=128), wo, writes=[wo])
    bgen = S.sb("bgen", [128, 16, 5, 128], BF16)
    bspec = S.sb("bspec", [128, 16, 5, 128], BF16)
    S.dma("pool", bgen[:, :, :, :], I["bt"][0], bgen, writes=[bgen])
    mods = S.sb("mods2", [128, 32], F32)
    tmpm = S.sb("tmpm2", [128, 24], F32)
    S.dma("sp", tmpm[:, 0:8], pp_view(I["l0_norm2"]), tmpm, writes=[tmpm], allow_slow_non_contiguous=True)
    g1b = []
    for s in range(2):
        load_mod_pp(S, tmpm, 1 + s, mod_d, 0, s, 4)
        load_mod_pp(S, mods, 2 * s + 1, mod_d, 0, s, 3)
        stt(S, "dve", mods[:, 16 * s:16 * s + 8], tmpm[:, 8 + 8 * s:16 + 8 * s], 1.0, tmpm[:, 0:8], ALU.add, ALU.mult,
            reads=[tmpm], writes=[mods])
        gb = S.sb(f"g1b{s}", [128, 1024], F32)
        S.dma("sp", gb[:, :], bc_view(mod_d[0, s, 2 * D:3 * D], D), gb, reads=[mod_d], writes=[gb])
        g1b.append(gb)
    Gs = [Buf("G2", mods[:, 0:8]), Buf("G2c", mods[:, 16:24])]
    Ss = [Buf("S2", mods[:, 8:16]), Buf("S2c", mods[:, 24:32])]
    RING = 6
    ktr = S.sb("ktr", [128, 8, RING, 128], BF16)
    vr = S.sb("vr", [128, RING, 16, 65], BF16)
    ktc = S.sb("ktc", [128, 8, 256], BF16)
    vc = S.sb("vc", [128, 2, 16, 65], BF16)
    kslots = [Buf(f"ks{i}", None) for i in range(RING)]
    vslots = [Buf(f"vs{i}", None) for i in range(RING)]
    S.add("pool", lambda e: e.memset(vr[:, :, :, 64:65], 1.0), writes=vslots)
    S.add("pool", lambda e: e.memset(vc[:, :, :, 64:65], 1.0), writes=[vc])
    S.dma("sp", ktc[:, :, :], KT[:, :, 0:256].rearrange("hp p t -> p hp t"), ktc, reads=[KT], writes=[ktc])
    for cc in range(2):
        S.dma("sp", vc[:, cc, :, 0:64], V[128 * cc:128 * cc + 128, :].rearrange("p (h d) -> p h d", h=16), vc,
              reads=[V], writes=[vc])
    qt = [S.sb(f"qt{i}", [128, 8, 128], BF16) for i in range(2)]
    xt_ = [S.sb(f"x2in{i}", [128, 1024], F32) for i in range(2)]
    tb = [S.sb(f"tb{i}", [128, 5, 128], F32) for i in range(2)]
    pt = [S.sb(f"pt{i}", [128, 7, 128], BF16) for i in range(2)]
    rec = [S.sb(f"rec{i}", [128, 1], F32) for i in range(4)]
    on = [S.sb(f"on{i}", [128, 16, 64], BF16) for i in range(2)]
    oT = [S.sb(f"oT{i}", [128, 8, 128], BF16) for i in range(2)]
    x1t = [S.sb(f"x1t{i}", [128, 1024], F32) for i in range(2)]
    xs = [S.sb(f"xs2{i}", [128, 1024], F32) for i in range(2)]
    ss = [S.sb(f"ss2{i}", [128, 1], F32) for i in range(2)]
    rstd = [S.sb(f"rstd2{i}", [128, 1], F32) for i in range(2)]
    h2 = [S.sb(f"h2t{i}", [128, 8, 128], BF16) for i in range(2)]
    pS = [S.ps(f"pS{i}", 1024 * i, 896) for i in range(2)]
    pO = [S.ps(f"pO{i}", 2048 + 128 * i, 65) for i in range(4)]
    pY = [S.ps(f"pY{i}", 2560 + 512 * i, 512) for i in range(2)]
    pOT = S.ps("pOT", 3584, 512, BF16)

    loaded = set()

    def ensure_chunk(g):
        if g in loaded:
            return
        loaded.add(g)
        sl = g % RING
        S.dma("sp", ktr[:, :, sl, :], KT[:, :, 128 * g:128 * g + 128].rearrange("hp p t -> p hp t"), kslots[sl],
              reads=[KT], writes=[kslots[sl]])
        S.dma("sp", vr[:, sl, :, 0:64], V[128 * g:128 * g + 128, :].rearrange("p (h d) -> p h d", h=16), vslots[sl],
              reads=[V], writes=[vslots[sl]])

    spec_lts = {1: 1, 2: 2, 31: 3, 32: 4}
    nh = 0
    for ti in range(NT1):
        is_ctx = ti < 2
        lt = ti - 2
        g = ti if is_ctx else lt + 4
        s = 1 if is_ctx else 0
        q = qt[ti % 2]
        S.dma("sp", q[:, :, :], QT[:, :, 128 * g:128 * g + 128].rearrange("hp p t -> p hp t"), q, reads=[QT], writes=[q])
        xt = xt_[ti % 2]
        S.dma("sp", xt[:, :], I["xk"][128 * g:128 * g + 128, :], xt, writes=[xt])
        nloc = 0 if is_ctx else 5
        if not is_ctx:
            for j in range(5):
                ensure_chunk(lt + 2 + j)
            if lt in spec_lts:
                S.dma("pool", bspec[:, :, :, :], I["bt"][spec_lts[lt]], bspec, writes=[bspec])
                bias = bspec
            else:
                bias = bgen
        onb = on[ti % 2]
        for h in range(NH):
            hp, half = h // 2, h % 2
            lo = 64 * half
            p = pS[nh % 2]
            ptb = pt[nh % 2]
            for j in range(nloc):
                sl = (lt + 2 + j) % RING
                mm(S, p, p[:, 128 * j:128 * j + 128], ktr[lo:lo + 64, hp, sl, :], q[lo:lo + 64, hp, :], True, True,
                   reads=[kslots[sl], q])
            for cc in range(2):
                jj = nloc + cc
                mm(S, p, p[:, 128 * jj:128 * jj + 128], ktc[lo:lo + 64, hp, 128 * cc:128 * cc + 128], q[lo:lo + 64, hp, :],
                   True, True, reads=[ktc, q])
            if nloc:
                t_ = tb[nh % 2]
                tt(S, "dve", t_[:, :, :], p[:, 0:640].rearrange("p (j q) -> p j q", j=5), bias[:, h, :, :], ALU.add,
                   reads=[p, bias], writes=[t_])
                act(S, ptb[:, 0:5, :], t_[:, :, :], AF.Exp, reads=[t_], writes=[ptb])
            act(S, ptb[:, nloc:nloc + 2, :], p[:, 128 * nloc:128 * nloc + 256].rearrange("p (j q) -> p j q", j=2), AF.Exp,
                reads=[p], writes=[ptb])
            po = pO[nh % 4]
            n = nloc + 2
            for j in range(nloc):
                sl = (lt + 2 + j) % RING
                mm(S, po, po[:, :], ptb[:, j, :], vr[:, sl, h, :], j == 0, False, reads=[ptb, vslots[sl]])
            for cc in range(2):
                mm(S, po, po[:, :], ptb[:, nloc + cc, :], vc[:, cc, h, :], (nloc + cc) == 0, cc == 1, reads=[ptb, vc])
            r_ = rec[nh % 4]
            S.add("dve", lambda e, r_=r_, po=po: e.reciprocal(out=r_[:, 0:1], in_=po[:, 64:65]), reads=[po], writes=[r_])
            ts(S, "dve", onb[:, h, :], po[:, 0:64], r_[:, 0:1], None, ALU.mult, None, reads=[po, r_], writes=[onb])
            nh += 1
        otb = oT[ti % 2]
        for hp in range(8):
            tr(S, pOT, pOT[:, 128 * hp:128 * hp + 128], onb[:, 2 * hp:2 * hp + 2, :].rearrange("p a b -> p (a b)"),
               C.identb[:, :], reads=[onb, C.identb])
        cp(S, "act", otb[:, 0:4, :], pOT[:, 0:512].rearrange("p (a b) -> p a b", a=4), reads=[pOT], writes=[otb])
        cp(S, "dve", otb[:, 4:8, :], pOT[:, 512:1024].rearrange("p (a b) -> p a b", a=4), reads=[pOT], writes=[otb])
        x1 = x1t[ti % 2]
        for cb in range(2):
            py = pY[cb]
            for hp in range(8):
                mm(S, py, py[:, :], otb[:, hp, :], wo[:, hp, 512 * cb:512 * cb + 512], hp == 0, hp == 7, reads=[otb, wo])
            tt(S, "dve", x1[:, 512 * cb:512 * cb + 512], py[:, :], g1b[s][:, 512 * cb:512 * cb + 512], ALU.mult,
               reads=[py, g1b[s]], writes=[x1])
        tt(S, "pool", x1[:, :], x1[:, :], xt[:, :], ALU.add, reads=[x1, xt], writes=[x1])
        S.dma("sp", x1a[128 * ti:128 * ti + 128, :], x1[:, :], x1, reads=[x1], writes=[x1a])
        hb = h2[ti % 2]
        norm_tile(S, C, x1, ss[ti % 2], rstd[ti % 2], xs[ti % 2], pY,
                  (hb, lambda kc, hb=hb: hb[:, kc, :]), Gs[s], Ss[s])
        S.dma("sp", h2T[:, :, 128 * ti:128 * ti + 128].rearrange("kc p t -> p kc t"), hb[:, :, :], hb, reads=[hb], writes=[h2T])
    S.barrier()
    S.off = m0


def phase3_ffn(S, C, I, mod_d, x1a, h2T, x1):
    m0 = S.off
    w1 = S.sb("w1", [128, 8, DFF], BF16)
    w3 = S.sb("w3", [128, 8, DFF], BF16)
    w2 = S.sb("w2", [128, NFC, 1024], BF16)
    for kc in range(8):
        S.dma("pool", w1[:, kc, :], I["l0_ffn_w1"][128 * kc:128 * kc + 128, :], w1, writes=[w1])
        S.dma("pool", w3[:, kc, :], I["l0_ffn_w3"][128 * kc:128 * kc + 128, :], w3, writes=[w3])
    for fc in range(NFC):
        S.dma("pool", w2[:, fc, :], I["l0_ffn_w2"][128 * fc:128 * fc + 128, :], w2, writes=[w2])
    g2b = []
    for s in range(2):
        gb = S.sb(f"g2b{s}", [128, 1024], F32)
        S.dma("sp", gb[:, :], bc_view(mod_d[0, s, 5 * D:6 * D], D), gb, reads=[mod_d], writes=[gb])
        g2b.append(gb)
    hb_ = [S.sb(f"h3b{i}", [128, 8, 512], BF16) for i in range(2)]
    hid = S.sb("hid", [128, NFC, 512], BF16)
    sa = [S.sb(f"sa{i}", [128, 512], F32) for i in range(2)]
    xa = [S.sb(f"xa{i}", [128, 1024], F32) for i in range(2)]
    xo = [S.sb(f"xo{i}", [128, 1024], F32) for i in range(2)]
    pA = [S.ps(f"pA{i}", 512 * i, 512) for i in range(2)]
    pB = [S.ps(f"pB{i}", 1024 + 512 * i, 512) for i in range(2)]
    pY = [S.ps(f"pY3{i}", 2048 + 512 * i, 512) for i in range(4)]
    nf = 0
    nt = 0
    for blk in range(NT1 // 4):
        hb = hb_[blk % 2]
        S.dma("sp", hb[:, :, :], h2T[:, :, 512 * blk:512 * blk + 512].rearrange("kc p t -> p kc t"), hb, reads=[h2T], writes=[hb])
        for fc in range(NFC):
            pa, pb = pA[nf % 2], pB[nf % 2]
            for kc in range(8):
                mm(S, pa, pa[:, :], w1[:, kc, 128 * fc:128 * fc + 128], hb[:, kc, :], kc == 0, kc == 7, reads=[w1, hb])
            for kc in range(8):
                mm(S, pb, pb[:, :], w3[:, kc, 128 * fc:128 * fc + 128], hb[:, kc, :], kc == 0, kc == 7, reads=[w3, hb])
            sb_ = sa[nf % 2]
            act(S, sb_[:, :], pa[:, :], AF.Silu, reads=[pa], writes=[sb_])
            tt(S, "dve", hid[:, fc, :], sb_[:, :], pb[:, :], ALU.mult, reads=[sb_, pb], writes=[hid])
            nf += 1
        for t in range(4):
            ti = blk * 4 + t
            s = 1 if ti < 2 else 0
            xab = xa[nt % 2]
            S.dma("sp", xab[:, :], x1a[128 * ti:128 * ti + 128, :], xab, reads=[x1a], writes=[xab])
            xob = xo[nt % 2]
            for cb in range(2):
                py = pY[(nt % 2) * 2 + cb]
                for fc in range(NFC):
                    mm(S, py, py[:, :], hid[:, fc, 128 * t:128 * t + 128], w2[:, fc, 512 * cb:512 * cb + 512],
                       fc == 0, fc == NFC - 1, reads=[hid, w2])
                tt(S, "dve", xob[:, 512 * cb:512 * cb + 512], py[:, :], g2b[s][:, 512 * cb:512 * cb + 512], ALU.mult,
                   reads=[py, g2b[s]], writes=[xob])
            tt(S, "pool", xob[:, :], xob[:, :], xab[:, :], ALU.add, reads=[xob, xab], writes=[xob])
            S.dma("sp", x1[128 * ti:128 * ti + 128, :], xob[:, :], xob, reads=[xob], writes=[x1])
            nt += 1
    S.barrier()
    S.off = m0


class RnnConsts:
    pass


def rnn_setup(S, C, I, mod_d, pass2):
    R = RnnConsts()
    R.win_x = S.sb("win_x", [128, 8, DRNN], BF16)
    S.dma("pool", R.win_x[:, :, :], I["l1_w_in"][:, DRNN:2 * DRNN].rearrange("(kc p) n -> p kc n", p=128), R.win_x,
          writes=[R.win_x])
    if pass2:
        R.win_g = S.sb("win_g", [128, 8, DRNN], BF16)
        S.dma("pool", R.win_g[:, :, :], I["l1_w_in"][:, 0:DRNN].rearrange("(kc p) n -> p kc n", p=128), R.win_g,
              writes=[R.win_g])
        R.wout = S.sb("wout", [RB, RCH, D], BF16)
        S.dma("pool", R.wout[:, :, :], I["l1_w_out"].rearrange("(ch p) n -> p ch n", p=RB), R.wout, writes=[R.wout])
    R.ga = S.sb("ga", [RB, 2, RCH, RB], BF16)
    R.gx = S.sb("gx", [RB, 2, RCH, RB], BF16)
    for d in range(2):
        S.dma("pool", R.ga[:, d, :, :], I["l1_gate_a_w"][d].rearrange("k c o -> c k o"), R.ga, writes=[R.ga])
        S.dma("pool", R.gx[:, d, :, :], I["l1_gate_x_w"][d].rearrange("k c o -> c k o"), R.gx, writes=[R.gx])
    R.cw = S.sb("cw", [RB, 5, RCH], F32)
    for j in range(4):
        S.dma("sp", R.cw[:, j, :], I["l1_conv_w"][j].rearrange("(ch p) -> p ch", p=RB), R.cw, writes=[R.cw],
              allow_slow_non_contiguous=True)
    S.dma("sp", R.cw[:, 4, :], I["l1_conv_b"].rearrange("(ch p) -> p ch", p=RB), R.cw, writes=[R.cw],
          allow_slow_non_contiguous=True)
    R.gb = S.sb("gb", [RB, 4, RCH], F32)
    R.c1 = S.sb("c1", [RB, 2, RCH], F32)
    lam = S.sb("lamt", [RB, 2 * RCH], F32)
    for d in range(2):
        S.dma("sp", R.gb[:, d, :], I["l1_gate_a_b"][d].rearrange("(ch p) -> p ch", p=RB), R.gb, writes=[R.gb],
              allow_slow_non_contiguous=True)
        S.dma("sp", R.gb[:, 2 + d, :], I["l1_gate_x_b"][d].rearrange("(ch p) -> p ch", p=RB), R.gb, writes=[R.gb],
              allow_slow_non_contiguous=True)
        S.dma("sp", lam[:, RCH * d:RCH * d + RCH], I["l1_lam"][d].rearrange("(ch p) -> p ch", p=RB), lam, writes=[lam],
              allow_slow_non_contiguous=True)
    n = 2 * RCH
    t = S.sb("sp_t", [RB, n], F32)
    w = S.sb("sp_w", [RB, n], F32)
    w2 = S.sb("sp_w2", [RB, n], F32)
    pl = S.sb("sp_pl", [RB, n], F32)
    s2 = S.sb("sp_s2", [RB, n], F32)
    mk = S.sb("sp_mk", [RB, n], F32)
    act(S, t[:, :], lam[:, :], AF.Exp, reads=[lam], writes=[t], scale=-1.0)
    ts(S, "dve", w[:, :], t[:, :], 2.0, None, ALU.add, None, reads=[t], writes=[w])
    S.add("dve", lambda e: e.reciprocal(out=w[:, :], in_=w[:, :]), reads=[w], writes=[w])
    tt(S, "dve", w[:, :], w[:, :], t[:, :], ALU.mult, reads=[w, t], writes=[w])
    tt(S, "dve", w2[:, :], w[:, :], w[:, :], ALU.mult, reads=[w], writes=[w2])
    ts(S, "dve", pl[:, :], w2[:, :], 1.0 / 11, 1.0 / 9, ALU.mult, ALU.add, reads=[w2], writes=[pl])
    for cf in (1.0 / 7, 1.0 / 5, 1.0 / 3, 1.0):
        tt(S, "dve", pl[:, :], pl[:, :], w2[:, :], ALU.mult, reads=[pl, w2], writes=[pl])
        ts(S, "dve", pl[:, :], pl[:, :], cf, None, ALU.add, None, reads=[pl], writes=[pl])
    tt(S, "dve", pl[:, :], pl[:, :], w[:, :], ALU.mult, reads=[pl, w], writes=[pl])
    ts(S, "dve", s2[:, :], t[:, :], 1.0, None, ALU.add, None, reads=[t], writes=[s2])
    act(S, s2[:, :], s2[:, :], AF.Ln, reads=[s2], writes=[s2])
    ts(S, "dve", mk[:, :], t[:, :], 0.5, None, ALU.is_lt, None, reads=[t], writes=[mk])
    stt(S, "dve", pl[:, :], pl[:, :], 2.0, s2[:, :], ALU.mult, ALU.subtract, reads=[pl, s2], writes=[pl])
    tt(S, "dve", pl[:, :], pl[:, :], mk[:, :], ALU.mult, reads=[pl, mk], writes=[pl])
    tt(S, "dve", pl[:, :], pl[:, :], s2[:, :], ALU.add, reads=[pl, s2], writes=[pl])
    ts(S, "dve", R.c1[:, :, :].rearrange("p a b -> p (a b)"), pl[:, :], -8.0, None, ALU.mult, None, reads=[pl], writes=[R.c1])
    R.ones = S.sb("ones_r", [RB, 1], F32)
    S.add("pool", lambda e: e.memset(R.ones[:, :], 1.0 + 2.0 ** -23), writes=[R.ones])
    R.ngb = S.sb("ngb", [RB, 4, RCH], F32)
    ts(S, "dve", R.ngb[:, :, :], R.gb[:, :, :], -1.0, None, ALU.mult, None, reads=[R.gb], writes=[R.ngb])
    R.msk = S.sb("msk", [128, 2], F32)
    S.dma("sp", R.msk[:, :], I["masks"], R.msk, writes=[R.msk])
    R.X = [S.sb(f"X{i}", [RB, 515], F32) for i in range(2)]
    R.xc = [S.sb(f"xc{i}", [RB, 512], F32) for i in range(2)]
    R.xcb = [S.sb(f"xcb{i}", [RB, 512], BF16) for i in range(2)]
    R.r = [S.sb(f"r{i}", [RB, 512], F32) for i in range(2)]
    R.iu = [S.sb(f"iu{i}", [RB, 512], F32) for i in range(2)]
    R.a = [S.sb(f"a{i}", [RB, 512], F32) for i in range(2)]
    R.m = [S.sb(f"m{i}", [RB, 512], F32) for i in range(2)]
    R.hs = [S.sb(f"hs{i}", [RB, 512], F32) for i in range(2)]
    R.win = [S.sb(f"hwin{i}", [128, 8, 516], BF16) for i in range(2)]
    R.pb = [S.ps(f"rb{i}", 512 * i, 512) for i in range(8)]
    R.pxB = [S.ps(f"pxB{i}", 3584 + 4 * i, 3, parts=RB) for i in range(2)]
    return R


def rnn_front(S, R, hwin, L, ch, nchunk, is_ctx, mask_before, mask_after):
    X = R.X[nchunk % 2]
    xc = R.xc[nchunk % 2]
    xcb = R.xcb[nchunk % 2]
    px = R.pb[nchunk % 2]
    col = 96 * ch
    if is_ctx:
        for kc in range(8):
            mm(S, px, px[0:RB, 0:L], R.win_x[:, kc, col:col + RB], hwin[:, kc, 0:L], kc == 0, kc == 7, reads=[R.win_x, hwin])
        S.add("pool", lambda e: e.memset(X[:, 0:2], 0.0), writes=[X])
        S.add("pool", lambda e: e.memset(X[:, L + 2:L + 3], 0.0), writes=[X])
        cp(S, "act", X[:, 2:L + 2], px[0:RB, 0:L], reads=[px], writes=[X])
    else:
        pxb = R.pxB[nchunk % 2]
        for kc in range(8):
            mm(S, px, px[0:RB, 0:512], R.win_x[:, kc, col:col + RB], hwin[:, kc, 0:512], kc == 0, kc == 7, reads=[R.win_x, hwin])
        for kc in range(8):
            mm(S, pxb, pxb[:, 0:3], R.win_x[:, kc, col:col + RB], hwin[:, kc, 512:515], kc == 0, kc == 7, reads=[R.win_x, hwin])
        cp(S, "act", X[:, 0:512], px[0:RB, 0:512], reads=[px], writes=[X])
        cp(S, "dve", X[:, 512:515], pxb[:, 0:3], reads=[pxb], writes=[X])
        if mask_before:
            ts(S, "dve", X[:, 0:2], X[:, 0:2], R.msk[0:RB, 0:1], None, ALU.mult, None, reads=[X, R.msk], writes=[X])
        if mask_after:
            ts(S, "dve", X[:, 514:515], X[:, 514:515], R.msk[0:RB, 1:2], None, ALU.mult, None, reads=[X, R.msk], writes=[X])
    ts(S, "dve", xc[:, 0:L], X[:, 0:L], R.cw[:, 0, ch:ch + 1], R.cw[:, 4, ch:ch + 1], ALU.mult, ALU.add,
       reads=[X, R.cw], writes=[xc])
    for j in range(1, 4):
        stt(S, "dve", xc[:, 0:L], X[:, j:j + L], R.cw[:, j, ch:ch + 1], xc[:, 0:L], ALU.mult, ALU.add,
            reads=[X, R.cw, xc], writes=[xc])
    cp(S, "pool", xcb[:, 0:L], xc[:, 0:L], reads=[xc], writes=[xcb])


def rnn_back(S, C, R, L, ch, nchunk, init, sumr, hs_out, extra_sig=None):
    xc = R.xc[nchunk % 2]
    xcb = R.xcb[nchunk % 2]
    for d in range(2):
        pr = R.pb[2 + d]
        pi = R.pb[4 + d]
        mm(S, pr, pr[0:RB, 0:L], R.ga[:, d, ch, :], xcb[:, 0:L], True, True, reads=[R.ga, xcb])
        mm(S, pi, pi[0:RB, 0:L], R.gx[:, d, ch, :], xcb[:, 0:L], True, True, reads=[R.gx, xcb])
    for d in range(2):
        pr = R.pb[2 + d]
        pi = R.pb[4 + d]
        r, iu = R.r[d], R.iu[d]
        if sumr is not None:
            act(S, r[:, 0:L], pr[0:RB, 0:L], AF.Sigmoid, reads=[pr, R.gb], writes=[r, sumr[d][0]],
                bias=R.gb[:, d, ch:ch + 1], accum_out=sumr[d][1])
        else:
            act(S, r[:, 0:L], pr[0:RB, 0:L], AF.Sigmoid, reads=[pr, R.gb], writes=[r], bias=R.gb[:, d, ch:ch + 1])
        act(S, iu[:, 0:L], pi[0:RB, 0:L], AF.Sigmoid, reads=[pi, R.gb], writes=[iu], bias=R.gb[:, 2 + d, ch:ch + 1])
    if extra_sig is not None:
        extra_sig()
    for d in range(2):
        r, a = R.r[d], R.a[d]
        act(S, a[:, 0:L], r[:, 0:L], AF.Exp, reads=[r, R.c1], writes=[a], scale=R.c1[:, d, ch:ch + 1])
    for d in range(2):
        a, m = R.a[d], R.m[d]
        act(S, m[:, 0:L], a[:, 0:L], AF.Square, reads=[a], writes=[m])
    for d in range(2):
        m = R.m[d]
        act(S, m[:, 0:L], m[:, 0:L], AF.Ln, reads=[m, R.ones], writes=[m], bias=R.ones[:, 0:1], scale=-1.0)
    for d in range(2):
        m = R.m[d]
        act(S, m[:, 0:L], m[:, 0:L], AF.Exp, reads=[m], writes=[m], scale=0.5)
    for d in range(2):
        r, iu, a, m, hs = R.r[d], R.iu[d], R.a[d], R.m[d], hs_out[d]
        tt(S, "dve", iu[:, 0:L], iu[:, 0:L], xc[:, 0:L], ALU.mult, reads=[iu, xc], writes=[iu])
        tt(S, "dve", iu[:, 0:L], iu[:, 0:L], m[:, 0:L], ALU.mult, reads=[iu, m], writes=[iu])
        ini = init[d]
        ini_reads = [] if isinstance(ini, float) else [ini[0]]
        ini_ap = ini if isinstance(ini, float) else ini[1]
        if d == 0:
            S.add("dve", lambda e, hs=hs, a=a, iu=iu, ini_ap=ini_ap: e.tensor_tensor_scan(
                out=hs[:, 0:L], data0=a[:, 0:L], data1=iu[:, 0:L], initial=ini_ap, op0=ALU.mult, op1=ALU.add),
                reads=[a, iu] + ini_reads, writes=[hs])
        else:
            def rv(buf):
                ap = buf[:, 0:L]
                return bass.AP(ap.tensor, ap.offset + (L - 1), [list(ap.ap[0]), [-1, L]])
            S.add("dve", lambda e, hs=hs, a=a, iu=iu, ini_ap=ini_ap: e.tensor_tensor_scan(
                out=rv(hs), data0=rv(a), data1=rv(iu), initial=ini_ap, op0=ALU.mult, op1=ALU.add),
                reads=[a, iu] + ini_reads, writes=[hs])


def phase4a_h1(S, C, I, mod_d, x1, hT1):
    m0 = S.off
    mods = S.sb("mods4", [128, 32], F32)
    tmpm = S.sb("tmpm4", [128, 24], F32)
    S.dma("sp", tmpm[:, 0:8], pp_view(I["l1_norm1"]), tmpm, writes=[tmpm], allow_slow_non_contiguous=True)
    for s in range(2):
        load_mod_pp(S, tmpm, 1 + s, mod_d, 1, s, 1)
        load_mod_pp(S, mods, 2 * s + 1, mod_d, 1, s, 0)
        stt(S, "dve", mods[:, 16 * s:16 * s + 8], tmpm[:, 8 + 8 * s:16 + 8 * s], 1.0, tmpm[:, 0:8], ALU.add, ALU.mult,
            reads=[tmpm], writes=[mods])
    Gs = [Buf("G4", mods[:, 0:8]), Buf("G4c", mods[:, 16:24])]
    Ss = [Buf("S4", mods[:, 8:16]), Buf("S4c", mods[:, 24:32])]
    xin = [S.sb(f"x4in{i}", [128, 1024], F32) for i in range(3)]
    xs = [S.sb(f"xs4{i}", [128, 1024], F32) for i in range(2)]
    ss = [S.sb(f"ss4{i}", [128, 1], F32) for i in range(2)]
    rstd = [S.sb(f"rstd4{i}", [128, 1], F32) for i in range(2)]
    hb_ = [S.sb(f"h4{i}", [128, 8, 128], BF16) for i in range(2)]
    pT = [S.ps(f"pT4{i}", 512 * i, 512) for i in range(2)]
    for ti in range(NT1):
        s = 1 if ti < 2 else 0
        xt = xin[ti % 3]
        S.dma("sp", xt[:, :], x1[128 * ti:128 * ti + 128, :], xt, reads=[x1], writes=[xt])
        hb = hb_[ti % 2]
        norm_tile(S, C, xt, ss[ti % 2], rstd[ti % 2], xs[ti % 2], pT, (hb, lambda kc, hb=hb: hb[:, kc, :]), Gs[s], Ss[s])
        S.dma("sp", hT1[:, :, 128 * ti:128 * ti + 128].rearrange("kc p t -> p kc t"), hb[:, :, :], hb, reads=[hb], writes=[hT1])
    S.barrier()
    S.off = m0


def seg_info(seg):
    if seg == 0:
        return True, 256, 0, False, False
    b = seg - 1
    s = 256 + 128 + 512 * b
    return False, 512, s - 2, b == 0, b == 7


def load_win(S, R, hT1, seg, n):
    is_ctx, L, w0, _, _ = seg_info(seg)
    hw = R.win[n % 2]
    wl = 256 if is_ctx else 515
    S.dma("sp", hw[:, :, 0:wl], hT1[:, :, w0:w0 + wl].rearrange("kc p t -> p kc t"), hw, reads=[hT1], writes=[hw])
    return hw


def phase4b_pass1(S, C, I, mod_d, hT1, SAB):
    m0 = S.off
    R = rnn_setup(S, C, I, mod_d, pass2=False)
    sab = S.sb("sab", [RB, 2, 9, 2, RCH], F32)
    S.add("pool", lambda e: e.memset(sab[:, 0, :, :, :], 0.0), writes=[sab])
    work = []
    for seg in range(9):
        for ch in range(RCH):
            work.append((seg, ch))
    wins = {0: load_win(S, R, hT1, 0, 0)}

    def front(n):
        seg, ch = work[n]
        is_ctx, L, w0, mb, ma = seg_info(seg)
        if ch == 0 and seg + 1 < 9:
            wins[seg + 1] = load_win(S, R, hT1, seg + 1, seg + 1)
        rnn_front(S, R, wins[seg], L, ch, n, is_ctx, mb, ma)

    front(0)
    for n, (seg, ch) in enumerate(work):
        is_ctx, L, w0, mb, ma = seg_info(seg)
        if n + 1 < len(work):
            front(n + 1)
        sumr = [(sab, sab[:, 0, seg, d, ch:ch + 1]) for d in range(2)]
        rnn_back(S, C, R, L, ch, n, [0.0, 0.0], sumr, R.hs)
        cp(S, "pool", sab[:, 1, seg, 0, ch:ch + 1], R.hs[0][:, L - 1:L], reads=[R.hs[0]], writes=[sab])
        cp(S, "pool", sab[:, 1, seg, 1, ch:ch + 1], R.hs[1][:, 0:1], reads=[R.hs[1]], writes=[sab])
    for seg in range(9):
        tt(S, "dve", sab[:, 0, seg, :, :], sab[:, 0, seg, :, :], R.c1[:, :, :], ALU.mult, reads=[sab, R.c1], writes=[sab])
    act(S, sab[:, 0, :, :, :], sab[:, 0, :, :, :], AF.Exp, reads=[sab], writes=[sab])
    S.dma("sp", SAB[:, :], sab[:, :, :, :, :].rearrange("p a s d c -> p (a s d c)"), sab, reads=[sab], writes=[SAB])
    S.barrier()
    S.off = m0


def phase5_fold(S, C, I, SALL, SOWN, hin, NR=4):
    m0 = S.off
    sall = S.sb("sall", [RB, NR, 2 * 9 * 2 * RCH], F32)
    sown = S.sb("sown", [RB, 2, 9, 2, RCH], F32)
    sel = S.sb("sel", [RB, 2 * NR], F32)
    S.dma("sp", sall[:, :, :], SALL.t.rearrange("(j p) f -> p j f", p=RB), sall, reads=[SALL], writes=[sall])
    S.dma("sp", sown[:, :, :, :, :].rearrange("p a s d c -> p (a s d c)"), SOWN[:, :], sown, reads=[SOWN], writes=[sown])
    S.dma("sp", sel[:, :], I["sel"], sel, writes=[sel])
    sv = sall[:, :, :].rearrange("p j (a s d c) -> p j a s d c", a=2, s=9, d=2)
    h = S.sb("hfold", [RB, RCH], F32)
    ae = S.sb("aeff", [RB, 8, RCH], F32)
    be = S.sb("beff", [RB, 8, RCH], F32)
    for d in range(2):
        cp(S, "dve", h[:, :], sown[:, 1, 0, d, :], reads=[sown], writes=[h])
        order = range(NR) if d == 0 else range(NR - 1, -1, -1)
        for j in order:
            mj = sel[:, d * NR + j:d * NR + j + 1]
            ts(S, "dve", ae[:, :, :], sv[:, j, 0, 1:9, d, :], -1.0, None, ALU.add, None, reads=[sall], writes=[ae])
            ts(S, "dve", ae[:, :, :], ae[:, :, :], mj, None, ALU.mult, None, reads=[ae, sel], writes=[ae])
            ts(S, "dve", ae[:, :, :], ae[:, :, :], 1.0, None, ALU.add, None, reads=[ae], writes=[ae])
            ts(S, "dve", be[:, :, :], sv[:, j, 1, 1:9, d, :], mj, None, ALU.mult, None, reads=[sall, sel], writes=[be])
            border = range(8) if d == 0 else range(7, -1, -1)
            for b in border:
                tt(S, "dve", h[:, :], h[:, :], ae[:, b, :], ALU.mult, reads=[h, ae], writes=[h])
                tt(S, "dve", h[:, :], h[:, :], be[:, b, :], ALU.add, reads=[h, be], writes=[h])
        border = list(range(8)) if d == 0 else list(range(7, -1, -1))
        for n, b in enumerate(border):
            cp(S, "dve", hin[:, d, b, :], h[:, :], reads=[h], writes=[hin])
            if n < 7:
                tt(S, "dve", h[:, :], h[:, :], sown[:, 0, 1 + b, d, :], ALU.mult, reads=[h, sown], writes=[h])
                tt(S, "dve", h[:, :], h[:, :], sown[:, 1, 1 + b, d, :], ALU.add, reads=[h, sown], writes=[h])
    S.barrier()
    S.off = m0


def phase6_pass2(S, C, I, mod_d, x1, hT1, hin, x2, h2T1, comb, XS=None, maskall=None):
    R = rnn_setup(S, C, I, mod_d, pass2=True)
    mods = S.sb("mods6", [128, 16], F32)
    tmpm = S.sb("tmpm6", [128, 16], F32)
    S.dma("sp", tmpm[:, 0:8], pp_view(I["l1_norm2"]), tmpm, writes=[tmpm], allow_slow_non_contiguous=True)
    load_mod_pp(S, tmpm, 1, mod_d, 1, 0, 4)
    load_mod_pp(S, mods, 1, mod_d, 1, 0, 3)
    stt(S, "dve", mods[:, 0:8], tmpm[:, 8:16], 1.0, tmpm[:, 0:8], ALU.add, ALU.mult, reads=[tmpm], writes=[mods])
    G2 = Buf("G6", mods[:, 0:8])
    S2 = Buf("S6", mods[:, 8:16])
    g1b = S.sb("g1b6", [128, 1024], F32)
    S.dma("sp", g1b[:, :], bc_view(mod_d[1, 0, 2 * D:3 * D], D), g1b, reads=[mod_d], writes=[g1b])
    wr = S.sb("wr", [128, 8, NE], F32)
    S.dma("sp", wr[:, :, :], I["l1_router_w"].rearrange("(kc p) e -> p kc e", p=128), wr, writes=[wr])
    brb = S.sb("brb", [128, NE], F32)
    S.dma("sp", brb[:, :], bc_view(I["l1_router_b"], NE), brb, writes=[brb])
    yin = S.sb("yin", [RB, RCH, 512], BF16)
    gg = [S.sb(f"gg{i}", [RB, 512], F32) for i in range(2)]
    xt_ = [S.sb(f"x6in{i}", [128, 1024], F32) for i in range(1)] * 2
    ytmp = [S.sb(f"y6{i}", [128, 1024], F32) for i in range(2)]
    xs = [S.sb(f"xs6{i}", [128, 1024], F32) for i in range(1)] * 2
    ss = [S.sb(f"ss6{i}", [128, 1], F32) for i in range(2)]
    rstd = [S.sb(f"rstd6{i}", [128, 1], F32) for i in range(2)]
    h2 = [S.sb(f"h6{i}", [128, 8, 128], BF16) for i in range(2)]
    h32 = [S.sb(f"h32{i}", [128, 8, 128], F32) for i in range(1)] * 2
    rt = [S.sb(f"rt{i}", [128, 48], F32) for i in range(2)]
    xsb = [S.sb(f"xsb{i}", [128, 1024], BF16) for i in range(2)] if XS is not None else None
    pg = R.pb[6]
    plog = S.ps("plog", 3584 + 16, 8)
    ntile = 0
    xg = [S.sb(f"xg{i}", [RB, 512], F32) for i in range(2)]
    work = [(seg, ch) for seg in range(1, 9) for ch in range(RCH)]
    wins = {1: load_win(S, R, hT1, 1, 0)}

    def front(n):
        seg, ch = work[n]
        is_ctx, L, w0, mb, ma = seg_info(seg)
        if ch == 0 and seg + 1 < 9:
            wins[seg + 1] = load_win(S, R, hT1, seg + 1, seg)
        rnn_front(S, R, wins[seg], L, ch, n, False, mb, ma)
        hw = wins[seg]
        for kc in range(8):
            mm(S, pg, pg[0:RB, :], R.win_g[:, kc, 96 * ch:96 * ch + RB], hw[:, kc, 2:514], kc == 0, kc == 7, reads=[R.win_g, hw])
        x_ = xg[n % 2]
        cp(S, "act", x_[:, :], pg[0:RB, :], reads=[pg], writes=[x_])

    front(0)
    for n, (seg, ch) in enumerate(work):
        b = seg - 1
        if True:
            init = [(hin, hin[:, d, b, ch:ch + 1]) for d in range(2)]
            x_ = xg[n % 2]
            g_ = gg[n % 2]
            act(S, g_[:, :], x_[:, :], AF.Square, reads=[x_], writes=[g_])
            ts(S, "dve", g_[:, :], g_[:, :], 0.044715, 1.0, ALU.mult, ALU.add, reads=[g_], writes=[g_])
            tt(S, "dve", g_[:, :], g_[:, :], x_[:, :], ALU.mult, reads=[g_, x_], writes=[g_])
            if n + 1 < len(work):
                front(n + 1)

            def gsig(g_=g_):
                act(S, g_[:, :], g_[:, :], AF.Sigmoid, reads=[g_], writes=[g_], scale=1.5957691216057308)
            rnn_back(S, C, R, 512, ch, n, init, None, R.hs, extra_sig=gsig)
            tt(S, "pool", g_[:, :], g_[:, :], x_[:, :], ALU.mult, reads=[g_, x_], writes=[g_])
            tt(S, "pool", R.hs[0][:, :], R.hs[0][:, :], R.hs[1][:, :], ALU.add, reads=[R.hs[0], R.hs[1]], writes=[R.hs[0]])
            tt(S, "pool", yin[:, ch, :], g_[:, :], R.hs[0][:, :], ALU.mult, reads=[g_, R.hs[0]], writes=[yin])
        if ch != RCH - 1:
            continue
        for t in range(4):
            mt = 4 * b + t
            ti = 2 + 1 + mt
            xt = xt_[ntile % 2]
            S.dma("sp", xt[:, :], x1[128 * ti:128 * ti + 128, :], xt, reads=[x1], writes=[xt])
            yt = ytmp[ntile % 2]
            for cb in range(2):
                py = R.pb[cb]
                for ch in range(RCH):
                    mm(S, py, py[:, :], yin[:, ch, 128 * t:128 * t + 128], R.wout[:, ch, 512 * cb:512 * cb + 512],
                       ch == 0, ch == RCH - 1, reads=[yin, R.wout])
                tt(S, "dve", yt[:, 512 * cb:512 * cb + 512], py[:, :], g1b[:, 512 * cb:512 * cb + 512], ALU.mult,
                   reads=[py, g1b], writes=[yt])
            tt(S, "pool", yt[:, :], yt[:, :], xt[:, :], ALU.add, reads=[yt, xt], writes=[yt])
            S.dma("sp", x2[128 * mt:128 * mt + 128, :], yt[:, :], yt, reads=[yt], writes=[x2])
            hb = h2[ntile % 2]
            hf = h32[ntile % 2]
            norm_tile(S, C, yt, ss[ntile % 2], rstd[ntile % 2], xs[ntile % 2], [R.pb[0], R.pb[1]],
                      (hb, lambda kc, hb=hb: hb[:, kc, :]), G2, S2,
                      hT32_dst=(hf, lambda kc, hf=hf: hf[:, kc, :]))
            if XS is None:
                S.dma("sp", h2T1[:, :, 128 * mt:128 * mt + 128].rearrange("kc p t -> p kc t"), hb[:, :, :], hb, reads=[hb], writes=[h2T1])
            else:
                xb_ = xsb[ntile % 2]
                cp(S, "pool", xb_[:, :], xs[ntile % 2][:, :], reads=[xs[ntile % 2]], writes=[xb_])
                S.dma("sp", XS[128 * mt:128 * mt + 128, :], xb_[:, :], xb_, reads=[xb_], writes=[XS])
            for kc in range(8):
                mm(S, plog, plog[:, :], hf[:, kc, :], wr[:, kc, :], kc == 0, kc == 7, reads=[hf, wr])
            r_ = rt[ntile % 2]
            lg, m8, ex, em, den, nv1 = r_[:, 0:8], r_[:, 8:16], r_[:, 16:24], r_[:, 24:32], r_[:, 32:33], r_[:, 33:34]
            tt(S, "dve", lg, plog[:, :], brb[:, :], ALU.add, reads=[plog, brb], writes=[r_])
            S.add("dve", lambda e, m8=m8, lg=lg: e.max(out=m8, in_=lg), reads=[r_], writes=[r_])
            ts(S, "dve", nv1, r_[:, 8:9], -1.0, None, ALU.mult, None, reads=[r_], writes=[r_])
            act(S, ex, lg, AF.Exp, reads=[r_], writes=[r_], bias=nv1)
            ts(S, "dve", em, lg, r_[:, 9:10], None, ALU.is_ge, None, reads=[r_], writes=[r_])
            if maskall is not None:
                cp(S, "dve", maskall[:, mt, :], em, reads=[r_], writes=[maskall])
            tt(S, "dve", em, em, ex, ALU.mult, reads=[r_], writes=[r_])
            S.add("dve", lambda e, den=den, em=em: e.reduce_sum(out=den, in_=em, axis=AX.X), reads=[r_], writes=[r_])
            S.add("dve", lambda e, den=den: e.reciprocal(out=den, in_=den), reads=[r_], writes=[r_])
            ts(S, "dve", comb[:, mt, :], em, den, None, ALU.mult, None, reads=[r_], writes=[comb])
            ntile += 1
    S.barrier()


def phase7_moe(S, C, I, mod_d, x2, h2T1, comb, out_d):
    m0 = S.off
    m5b = S.sb("m5b", [128, 1024], F32)
    S.dma("sp", m5b[:, :], bc_view(mod_d[1, 0, 5 * D:6 * D], D), m5b, reads=[mod_d], writes=[m5b])
    hT = S.sb("hTg", [128, 8, 2048], BF16)
    acc = [S.sb(f"acc{i}", [128, 1024], F32) for i in range(16)]
    NWB = 2
    w1g = [S.sb(f"w1g{i}", [128, 8, 512], BF16) for i in range(NWB)]
    w3g = [S.sb(f"w3g{i}", [128, 8, 512], BF16) for i in range(NWB)]
    w2g = [S.sb(f"w2g{i}", [128, 4, 1024], BF16) for i in range(NWB)]
    hid = [S.sb(f"hidm{i}", [128, 4, 512], BF16) for i in range(2)]
    sa = [S.sb(f"sam{i}", [128, 512], F32) for i in range(2)]
    xin = [S.sb(f"x7in{i}", [128, 1024], F32) for i in range(2)]
    pA = [S.ps(f"pA7{i}", 512 * i, 512) for i in range(2)]
    pB = [S.ps(f"pB7{i}", 1024 + 512 * i, 512) for i in range(2)]
    pY = [S.ps(f"pY7{i}", 2048 + 512 * i, 512) for i in range(4)]
    nw = 0
    nf = 0
    nh = 0
    nt = 0

    def load_w(e, fg, n):
        S.dma("pool", w1g[n % NWB][:, :, :], I["l1_moe_w1"][e, :, 512 * fg:512 * fg + 512].rearrange("(kc p) n -> p kc n", p=128),
              w1g[n % NWB], writes=[w1g[n % NWB]])
        S.dma("pool", w3g[n % NWB][:, :, :], I["l1_moe_w3"][e, :, 512 * fg:512 * fg + 512].rearrange("(kc p) n -> p kc n", p=128),
              w3g[n % NWB], writes=[w3g[n % NWB]])
        S.dma("pool", w2g[n % NWB][:, :, :], I["l1_moe_w2"][e, 512 * fg:512 * fg + 512, :].rearrange("(fc p) n -> p fc n", p=128),
              w2g[n % NWB], writes=[w2g[n % NWB]])

    steps = [(G, e, fg) for G in range(2) for e in range(NE) for fg in range(7)]
    load_w(steps[0][1], steps[0][2], 0)
    for si, (G, e, fg) in enumerate(steps):
        if e == 0 and fg == 0:
            S.dma("sp", hT[:, :, :], h2T1[:, :, 2048 * G:2048 * G + 2048].rearrange("kc p t -> p kc t"), hT, reads=[h2T1], writes=[hT])
        if si + 1 < len(steps):
            load_w(steps[si + 1][1], steps[si + 1][2], si + 1)
        a1, a3, a2 = w1g[si % NWB], w3g[si % NWB], w2g[si % NWB]
        first = (e == 0 and fg == 0)
        for tb in range(4):
            hd = hid[nh % 2]
            for fc in range(4):
                pa, pb = pA[nf % 2], pB[nf % 2]
                for kc in range(8):
                    mm(S, pa, pa[:, :], a1[:, kc, 128 * fc:128 * fc + 128], hT[:, kc, 512 * tb:512 * tb + 512], kc == 0, kc == 7,
                       reads=[a1, hT])
                for kc in range(8):
                    mm(S, pb, pb[:, :], a3[:, kc, 128 * fc:128 * fc + 128], hT[:, kc, 512 * tb:512 * tb + 512], kc == 0, kc == 7,
                       reads=[a3, hT])
                sb_ = sa[nf % 2]
                act(S, sb_[:, :], pa[:, :], AF.Silu, reads=[pa], writes=[sb_])
                tt(S, "dve", hd[:, fc, :], sb_[:, :], pb[:, :], ALU.mult, reads=[sb_, pb], writes=[hd])
                nf += 1
            for t in range(4):
                tl = 4 * tb + t
                mt = 16 * G + tl
                ac = acc[tl]
                for cb in range(2):
                    py = pY[(nt % 2) * 2 + cb]
                    for fc in range(4):
                        mm(S, py, py[:, :], hd[:, fc, 128 * t:128 * t + 128], a2[:, fc, 512 * cb:512 * cb + 512], fc == 0, fc == 3,
                           reads=[hd, a2])
                    sl = slice(512 * cb, 512 * cb + 512)
                    if first:
                        ts(S, "dve", ac[:, sl], py[:, :], comb[:, mt, e:e + 1], None, ALU.mult, None, reads=[py, comb], writes=[ac])
                    else:
                        stt(S, "dve", ac[:, sl], py[:, :], comb[:, mt, e:e + 1], ac[:, sl], ALU.mult, ALU.add,
                            reads=[py, comb, ac], writes=[ac])
                nt += 1
            nh += 1
        if e == NE - 1 and fg == 6:
            for tl in range(16):
                mt = 16 * G + tl
                xt = xin[tl % 2]
                S.dma("sp", xt[:, :], x2[128 * mt:128 * mt + 128, :], xt, reads=[x2], writes=[xt])
                ac = acc[tl]
                tt(S, "pool", ac[:, :], ac[:, :], m5b[:, :], ALU.mult, reads=[ac, m5b], writes=[ac])
                tt(S, "pool", xt[:, :], xt[:, :], ac[:, :], ALU.add, reads=[xt, ac], writes=[xt])
                S.dma("sp", out_d[128 * mt:128 * mt + 128, :], xt[:, :], xt, reads=[xt], writes=[out_d])
    S.barrier()
    S.off = m0


I32 = mybir.dt.int32
GSZ = 768
NGRP = 18
GT = GSZ // 128
TBW = GSZ // 2
TPB = TBW // 128


class Route:
    pass


def phase_route(S, C, I, comb, maskall):
    Rt = Route()
    Rt.slotA_i = S.sb("slotA_i", [128, 32], I32)
    Rt.slotB_i = S.sb("slotB_i", [128, 32], I32)
    Rt.gA = S.sb("gA", [128, 32], F32)
    Rt.gB = S.sb("gB", [128, 32], F32)
    Rt.Eg_i = S.sb("Eg_i", [128, NGRP], I32)
    Rt.tokA = S.sb("tokA", [128, 32, 8], I32)
    Rt.tokB = S.sb("tokB", [128, 32, 8], I32)
    m0 = S.off
    rc = S.sb("rc", [128, 216], F32)
    S.dma("sp", rc[:, :], I["rconst"], rc, writes=[rc])
    Lb = S.sb("Lb", [128, 128], BF16)
    ob = S.sb("ob", [128, 128], BF16)
    mb = S.sb("mb", [128, 256], BF16)
    cp(S, "dve", Lb[:, :], rc[:, 0:128], reads=[rc], writes=[Lb])
    S.add("pool", lambda e: e.memset(ob[:, :], 1.0), writes=[ob])
    cp(S, "dve", mb[:, :], maskall[:, :, :].rearrange("p t e -> p (t e)"), reads=[maskall], writes=[mb])
    p_r = S.ps("p_rin", 0, 256)
    p_c = S.ps("p_cnt", 512, 256)
    mm(S, p_r, p_r[:, :], Lb[:, :], mb[:, :], True, True, reads=[Lb, mb])
    mm(S, p_c, p_c[:, :], ob[:, :], mb[:, :], True, True, reads=[ob, mb])
    rin = S.sb("rin", [128, 32, NE], F32)
    cnt = S.sb("cnt", [128, 32, NE], F32)
    cp(S, "dve", rin[:, :, :].rearrange("p t e -> p (t e)"), p_r[:, :], reads=[p_r], writes=[rin])
    cp(S, "dve", cnt[:, :, :].rearrange("p t e -> p (t e)"), p_c[:, :], reads=[p_c], writes=[cnt])
    ones32 = S.sb("ones32", [128, 32], F32)
    S.add("pool", lambda e: e.memset(ones32[:, :], 1.0), writes=[ones32])
    inc = S.sb("inc", [128, NE, 32], F32)
    for e_ in range(NE):
        S.add("dve", lambda e, e_=e_: e.tensor_tensor_scan(out=inc[:, e_, :], data0=ones32[:, :], data1=cnt[:, :, e_],
                                                           initial=0.0, op0=ALU.mult, op1=ALU.add),
              reads=[ones32, cnt], writes=[inc])
    pre = S.sb("pre", [128, NE, 32], F32)
    tt(S, "dve", pre[:, :, :], inc[:, :, :], cnt[:, :, :].rearrange("p t e -> p e t"), ALU.subtract, reads=[inc, cnt], writes=[pre])
    sm = S.sb("route_sm", [128, 64], F32)
    n_e, G_, gend, gstart, sbase, tmp8 = (sm[:, 0:8], sm[:, 8:16], sm[:, 16:24], sm[:, 24:32], sm[:, 32:40], sm[:, 40:48])
    cp(S, "dve", n_e, inc[:, :, 31], reads=[inc], writes=[sm])
    ts(S, "dve", G_, n_e, 0.0, None, ALU.is_gt, None, reads=[sm], writes=[sm])
    for k in range(1, (4096 + GSZ - 1) // GSZ):
        ts(S, "dve", tmp8, n_e, float(GSZ * k), None, ALU.is_gt, None, reads=[sm], writes=[sm])
        tt(S, "dve", G_, G_, tmp8, ALU.add, reads=[sm], writes=[sm])
    S.add("dve", lambda e: e.tensor_tensor_scan(out=gend, data0=ones32[:, 0:8], data1=G_, initial=0.0, op0=ALU.mult, op1=ALU.add),
          reads=[sm, ones32], writes=[sm])
    tt(S, "dve", gstart, gend, G_, ALU.subtract, reads=[sm], writes=[sm])
    ts(S, "dve", sbase, gstart, float(GSZ), None, ALU.mult, None, reads=[sm], writes=[sm])
    v = S.sb("route_v", [128, 32, NE], F32)
    tt(S, "dve", v[:, :, :], rin[:, :, :], pre[:, :, :].rearrange("p e t -> p t e"), ALU.add, reads=[rin, pre], writes=[v])
    sb_b = bass.AP(sbase.tensor, sbase.offset, [list(sbase.ap[0]), [0, 32], [1, NE]])
    tt(S, "dve", v[:, :, :], v[:, :, :], sb_b, ALU.add, reads=[v, sm], writes=[v])
    stt(S, "dve", v[:, :, :], v[:, :, :], 1.0, maskall[:, :, :], ALU.add, ALU.mult, reads=[v, maskall], writes=[v])
    m8 = S.sb("route_m8", [128, 32, NE], F32)
    for t_ in range(32):
        S.add("dve", lambda e, t_=t_: e.max(out=m8[:, t_, :], in_=v[:, t_, :]), reads=[v], writes=[m8])
    sf = S.sb("route_sf", [128, 2, 32], F32)
    oh = S.sb("route_oh", [128, 32, NE], F32)
    for which, (sl_i, g_) in enumerate(((Rt.slotA_i, Rt.gA), (Rt.slotB_i, Rt.gB))):
        top = m8[:, :, which]
        ts(S, "dve", sf[:, which, :], top, -1.0, None, ALU.add, None, reads=[m8], writes=[sf])
        cp(S, "dve", sl_i[:, :], sf[:, which, :], reads=[sf], writes=[sl_i])
        top_b = bass.AP(top.tensor, top.offset, [list(top.ap[0]), list(top.ap[1]), [0, NE]])
        tt(S, "dve", oh[:, :, :], v[:, :, :], top_b, ALU.is_equal, reads=[v, m8], writes=[oh])
        tt(S, "dve", oh[:, :, :], oh[:, :, :], comb[:, :, :], ALU.mult, reads=[oh, comb], writes=[oh])
        S.add("dve", lambda e, g_=g_: e.reduce_sum(out=g_[:, :], in_=oh[:, :, :], axis=AX.X), reads=[oh], writes=[g_])
    Eg = S.sb("Eg_f", [128, NGRP], F32)
    ind = S.sb("route_ind", [128, 2, NGRP], F32)
    gio = rc[:, 184:184 + NGRP]
    S.add("pool", lambda e: e.memset(Eg[:, :], 0.0), writes=[Eg])
    for e_ in range(1, NE):
        ts(S, "dve", ind[:, 0, :], gio, gstart[:, e_:e_ + 1], None, ALU.is_ge, None, reads=[rc, sm], writes=[ind])
        ts(S, "dve", ind[:, 1, :], gio, gend[:, e_:e_ + 1], None, ALU.is_lt, None, reads=[rc, sm], writes=[ind])
        tt(S, "dve", ind[:, 0, :], ind[:, 0, :], ind[:, 1, :], ALU.mult, reads=[ind], writes=[ind])
        stt(S, "dve", Eg[:, :], ind[:, 0, :], float(e_), Eg[:, :], ALU.mult, ALU.add, reads=[ind, Eg], writes=[Eg])
    cp(S, "dve", Rt.Eg_i[:, :], Eg[:, :], reads=[Eg], writes=[Rt.Eg_i])
    tk = rc[:, 152:184]
    tk_b = bass.AP(tk.tensor, tk.offset, [list(tk.ap[0]), list(tk.ap[1]), [0, 8]])
    tkf = S.sb("tkf", [128, 32, 8], F32)
    cp(S, "dve", tkf[:, :, :], tk_b, reads=[rc], writes=[tkf])
    cp(S, "dve", Rt.tokA[:, :, :], tkf[:, :, :], reads=[tkf], writes=[Rt.tokA])
    ts(S, "dve", tkf[:, :, :], tkf[:, :, :], 4096.0, None, ALU.add, None, reads=[tkf], writes=[tkf])
    cp(S, "dve", Rt.tokB[:, :, :], tkf[:, :, :], reads=[tkf], writes=[Rt.tokB])
    S.barrier()
    S.off = m0
    return Rt


def phase_permute(S, C, I, Rt, XS, Hslot, Tslot):
    m0 = S.off
    zer = S.sb("zer", [128, GT * 1024], BF16)
    S.add("pool", lambda e: e.memset(zer[:, :], 0.0), writes=[zer])
    for i in range(NGRP):
        S.dma("sp", Hslot[GSZ * i:GSZ * i + GSZ, :].rearrange("(a p) n -> p a n", p=128),
              zer[:, :].rearrange("p (a n) -> p a n", a=GT), zer, reads=[zer], writes=[Hslot])
    dump = S.sb("dumpi", [128, 1024], I32)
    S.add("pool", lambda e: e.memset(dump[:, :], 8192), writes=[dump])
    S.dma("sp", Tslot.t.rearrange("(p a) o -> p (a o)", p=128), dump[:, 0:NGRP * GSZ // 128 * 8], dump, reads=[dump], writes=[Tslot])
    for mt in range(32):
        for sl, tk_ in ((Rt.slotA_i, Rt.tokA), (Rt.slotB_i, Rt.tokB)):
            S.add("pool", lambda e, sl=sl, tk_=tk_, mt=mt: e.indirect_dma_start(
                out=Tslot[:, :], out_offset=bass.IndirectOffsetOnAxis(ap=sl[:, mt:mt + 1], axis=0),
                in_=tk_[:, mt, :], in_offset=None, bounds_check=None),
                reads=[tk_, sl, Tslot], writes=[Tslot], dma_buf=tk_)
    xt = [S.sb(f"xperm{i}", [128, 1024], BF16) for i in range(3)]
    for mt in range(32):
        x_ = xt[mt % 3]
        S.dma("sp", x_[:, :], XS[128 * mt:128 * mt + 128, :], x_, reads=[XS], writes=[x_])
        for sl in (Rt.slotA_i, Rt.slotB_i):
            S.add("pool", lambda e, x_=x_, sl=sl, mt=mt: e.indirect_dma_start(
                out=Hslot[:, :], out_offset=bass.IndirectOffsetOnAxis(ap=sl[:, mt:mt + 1], axis=0),
                in_=x_[:, :], in_offset=None, bounds_check=None),
                reads=[x_, sl, Hslot], writes=[Hslot], dma_buf=x_)
    S.barrier()
    S.off = m0


def phase7s_moe(S, C, I, mod_d, Rt, Hslot, Tslot, Yab):
    m0 = S.off
    mods = S.sb("mods7", [128, 16], F32)
    tmpm = S.sb("tmpm7", [128, 16], F32)
    S.dma("sp", tmpm[:, 0:8], pp_view(I["l1_norm2"]), tmpm, writes=[tmpm], allow_slow_non_contiguous=True)
    load_mod_pp(S, tmpm, 1, mod_d, 1, 0, 4)
    load_mod_pp(S, mods, 1, mod_d, 1, 0, 3)
    stt(S, "dve", mods[:, 0:8], tmpm[:, 8:16], 1.0, tmpm[:, 0:8], ALU.add, ALU.mult, reads=[tmpm], writes=[mods])
    hT = [S.sb(f"hTs{i}", [128, 8, GSZ], BF16) for i in range(2)]
    acc = [S.sb(f"accs{i}", [128, 1024], F32) for i in range(GT)]
    NWB = 3
    w1g = [S.sb(f"w1s{i}", [128, 8, 512], BF16) for i in range(NWB)]
    w3g = [S.sb(f"w3s{i}", [128, 8, 512], BF16) for i in range(NWB)]
    w2g = [S.sb(f"w2s{i}", [128, 4, 1024], BF16) for i in range(NWB)]
    hid = [S.sb(f"hids{i}", [128, 4, 512], BF16) for i in range(2)]
    sa = [S.sb(f"sas{i}", [128, 512], F32) for i in range(2)]
    st_ = [S.sb(f"slt{i}", [128, 1024], BF16) for i in range(4)]
    tix = [S.sb(f"tix{i}", [128, 8], I32) for i in range(4)]
    pA = [S.ps(f"pA8{i}", 512 * i, 512) for i in range(2)]
    pB = [S.ps(f"pB8{i}", 1024 + 512 * i, 512) for i in range(2)]
    pY = [S.ps(f"pY8{i}", 2048 + 512 * i, 512) for i in range(4)]
    w1t, w3t, w2t = I["l1_moe_w1"], I["l1_moe_w3"], I["l1_moe_w2"]

    nreg = [0]

    def dyn_load(dst, static_ap, estride, g):
        off0 = static_ap.offset
        pat = [list(x) for x in static_ap.ap]
        tens = static_ap.tensor

        nreg[0] += 1
        rname = f"er{nreg[0]}"

        def allregs(e):
            hs = []
            try:
                while True:
                    nreg[0] += 1
                    hs.append(e.alloc_register(f"gc{nreg[0]}"))
            except ValueError:
                pass
            for h in hs:
                e.free_register(h)
            return hs

        def fn(e):
            before = allregs(e)
            with e.register(rname) as er:
                e.reg_load(er, Rt.Eg_i[0:1, g:g + 1])
                e.reg_mul(er, er, estride)
                e.reg_add(er, er, off0)
                ins = e.dma_start(out=dst[:, :, :], in_=bass.AP(tens, er, pat))
            after = {h.regnum for h in allregs(e)}
            for h in before:
                if h.regnum not in after:
                    e.free_register(h)
            return ins
        S.add("pool", fn, reads=[Rt.Eg_i], writes=[dst], dma_buf=dst)

    def load_w(g, fg, n):
        dyn_load(w1g[n % NWB], w1t[0, :, 512 * fg:512 * fg + 512].rearrange("(kc p) n -> p kc n", p=128), D * DFE, g)
        dyn_load(w3g[n % NWB], w3t[0, :, 512 * fg:512 * fg + 512].rearrange("(kc p) n -> p kc n", p=128), D * DFE, g)
        dyn_load(w2g[n % NWB], w2t[0, 512 * fg:512 * fg + 512, :].rearrange("(fc p) n -> p fc n", p=128), DFE * D, g)

    nst = [0]
    ntr = [0]

    def prologue(g):
        hb = hT[g % 2]
        for t in range(GT):
            x_ = st_[nst[0] % 4]
            nst[0] += 1
            S.dma("sp", x_[:, :], Hslot[GSZ * g + 128 * t:GSZ * g + 128 * t + 128, :], x_, reads=[Hslot], writes=[x_])
            p = pA[ntr[0] % 2]
            ntr[0] += 1
            pv = p.t.bitcast(BF16)
            for kc in range(8):
                tr(S, p, pv[:, 128 * kc:128 * kc + 128], x_[:, 128 * kc:128 * kc + 128], C.identb[:, :], reads=[x_, C.identb])
            for kc in range(8):
                src = pv[:, 128 * kc:128 * kc + 128]
                if kc % 2 == 0:
                    ts(S, "dve", hb[:, kc, 128 * t:128 * t + 128], src, mods[:, kc:kc + 1], mods[:, 8 + kc:9 + kc], ALU.mult, ALU.add,
                       reads=[p, mods], writes=[hb])
                else:
                    act(S, hb[:, kc, 128 * t:128 * t + 128], src, AF.Identity, reads=[p, mods], writes=[hb],
                        bias=mods[:, 8 + kc:9 + kc], scale=mods[:, kc:kc + 1])

    steps = [(g, fg) for g in range(NGRP) for fg in range(7)]
    load_w(0, 0, 0)
    load_w(0, 1, 1)
    prologue(0)
    nf = 0
    nh = 0
    nt = 0
    for si, (g, fg) in enumerate(steps):
        if si + 2 < len(steps):
            load_w(steps[si + 2][0], steps[si + 2][1], si + 2)
        if fg == 6 and g + 1 < NGRP:
            prologue(g + 1)
        hb = hT[g % 2]
        a1, a3, a2 = w1g[si % NWB], w3g[si % NWB], w2g[si % NWB]
        for tb in range(2):
            hd = hid[nh % 2]
            for fc in range(4):
                pa, pb = pA[nf % 2], pB[nf % 2]
                for kc in range(8):
                    mm(S, pa, pa[:, 0:TBW], a1[:, kc, 128 * fc:128 * fc + 128], hb[:, kc, TBW * tb:TBW * tb + TBW], kc == 0, kc == 7,
                       reads=[a1, hb])
                for kc in range(8):
                    mm(S, pb, pb[:, 0:TBW], a3[:, kc, 128 * fc:128 * fc + 128], hb[:, kc, TBW * tb:TBW * tb + TBW], kc == 0, kc == 7,
                       reads=[a3, hb])
                sb_ = sa[nf % 2]
                act(S, sb_[:, 0:TBW], pa[:, 0:TBW], AF.Silu, reads=[pa], writes=[sb_])
                tt(S, "dve", hd[:, fc, 0:TBW], sb_[:, 0:TBW], pb[:, 0:TBW], ALU.mult, reads=[sb_, pb], writes=[hd])
                nf += 1
            for t in range(TPB):
                tl = TPB * tb + t
                ac = acc[tl]
                for cb in range(2):
                    py = pY[(nt % 2) * 2 + cb]
                    for fc in range(4):
                        mm(S, py, py[:, :], hd[:, fc, 128 * t:128 * t + 128], a2[:, fc, 512 * cb:512 * cb + 512], fc == 0, fc == 3,
                           reads=[hd, a2])
                    sl = slice(512 * cb, 512 * cb + 512)
                    if fg == 0:
                        cp(S, "dve", ac[:, sl], py[:, :], reads=[py], writes=[ac])
                    else:
                        tt(S, "dve", ac[:, sl], py[:, :], ac[:, sl], ALU.add, reads=[py, ac], writes=[ac])
                if fg == 6:
                    tx = tix[(GT * g + tl) % 4]
                    S.dma("sp", tx[:, :], Tslot[GSZ * g + 128 * tl:GSZ * g + 128 * tl + 128, :], tx, reads=[Tslot], writes=[tx])
                    S.add("pool", lambda e, ac=ac, tx=tx: e.indirect_dma_start(
                        out=Yab[:, :], out_offset=bass.IndirectOffsetOnAxis(ap=tx[:, 0:1], axis=0),
                        in_=ac[:, :], in_offset=None, bounds_check=None),
                        reads=[ac, tx, Yab], writes=[Yab], dma_buf=ac)
                nt += 1
            nh += 1
    S.barrier()
    S.off = m0


def phase8_combine(S, C, I, mod_d, Rt, Yab, x2, out_d):
    m0 = S.off
    m5b = S.sb("m5bs", [128, 1024], F32)
    S.dma("sp", m5b[:, :], bc_view(mod_d[1, 0, 5 * D:6 * D], D), m5b, reads=[mod_d], writes=[m5b])
    ya = [S.sb(f"ya{i}", [128, 1024], F32) for i in range(2)]
    yb = [S.sb(f"yb{i}", [128, 1024], F32) for i in range(2)]
    xi = [S.sb(f"xc8{i}", [128, 1024], F32) for i in range(2)]
    for mt in range(32):
        a_, b_, x_ = ya[mt % 2], yb[mt % 2], xi[mt % 2]
        S.dma("sp", x_[:, :], x2[128 * mt:128 * mt + 128, :], x_, reads=[x2], writes=[x_])
        S.dma("sp", a_[:, :], Yab[128 * mt:128 * mt + 128, :], a_, reads=[Yab], writes=[a_])
        S.dma("sp", b_[:, :], Yab[4096 + 128 * mt:4096 + 128 * mt + 128, :], b_, reads=[Yab], writes=[b_])
        ts(S, "dve", a_[:, :], a_[:, :], Rt.gA[:, mt:mt + 1], None, ALU.mult, None, reads=[a_, Rt.gA], writes=[a_])
        stt(S, "dve", a_[:, :], b_[:, :], Rt.gB[:, mt:mt + 1], a_[:, :], ALU.mult, ALU.add, reads=[b_, Rt.gB, a_], writes=[a_])
        tt(S, "dve", a_[:, :], a_[:, :], m5b[:, :], ALU.mult, reads=[a_, m5b], writes=[a_])
        tt(S, "pool", x_[:, :], x_[:, :], a_[:, :], ALU.add, reads=[x_, a_], writes=[x_])
        S.dma("sp", out_d[128 * mt:128 * mt + 128, :], x_[:, :], x_, reads=[x_], writes=[out_d])
    S.barrier()
    S.off = m0


def declare_inputs(nc, names_shapes):
    I = {}
    for name, shape in names_shapes:
        I[name] = nc.dram_tensor(name, list(shape), F32, kind="ExternalInput").ap()
    return I


A_INPUTS = [
    ("xk", (NCH * 128, D)), ("cv", (2, D)), ("bt", (5, 128, 16, 5, 128)),
    ("l0_w_mod", (D, 6 * D)), ("l0_b_mod", (6 * D,)), ("l0_norm1", (D,)), ("l0_norm2", (D,)),
    ("l0_w_qkv", (D, 3 * D)), ("l0_q_gain", (HD,)), ("l0_k_gain", (HD,)), ("l0_w_o", (D, D)),
    ("l0_ffn_w1", (D, DFF)), ("l0_ffn_w3", (D, DFF)), ("l0_ffn_w2", (DFF, D)),
    ("l1_w_mod", (D, 6 * D)), ("l1_b_mod", (6 * D,)),
]


A_INPUTS2 = [
    ("l1_norm1", (D,)), ("l1_w_in", (D, 2 * DRNN)), ("l1_conv_w", (4, DRNN)), ("l1_conv_b", (DRNN,)),
    ("l1_gate_a_w", (2, RCH, RB, RB)), ("l1_gate_a_b", (2, DRNN)), ("l1_gate_x_w", (2, RCH, RB, RB)),
    ("l1_gate_x_b", (2, DRNN)), ("l1_lam", (2, DRNN)), ("masks", (128, 2)),
]
B_INPUTS = A_INPUTS2 + [
    ("x1", (NT1 * 128, D)), ("mod_in", (2, 2, 6 * D)), ("sall", (4 * RB, 576)), ("sown", (RB, 576)),
    ("sel", (RB, 8)), ("l1_norm2", (D,)), ("l1_w_out", (DRNN, D)), ("l1_router_w", (D, NE)), ("l1_router_b", (NE,)),
    ("l1_moe_w1", (NE, D, DFE)), ("l1_moe_w3", (NE, D, DFE)), ("l1_moe_w2", (NE, DFE, D)),
]


def build_A(debug=False):
    nc = bass.Bass("TRN2", target_bir_lowering=False)
    I = declare_inputs(nc, A_INPUTS + A_INPUTS2)
    S = Sched(nc)
    C = Common(S)
    mod_d = S.dram("mod_d", [2, 2, 6 * D], F32, kind="ExternalOutput")
    QT = S.dram("QT", [8, 128, NCH * 128], BF16)
    KT = S.dram("KT", [8, 128, NCH * 128], BF16)
    V = S.dram("V", [NCH * 128, D], BF16)
    x1a = S.dram("x1a", [NT1 * 128, D], F32, kind="ExternalOutput" if debug else "Internal")
    h2T = S.dram("h2T", [8, 128, NT1 * 128], BF16)
    x1 = S.dram("x1", [NT1 * 128, D], F32, kind="ExternalOutput")
    hT1 = S.dram("hT1", [8, 128, NT1 * 128], BF16)
    SAB = S.dram("sab_out", [RB, 576], F32, kind="ExternalOutput")
    phase0_adaln(S, C, I, mod_d)
    phase1_qkv(S, C, I, mod_d, QT, KT, V)
    phase2_attn(S, C, I, mod_d, QT, KT, V, x1a, h2T)
    phase3_ffn(S, C, I, mod_d, x1a, h2T, x1)
    phase4a_h1(S, C, I, mod_d, x1, hT1)
    phase4b_pass1(S, C, I, mod_d, hT1, SAB)
    S.emit()
    return nc


def build_B(debug=False):
    nc = bass.Bass("TRN2", target_bir_lowering=False)
    I = declare_inputs(nc, B_INPUTS)
    S = Sched(nc)
    C = Common(S)
    mod_d = Buf("mod_in", I["mod_in"])
    x1 = Buf("x1", I["x1"])
    SALL = Buf("sall", I["sall"])
    SOWN = Buf("sown", I["sown"])
    hT1 = S.dram("hT1", [8, 128, NT1 * 128], BF16)
    x2 = S.dram("x2", [32 * 128, D], F32, kind="ExternalOutput" if debug else "Internal")
    h2T1 = S.dram("h2T1", [8, 128, 32 * 128], BF16)
    out_d = S.dram("out", [32 * 128, D], F32, kind="ExternalOutput")
    hin = S.sb("hin", [RB, 2, 8, RCH], F32)
    comb = S.sb("comb", [128, 32, NE], F32)
    phase4a_h1(S, C, I, mod_d, x1, hT1)
    phase5_fold(S, C, I, SALL, SOWN, hin)
    m = S.off
    phase6_pass2(S, C, I, mod_d, x1, hT1, hin, x2, h2T1, comb)
    S.off = m
    phase7_moe(S, C, I, mod_d, x2, h2T1, comb, out_d)
    S.emit()
    return nc


F_INPUTS = A_INPUTS + A_INPUTS2 + [
    ("sel", (RB, 8)), ("l1_norm2", (D,)), ("l1_w_out", (DRNN, D)), ("l1_router_w", (D, NE)), ("l1_router_b", (NE,)),
    ("l1_moe_w1", (NE, D, DFE)), ("l1_moe_w3", (NE, D, DFE)), ("l1_moe_w2", (NE, DFE, D)), ("rconst", (128, 216)),
]


SPARSE = True


def build_fused():
    nc = bass.Bass("TRN2", target_bir_lowering=False)
    I = declare_inputs(nc, F_INPUTS)
    S = Sched(nc)
    C = Common(S)
    mod_d = S.dram("mod_d", [2, 2, 6 * D], F32)
    QT = S.dram("QT", [8, 128, NCH * 128], BF16)
    KT = S.dram("KT", [8, 128, NCH * 128], BF16)
    V = S.dram("V", [NCH * 128, D], BF16)
    x1a = S.dram("x1a", [NT1 * 128, D], F32)
    h2T = S.dram("h2T", [8, 128, NT1 * 128], BF16)
    x1 = S.dram("x1", [NT1 * 128, D], F32)
    hT1 = S.dram("hT1", [8, 128, NT1 * 128], BF16)
    SAB = S.dram("sab_b", [RB, 576], F32)
    SALL = S.dram("sall_g", [4 * RB, 576], F32)
    x2 = S.dram("x2", [32 * 128, D], F32)
    h2T1 = S.dram("h2T1", [8, 128, 32 * 128], BF16)
    out_d = S.dram("out", [32 * 128, D], F32, kind="ExternalOutput")
    phase0_adaln(S, C, I, mod_d)
    phase1_qkv(S, C, I, mod_d, QT, KT, V)
    phase2_attn(S, C, I, mod_d, QT, KT, V, x1a, h2T)
    phase3_ffn(S, C, I, mod_d, x1a, h2T, x1)
    phase4a_h1(S, C, I, mod_d, x1, hT1)
    phase4b_pass1(S, C, I, mod_d, hT1, SAB)
    cc = Buf("cc")
    S.add("pool", lambda e: e.collective_compute("AllGather", ALU.bypass, replica_groups=[[0, 1, 2, 3], [4, 5, 6, 7]],
                                                  ins=[SAB.t.opt()], outs=[SALL.t.opt()]),
          reads=[SAB], writes=[SALL], dma_buf=cc, inc=1)
    hin = S.sb("hin", [RB, 2, 8, RCH], F32)
    comb = S.sb("comb", [128, 32, NE], F32)
    phase5_fold(S, C, I, SALL, SAB, hin)
    if not SPARSE:
        m = S.off
        phase6_pass2(S, C, I, mod_d, x1, hT1, hin, x2, h2T1, comb)
        S.off = m
        phase7_moe(S, C, I, mod_d, x2, h2T1, comb, out_d)
    else:
        maskall = S.sb("maskall", [128, 32, NE], F32)
        XS = S.dram("XS", [32 * 128, D], BF16)
        Hslot = S.dram("Hslot", [NGRP * GSZ, D], BF16)
        Tslot = S.dram("Tslot", [NGRP * GSZ, 8], I32)
        Yab = S.dram("Yab", [8192 + 128, D], F32)
        m = S.off
        phase6_pass2(S, C, I, mod_d, x1, hT1, hin, x2, h2T1, comb, XS=XS, maskall=maskall)
        S.off = m
        Rt = phase_route(S, C, I, comb, maskall)
        phase_permute(S, C, I, Rt, XS, Hslot, Tslot)
        phase7s_moe(S, C, I, mod_d, Rt, Hslot, Tslot, Yab)
        phase8_combine(S, C, I, mod_d, Rt, Yab, x2, out_d)
    S.emit()
    return nc


def make_bias_tables(rpb, k):
    T0 = 32 * k
    out = np.empty((5, 128, 16, 5, 128), np.float32)
    p = np.arange(128)

    def table(gt, kts):
        qr = 2 * gt + p // 64
        qc = p % 64
        rs_ = np.clip(qr - 4, 0, 248)
        cs_ = np.clip(qc - 8, 0, 48)
        tab = np.full((128, 16, 5, 128), NEG, np.float32)
        for j, kt in enumerate(kts):
            if kt < 0 or kt > 127:
                continue
            kr = (2 * kt + p // 64)[:, None]
            kcol = (p % 64)[:, None]
            inwin = (kr >= rs_[None]) & (kr < rs_[None] + 8) & (kcol >= cs_[None]) & (kcol < cs_[None] + 16)
            dr = np.clip(kr - qr[None] + 7, 0, 14)
            dc = np.clip(kcol - qc[None] + 15, 0, 30)
            vals = rpb[:, dr, dc]
            tab[:, :, j, :] = np.where(inwin[:, None, :], vals.transpose(1, 0, 2), NEG)
        return tab

    def kts_for(lt):
        gt = T0 - 1 + lt
        kts = [gt - 2 + j for j in range(5)]
        if gt == 0:
            kts[0] = 3
        if gt == 127:
            kts[4] = 124
        return gt, kts

    out[0] = table(10, [8, 9, 10, 11, 12])
    for i, lt in enumerate((1, 2, 31, 32)):
        gt, kts = kts_for(lt)
        if gt < 0 or gt > 127:
            out[1 + i] = out[0]
        else:
            out[1 + i] = table(gt, kts)
    return out


def make_xk(x, ctx, b, k):
    T0 = 32 * k
    xk = np.zeros((NCH * 128, D), np.float32)
    xk[0:256] = ctx[b]
    for j in range(NKC):
        gt = T0 - 3 + j
        if k == 0 and j == 1:
            gt = 3
        if k == 3 and j == 36:
            gt = 124
        if 0 <= gt < 128:
            xk[256 + 128 * j:256 + 128 * j + 128] = x[b, 128 * gt:128 * gt + 128]
    return xk


_CACHE = {}


def _make_rconst():
    rc = np.zeros((128, 216), np.float32)
    p = np.arange(128)
    rc[:, 0:128] = (p[:, None] < p[None, :]).astype(np.float32)
    rc[:, 128:144] = np.arange(16, dtype=np.float32)[None, :]
    rc[:, 144:152] = np.arange(8, dtype=np.float32)[None, :]
    rc[:, 152:184] = (np.arange(32)[None, :] * 128 + p[:, None]).astype(np.float32)
    rc[:, 184:216] = np.arange(32, dtype=np.float32)[None, :]
    return rc


RCONST = _make_rconst()


def kernel(**inputs):
    inp = {k: np.ascontiguousarray(np.asarray(v, dtype=np.float32)) for k, v in inputs.items()}
    if "F" not in _CACHE:
        _CACHE["F"] = build_fused()
    nc = _CACHE["F"]
    n = 8
    maps = []
    for i in range(n):
        b, k = i // 4, i % 4
        sel = np.zeros((RB, 8), np.float32)
        for j in range(4):
            if j < k:
                sel[:, j] = 1.0
            if j > k:
                sel[:, 4 + j] = 1.0
        m = {"xk": make_xk(inp["x"], inp["ctx"], b, k),
             "cv": np.stack([inp["c"][b], inp["c_ctx"]]).astype(np.float32),
             "bt": make_bias_tables(inp["l0_rpb"], k),
             "masks": np.tile(np.array([[0.0 if k == 0 else 1.0, 0.0 if k == 3 else 1.0]], np.float32), (128, 1)),
             "sel": sel, "rconst": RCONST}
        for name, _ in F_INPUTS:
            if name not in m:
                m[name] = inp[name]
        maps.append(m)
    res = run_bass_kernel_spmd(nc, maps, core_ids=list(range(n)))
    out = np.empty((2, 16384, D), np.float32)
    for i in range(n):
        b, k = i // 4, i % 4
        out[b, 4096 * k:4096 * k + 4096] = np.asarray(res.results[i]["out"])
    return out
```

```python
import numpy as np
from contextlib import ExitStack
import concourse.bass as bass
import concourse.mybir as mybir
from concourse.bass_utils import run_bass_kernel_spmd

F32 = mybir.dt.float32
BF16 = mybir.dt.bfloat16
AF = mybir.ActivationFunctionType
ALU = mybir.AluOpType
AX = mybir.AxisListType

ENGS = ("pe", "act", "dve", "pool", "sp")


class Buf:
    __slots__ = ("name", "t", "last_w", "reads")

    def __init__(self, name, t=None):
        self.name = name
        self.t = t
        self.last_w = None
        self.reads = []

    def __getitem__(self, k):
        return self.t[k]


class Op:
    __slots__ = ("eng", "fn", "deps", "signal", "pos", "dma_key", "val", "is_dma", "inc")

    def __init__(self, eng, fn):
        self.eng = eng
        self.fn = fn
        self.deps = []
        self.signal = False
        self.pos = 0
        self.is_dma = False
        self.dma_key = None
        self.val = 0
        self.inc = 16


class Sched:
    ARENA_F32 = 53000

    def __init__(self, nc, same_engine_sync=True):
        self.nc = nc
        self.ops = {e: [] for e in ENGS}
        self.same = same_engine_sync
        self.waited = {e: {} for e in ENGS}
        self.dma_cnt = {}
        self.dma_keys = []
        self.slot_of = {}
        self.bar_pos = {}
        self.off = 0
        self.arena = None
        self.peak = 0
        self.psum = None
        self.ndram = 0

    def sb(self, name, shape, dtype, off=None):
        if self.arena is None:
            self.arena = self.nc.alloc_sbuf_tensor("arena", [128, self.ARENA_F32], F32)
        esz = 2 if dtype == BF16 else 4
        nel = int(np.prod(shape[1:]))
        nbytes = (nel * esz + 63) // 64 * 64
        if off is None:
            off = self.off
            self.off += nbytes
        assert off + nbytes <= self.ARENA_F32 * 4, (name, off, nbytes)
        self.peak = max(self.peak, off + nbytes)
        a = self.arena[0:shape[0], off // 4: off // 4 + nbytes // 4]
        if dtype != F32:
            a = a.bitcast(dtype)
        a = a[:, 0:nel]
        if len(shape) > 2:
            names = "abcdefg"[:len(shape) - 1]
            pat = "p (" + " ".join(names) + ") -> p " + " ".join(names)
            a = a.rearrange(pat, **{nm: shape[1 + i] for i, nm in enumerate(names[:-1])})
        return Buf(name, a)

    def ps(self, name, col, ncols, dtype=F32, parts=128):
        if self.psum is None:
            self.psum = self.nc.alloc_psum_tensor("psum_all", [128, 4096], F32).ap()
        a = self.psum[0:parts, col:col + ncols]
        if dtype != F32:
            a = a.bitcast(dtype)
        return Buf(name, a)

    def dram(self, name, shape, dtype, kind="Internal"):
        return Buf(name, self.nc.dram_tensor(name, list(shape), dtype, kind=kind).ap())

    def _need(self, op, prod):
        if prod is None or prod is op:
            return
        e = op.eng
        w = self.waited[e]
        if prod.is_dma:
            k = ("d", prod.dma_key)
            if w.get(k, 0) >= prod.val:
                return
            w[k] = prod.val
            op.deps.append(prod)
            return
        if prod.eng == e and (not self.same or e == "pe"):
            return
        if w.get(prod.eng, -1) >= prod.pos:
            return
        w[prod.eng] = prod.pos
        prod.signal = True
        op.deps.append(prod)

    def add(self, eng, fn, reads=(), writes=(), dma_buf=None, inc=16):
        op = Op(eng, fn)
        op.inc = inc
        op.pos = len(self.ops[eng])
        if dma_buf is not None:
            op.is_dma = True
            bid = id(dma_buf)
            if bid not in self.slot_of:
                slot = len(self.slot_of)
                self.slot_of[bid] = slot
                if slot >= len(self.dma_keys):
                    self.dma_keys.append(slot)
                    self.dma_cnt[slot] = 0
            key = self.slot_of[bid]
            self.dma_cnt[key] += inc
            op.dma_key = key
            op.val = self.dma_cnt[key]
        for b in reads:
            self._need(op, b.last_w)
        for b in writes:
            self._need(op, b.last_w)
            for r in b.reads:
                self._need(op, r)
        for b in reads:
            b.reads.append(op)
        for b in writes:
            b.last_w = op
            b.reads = []
        self.ops[eng].append(op)
        return op

    def dma(self, eng, out, in_, sbuf, reads=(), writes=(), **kw):
        return self.add(eng, lambda e: e.dma_start(out=out, in_=in_, **kw),
                        reads=reads, writes=writes, dma_buf=sbuf)

    def barrier(self):
        lasts = []
        for e in ENGS:
            for op in reversed(self.ops[e]):
                if not op.is_dma and op.fn is not None:
                    lasts.append(op)
                    break
        last_dma = {}
        for e in ENGS:
            for op in self.ops[e][self.bar_pos.get(e, 0):]:
                if op.is_dma:
                    last_dma[op.dma_key] = op
        for e in ENGS:
            op = Op(e, None)
            op.pos = len(self.ops[e])
            for p in lasts:
                if p.eng != e:
                    self._need(op, p)
            for p in last_dma.values():
                self._need(op, p)
            self.ops[e].append(op)
            self.bar_pos[e] = len(self.ops[e])
        self.slot_of = {}

    def emit(self):
        nc = self.nc
        with ExitStack() as st:
            esem = {e: st.enter_context(nc.semaphore(f"s_{e}")) for e in ENGS}
            dsem = {k: st.enter_context(nc.semaphore(f"d_{i}")) for i, k in enumerate(self.dma_keys)}
            for e in ENGS:
                c = 0
                for op in self.ops[e]:
                    if op.is_dma:
                        continue
                    if op.signal:
                        c += 1
                        op.val = c
            block = st.enter_context(nc.Block())

            def run(ename, eng):
                for op in self.ops[ename]:
                    for p in op.deps:
                        if p.is_dma:
                            eng.wait_ge(dsem[p.dma_key], p.val)
                        else:
                            eng.wait_ge(esem[p.eng], p.val)
                    if op.fn is None:
                        continue
                    ins = op.fn(eng)
                    if op.is_dma:
                        ins.then_inc(dsem[op.dma_key], op.inc)
                    elif op.signal:
                        ins.then_inc(esem[ename], 1)

            @block.tensor
            def _(eng):
                run("pe", eng)

            @block.scalar
            def _(eng):
                run("act", eng)

            @block.vector
            def _(eng):
                run("dve", eng)

            @block.gpsimd
            def _(eng):
                run("pool", eng)

            @block.sync
            def _(eng):
                run("sp", eng)


D = 1024
KC = 8
NH = 16
HD = 64
NQT = 34
NKC = 38
NCH = 40
NT1 = 36
DFF = 2816
NFC = 22
DRNN = 1536
RCH = 16
RB = 96
NE = 8
DFE = 3584
EPS = 1e-6
NEG = -30000.0


def mm(S, ps, out, lhsT, rhs, start, stop, reads):
    S.add("pe", lambda e: e.matmul(out, lhsT=lhsT, rhs=rhs, start=start, stop=stop), reads=reads, writes=[ps])


def tr(S, ps, out, in_, ident, reads):
    S.add("pe", lambda e: e.transpose(out=out, in_=in_, identity=ident), reads=reads, writes=[ps])


def act(S, out, in_, func, reads, writes, bias=None, scale=None, accum_out=None):
    kw = {}
    if bias is not None:
        kw["bias"] = bias
    if scale is not None:
        kw["scale"] = scale
    if accum_out is not None:
        kw["accum_out"] = accum_out
    S.add("act", lambda e: e.activation(out=out, in_=in_, func=func, **kw), reads=reads, writes=writes)


def ts(S, eng, out, in0, s1, s2, op0, op1, reads, writes):
    if op1 is None:
        S.add(eng, lambda e: e.tensor_scalar(out=out, in0=in0, scalar1=s1, scalar2=None, op0=op0), reads=reads, writes=writes)
    else:
        S.add(eng, lambda e: e.tensor_scalar(out=out, in0=in0, scalar1=s1, scalar2=s2, op0=op0, op1=op1), reads=reads, writes=writes)


def stt(S, eng, out, in0, scalar, in1, op0, op1, reads, writes):
    S.add(eng, lambda e: e.scalar_tensor_tensor(out=out, in0=in0, scalar=scalar, in1=in1, op0=op0, op1=op1),
          reads=reads, writes=writes)


def tt(S, eng, out, in0, in1, op, reads, writes):
    S.add(eng, lambda e: e.tensor_tensor(out=out, in0=in0, in1=in1, op=op), reads=reads, writes=writes)


def cp(S, eng, out, in_, reads, writes):
    if eng == "act":
        S.add("act", lambda e: e.copy(out=out, in_=in_), reads=reads, writes=writes)
    else:
        S.add(eng, lambda e: e.tensor_copy(out=out, in_=in_), reads=reads, writes=writes)


def pp_view(vec_ap):
    return vec_ap.rearrange("(c p) -> p c", p=128)


def bc_view(row_ap, n):
    return bass.AP(row_ap.tensor, row_ap.offset, [[0, 128], [1, n]])


class Common:
    def __init__(self, S):
        self.identf = S.sb("identf", [128, 128], F32)
        self.identb = S.sb("identb", [128, 128], BF16)
        self.junk = S.sb("junk", [128, 1024], BF16)
        self.epsb = S.sb("epsb", [128, 1], F32)
        S.add("pool", lambda e: e.memset(self.epsb[:, :], EPS), writes=[self.epsb])
        for b, in (self.identf,), (self.identb,):
            S.add("pool", lambda e, b=b: e.memset(b[:], 1.0), writes=[b])
            S.add("pool", lambda e, b=b: e.affine_select(out=b[:], in_=b[:], pattern=[[-1, 128]], compare_op=ALU.is_equal,
                                                         fill=0.0, base=0, channel_multiplier=1), reads=[b], writes=[b])


def norm_tile(S, C, xt, ss, rstd, xs, pst, hT_dst, G, Sft, reads_extra=(), hT32_dst=None):
    hbuf, hfn = hT_dst
    act(S, C.junk[:, :], xt[:, :], AF.Square, reads=[xt], writes=[C.junk, ss], accum_out=ss[:, 0:1])
    ts(S, "dve", rstd[:, 0:1], ss[:, 0:1], 1.0 / D, EPS, ALU.mult, ALU.add, reads=[ss], writes=[rstd])
    act(S, rstd[:, 0:1], rstd[:, 0:1], AF.Sqrt, reads=[rstd], writes=[rstd])
    S.add("dve", lambda e: e.reciprocal(out=rstd[:, 0:1], in_=rstd[:, 0:1]), reads=[rstd], writes=[rstd])
    act(S, xs[:, :], xt[:, :], AF.Identity, reads=[xt, rstd], writes=[xs], scale=rstd[:, 0:1])
    for half in range(2):
        p = pst[half]
        for q in range(4):
            kc = half * 4 + q
            tr(S, p, p[:, 128 * q:128 * q + 128], xs[:, 128 * kc:128 * kc + 128], C.identf[:, :], reads=[xs, C.identf])
        for q in range(4):
            kc = half * 4 + q
            src = p[:, 128 * q:128 * q + 128]
            if hT32_dst is not None:
                b32, f32fn = hT32_dst
                if kc % 2 == 0:
                    ts(S, "dve", f32fn(kc), src, G[:, kc:kc + 1], Sft[:, kc:kc + 1], ALU.mult, ALU.add,
                       reads=[p, G, Sft], writes=[b32])
                else:
                    act(S, f32fn(kc), src, AF.Identity, reads=[p, G, Sft], writes=[b32],
                        bias=Sft[:, kc:kc + 1], scale=G[:, kc:kc + 1])
                cp(S, "pool", hfn(kc), f32fn(kc), reads=[b32], writes=[hbuf])
            elif kc % 2 == 0:
                ts(S, "dve", hfn(kc), src, G[:, kc:kc + 1], Sft[:, kc:kc + 1], ALU.mult, ALU.add,
                   reads=[p, G, Sft], writes=[hbuf])
            else:
                act(S, hfn(kc), src, AF.Identity, reads=[p, G, Sft], writes=[hbuf],
                    bias=Sft[:, kc:kc + 1], scale=G[:, kc:kc + 1])


def load_mod_pp(S, dst, col, mod_d, layer, stream, which, tmp_ok=True):
    src = pp_view(mod_d[layer, stream, which * D:(which + 1) * D])
    S.dma("sp", dst[:, col * 8:col * 8 + 8], src, dst, reads=[mod_d], writes=[dst], allow_slow_non_contiguous=True)


def phase0_adaln(S, C, I, mod_d):
    m0 = S.off
    cT = S.sb("cT", [128, 8, 2], F32)
    sc = S.sb("sc", [128, 8, 2], F32)
    rep = S.sb("rep", [128, 16, 128], F32)
    wblk = [S.sb(f"wblk{i}", [128, 8, 512], F32) for i in range(2)]
    bblk = [S.sb(f"bblk{i}", [128, 512], F32) for i in range(2)]
    res = [S.sb(f"res{i}", [128, 512], F32) for i in range(4)]
    pss = [S.ps(f"p0ps{i}", 512 * i, 512) for i in range(4)]
    for s in range(2):
        S.dma("sp", cT[:, :, s], pp_view(I["cv"][s, :]), cT, writes=[cT], allow_slow_non_contiguous=True)
    act(S, sc[:, :, :], cT[:, :, :], AF.Silu, reads=[cT], writes=[sc])
    for kc in range(8):
        for s in range(2):
            cp(S, "dve", rep[:, kc * 2 + s, :], sc[:, kc, s:s + 1].to_broadcast([128, 128]), reads=[sc], writes=[rep])
    it = 0
    for l in range(2):
        wm = I[f"l{l}_w_mod"]
        bm = I[f"l{l}_b_mod"]
        for j in range(12):
            wb = wblk[it % 2]
            bb = bblk[it % 2]
            S.dma("sp", wb[:, :, :], wm[:, 512 * j:512 * j + 512].rearrange("(kc p) n -> p kc n", p=128), wb, writes=[wb])
            S.dma("sp", bb[:, :], bc_view(bm[512 * j:512 * j + 512], 512), bb, writes=[bb])
            for s in range(2):
                p = pss[(it % 2) * 2 + s]
                r = res[(it % 2) * 2 + s]
                for kc in range(8):
                    mm(S, p, p[:, :], rep[:, kc * 2 + s, :], wb[:, kc, :], kc == 0, kc == 7, reads=[rep, wb])
                tt(S, "dve", r[:, :], p[:, :], bb[:, :], ALU.add, reads=[p, bb], writes=[r])
                S.dma("sp", mod_d[l, s:s + 1, 512 * j:512 * j + 512], r[0:1, :], r, reads=[r], writes=[mod_d])
            it += 1
    S.barrier()
    S.off = m0


def phase1_qkv(S, C, I, mod_d, QT, KT, V):
    m0 = S.off
    wq = S.sb("wqkv", [128, 8, 3072], BF16)
    S.dma("pool", wq[:, :, :], I["l0_w_qkv"].rearrange("(kc p) n -> p kc n", p=128), wq, writes=[wq])
    mods = S.sb("mods1", [128, 32], F32)
    tmpm = S.sb("tmpm1", [128, 24], F32)
    S.dma("sp", tmpm[:, 0:8], pp_view(I["l0_norm1"]), tmpm, writes=[tmpm], allow_slow_non_contiguous=True)
    for s in range(2):
        load_mod_pp(S, tmpm, 1 + s, mod_d, 0, s, 1)
        load_mod_pp(S, mods, 2 * s + 1, mod_d, 0, s, 0)
        stt(S, "dve", mods[:, 16 * s:16 * s + 8], tmpm[:, 8 + 8 * s:16 + 8 * s], 1.0, tmpm[:, 0:8], ALU.add, ALU.mult,
            reads=[tmpm], writes=[mods])
    Gs = [Buf("G", mods[:, 0:8]), Buf("Gc", mods[:, 16:24])]
    Ss = [Buf("S", mods[:, 8:16]), Buf("Sc", mods[:, 24:32])]
    gains = S.sb("gains", [128, 2], F32)
    for half in range(2):
        S.dma("sp", gains[64 * half:64 * half + 64, 0:1], I["l0_q_gain"].rearrange("(p o) -> p o", o=1), gains, writes=[gains])
        S.dma("sp", gains[64 * half:64 * half + 64, 1:2], I["l0_k_gain"].rearrange("(p o) -> p o", o=1), gains, writes=[gains])
    ts(S, "dve", gains[:, 0:1], gains[:, 0:1], HD ** -0.5, None, ALU.mult, None, reads=[gains], writes=[gains])
    bd = S.sb("bd", [128, 128], BF16)
    S.add("pool", lambda e: e.memset(bd[:, :], 0.0), writes=[bd])
    S.add("pool", lambda e: e.memset(bd[0:64, 0:64], 1.0 / 64), reads=[bd], writes=[bd])
    S.add("pool", lambda e: e.memset(bd[64:128, 64:128], 1.0 / 64), reads=[bd], writes=[bd])
    xin = [S.sb(f"xin{i}", [128, 1024], F32) for i in range(3)]
    xs = [S.sb(f"xs{i}", [128, 1024], F32) for i in range(2)]
    ss = [S.sb(f"ss{i}", [128, 1], F32) for i in range(2)]
    rstd = [S.sb(f"rstd{i}", [128, 1], F32) for i in range(2)]
    hT = [S.sb(f"hT{i}", [128, 8, 512], BF16) for i in range(2)]
    sq = [S.sb(f"sq{i}", [128, 512], BF16) for i in range(2)]
    rs = [S.sb(f"rs{i}", [128, 512], F32) for i in range(2)]
    qn = [S.sb(f"qn{i}", [128, 512], BF16) for i in range(3)]
    vt = [S.sb(f"vt{i}", [128, 1024], BF16) for i in range(2)]
    pT = [S.ps(f"pT{i}", 512 * i, 512) for i in range(2)]
    pQ = [S.ps(f"pQ{i}", 1024 + 512 * i, 512) for i in range(2)]
    pR = [S.ps(f"pR{i}", 2048 + 512 * i, 512) for i in range(2)]
    pV = [S.ps(f"pV{i}", 3072 + 512 * i, 512) for i in range(2)]
    nt = 0
    nqk = 0
    for blk in range(NCH // 4):
        hb = hT[blk % 2]
        for t in range(4):
            g = blk * 4 + t
            s = 0 if g >= 2 else 1
            xt = xin[nt % 3]
            S.dma("sp", xt[:, :], I["xk"][128 * g:128 * g + 128, :], xt, writes=[xt])
            norm_tile(S, C, xt, ss[nt % 2], rstd[nt % 2], xs[nt % 2], pT,
                      (hb, lambda kc, hb=hb, t=t: hb[:, kc, 128 * t:128 * t + 128]), Gs[s], Ss[s])
            nt += 1
        for which, dst_d in ((0, QT), (1, KT)):
            for hp in range(8):
                p = pQ[nqk % 2]
                pr = pR[nqk % 2]
                col = which * 1024 + 128 * hp
                for kc in range(8):
                    mm(S, p, p[:, :], wq[:, kc, col:col + 128], hb[:, kc, :], kc == 0, kc == 7, reads=[wq, hb])
                sqb = sq[nqk % 2]
                act(S, sqb[:, :], p[:, :], AF.Square, reads=[p], writes=[sqb])
                mm(S, pr, pr[:, :], bd[:, :], sqb[:, :], True, True, reads=[bd, sqb])
                rsb = rs[nqk % 2]
                act(S, rsb[:, :], pr[:, :], AF.Sqrt, reads=[pr, C.epsb], writes=[rsb], bias=C.epsb[:, 0:1])
                S.add("dve", lambda e, rsb=rsb: e.reciprocal(out=rsb[:, :], in_=rsb[:, :]), reads=[rsb], writes=[rsb])
                qb = qn[nqk % 3]
                stt(S, "dve", qb[:, :], p[:, :], gains[:, which:which + 1], rsb[:, :], ALU.mult, ALU.mult,
                    reads=[p, gains, rsb], writes=[qb])
                S.dma("sp", dst_d[hp, :, 512 * blk:512 * blk + 512], qb[:, :], qb, reads=[qb], writes=[dst_d])
                nqk += 1
        for t in range(4):
            g = blk * 4 + t
            vb = vt[g % 2]
            for cb in range(2):
                p = pV[cb]
                for kc in range(8):
                    mm(S, p, p[:, :], hb[:, kc, 128 * t:128 * t + 128], wq[:, kc, 2048 + 512 * cb:2048 + 512 * cb + 512],
                       kc == 0, kc == 7, reads=[hb, wq])
                cp(S, "act", vb[:, 512 * cb:512 * cb + 512], p[:, :], reads=[p], writes=[vb])
            S.dma("sp", V[128 * g:128 * g + 128, :], vb[:, :], vb, reads=[vb], writes=[V])
    S.barrier()
    S.off = m0


def phase2_attn(S, C, I, mod_d, QT, KT, V, x1a, h2T):
    m0 = S.off
    wo = S.sb("wo", [128, 8, 1024], BF16)
    S.dma("pool", wo[:, :, :], I["l0_w_o"].rearrange("(hp p) n -> p hp n", p=128), wo, writes=[wo])
    bgen = S.sb("bgen", [128, 16, 5, 128], BF16)
    bspec = S.sb("bspec", [128, 16, 5, 128], BF16)
    S.dma("pool", bgen[:, :, :, :], I["bt"][0], bgen, writes=[bgen])
    mods = S.sb("mods2", [128, 32], F32)
    tmpm = S.sb("tmpm2", [128, 24], F32)
    S.dma("sp", tmpm[:, 0:8], pp_view(I["l0_norm2"]), tmpm, writes=[tmpm], allow_slow_non_contiguous=True)
    g1b = []
    for s in range(2):
        load_mod_pp(S, tmpm, 1 + s, mod_d, 0, s, 4)
        load_mod_pp(S, mods, 2 * s + 1, mod_d, 0, s, 3)
        stt(S, "dve", mods[:, 16 * s:16 * s + 8], tmpm[:, 8 + 8 * s:16 + 8 * s], 1.0, tmpm[:, 0:8], ALU.add, ALU.mult,
            reads=[tmpm], writes=[mods])
        gb = S.sb(f"g1b{s}", [128, 1024], F32)
        S.dma("sp", gb[:, :], bc_view(mod_d[0, s, 2 * D:3 * D], D), gb, reads=[mod_d], writes=[gb])
        g1b.append(gb)
    Gs = [Buf("G2", mods[:, 0:8]), Buf("G2c", mods[:, 16:24])]
    Ss = [Buf("S2", mods[:, 8:16]), Buf("S2c", mods[:, 24:32])]
    RING = 6
    ktr = S.sb("ktr", [128, 8, RING, 128], BF16)
    vr = S.sb("vr", [128, RING, 16, 65], BF16)
    ktc = S.sb("ktc", [128, 8, 256], BF16)
    vc = S.sb("vc", [128, 2, 16, 65], BF16)
    kslots = [Buf(f"ks{i}", None) for i in range(RING)]
    vslots = [Buf(f"vs{i}", None) for i in range(RING)]
    S.add("pool", lambda e: e.memset(vr[:, :, :, 64:65], 1.0), writes=vslots)
    S.add("pool", lambda e: e.memset(vc[:, :, :, 64:65], 1.0), writes=[vc])
    S.dma("sp", ktc[:, :, :], KT[:, :, 0:256].rearrange("hp p t -> p hp t"), ktc, reads=[KT], writes=[ktc])
    for cc in range(2):
        S.dma("sp", vc[:, cc, :, 0:64], V[128 * cc:128 * cc + 128, :].rearrange("p (h d) -> p h d", h=16), vc,
              reads=[V], writes=[vc])
    qt = [S.sb(f"qt{i}", [128, 8, 128], BF16) for i in range(2)]
    xt_ = [S.sb(f"x2in{i}", [128, 1024], F32) for i in range(2)]
    tb = [S.sb(f"tb{i}", [128, 5, 128], F32) for i in range(2)]
    pt = [S.sb(f"pt{i}", [128, 7, 128], BF16) for i in range(2)]
    rec = [S.sb(f"rec{i}", [128, 1], F32) for i in range(4)]
    on = [S.sb(f"on{i}", [128, 16, 64], BF16) for i in range(2)]
    oT = [S.sb(f"oT{i}", [128, 8, 128], BF16) for i in range(2)]
    x1t = [S.sb(f"x1t{i}", [128, 1024], F32) for i in range(2)]
    xs = [S.sb(f"xs2{i}", [128, 1024], F32) for i in range(2)]
    ss = [S.sb(f"ss2{i}", [128, 1], F32) for i in range(2)]
    rstd = [S.sb(f"rstd2{i}", [128, 1], F32) for i in range(2)]
    h2 = [S.sb(f"h2t{i}", [128, 8, 128], BF16) for i in range(2)]
    pS = [S.ps(f"pS{i}", 1024 * i, 896) for i in range(2)]
    pO = [S.ps(f"pO{i}", 2048 + 128 * i, 65) for i in range(4)]
    pY = [S.ps(f"pY{i}", 2560 + 512 * i, 512) for i in range(2)]
    pOT = S.ps("pOT", 3584, 512, BF16)

    loaded = set()

    def ensure_chunk(g):
        if g in loaded:
            return
        loaded.add(g)
        sl = g % RING
        S.dma("sp", ktr[:, :, sl, :], KT[:, :, 128 * g:128 * g + 128].rearrange("hp p t -> p hp t"), kslots[sl],
              reads=[KT], writes=[kslots[sl]])
        S.dma("sp", vr[:, sl, :, 0:64], V[128 * g:128 * g + 128, :].rearrange("p (h d) -> p h d", h=16), vslots[sl],
              reads=[V], writes=[vslots[sl]])

    spec_lts = {1: 1, 2: 2, 31: 3, 32: 4}
    nh = 0
    for ti in range(NT1):
        is_ctx = ti < 2
        lt = ti - 2
        g = ti if is_ctx else lt + 4
        s = 1 if is_ctx else 0
        q = qt[ti % 2]
        S.dma("sp", q[:, :, :], QT[:, :, 128 * g:128 * g + 128].rearrange("hp p t -> p hp t"), q, reads=[QT], writes=[q])
        xt = xt_[ti % 2]
        S.dma("sp", xt[:, :], I["xk"][128 * g:128 * g + 128, :], xt, writes=[xt])
        nloc = 0 if is_ctx else 5
        if not is_ctx:
            for j in range(5):
                ensure_chunk(lt + 2 + j)
            if lt in spec_lts:
                S.dma("pool", bspec[:, :, :, :], I["bt"][spec_lts[lt]], bspec, writes=[bspec])
                bias = bspec
            else:
                bias = bgen
        onb = on[ti % 2]
        for h in range(NH):
            hp, half = h // 2, h % 2
            lo = 64 * half
            p = pS[nh % 2]
            ptb = pt[nh % 2]
            for j in range(nloc):
                sl = (lt + 2 + j) % RING
                mm(S, p, p[:, 128 * j:128 * j + 128], ktr[lo:lo + 64, hp, sl, :], q[lo:lo + 64, hp, :], True, True,
                   reads=[kslots[sl], q])
            for cc in range(2):
                jj = nloc + cc
                mm(S, p, p[:, 128 * jj:128 * jj + 128], ktc[lo:lo + 64, hp, 128 * cc:128 * cc + 128], q[lo:lo + 64, hp, :],
                   True, True, reads=[ktc, q])
            if nloc:
                t_ = tb[nh % 2]
                tt(S, "dve", t_[:, :, :], p[:, 0:640].rearrange("p (j q) -> p j q", j=5), bias[:, h, :, :], ALU.add,
                   reads=[p, bias], writes=[t_])
                act(S, ptb[:, 0:5, :], t_[:, :, :], AF.Exp, reads=[t_], writes=[ptb])
            act(S, ptb[:, nloc:nloc + 2, :], p[:, 128 * nloc:128 * nloc + 256].rearrange("p (j q) -> p j q", j=2), AF.Exp,
                reads=[p], writes=[ptb])
            po = pO[nh % 4]
            n = nloc + 2
            for j in range(nloc):
                sl = (lt + 2 + j) % RING
                mm(S, po, po[:, :], ptb[:, j, :], vr[:, sl, h, :], j == 0, False, reads=[ptb, vslots[sl]])
            for cc in range(2):
                mm(S, po, po[:, :], ptb[:, nloc + cc, :], vc[:, cc, h, :], (nloc + cc) == 0, cc == 1, reads=[ptb, vc])
            r_ = rec[nh % 4]
            S.add("dve", lambda e, r_=r_, po=po: e.reciprocal(out=r_[:, 0:1], in_=po[:, 64:65]), reads=[po], writes=[r_])
            ts(S, "dve", onb[:, h, :], po[:, 0:64], r_[:, 0:1], None, ALU.mult, None, reads=[po, r_], writes=[onb])
            nh += 1
        otb = oT[ti % 2]
        for hp in range(8):
            tr(S, pOT, pOT[:, 128 * hp:128 * hp + 128], onb[:, 2 * hp:2 * hp + 2, :].rearrange("p a b -> p (a b)"),
               C.identb[:, :], reads=[onb, C.identb])
        cp(S, "act", otb[:, 0:4, :], pOT[:, 0:512].rearrange("p (a b) -> p a b", a=4), reads=[pOT], writes=[otb])
        cp(S, "dve", otb[:, 4:8, :], pOT[:, 512:1024].rearrange("p (a b) -> p a b", a=4), reads=[pOT], writes=[otb])
        x1 = x1t[ti % 2]
        for cb in range(2):
            py = pY[cb]
            for hp in range(8):
                mm(S, py, py[:, :], otb[:, hp, :], wo[:, hp, 512 * cb:512 * cb + 512], hp == 0, hp == 7, reads=[otb, wo])
            tt(S, "dve", x1[:, 512 * cb:512 * cb + 512], py[:, :], g1b[s][:, 512 * cb:512 * cb + 512], ALU.mult,
               reads=[py, g1b[s]], writes=[x1])
        tt(S, "pool", x1[:, :], x1[:, :], xt[:, :], ALU.add, reads=[x1, xt], writes=[x1])
        S.dma("sp", x1a[128 * ti:128 * ti + 128, :], x1[:, :], x1, reads=[x1], writes=[x1a])
        hb = h2[ti % 2]
        norm_tile(S, C, x1, ss[ti % 2], rstd[ti % 2], xs[ti % 2], pY,
                  (hb, lambda kc, hb=hb: hb[:, kc, :]), Gs[s], Ss[s])
        S.dma("sp", h2T[:, :, 128 * ti:128 * ti + 128].rearrange("kc p t -> p kc t"), hb[:, :, :], hb, reads=[hb], writes=[h2T])
    S.barrier()
    S.off = m0


def phase3_ffn(S, C, I, mod_d, x1a, h2T, x1):
    m0 = S.off
    w1 = S.sb("w1", [128, 8, DFF], BF16)
    w3 = S.sb("w3", [128, 8, DFF], BF16)
    w2 = S.sb("w2", [128, NFC, 1024], BF16)
    for kc in range(8):
        S.dma("pool", w1[:, kc, :], I["l0_ffn_w1"][128 * kc:128 * kc + 128, :], w1, writes=[w1])
        S.dma("pool", w3[:, kc, :], I["l0_ffn_w3"][128 * kc:128 * kc + 128, :], w3, writes=[w3])
    for fc in range(NFC):
        S.dma("pool", w2[:, fc, :], I["l0_ffn_w2"][128 * fc:128 * fc + 128, :], w2, writes=[w2])
    g2b = []
    for s in range(2):
        gb = S.sb(f"g2b{s}", [128, 1024], F32)
        S.dma("sp", gb[:, :], bc_view(mod_d[0, s, 5 * D:6 * D], D), gb, reads=[mod_d], writes=[gb])
        g2b.append(gb)
    hb_ = [S.sb(f"h3b{i}", [128, 8, 512], BF16) for i in range(2)]
    hid = S.sb("hid", [128, NFC, 512], BF16)
    sa = [S.sb(f"sa{i}", [128, 512], F32) for i in range(2)]
    xa = [S.sb(f"xa{i}", [128, 1024], F32) for i in range(2)]
    xo = [S.sb(f"xo{i}", [128, 1024], F32) for i in range(2)]
    pA = [S.ps(f"pA{i}", 512 * i, 512) for i in range(2)]
    pB = [S.ps(f"pB{i}", 1024 + 512 * i, 512) for i in range(2)]
    pY = [S.ps(f"pY3{i}", 2048 + 512 * i, 512) for i in range(4)]
    nf = 0
    nt = 0
    for blk in range(NT1 // 4):
        hb = hb_[blk % 2]
        S.dma("sp", hb[:, :, :], h2T[:, :, 512 * blk:512 * blk + 512].rearrange("kc p t -> p kc t"), hb, reads=[h2T], writes=[hb])
        for fc in range(NFC):
            pa, pb = pA[nf % 2], pB[nf % 2]
            for kc in range(8):
                mm(S, pa, pa[:, :], w1[:, kc, 128 * fc:128 * fc + 128], hb[:, kc, :], kc == 0, kc == 7, reads=[w1, hb])
            for kc in range(8):
                mm(S, pb, pb[:, :], w3[:, kc, 128 * fc:128 * fc + 128], hb[:, kc, :], kc == 0, kc == 7, reads=[w3, hb])
            sb_ = sa[nf % 2]
            act(S, sb_[:, :], pa[:, :], AF.Silu, reads=[pa], writes=[sb_])
            tt(S, "dve", hid[:, fc, :], sb_[:, :], pb[:, :], ALU.mult, reads=[sb_, pb], writes=[hid])
            nf += 1
        for t in range(4):
            ti = blk * 4 + t
            s = 1 if ti < 2 else 0
            xab = xa[nt % 2]
            S.dma("sp", xab[:, :], x1a[128 * ti:128 * ti + 128, :], xab, reads=[x1a], writes=[xab])
            xob = xo[nt % 2]
            for cb in range(2):
                py = pY[(nt % 2) * 2 + cb]
                for fc in range(NFC):
                    mm(S, py, py[:, :], hid[:, fc, 128 * t:128 * t + 128], w2[:, fc, 512 * cb:512 * cb + 512],
                       fc == 0, fc == NFC - 1, reads=[hid, w2])
                tt(S, "dve", xob[:, 512 * cb:512 * cb + 512], py[:, :], g2b[s][:, 512 * cb:512 * cb + 512], ALU.mult,
                   reads=[py, g2b[s]], writes=[xob])
            tt(S, "pool", xob[:, :], xob[:, :], xab[:, :], ALU.add, reads=[xob, xab], writes=[xob])
            S.dma("sp", x1[128 * ti:128 * ti + 128, :], xob[:, :], xob, reads=[xob], writes=[x1])
            nt += 1
    S.barrier()
    S.off = m0


class RnnConsts:
    pass


def rnn_setup(S, C, I, mod_d, pass2):
    R = RnnConsts()
    R.win_x = S.sb("win_x", [128, 8, DRNN], BF16)
    S.dma("pool", R.win_x[:, :, :], I["l1_w_in"][:, DRNN:2 * DRNN].rearrange("(kc p) n -> p kc n", p=128), R.win_x,
          writes=[R.win_x])
    if pass2:
        R.win_g = S.sb("win_g", [128, 8, DRNN], BF16)
        S.dma("pool", R.win_g[:, :, :], I["l1_w_in"][:, 0:DRNN].rearrange("(kc p) n -> p kc n", p=128), R.win_g,
              writes=[R.win_g])
        R.wout = S.sb("wout", [RB, RCH, D], BF16)
        S.dma("pool", R.wout[:, :, :], I["l1_w_out"].rearrange("(ch p) n -> p ch n", p=RB), R.wout, writes=[R.wout])
    R.ga = S.sb("ga", [RB, 2, RCH, RB], BF16)
    R.gx = S.sb("gx", [RB, 2, RCH, RB], BF16)
    for d in range(2):
        S.dma("pool", R.ga[:, d, :, :], I["l1_gate_a_w"][d].rearrange("k c o -> c k o"), R.ga, writes=[R.ga])
        S.dma("pool", R.gx[:, d, :, :], I["l1_gate_x_w"][d].rearrange("k c o -> c k o"), R.gx, writes=[R.gx])
    R.cw = S.sb("cw", [RB, 5, RCH], F32)
    for j in range(4):
        S.dma("sp", R.cw[:, j, :], I["l1_conv_w"][j].rearrange("(ch p) -> p ch", p=RB), R.cw, writes=[R.cw],
              allow_slow_non_contiguous=True)
    S.dma("sp", R.cw[:, 4, :], I["l1_conv_b"].rearrange("(ch p) -> p ch", p=RB), R.cw, writes=[R.cw],
          allow_slow_non_contiguous=True)
    R.gb = S.sb("gb", [RB, 4, RCH], F32)
    R.c1 = S.sb("c1", [RB, 2, RCH], F32)
    lam = S.sb("lamt", [RB, 2 * RCH], F32)
    for d in range(2):
        S.dma("sp", R.gb[:, d, :], I["l1_gate_a_b"][d].rearrange("(ch p) -> p ch", p=RB), R.gb, writes=[R.gb],
              allow_slow_non_contiguous=True)
        S.dma("sp", R.gb[:, 2 + d, :], I["l1_gate_x_b"][d].rearrange("(ch p) -> p ch", p=RB), R.gb, writes=[R.gb],
              allow_slow_non_contiguous=True)
        S.dma("sp", lam[:, RCH * d:RCH * d + RCH], I["l1_lam"][d].rearrange("(ch p) -> p ch", p=RB), lam, writes=[lam],
              allow_slow_non_contiguous=True)
    n = 2 * RCH
    t = S.sb("sp_t", [RB, n], F32)
    w = S.sb("sp_w", [RB, n], F32)
    w2 = S.sb("sp_w2", [RB, n], F32)
    pl = S.sb("sp_pl", [RB, n], F32)
    s2 = S.sb("sp_s2", [RB, n], F32)
    mk = S.sb("sp_mk", [RB, n], F32)
    act(S, t[:, :], lam[:, :], AF.Exp, reads=[lam], writes=[t], scale=-1.0)
    ts(S, "dve", w[:, :], t[:, :], 2.0, None, ALU.add, None, reads=[t], writes=[w])
    S.add("dve", lambda e: e.reciprocal(out=w[:, :], in_=w[:, :]), reads=[w], writes=[w])
    tt(S, "dve", w[:, :], w[:, :], t[:, :], ALU.mult, reads=[w, t], writes=[w])
    tt(S, "dve", w2[:, :], w[:, :], w[:, :], ALU.mult, reads=[w], writes=[w2])
    ts(S, "dve", pl[:, :], w2[:, :], 1.0 / 11, 1.0 / 9, ALU.mult, ALU.add, reads=[w2], writes=[pl])
    for cf in (1.0 / 7, 1.0 / 5, 1.0 / 3, 1.0):
        tt(S, "dve", pl[:, :], pl[:, :], w2[:, :], ALU.mult, reads=[pl, w2], writes=[pl])
        ts(S, "dve", pl[:, :], pl[:, :], cf, None, ALU.add, None, reads=[pl], writes=[pl])
    tt(S, "dve", pl[:, :], pl[:, :], w[:, :], ALU.mult, reads=[pl, w], writes=[pl])
    ts(S, "dve", s2[:, :], t[:, :], 1.0, None, ALU.add, None, reads=[t], writes=[s2])
    act(S, s2[:, :], s2[:, :], AF.Ln, reads=[s2], writes=[s2])
    ts(S, "dve", mk[:, :], t[:, :], 0.5, None, ALU.is_lt, None, reads=[t], writes=[mk])
    stt(S, "dve", pl[:, :], pl[:, :], 2.0, s2[:, :], ALU.mult, ALU.subtract, reads=[pl, s2], writes=[pl])
    tt(S, "dve", pl[:, :], pl[:, :], mk[:, :], ALU.mult, reads=[pl, mk], writes=[pl])
    tt(S, "dve", pl[:, :], pl[:, :], s2[:, :], ALU.add, reads=[pl, s2], writes=[pl])
    ts(S, "dve", R.c1[:, :, :].rearrange("p a b -> p (a b)"), pl[:, :], -8.0, None, ALU.mult, None, reads=[pl], writes=[R.c1])
    R.ones = S.sb("ones_r", [RB, 1], F32)
    S.add("pool", lambda e: e.memset(R.ones[:, :], 1.0 + 2.0 ** -23), writes=[R.ones])
    R.ngb = S.sb("ngb", [RB, 4, RCH], F32)
    ts(S, "dve", R.ngb[:, :, :], R.gb[:, :, :], -1.0, None, ALU.mult, None, reads=[R.gb], writes=[R.ngb])
    R.msk = S.sb("msk", [128, 2], F32)
    S.dma("sp", R.msk[:, :], I["masks"], R.msk, writes=[R.msk])
    R.X = [S.sb(f"X{i}", [RB, 515], F32) for i in range(2)]
    R.xc = [S.sb(f"xc{i}", [RB, 512], F32) for i in range(2)]
    R.xcb = [S.sb(f"xcb{i}", [RB, 512], BF16) for i in range(2)]
    R.r = [S.sb(f"r{i}", [RB, 512], F32) for i in range(2)]
    R.iu = [S.sb(f"iu{i}", [RB, 512], F32) for i in range(2)]
    R.a = [S.sb(f"a{i}", [RB, 512], F32) for i in range(2)]
    R.m = [S.sb(f"m{i}", [RB, 512], F32) for i in range(2)]
    R.hs = [S.sb(f"hs{i}", [RB, 512], F32) for i in range(2)]
    R.win = [S.sb(f"hwin{i}", [128, 8, 516], BF16) for i in range(2)]
    R.pb = [S.ps(f"rb{i}", 512 * i, 512) for i in range(8)]
    R.pxB = [S.ps(f"pxB{i}", 3584 + 4 * i, 3, parts=RB) for i in range(2)]
    return R


def rnn_front(S, R, hwin, L, ch, nchunk, is_ctx, mask_before, mask_after):
    X = R.X[nchunk % 2]
    xc = R.xc[nchunk % 2]
    xcb = R.xcb[nchunk % 2]
    px = R.pb[nchunk % 2]
    col = 96 * ch
    if is_ctx:
        for kc in range(8):
            mm(S, px, px[0:RB, 0:L], R.win_x[:, kc, col:col + RB], hwin[:, kc, 0:L], kc == 0, kc == 7, reads=[R.win_x, hwin])
        S.add("pool", lambda e: e.memset(X[:, 0:2], 0.0), writes=[X])
        S.add("pool", lambda e: e.memset(X[:, L + 2:L + 3], 0.0), writes=[X])
        cp(S, "dve", X[:, 2:L + 2], px[0:RB, 0:L], reads=[px], writes=[X])
    else:
        pxb = R.pxB[nchunk % 2]
        for kc in range(8):
            mm(S, px, px[0:RB, 0:512], R.win_x[:, kc, col:col + RB], hwin[:, kc, 0:512], kc == 0, kc == 7, reads=[R.win_x, hwin])
        for kc in range(8):
            mm(S, pxb, pxb[:, 0:3], R.win_x[:, kc, col:col + RB], hwin[:, kc, 512:515], kc == 0, kc == 7, reads=[R.win_x, hwin])
        cp(S, "dve", X[:, 0:512], px[0:RB, 0:512], reads=[px], writes=[X])
        cp(S, "dve", X[:, 512:515], pxb[:, 0:3], reads=[pxb], writes=[X])
        if mask_before:
            ts(S, "dve", X[:, 0:2], X[:, 0:2], R.msk[0:RB, 0:1], None, ALU.mult, None, reads=[X, R.msk], writes=[X])
        if mask_after:
            ts(S, "dve", X[:, 514:515], X[:, 514:515], R.msk[0:RB, 1:2], None, ALU.mult, None, reads=[X, R.msk], writes=[X])
    ts(S, "dve", xc[:, 0:L], X[:, 0:L], R.cw[:, 0, ch:ch + 1], R.cw[:, 4, ch:ch + 1], ALU.mult, ALU.add,
       reads=[X, R.cw], writes=[xc])
    for j in range(1, 4):
        stt(S, "dve", xc[:, 0:L], X[:, j:j + L], R.cw[:, j, ch:ch + 1], xc[:, 0:L], ALU.mult, ALU.add,
            reads=[X, R.cw, xc], writes=[xc])
    cp(S, "pool", xcb[:, 0:L], xc[:, 0:L], reads=[xc], writes=[xcb])


def rnn_back(S, C, R, L, ch, nchunk, init, sumr, hs_out, extra_sig=None):
    xc = R.xc[nchunk % 2]
    xcb = R.xcb[nchunk % 2]
    for d in range(2):
        pr = R.pb[2 + d]
        pi = R.pb[4 + d]
        mm(S, pr, pr[0:RB, 0:L], R.ga[:, d, ch, :], xcb[:, 0:L], True, True, reads=[R.ga, xcb])
        mm(S, pi, pi[0:RB, 0:L], R.gx[:, d, ch, :], xcb[:, 0:L], True, True, reads=[R.gx, xcb])
    for d in range(2):
        pr = R.pb[2 + d]
        pi = R.pb[4 + d]
        r, iu = R.r[d], R.iu[d]
        if sumr is not None:
            act(S, r[:, 0:L], pr[0:RB, 0:L], AF.Sigmoid, reads=[pr, R.gb], writes=[r, sumr[d][0]],
                bias=R.gb[:, d, ch:ch + 1], accum_out=sumr[d][1])
        else:
            act(S, r[:, 0:L], pr[0:RB, 0:L], AF.Sigmoid, reads=[pr, R.gb], writes=[r], bias=R.gb[:, d, ch:ch + 1])
        act(S, iu[:, 0:L], pi[0:RB, 0:L], AF.Sigmoid, reads=[pi, R.gb], writes=[iu], bias=R.gb[:, 2 + d, ch:ch + 1])
    if extra_sig is not None:
        extra_sig()
    for d in range(2):
        r, a = R.r[d], R.a[d]
        act(S, a[:, 0:L], r[:, 0:L], AF.Exp, reads=[r, R.c1], writes=[a], scale=R.c1[:, d, ch:ch + 1])
    for d in range(2):
        a, m = R.a[d], R.m[d]
        act(S, m[:, 0:L], a[:, 0:L], AF.Square, reads=[a], writes=[m])
    for d in range(2):
        m = R.m[d]
        act(S, m[:, 0:L], m[:, 0:L], AF.Ln, reads=[m, R.ones], writes=[m], bias=R.ones[:, 0:1], scale=-1.0)
    for d in range(2):
        m = R.m[d]
        act(S, m[:, 0:L], m[:, 0:L], AF.Exp, reads=[m], writes=[m], scale=0.5)
    for d in range(2):
        r, iu, a, m, hs = R.r[d], R.iu[d], R.a[d], R.m[d], hs_out[d]
        tt(S, "dve", iu[:, 0:L], iu[:, 0:L], xc[:, 0:L], ALU.mult, reads=[iu, xc], writes=[iu])
        tt(S, "dve", iu[:, 0:L], iu[:, 0:L], m[:, 0:L], ALU.mult, reads=[iu, m], writes=[iu])
        ini = init[d]
        ini_reads = [] if isinstance(ini, float) else [ini[0]]
        ini_ap = ini if isinstance(ini, float) else ini[1]
        if d == 0:
            S.add("dve", lambda e, hs=hs, a=a, iu=iu, ini_ap=ini_ap: e.tensor_tensor_scan(
                out=hs[:, 0:L], data0=a[:, 0:L], data1=iu[:, 0:L], initial=ini_ap, op0=ALU.mult, op1=ALU.add),
                reads=[a, iu] + ini_reads, writes=[hs])
        else:
            def rv(buf):
                ap = buf[:, 0:L]
                return bass.AP(ap.tensor, ap.offset + (L - 1), [list(ap.ap[0]), [-1, L]])
            S.add("dve", lambda e, hs=hs, a=a, iu=iu, ini_ap=ini_ap: e.tensor_tensor_scan(
                out=rv(hs), data0=rv(a), data1=rv(iu), initial=ini_ap, op0=ALU.mult, op1=ALU.add),
                reads=[a, iu] + ini_reads, writes=[hs])


def phase4a_h1(S, C, I, mod_d, x1, hT1):
    m0 = S.off
    mods = S.sb("mods4", [128, 32], F32)
    tmpm = S.sb("tmpm4", [128, 24], F32)
    S.dma("sp", tmpm[:, 0:8], pp_view(I["l1_norm1"]), tmpm, writes=[tmpm], allow_slow_non_contiguous=True)
    for s in range(2):
        load_mod_pp(S, tmpm, 1 + s, mod_d, 1, s, 1)
        load_mod_pp(S, mods, 2 * s + 1, mod_d, 1, s, 0)
        stt(S, "dve", mods[:, 16 * s:16 * s + 8], tmpm[:, 8 + 8 * s:16 + 8 * s], 1.0, tmpm[:, 0:8], ALU.add, ALU.mult,
            reads=[tmpm], writes=[mods])
    Gs = [Buf("G4", mods[:, 0:8]), Buf("G4c", mods[:, 16:24])]
    Ss = [Buf("S4", mods[:, 8:16]), Buf("S4c", mods[:, 24:32])]
    xin = [S.sb(f"x4in{i}", [128, 1024], F32) for i in range(3)]
    xs = [S.sb(f"xs4{i}", [128, 1024], F32) for i in range(2)]
    ss = [S.sb(f"ss4{i}", [128, 1], F32) for i in range(2)]
    rstd = [S.sb(f"rstd4{i}", [128, 1], F32) for i in range(2)]
    hb_ = [S.sb(f"h4{i}", [128, 8, 128], BF16) for i in range(2)]
    pT = [S.ps(f"pT4{i}", 512 * i, 512) for i in range(2)]
    for ti in range(NT1):
        s = 1 if ti < 2 else 0
        xt = xin[ti % 3]
        S.dma("sp", xt[:, :], x1[128 * ti:128 * ti + 128, :], xt, reads=[x1], writes=[xt])
        hb = hb_[ti % 2]
        norm_tile(S, C, xt, ss[ti % 2], rstd[ti % 2], xs[ti % 2], pT, (hb, lambda kc, hb=hb: hb[:, kc, :]), Gs[s], Ss[s])
        S.dma("sp", hT1[:, :, 128 * ti:128 * ti + 128].rearrange("kc p t -> p kc t"), hb[:, :, :], hb, reads=[hb], writes=[hT1])
    S.barrier()
    S.off = m0


def seg_info(seg):
    if seg == 0:
        return True, 256, 0, False, False
    b = seg - 1
    s = 256 + 128 + 512 * b
    return False, 512, s - 2, b == 0, b == 7


def load_win(S, R, hT1, seg, n):
    is_ctx, L, w0, _, _ = seg_info(seg)
    hw = R.win[n % 2]
    wl = 256 if is_ctx else 515
    S.dma("sp", hw[:, :, 0:wl], hT1[:, :, w0:w0 + wl].rearrange("kc p t -> p kc t"), hw, reads=[hT1], writes=[hw])
    return hw


def phase4b_pass1(S, C, I, mod_d, hT1, SAB):
    m0 = S.off
    R = rnn_setup(S, C, I, mod_d, pass2=False)
    sab = S.sb("sab", [RB, 2, 9, 2, RCH], F32)
    S.add("pool", lambda e: e.memset(sab[:, 0, :, :, :], 0.0), writes=[sab])
    work = []
    for seg in range(9):
        for ch in range(RCH):
            work.append((seg, ch))
    wins = {0: load_win(S, R, hT1, 0, 0)}

    def front(n):
        seg, ch = work[n]
        is_ctx, L, w0, mb, ma = seg_info(seg)
        if ch == 0 and seg + 1 < 9:
            wins[seg + 1] = load_win(S, R, hT1, seg + 1, seg + 1)
        rnn_front(S, R, wins[seg], L, ch, n, is_ctx, mb, ma)

    front(0)
    for n, (seg, ch) in enumerate(work):
        is_ctx, L, w0, mb, ma = seg_info(seg)
        if n + 1 < len(work):
            front(n + 1)
        sumr = [(sab, sab[:, 0, seg, d, ch:ch + 1]) for d in range(2)]
        rnn_back(S, C, R, L, ch, n, [0.0, 0.0], sumr, R.hs)
        cp(S, "pool", sab[:, 1, seg, 0, ch:ch + 1], R.hs[0][:, L - 1:L], reads=[R.hs[0]], writes=[sab])
        cp(S, "pool", sab[:, 1, seg, 1, ch:ch + 1], R.hs[1][:, 0:1], reads=[R.hs[1]], writes=[sab])
    for seg in range(9):
        tt(S, "dve", sab[:, 0, seg, :, :], sab[:, 0, seg, :, :], R.c1[:, :, :], ALU.mult, reads=[sab, R.c1], writes=[sab])
    act(S, sab[:, 0, :, :, :], sab[:, 0, :, :, :], AF.Exp, reads=[sab], writes=[sab])
    S.dma("sp", SAB[:, :], sab[:, :, :, :, :].rearrange("p a s d c -> p (a s d c)"), sab, reads=[sab], writes=[SAB])
    S.barrier()
    S.off = m0


def phase5_fold(S, C, I, SALL, SOWN, hin, NR=4):
    m0 = S.off
    sall = S.sb("sall", [RB, NR, 2 * 9 * 2 * RCH], F32)
    sown = S.sb("sown", [RB, 2, 9, 2, RCH], F32)
    sel = S.sb("sel", [RB, 2 * NR], F32)
    S.dma("sp", sall[:, :, :], SALL.t.rearrange("(j p) f -> p j f", p=RB), sall, reads=[SALL], writes=[sall])
    S.dma("sp", sown[:, :, :, :, :].rearrange("p a s d c -> p (a s d c)"), SOWN[:, :], sown, reads=[SOWN], writes=[sown])
    S.dma("sp", sel[:, :], I["sel"], sel, writes=[sel])
    sv = sall[:, :, :].rearrange("p j (a s d c) -> p j a s d c", a=2, s=9, d=2)
    h = S.sb("hfold", [RB, RCH], F32)
    ae = S.sb("aeff", [RB, 8, RCH], F32)
    be = S.sb("beff", [RB, 8, RCH], F32)
    for d in range(2):
        cp(S, "dve", h[:, :], sown[:, 1, 0, d, :], reads=[sown], writes=[h])
        order = range(NR) if d == 0 else range(NR - 1, -1, -1)
        for j in order:
            mj = sel[:, d * NR + j:d * NR + j + 1]
            ts(S, "dve", ae[:, :, :], sv[:, j, 0, 1:9, d, :], -1.0, None, ALU.add, None, reads=[sall], writes=[ae])
            ts(S, "dve", ae[:, :, :], ae[:, :, :], mj, None, ALU.mult, None, reads=[ae, sel], writes=[ae])
            ts(S, "dve", ae[:, :, :], ae[:, :, :], 1.0, None, ALU.add, None, reads=[ae], writes=[ae])
            ts(S, "dve", be[:, :, :], sv[:, j, 1, 1:9, d, :], mj, None, ALU.mult, None, reads=[sall, sel], writes=[be])
            border = range(8) if d == 0 else range(7, -1, -1)
            for b in border:
                tt(S, "dve", h[:, :], h[:, :], ae[:, b, :], ALU.mult, reads=[h, ae], writes=[h])
                tt(S, "dve", h[:, :], h[:, :], be[:, b, :], ALU.add, reads=[h, be], writes=[h])
        border = list(range(8)) if d == 0 else list(range(7, -1, -1))
        for n, b in enumerate(border):
            cp(S, "dve", hin[:, d, b, :], h[:, :], reads=[h], writes=[hin])
            if n < 7:
                tt(S, "dve", h[:, :], h[:, :], sown[:, 0, 1 + b, d, :], ALU.mult, reads=[h, sown], writes=[h])
                tt(S, "dve", h[:, :], h[:, :], sown[:, 1, 1 + b, d, :], ALU.add, reads=[h, sown], writes=[h])
    S.barrier()
    S.off = m0


def phase6_pass2(S, C, I, mod_d, x1, hT1, hin, x2, h2T1, comb, XS=None, maskall=None):
    R = rnn_setup(S, C, I, mod_d, pass2=True)
    mods = S.sb("mods6", [128, 16], F32)
    tmpm = S.sb("tmpm6", [128, 16], F32)
    S.dma("sp", tmpm[:, 0:8], pp_view(I["l1_norm2"]), tmpm, writes=[tmpm], allow_slow_non_contiguous=True)
    load_mod_pp(S, tmpm, 1, mod_d, 1, 0, 4)
    load_mod_pp(S, mods, 1, mod_d, 1, 0, 3)
    stt(S, "dve", mods[:, 0:8], tmpm[:, 8:16], 1.0, tmpm[:, 0:8], ALU.add, ALU.mult, reads=[tmpm], writes=[mods])
    G2 = Buf("G6", mods[:, 0:8])
    S2 = Buf("S6", mods[:, 8:16])
    g1b = S.sb("g1b6", [128, 1024], F32)
    S.dma("sp", g1b[:, :], bc_view(mod_d[1, 0, 2 * D:3 * D], D), g1b, reads=[mod_d], writes=[g1b])
    wr = S.sb("wr", [128, 8, NE], F32)
    S.dma("sp", wr[:, :, :], I["l1_router_w"].rearrange("(kc p) e -> p kc e", p=128), wr, writes=[wr])
    brb = S.sb("brb", [128, NE], F32)
    S.dma("sp", brb[:, :], bc_view(I["l1_router_b"], NE), brb, writes=[brb])
    yin = S.sb("yin", [RB, RCH, 512], BF16)
    gg = [S.sb(f"gg{i}", [RB, 512], F32) for i in range(2)]
    xt_ = [S.sb(f"x6in{i}", [128, 1024], F32) for i in range(1)] * 2
    ytmp = [S.sb(f"y6{i}", [128, 1024], F32) for i in range(2)]
    xs = [S.sb(f"xs6{i}", [128, 1024], F32) for i in range(1)] * 2
    ss = [S.sb(f"ss6{i}", [128, 1], F32) for i in range(2)]
    rstd = [S.sb(f"rstd6{i}", [128, 1], F32) for i in range(2)]
    h2 = [S.sb(f"h6{i}", [128, 8, 128], BF16) for i in range(2)]
    h32 = [S.sb(f"h32{i}", [128, 8, 128], F32) for i in range(1)] * 2
    rt = [S.sb(f"rt{i}", [128, 48], F32) for i in range(2)]
    xsb = [S.sb(f"xsb{i}", [128, 1024], BF16) for i in range(2)] if XS is not None else None
    pg = R.pb[6]
    plog = S.ps("plog", 3584 + 16, 8)
    ntile = 0
    xg = [S.sb(f"xg{i}", [RB, 512], F32) for i in range(2)]
    work = [(seg, ch) for seg in range(1, 9) for ch in range(RCH)]
    wins = {1: load_win(S, R, hT1, 1, 0)}

    def front(n):
        seg, ch = work[n]
        is_ctx, L, w0, mb, ma = seg_info(seg)
        if ch == 0 and seg + 1 < 9:
            wins[seg + 1] = load_win(S, R, hT1, seg + 1, seg)
        rnn_front(S, R, wins[seg], L, ch, n, False, mb, ma)
        hw = wins[seg]
        for kc in range(8):
            mm(S, pg, pg[0:RB, :], R.win_g[:, kc, 96 * ch:96 * ch + RB], hw[:, kc, 2:514], kc == 0, kc == 7, reads=[R.win_g, hw])
        x_ = xg[n % 2]
        cp(S, "act", x_[:, :], pg[0:RB, :], reads=[pg], writes=[x_])

    front(0)
    for n, (seg, ch) in enumerate(work):
        b = seg - 1
        if True:
            init = [(hin, hin[:, d, b, ch:ch + 1]) for d in range(2)]
            x_ = xg[n % 2]
            g_ = gg[n % 2]
            act(S, g_[:, :], x_[:, :], AF.Square, reads=[x_], writes=[g_])
            ts(S, "dve", g_[:, :], g_[:, :], 0.044715, 1.0, ALU.mult, ALU.add, reads=[g_], writes=[g_])
            tt(S, "dve", g_[:, :], g_[:, :], x_[:, :], ALU.mult, reads=[g_, x_], writes=[g_])
            if n + 1 < len(work):
                front(n + 1)

            def gsig(g_=g_):
                act(S, g_[:, :], g_[:, :], AF.Sigmoid, reads=[g_], writes=[g_], scale=1.5957691216057308)
            rnn_back(S, C, R, 512, ch, n, init, None, R.hs, extra_sig=gsig)
            tt(S, "pool", g_[:, :], g_[:, :], x_[:, :], ALU.mult, reads=[g_, x_], writes=[g_])
            tt(S, "pool", R.hs[0][:, :], R.hs[0][:, :], R.hs[1][:, :], ALU.add, reads=[R.hs[0], R.hs[1]], writes=[R.hs[0]])
            tt(S, "pool", yin[:, ch, :], g_[:, :], R.hs[0][:, :], ALU.mult, reads=[g_, R.hs[0]], writes=[yin])
        if ch != RCH - 1:
            continue
        for t in range(4):
            mt = 4 * b + t
            ti = 2 + 1 + mt
            xt = xt_[ntile % 2]
            S.dma("sp", xt[:, :], x1[128 * ti:128 * ti + 128, :], xt, reads=[x1], writes=[xt])
            yt = ytmp[ntile % 2]
            for cb in range(2):
                py = R.pb[cb]
                for ch in range(RCH):
                    mm(S, py, py[:, :], yin[:, ch, 128 * t:128 * t + 128], R.wout[:, ch, 512 * cb:512 * cb + 512],
                       ch == 0, ch == RCH - 1, reads=[yin, R.wout])
                tt(S, "dve", yt[:, 512 * cb:512 * cb + 512], py[:, :], g1b[:, 512 * cb:512 * cb + 512], ALU.mult,
                   reads=[py, g1b], writes=[yt])
            tt(S, "pool", yt[:, :], yt[:, :], xt[:, :], ALU.add, reads=[yt, xt], writes=[yt])
            S.dma("sp", x2[128 * mt:128 * mt + 128, :], yt[:, :], yt, reads=[yt], writes=[x2])
            hb = h2[ntile % 2]
            hf = h32[ntile % 2]
            norm_tile(S, C, yt, ss[ntile % 2], rstd[ntile % 2], xs[ntile % 2], [R.pb[0], R.pb[1]],
                      (hb, lambda kc, hb=hb: hb[:, kc, :]), G2, S2,
                      hT32_dst=(hf, lambda kc, hf=hf: hf[:, kc, :]))
            if XS is None:
                S.dma("sp", h2T1[:, :, 128 * mt:128 * mt + 128].rearrange("kc p t -> p kc t"), hb[:, :, :], hb, reads=[hb], writes=[h2T1])
            else:
                xb_ = xsb[ntile % 2]
                cp(S, "pool", xb_[:, :], xs[ntile % 2][:, :], reads=[xs[ntile % 2]], writes=[xb_])
                S.dma("sp", XS[128 * mt:128 * mt + 128, :], xb_[:, :], xb_, reads=[xb_], writes=[XS])
            for kc in range(8):
                mm(S, plog, plog[:, :], hf[:, kc, :], wr[:, kc, :], kc == 0, kc == 7, reads=[hf, wr])
            r_ = rt[ntile % 2]
            lg, m8, ex, em, den, nv1 = r_[:, 0:8], r_[:, 8:16], r_[:, 16:24], r_[:, 24:32], r_[:, 32:33], r_[:, 33:34]
            tt(S, "dve", lg, plog[:, :], brb[:, :], ALU.add, reads=[plog, brb], writes=[r_])
            S.add("dve", lambda e, m8=m8, lg=lg: e.max(out=m8, in_=lg), reads=[r_], writes=[r_])
            ts(S, "dve", nv1, r_[:, 8:9], -1.0, None, ALU.mult, None, reads=[r_], writes=[r_])
            act(S, ex, lg, AF.Exp, reads=[r_], writes=[r_], bias=nv1)
            ts(S, "dve", em, lg, r_[:, 9:10], None, ALU.is_ge, None, reads=[r_], writes=[r_])
            if maskall is not None:
                cp(S, "dve", maskall[:, mt, :], em, reads=[r_], writes=[maskall])
            tt(S, "dve", em, em, ex, ALU.mult, reads=[r_], writes=[r_])
            S.add("dve", lambda e, den=den, em=em: e.reduce_sum(out=den, in_=em, axis=AX.X), reads=[r_], writes=[r_])
            S.add("dve", lambda e, den=den: e.reciprocal(out=den, in_=den), reads=[r_], writes=[r_])
            ts(S, "dve", comb[:, mt, :], em, den, None, ALU.mult, None, reads=[r_], writes=[comb])
            ntile += 1
    S.barrier()


def phase7_moe(S, C, I, mod_d, x2, h2T1, comb, out_d):
    m0 = S.off
    m5b = S.sb("m5b", [128, 1024], F32)
    S.dma("sp", m5b[:, :], bc_view(mod_d[1, 0, 5 * D:6 * D], D), m5b, reads=[mod_d], writes=[m5b])
    hT = S.sb("hTg", [128, 8, 2048], BF16)
    acc = [S.sb(f"acc{i}", [128, 1024], F32) for i in range(16)]
    NWB = 2
    w1g = [S.sb(f"w1g{i}", [128, 8, 512], BF16) for i in range(NWB)]
    w3g = [S.sb(f"w3g{i}", [128, 8, 512], BF16) for i in range(NWB)]
    w2g = [S.sb(f"w2g{i}", [128, 4, 1024], BF16) for i in range(NWB)]
    hid = [S.sb(f"hidm{i}", [128, 4, 512], BF16) for i in range(2)]
    sa = [S.sb(f"sam{i}", [128, 512], F32) for i in range(2)]
    xin = [S.sb(f"x7in{i}", [128, 1024], F32) for i in range(2)]
    pA = [S.ps(f"pA7{i}", 512 * i, 512) for i in range(2)]
    pB = [S.ps(f"pB7{i}", 1024 + 512 * i, 512) for i in range(2)]
    pY = [S.ps(f"pY7{i}", 2048 + 512 * i, 512) for i in range(4)]
    nw = 0
    nf = 0
    nh = 0
    nt = 0

    def load_w(e, fg, n):
        S.dma("pool", w1g[n % NWB][:, :, :], I["l1_moe_w1"][e, :, 512 * fg:512 * fg + 512].rearrange("(kc p) n -> p kc n", p=128),
              w1g[n % NWB], writes=[w1g[n % NWB]])
        S.dma("pool", w3g[n % NWB][:, :, :], I["l1_moe_w3"][e, :, 512 * fg:512 * fg + 512].rearrange("(kc p) n -> p kc n", p=128),
              w3g[n % NWB], writes=[w3g[n % NWB]])
        S.dma("pool", w2g[n % NWB][:, :, :], I["l1_moe_w2"][e, 512 * fg:512 * fg + 512, :].rearrange("(fc p) n -> p fc n", p=128),
              w2g[n % NWB], writes=[w2g[n % NWB]])

    steps = [(G, e, fg) for G in range(2) for e in range(NE) for fg in range(7)]
    load_w(steps[0][1], steps[0][2], 0)
    for si, (G, e, fg) in enumerate(steps):
        if e == 0 and fg == 0:
            S.dma("sp", hT[:, :, :], h2T1[:, :, 2048 * G:2048 * G + 2048].rearrange("kc p t -> p kc t"), hT, reads=[h2T1], writes=[hT])
        if si + 1 < len(steps):
            load_w(steps[si + 1][1], steps[si + 1][2], si + 1)
        a1, a3, a2 = w1g[si % NWB], w3g[si % NWB], w2g[si % NWB]
        first = (e == 0 and fg == 0)
        for tb in range(4):
            hd = hid[nh % 2]
            for fc in range(4):
                pa, pb = pA[nf % 2], pB[nf % 2]
                for kc in range(8):
                    mm(S, pa, pa[:, :], a1[:, kc, 128 * fc:128 * fc + 128], hT[:, kc, 512 * tb:512 * tb + 512], kc == 0, kc == 7,
                       reads=[a1, hT])
                for kc in range(8):
                    mm(S, pb, pb[:, :], a3[:, kc, 128 * fc:128 * fc + 128], hT[:, kc, 512 * tb:512 * tb + 512], kc == 0, kc == 7,
                       reads=[a3, hT])
                sb_ = sa[nf % 2]
                act(S, sb_[:, :], pa[:, :], AF.Silu, reads=[pa], writes=[sb_])
                tt(S, "dve", hd[:, fc, :], sb_[:, :], pb[:, :], ALU.mult, reads=[sb_, pb], writes=[hd])
                nf += 1
            for t in range(4):
                tl = 4 * tb + t
                mt = 16 * G + tl
                ac = acc[tl]
                for cb in range(2):
                    py = pY[(nt % 2) * 2 + cb]
                    for fc in range(4):
                        mm(S, py, py[:, :], hd[:, fc, 128 * t:128 * t + 128], a2[:, fc, 512 * cb:512 * cb + 512], fc == 0, fc == 3,
                           reads=[hd, a2])
                    sl = slice(512 * cb, 512 * cb + 512)
                    if first:
                        ts(S, "dve", ac[:, sl], py[:, :], comb[:, mt, e:e + 1], None, ALU.mult, None, reads=[py, comb], writes=[ac])
                    else:
                        stt(S, "dve", ac[:, sl], py[:, :], comb[:, mt, e:e + 1], ac[:, sl], ALU.mult, ALU.add,
                            reads=[py, comb, ac], writes=[ac])
                nt += 1
            nh += 1
        if e == NE - 1 and fg == 6:
            for tl in range(16):
                mt = 16 * G + tl
                xt = xin[tl % 2]
                S.dma("sp", xt[:, :], x2[128 * mt:128 * mt + 128, :], xt, reads=[x2], writes=[xt])
                ac = acc[tl]
                tt(S, "pool", ac[:, :], ac[:, :], m5b[:, :], ALU.mult, reads=[ac, m5b], writes=[ac])
                tt(S, "pool", xt[:, :], xt[:, :], ac[:, :], ALU.add, reads=[xt, ac], writes=[xt])
                S.dma("sp", out_d[128 * mt:128 * mt + 128, :], xt[:, :], xt, reads=[xt], writes=[out_d])
    S.barrier()
    S.off = m0


I32 = mybir.dt.int32
GSZ = 512
NGRP = 24
GT = GSZ // 128
NTB = 1
TBW = GSZ // NTB
TPB = TBW // 128


class Route:
    pass


def phase_route(S, C, I, comb, maskall):
    Rt = Route()
    Rt.slotA_i = S.sb("slotA_i", [128, 32], I32)
    Rt.slotB_i = S.sb("slotB_i", [128, 32], I32)
    Rt.gA = S.sb("gA", [128, 32], F32)
    Rt.gB = S.sb("gB", [128, 32], F32)
    Rt.Eg_i = S.sb("Eg_i", [128, NGRP], I32)
    Rt.tokA = S.sb("tokA", [128, 32, 8], I32)
    Rt.tokB = S.sb("tokB", [128, 32, 8], I32)
    m0 = S.off
    rc = S.sb("rc", [128, 216], F32)
    S.dma("sp", rc[:, :], I["rconst"], rc, writes=[rc])
    Lb = S.sb("Lb", [128, 128], BF16)
    ob = S.sb("ob", [128, 128], BF16)
    mb = S.sb("mb", [128, 256], BF16)
    cp(S, "dve", Lb[:, :], rc[:, 0:128], reads=[rc], writes=[Lb])
    S.add("pool", lambda e: e.memset(ob[:, :], 1.0), writes=[ob])
    cp(S, "dve", mb[:, :], maskall[:, :, :].rearrange("p t e -> p (t e)"), reads=[maskall], writes=[mb])
    p_r = S.ps("p_rin", 0, 256)
    p_c = S.ps("p_cnt", 512, 256)
    mm(S, p_r, p_r[:, :], Lb[:, :], mb[:, :], True, True, reads=[Lb, mb])
    mm(S, p_c, p_c[:, :], ob[:, :], mb[:, :], True, True, reads=[ob, mb])
    rin = S.sb("rin", [128, 32, NE], F32)
    cnt = S.sb("cnt", [128, 32, NE], F32)
    cp(S, "dve", rin[:, :, :].rearrange("p t e -> p (t e)"), p_r[:, :], reads=[p_r], writes=[rin])
    cp(S, "dve", cnt[:, :, :].rearrange("p t e -> p (t e)"), p_c[:, :], reads=[p_c], writes=[cnt])
    ones32 = S.sb("ones32", [128, 32], F32)
    S.add("pool", lambda e: e.memset(ones32[:, :], 1.0), writes=[ones32])
    inc = S.sb("inc", [128, NE, 32], F32)
    for e_ in range(NE):
        S.add("dve", lambda e, e_=e_: e.tensor_tensor_scan(out=inc[:, e_, :], data0=ones32[:, :], data1=cnt[:, :, e_],
                                                           initial=0.0, op0=ALU.mult, op1=ALU.add),
              reads=[ones32, cnt], writes=[inc])
    pre = S.sb("pre", [128, NE, 32], F32)
    tt(S, "dve", pre[:, :, :], inc[:, :, :], cnt[:, :, :].rearrange("p t e -> p e t"), ALU.subtract, reads=[inc, cnt], writes=[pre])
    sm = S.sb("route_sm", [128, 64], F32)
    n_e, G_, gend, gstart, sbase, tmp8 = (sm[:, 0:8], sm[:, 8:16], sm[:, 16:24], sm[:, 24:32], sm[:, 32:40], sm[:, 40:48])
    cp(S, "dve", n_e, inc[:, :, 31], reads=[inc], writes=[sm])
    ts(S, "dve", G_, n_e, 0.0, None, ALU.is_gt, None, reads=[sm], writes=[sm])
    for k in range(1, (4096 + GSZ - 1) // GSZ):
        ts(S, "dve", tmp8, n_e, float(GSZ * k), None, ALU.is_gt, None, reads=[sm], writes=[sm])
        tt(S, "dve", G_, G_, tmp8, ALU.add, reads=[sm], writes=[sm])
    S.add("dve", lambda e: e.tensor_tensor_scan(out=gend, data0=ones32[:, 0:8], data1=G_, initial=0.0, op0=ALU.mult, op1=ALU.add),
          reads=[sm, ones32], writes=[sm])
    tt(S, "dve", gstart, gend, G_, ALU.subtract, reads=[sm], writes=[sm])
    ts(S, "dve", sbase, gstart, float(GSZ), None, ALU.mult, None, reads=[sm], writes=[sm])
    v = S.sb("route_v", [128, 32, NE], F32)
    tt(S, "dve", v[:, :, :], rin[:, :, :], pre[:, :, :].rearrange("p e t -> p t e"), ALU.add, reads=[rin, pre], writes=[v])
    sb_b = bass.AP(sbase.tensor, sbase.offset, [list(sbase.ap[0]), [0, 32], [1, NE]])
    tt(S, "dve", v[:, :, :], v[:, :, :], sb_b, ALU.add, reads=[v, sm], writes=[v])
    stt(S, "dve", v[:, :, :], v[:, :, :], 1.0, maskall[:, :, :], ALU.add, ALU.mult, reads=[v, maskall], writes=[v])
    m8 = S.sb("route_m8", [128, 32, NE], F32)
    for t_ in range(32):
        S.add("dve", lambda e, t_=t_: e.max(out=m8[:, t_, :], in_=v[:, t_, :]), reads=[v], writes=[m8])
    sf = S.sb("route_sf", [128, 2, 32], F32)
    oh = S.sb("route_oh", [128, 32, NE], F32)
    for which, (sl_i, g_) in enumerate(((Rt.slotA_i, Rt.gA), (Rt.slotB_i, Rt.gB))):
        top = m8[:, :, which]
        ts(S, "dve", sf[:, which, :], top, -1.0, None, ALU.add, None, reads=[m8], writes=[sf])
        cp(S, "dve", sl_i[:, :], sf[:, which, :], reads=[sf], writes=[sl_i])
        top_b = bass.AP(top.tensor, top.offset, [list(top.ap[0]), list(top.ap[1]), [0, NE]])
        tt(S, "dve", oh[:, :, :], v[:, :, :], top_b, ALU.is_equal, reads=[v, m8], writes=[oh])
        tt(S, "dve", oh[:, :, :], oh[:, :, :], comb[:, :, :], ALU.mult, reads=[oh, comb], writes=[oh])
        S.add("dve", lambda e, g_=g_: e.reduce_sum(out=g_[:, :], in_=oh[:, :, :], axis=AX.X), reads=[oh], writes=[g_])
    Eg = S.sb("Eg_f", [128, NGRP], F32)
    ind = S.sb("route_ind", [128, 2, NGRP], F32)
    gio = rc[:, 184:184 + NGRP]
    S.add("pool", lambda e: e.memset(Eg[:, :], 0.0), writes=[Eg])
    for e_ in range(1, NE):
        ts(S, "dve", ind[:, 0, :], gio, gstart[:, e_:e_ + 1], None, ALU.is_ge, None, reads=[rc, sm], writes=[ind])
        ts(S, "dve", ind[:, 1, :], gio, gend[:, e_:e_ + 1], None, ALU.is_lt, None, reads=[rc, sm], writes=[ind])
        tt(S, "dve", ind[:, 0, :], ind[:, 0, :], ind[:, 1, :], ALU.mult, reads=[ind], writes=[ind])
        stt(S, "dve", Eg[:, :], ind[:, 0, :], float(e_), Eg[:, :], ALU.mult, ALU.add, reads=[ind, Eg], writes=[Eg])
    cp(S, "dve", Rt.Eg_i[:, :], Eg[:, :], reads=[Eg], writes=[Rt.Eg_i])
    tk = rc[:, 152:184]
    tk_b = bass.AP(tk.tensor, tk.offset, [list(tk.ap[0]), list(tk.ap[1]), [0, 8]])
    tkf = S.sb("tkf", [128, 32, 8], F32)
    cp(S, "dve", tkf[:, :, :], tk_b, reads=[rc], writes=[tkf])
    cp(S, "dve", Rt.tokA[:, :, :], tkf[:, :, :], reads=[tkf], writes=[Rt.tokA])
    ts(S, "dve", tkf[:, :, :], tkf[:, :, :], 4096.0, None, ALU.add, None, reads=[tkf], writes=[tkf])
    cp(S, "dve", Rt.tokB[:, :, :], tkf[:, :, :], reads=[tkf], writes=[Rt.tokB])
    S.barrier()
    S.off = m0
    return Rt


def phase_permute(S, C, I, Rt, XS, Hslot, Tslot):
    m0 = S.off
    zer = S.sb("zer", [128, GT * 1024], BF16)
    S.add("pool", lambda e: e.memset(zer[:, :], 0.0), writes=[zer])
    for i in range(NGRP):
        S.dma("sp", Hslot[GSZ * i:GSZ * i + GSZ, :].rearrange("(a p) n -> p a n", p=128),
              zer[:, :].rearrange("p (a n) -> p a n", a=GT), zer, reads=[zer], writes=[Hslot])
    dump = S.sb("dumpi", [128, 1024], I32)
    S.add("pool", lambda e: e.memset(dump[:, :], 8192), writes=[dump])
    S.dma("sp", Tslot.t.rearrange("(p a) o -> p (a o)", p=128), dump[:, 0:NGRP * GSZ // 128 * 8], dump, reads=[dump], writes=[Tslot])
    for mt in range(32):
        for sl, tk_ in ((Rt.slotA_i, Rt.tokA), (Rt.slotB_i, Rt.tokB)):
            S.add("pool", lambda e, sl=sl, tk_=tk_, mt=mt: e.indirect_dma_start(
                out=Tslot[:, :], out_offset=bass.IndirectOffsetOnAxis(ap=sl[:, mt:mt + 1], axis=0),
                in_=tk_[:, mt, :], in_offset=None, bounds_check=None),
                reads=[tk_, sl, Tslot], writes=[Tslot], dma_buf=tk_)
    xt = [S.sb(f"xperm{i}", [128, 1024], BF16) for i in range(3)]
    for mt in range(32):
        x_ = xt[mt % 3]
        S.dma("sp", x_[:, :], XS[128 * mt:128 * mt + 128, :], x_, reads=[XS], writes=[x_])
        for sl in (Rt.slotA_i, Rt.slotB_i):
            S.add("pool", lambda e, x_=x_, sl=sl, mt=mt: e.indirect_dma_start(
                out=Hslot[:, :], out_offset=bass.IndirectOffsetOnAxis(ap=sl[:, mt:mt + 1], axis=0),
                in_=x_[:, :], in_offset=None, bounds_check=None),
                reads=[x_, sl, Hslot], writes=[Hslot], dma_buf=x_)
    S.barrier()
    S.off = m0


def phase7s_moe(S, C, I, mod_d, Rt, Hslot, Tslot, Yab):
    m0 = S.off
    mods = S.sb("mods7", [128, 16], F32)
    tmpm = S.sb("tmpm7", [128, 16], F32)
    S.dma("sp", tmpm[:, 0:8], pp_view(I["l1_norm2"]), tmpm, writes=[tmpm], allow_slow_non_contiguous=True)
    load_mod_pp(S, tmpm, 1, mod_d, 1, 0, 4)
    load_mod_pp(S, mods, 1, mod_d, 1, 0, 3)
    stt(S, "dve", mods[:, 0:8], tmpm[:, 8:16], 1.0, tmpm[:, 0:8], ALU.add, ALU.mult, reads=[tmpm], writes=[mods])
    hT = [S.sb(f"hTs{i}", [128, 8, GSZ], BF16) for i in range(2)]
    acc = [S.sb(f"accs{i}", [128, 1024], F32) for i in range(GT)]
    NWB = 3
    w1g = [S.sb(f"w1s{i}", [128, 8, 512], BF16) for i in range(NWB)]
    w3g = [S.sb(f"w3s{i}", [128, 8, 512], BF16) for i in range(NWB)]
    w2g = [S.sb(f"w2s{i}", [128, 4, 1024], BF16) for i in range(NWB)]
    hid = [S.sb(f"hids{i}", [128, 4, 512], BF16) for i in range(2)]
    sa = [S.sb(f"sas{i}", [128, 512], F32) for i in range(2)]
    st_ = [S.sb(f"slt{i}", [128, 1024], BF16) for i in range(4)]
    tix = [S.sb(f"tix{i}", [128, 8], I32) for i in range(4)]
    pA = [S.ps(f"pA8{i}", 512 * i, 512) for i in range(2)]
    pB = [S.ps(f"pB8{i}", 1024 + 512 * i, 512) for i in range(2)]
    pY = [S.ps(f"pY8{i}", 2048 + 512 * i, 512) for i in range(4)]
    w1t, w3t, w2t = I["l1_moe_w1"], I["l1_moe_w3"], I["l1_moe_w2"]

    nreg = [0]

    def dyn_load(dst, static_ap, estride, g):
        off0 = static_ap.offset
        pat = [list(x) for x in static_ap.ap]
        tens = static_ap.tensor

        nreg[0] += 1
        rname = f"er{nreg[0]}"

        def allregs(e):
            hs = []
            try:
                while True:
                    nreg[0] += 1
                    hs.append(e.alloc_register(f"gc{nreg[0]}"))
            except ValueError:
                pass
            for h in hs:
                e.free_register(h)
            return hs

        def fn(e):
            before = allregs(e)
            with e.register(rname) as er:
                e.reg_load(er, Rt.Eg_i[0:1, g:g + 1])
                e.reg_mul(er, er, estride)
                e.reg_add(er, er, off0)
                ins = e.dma_start(out=dst[:, :, :], in_=bass.AP(tens, er, pat))
            after = {h.regnum for h in allregs(e)}
            for h in before:
                if h.regnum not in after:
                    e.free_register(h)
            return ins
        S.add("pool", fn, reads=[Rt.Eg_i], writes=[dst], dma_buf=dst)

    def load_w(g, fg, n):
        dyn_load(w1g[n % NWB], w1t[0, :, 512 * fg:512 * fg + 512].rearrange("(kc p) n -> p kc n", p=128), D * DFE, g)
        dyn_load(w3g[n % NWB], w3t[0, :, 512 * fg:512 * fg + 512].rearrange("(kc p) n -> p kc n", p=128), D * DFE, g)
        dyn_load(w2g[n % NWB], w2t[0, 512 * fg:512 * fg + 512, :].rearrange("(fc p) n -> p fc n", p=128), DFE * D, g)

    nst = [0]
    ntr = [0]

    def prologue(g):
        hb = hT[g % 2]
        for t in range(GT):
            x_ = st_[nst[0] % 4]
            nst[0] += 1
            S.dma("sp", x_[:, :], Hslot[GSZ * g + 128 * t:GSZ * g + 128 * t + 128, :], x_, reads=[Hslot], writes=[x_])
            p = pA[ntr[0] % 2]
            ntr[0] += 1
            pv = p.t.bitcast(BF16)
            for kc in range(8):
                tr(S, p, pv[:, 128 * kc:128 * kc + 128], x_[:, 128 * kc:128 * kc + 128], C.identb[:, :], reads=[x_, C.identb])
            for kc in range(8):
                src = pv[:, 128 * kc:128 * kc + 128]
                if kc % 2 == 0:
                    ts(S, "dve", hb[:, kc, 128 * t:128 * t + 128], src, mods[:, kc:kc + 1], mods[:, 8 + kc:9 + kc], ALU.mult, ALU.add,
                       reads=[p, mods], writes=[hb])
                else:
                    act(S, hb[:, kc, 128 * t:128 * t + 128], src, AF.Identity, reads=[p, mods], writes=[hb],
                        bias=mods[:, 8 + kc:9 + kc], scale=mods[:, kc:kc + 1])

    steps = [(g, fg) for g in range(NGRP) for fg in range(7)]
    load_w(0, 0, 0)
    load_w(0, 1, 1)
    prologue(0)
    nf = 0
    nh = 0
    nt = 0
    for si, (g, fg) in enumerate(steps):
        if si + 2 < len(steps):
            load_w(steps[si + 2][0], steps[si + 2][1], si + 2)
        if fg == 6 and g + 1 < NGRP:
            prologue(g + 1)
        hb = hT[g % 2]
        a1, a3, a2 = w1g[si % NWB], w3g[si % NWB], w2g[si % NWB]
        for tb in range(NTB):
            hd = hid[nh % 2]
            for fc in range(4):
                pa, pb = pA[nf % 2], pB[nf % 2]
                for kc in range(8):
                    mm(S, pa, pa[:, 0:TBW], a1[:, kc, 128 * fc:128 * fc + 128], hb[:, kc, TBW * tb:TBW * tb + TBW], kc == 0, kc == 7,
                       reads=[a1, hb])
                for kc in range(8):
                    mm(S, pb, pb[:, 0:TBW], a3[:, kc, 128 * fc:128 * fc + 128], hb[:, kc, TBW * tb:TBW * tb + TBW], kc == 0, kc == 7,
                       reads=[a3, hb])
                sb_ = sa[nf % 2]
                act(S, sb_[:, 0:TBW], pa[:, 0:TBW], AF.Silu, reads=[pa], writes=[sb_])
                tt(S, "dve", hd[:, fc, 0:TBW], sb_[:, 0:TBW], pb[:, 0:TBW], ALU.mult, reads=[sb_, pb], writes=[hd])
                nf += 1
            for t in range(TPB):
                tl = TPB * tb + t
                ac = acc[tl]
                for cb in range(2):
                    py = pY[(nt % 2) * 2 + cb]
                    for fc in range(4):
                        mm(S, py, py[:, :], hd[:, fc, 128 * t:128 * t + 128], a2[:, fc, 512 * cb:512 * cb + 512], fc == 0, fc == 3,
                           reads=[hd, a2])
                    sl = slice(512 * cb, 512 * cb + 512)
                    if fg == 0:
                        cp(S, "dve", ac[:, sl], py[:, :], reads=[py], writes=[ac])
                    else:
                        tt(S, "dve", ac[:, sl], py[:, :], ac[:, sl], ALU.add, reads=[py, ac], writes=[ac])
                if fg == 6:
                    tx = tix[(GT * g + tl) % 4]
                    S.dma("sp", tx[:, :], Tslot[GSZ * g + 128 * tl:GSZ * g + 128 * tl + 128, :], tx, reads=[Tslot], writes=[tx])
                    S.add("pool", lambda e, ac=ac, tx=tx: e.indirect_dma_start(
                        out=Yab[:, :], out_offset=bass.IndirectOffsetOnAxis(ap=tx[:, 0:1], axis=0),
                        in_=ac[:, :], in_offset=None, bounds_check=None),
                        reads=[ac, tx, Yab], writes=[Yab], dma_buf=ac)
                nt += 1
            nh += 1
    S.barrier()
    S.off = m0


def phase8_combine(S, C, I, mod_d, Rt, Yab, x2, out_d):
    m0 = S.off
    m5b = S.sb("m5bs", [128, 1024], F32)
    S.dma("sp", m5b[:, :], bc_view(mod_d[1, 0, 5 * D:6 * D], D), m5b, reads=[mod_d], writes=[m5b])
    ya = [S.sb(f"ya{i}", [128, 1024], F32) for i in range(2)]
    yb = [S.sb(f"yb{i}", [128, 1024], F32) for i in range(2)]
    xi = [S.sb(f"xc8{i}", [128, 1024], F32) for i in range(2)]
    for mt in range(32):
        a_, b_, x_ = ya[mt % 2], yb[mt % 2], xi[mt % 2]
        S.dma("sp", x_[:, :], x2[128 * mt:128 * mt + 128, :], x_, reads=[x2], writes=[x_])
        S.dma("sp", a_[:, :], Yab[128 * mt:128 * mt + 128, :], a_, reads=[Yab], writes=[a_])
        S.dma("sp", b_[:, :], Yab[4096 + 128 * mt:4096 + 128 * mt + 128, :], b_, reads=[Yab], writes=[b_])
        ts(S, "dve", a_[:, :], a_[:, :], Rt.gA[:, mt:mt + 1], None, ALU.mult, None, reads=[a_, Rt.gA], writes=[a_])
        stt(S, "dve", a_[:, :], b_[:, :], Rt.gB[:, mt:mt + 1], a_[:, :], ALU.mult, ALU.add, reads=[b_, Rt.gB, a_], writes=[a_])
        tt(S, "dve", a_[:, :], a_[:, :], m5b[:, :], ALU.mult, reads=[a_, m5b], writes=[a_])
        tt(S, "pool", x_[:, :], x_[:, :], a_[:, :], ALU.add, reads=[x_, a_], writes=[x_])
        S.dma("sp", out_d[128 * mt:128 * mt + 128, :], x_[:, :], x_, reads=[x_], writes=[out_d])
    S.barrier()
    S.off = m0


def declare_inputs(nc, names_shapes):
    I = {}
    for name, shape in names_shapes:
        I[name] = nc.dram_tensor(name, list(shape), F32, kind="ExternalInput").ap()
    return I


A_INPUTS = [
    ("xk", (NCH * 128, D)), ("cv", (2, D)), ("bt", (5, 128, 16, 5, 128)),
    ("l0_w_mod", (D, 6 * D)), ("l0_b_mod", (6 * D,)), ("l0_norm1", (D,)), ("l0_norm2", (D,)),
    ("l0_w_qkv", (D, 3 * D)), ("l0_q_gain", (HD,)), ("l0_k_gain", (HD,)), ("l0_w_o", (D, D)),
    ("l0_ffn_w1", (D, DFF)), ("l0_ffn_w3", (D, DFF)), ("l0_ffn_w2", (DFF, D)),
    ("l1_w_mod", (D, 6 * D)), ("l1_b_mod", (6 * D,)),
]


A_INPUTS2 = [
    ("l1_norm1", (D,)), ("l1_w_in", (D, 2 * DRNN)), ("l1_conv_w", (4, DRNN)), ("l1_conv_b", (DRNN,)),
    ("l1_gate_a_w", (2, RCH, RB, RB)), ("l1_gate_a_b", (2, DRNN)), ("l1_gate_x_w", (2, RCH, RB, RB)),
    ("l1_gate_x_b", (2, DRNN)), ("l1_lam", (2, DRNN)), ("masks", (128, 2)),
]
B_INPUTS = A_INPUTS2 + [
    ("x1", (NT1 * 128, D)), ("mod_in", (2, 2, 6 * D)), ("sall", (4 * RB, 576)), ("sown", (RB, 576)),
    ("sel", (RB, 8)), ("l1_norm2", (D,)), ("l1_w_out", (DRNN, D)), ("l1_router_w", (D, NE)), ("l1_router_b", (NE,)),
    ("l1_moe_w1", (NE, D, DFE)), ("l1_moe_w3", (NE, D, DFE)), ("l1_moe_w2", (NE, DFE, D)),
]


def build_A(debug=False):
    nc = bass.Bass("TRN2", target_bir_lowering=False)
    I = declare_inputs(nc, A_INPUTS + A_INPUTS2)
    S = Sched(nc)
    C = Common(S)
    mod_d = S.dram("mod_d", [2, 2, 6 * D], F32, kind="ExternalOutput")
    QT = S.dram("QT", [8, 128, NCH * 128], BF16)
    KT = S.dram("KT", [8, 128, NCH * 128], BF16)
    V = S.dram("V", [NCH * 128, D], BF16)
    x1a = S.dram("x1a", [NT1 * 128, D], F32, kind="ExternalOutput" if debug else "Internal")
    h2T = S.dram("h2T", [8, 128, NT1 * 128], BF16)
    x1 = S.dram("x1", [NT1 * 128, D], F32, kind="ExternalOutput")
    hT1 = S.dram("hT1", [8, 128, NT1 * 128], BF16)
    SAB = S.dram("sab_out", [RB, 576], F32, kind="ExternalOutput")
    phase0_adaln(S, C, I, mod_d)
    phase1_qkv(S, C, I, mod_d, QT, KT, V)
    phase2_attn(S, C, I, mod_d, QT, KT, V, x1a, h2T)
    phase3_ffn(S, C, I, mod_d, x1a, h2T, x1)
    phase4a_h1(S, C, I, mod_d, x1, hT1)
    phase4b_pass1(S, C, I, mod_d, hT1, SAB)
    S.emit()
    return nc


def build_B(debug=False):
    nc = bass.Bass("TRN2", target_bir_lowering=False)
    I = declare_inputs(nc, B_INPUTS)
    S = Sched(nc)
    C = Common(S)
    mod_d = Buf("mod_in", I["mod_in"])
    x1 = Buf("x1", I["x1"])
    SALL = Buf("sall", I["sall"])
    SOWN = Buf("sown", I["sown"])
    hT1 = S.dram("hT1", [8, 128, NT1 * 128], BF16)
    x2 = S.dram("x2", [32 * 128, D], F32, kind="ExternalOutput" if debug else "Internal")
    h2T1 = S.dram("h2T1", [8, 128, 32 * 128], BF16)
    out_d = S.dram("out", [32 * 128, D], F32, kind="ExternalOutput")
    hin = S.sb("hin", [RB, 2, 8, RCH], F32)
    comb = S.sb("comb", [128, 32, NE], F32)
    phase4a_h1(S, C, I, mod_d, x1, hT1)
    phase5_fold(S, C, I, SALL, SOWN, hin)
    m = S.off
    phase6_pass2(S, C, I, mod_d, x1, hT1, hin, x2, h2T1, comb)
    S.off = m
    phase7_moe(S, C, I, mod_d, x2, h2T1, comb, out_d)
    S.emit()
    return nc


F_INPUTS = A_INPUTS + A_INPUTS2 + [
    ("sel", (RB, 8)), ("l1_norm2", (D,)), ("l1_w_out", (DRNN, D)), ("l1_router_w", (D, NE)), ("l1_router_b", (NE,)),
    ("l1_moe_w1", (NE, D, DFE)), ("l1_moe_w3", (NE, D, DFE)), ("l1_moe_w2", (NE, DFE, D)), ("rconst", (128, 216)),
]


SPARSE = True


def build_fused():
    nc = bass.Bass("TRN2", target_bir_lowering=False)
    I = declare_inputs(nc, F_INPUTS)
    S = Sched(nc)
    C = Common(S)
    mod_d = S.dram("mod_d", [2, 2, 6 * D], F32)
    QT = S.dram("QT", [8, 128, NCH * 128], BF16)
    KT = S.dram("KT", [8, 128, NCH * 128], BF16)
    V = S.dram("V", [NCH * 128, D], BF16)
    x1a = S.dram("x1a", [NT1 * 128, D], F32)
    h2T = S.dram("h2T", [8, 128, NT1 * 128], BF16)
    x1 = S.dram("x1", [NT1 * 128, D], F32)
    hT1 = S.dram("hT1", [8, 128, NT1 * 128], BF16)
    SAB = S.dram("sab_b", [RB, 576], F32)
    SALL = S.dram("sall_g", [4 * RB, 576], F32)
    x2 = S.dram("x2", [32 * 128, D], F32)
    h2T1 = S.dram("h2T1", [8, 128, 32 * 128], BF16)
    out_d = S.dram("out", [32 * 128, D], F32, kind="ExternalOutput")
    phase0_adaln(S, C, I, mod_d)
    phase1_qkv(S, C, I, mod_d, QT, KT, V)
    phase2_attn(S, C, I, mod_d, QT, KT, V, x1a, h2T)
    phase3_ffn(S, C, I, mod_d, x1a, h2T, x1)
    phase4a_h1(S, C, I, mod_d, x1, hT1)
    phase4b_pass1(S, C, I, mod_d, hT1, SAB)
    cc = Buf("cc")
    S.add("pool", lambda e: e.collective_compute("AllGather", ALU.bypass, replica_groups=[[0, 1, 2, 3], [4, 5, 6, 7]],
                                                  ins=[SAB.t.opt()], outs=[SALL.t.opt()]),
          reads=[SAB], writes=[SALL], dma_buf=cc, inc=1)
    hin = S.sb("hin", [RB, 2, 8, RCH], F32)
    comb = S.sb("comb", [128, 32, NE], F32)
    phase5_fold(S, C, I, SALL, SAB, hin)
    if not SPARSE:
        m = S.off
        phase6_pass2(S, C, I, mod_d, x1, hT1, hin, x2, h2T1, comb)
        S.off = m
        phase7_moe(S, C, I, mod_d, x2, h2T1, comb, out_d)
    else:
        maskall = S.sb("maskall", [128, 32, NE], F32)
        XS = S.dram("XS", [32 * 128, D], BF16)
        Hslot = S.dram("Hslot", [NGRP * GSZ, D], BF16)
        Tslot = S.dram("Tslot", [NGRP * GSZ, 8], I32)
        Yab = S.dram("Yab", [8192 + 128, D], F32)
        m = S.off
        phase6_pass2(S, C, I, mod_d, x1, hT1, hin, x2, h2T1, comb, XS=XS, maskall=maskall)
        S.off = m
        Rt = phase_route(S, C, I, comb, maskall)
        phase_permute(S, C, I, Rt, XS, Hslot, Tslot)
        phase7s_moe(S, C, I, mod_d, Rt, Hslot, Tslot, Yab)
        phase8_combine(S, C, I, mod_d, Rt, Yab, x2, out_d)
    S.emit()
    return nc


def make_bias_tables(rpb, k):
    T0 = 32 * k
    out = np.empty((5, 128, 16, 5, 128), np.float32)
    p = np.arange(128)

    def table(gt, kts):
        qr = 2 * gt + p // 64
        qc = p % 64
        rs_ = np.clip(qr - 4, 0, 248)
        cs_ = np.clip(qc - 8, 0, 48)
        tab = np.full((128, 16, 5, 128), NEG, np.float32)
        for j, kt in enumerate(kts):
            if kt < 0 or kt > 127:
                continue
            kr = (2 * kt + p // 64)[:, None]
            kcol = (p % 64)[:, None]
            inwin = (kr >= rs_[None]) & (kr < rs_[None] + 8) & (kcol >= cs_[None]) & (kcol < cs_[None] + 16)
            dr = np.clip(kr - qr[None] + 7, 0, 14)
            dc = np.clip(kcol - qc[None] + 15, 0, 30)
            vals = rpb[:, dr, dc]
            tab[:, :, j, :] = np.where(inwin[:, None, :], vals.transpose(1, 0, 2), NEG)
        return tab

    def kts_for(lt):
        gt = T0 - 1 + lt
        kts = [gt - 2 + j for j in range(5)]
        if gt == 0:
            kts[0] = 3
        if gt == 127:
            kts[4] = 124
        return gt, kts

    out[0] = table(10, [8, 9, 10, 11, 12])
    for i, lt in enumerate((1, 2, 31, 32)):
        gt, kts = kts_for(lt)
        if gt < 0 or gt > 127:
            out[1 + i] = out[0]
        else:
            out[1 + i] = table(gt, kts)
    return out


def make_xk(x, ctx, b, k):
    T0 = 32 * k
    xk = np.zeros((NCH * 128, D), np.float32)
    xk[0:256] = ctx[b]
    for j in range(NKC):
        gt = T0 - 3 + j
        if k == 0 and j == 1:
            gt = 3
        if k == 3 and j == 36:
            gt = 124
        if 0 <= gt < 128:
            xk[256 + 128 * j:256 + 128 * j + 128] = x[b, 128 * gt:128 * gt + 128]
    return xk


_CACHE = {}


def _make_rconst():
    rc = np.zeros((128, 216), np.float32)
    p = np.arange(128)
    rc[:, 0:128] = (p[:, None] < p[None, :]).astype(np.float32)
    rc[:, 128:144] = np.arange(16, dtype=np.float32)[None, :]
    rc[:, 144:152] = np.arange(8, dtype=np.float32)[None, :]
    rc[:, 152:184] = (np.arange(32)[None, :] * 128 + p[:, None]).astype(np.float32)
    rc[:, 184:216] = np.arange(32, dtype=np.float32)[None, :]
    return rc


RCONST = _make_rconst()


def kernel(**inputs):
    inp = {k: np.ascontiguousarray(np.asarray(v, dtype=np.float32)) for k, v in inputs.items()}
    if "F" not in _CACHE:
        _CACHE["F"] = build_fused()
    nc = _CACHE["F"]
    n = 8
    maps = []
    for i in range(n):
        b, k = i // 4, i % 4
        sel = np.zeros((RB, 8), np.float32)
        for j in range(4):
            if j < k:
                sel[:, j] = 1.0
            if j > k:
                sel[:, 4 + j] = 1.0
        m = {"xk": make_xk(inp["x"], inp["ctx"], b, k),
             "cv": np.stack([inp["c"][b], inp["c_ctx"]]).astype(np.float32),
             "bt": make_bias_tables(inp["l0_rpb"], k),
             "masks": np.tile(np.array([[0.0 if k == 0 else 1.0, 0.0 if k == 3 else 1.0]], np.float32), (128, 1)),
             "sel": sel, "rconst": RCONST}
        for name, _ in F_INPUTS:
            if name not in m:
                m[name] = inp[name]
        maps.append(m)
    res = run_bass_kernel_spmd(nc, maps, core_ids=list(range(n)))
    out = np.empty((2, 16384, D), np.float32)
    for i in range(n):
        b, k = i // 4, i % 4
        out[b, 4096 * k:4096 * k + 4096] = np.asarray(res.results[i]["out"])
    return out
```

```python
import numpy as np
from contextlib import ExitStack
import concourse.bass as bass
import concourse.mybir as mybir
from concourse.bass_utils import run_bass_kernel_spmd

F32 = mybir.dt.float32
BF16 = mybir.dt.bfloat16
AF = mybir.ActivationFunctionType
ALU = mybir.AluOpType
AX = mybir.AxisListType

ENGS = ("pe", "act", "dve", "pool", "sp")


class Buf:
    __slots__ = ("name", "t", "last_w", "reads")

    def __init__(self, name, t=None):
        self.name = name
        self.t = t
        self.last_w = None
        self.reads = []

    def __getitem__(self, k):
        return self.t[k]


class Op:
    __slots__ = ("eng", "fn", "deps", "signal", "pos", "dma_key", "val", "is_dma", "inc")

    def __init__(self, eng, fn):
        self.eng = eng
        self.fn = fn
        self.deps = []
        self.signal = False
        self.pos = 0
        self.is_dma = False
        self.dma_key = None
        self.val = 0
        self.inc = 16


class Sched:
    ARENA_F32 = 53000

    def __init__(self, nc, same_engine_sync=True):
        self.nc = nc
        self.ops = {e: [] for e in ENGS}
        self.same = same_engine_sync
        self.waited = {e: {} for e in ENGS}
        self.dma_cnt = {}
        self.dma_keys = []
        self.slot_of = {}
        self.bar_pos = {}
        self.off = 0
        self.arena = None
        self.peak = 0
        self.psum = None
        self.ndram = 0

    def sb(self, name, shape, dtype, off=None):
        if self.arena is None:
            self.arena = self.nc.alloc_sbuf_tensor("arena", [128, self.ARENA_F32], F32)
        esz = 2 if dtype == BF16 else 4
        nel = int(np.prod(shape[1:]))
        nbytes = (nel * esz + 63) // 64 * 64
        if off is None:
            off = self.off
            self.off += nbytes
        assert off + nbytes <= self.ARENA_F32 * 4, (name, off, nbytes)
        self.peak = max(self.peak, off + nbytes)
        a = self.arena[0:shape[0], off // 4: off // 4 + nbytes // 4]
        if dtype != F32:
            a = a.bitcast(dtype)
        a = a[:, 0:nel]
        if len(shape) > 2:
            names = "abcdefg"[:len(shape) - 1]
            pat = "p (" + " ".join(names) + ") -> p " + " ".join(names)
            a = a.rearrange(pat, **{nm: shape[1 + i] for i, nm in enumerate(names[:-1])})
        return Buf(name, a)

    def ps(self, name, col, ncols, dtype=F32, parts=128):
        if self.psum is None:
            self.psum = self.nc.alloc_psum_tensor("psum_all", [128, 4096], F32).ap()
        a = self.psum[0:parts, col:col + ncols]
        if dtype != F32:
            a = a.bitcast(dtype)
        return Buf(name, a)

    def dram(self, name, shape, dtype, kind="Internal"):
        return Buf(name, self.nc.dram_tensor(name, list(shape), dtype, kind=kind).ap())

    def _need(self, op, prod):
        if prod is None or prod is op:
            return
        e = op.eng
        w = self.waited[e]
        if prod.is_dma:
            k = ("d", prod.dma_key)
            if w.get(k, 0) >= prod.val:
                return
            w[k] = prod.val
            op.deps.append(prod)
            return
        if prod.eng == e and (not self.same or e == "pe"):
            return
        if w.get(prod.eng, -1) >= prod.pos:
            return
        w[prod.eng] = prod.pos
        prod.signal = True
        op.deps.append(prod)

    def add(self, eng, fn, reads=(), writes=(), dma_buf=None, inc=16):
        op = Op(eng, fn)
        op.inc = inc
        op.pos = len(self.ops[eng])
        if dma_buf is not None:
            op.is_dma = True
            bid = id(dma_buf)
            if bid not in self.slot_of:
                slot = len(self.slot_of)
                self.slot_of[bid] = slot
                if slot >= len(self.dma_keys):
                    self.dma_keys.append(slot)
                    self.dma_cnt[slot] = 0
            key = self.slot_of[bid]
            self.dma_cnt[key] += inc
            op.dma_key = key
            op.val = self.dma_cnt[key]
        for b in reads:
            self._need(op, b.last_w)
        for b in writes:
            self._need(op, b.last_w)
            for r in b.reads:
                self._need(op, r)
        for b in reads:
            b.reads.append(op)
        for b in writes:
            b.last_w = op
            b.reads = []
        self.ops[eng].append(op)
        return op

    def dma(self, eng, out, in_, sbuf, reads=(), writes=(), **kw):
        return self.add(eng, lambda e: e.dma_start(out=out, in_=in_, **kw),
                        reads=reads, writes=writes, dma_buf=sbuf)

    def barrier(self):
        lasts = []
        for e in ENGS:
            for op in reversed(self.ops[e]):
                if not op.is_dma and op.fn is not None:
                    lasts.append(op)
                    break
        last_dma = {}
        for e in ENGS:
            for op in self.ops[e][self.bar_pos.get(e, 0):]:
                if op.is_dma:
                    last_dma[op.dma_key] = op
        for e in ENGS:
            op = Op(e, None)
            op.pos = len(self.ops[e])
            for p in lasts:
                if p.eng != e:
                    self._need(op, p)
            for p in last_dma.values():
                self._need(op, p)
            self.ops[e].append(op)
            self.bar_pos[e] = len(self.ops[e])
        self.slot_of = {}

    def emit(self):
        nc = self.nc
        with ExitStack() as st:
            esem = {e: st.enter_context(nc.semaphore(f"s_{e}")) for e in ENGS}
            dsem = {k: st.enter_context(nc.semaphore(f"d_{i}")) for i, k in enumerate(self.dma_keys)}
            for e in ENGS:
                c = 0
                for op in self.ops[e]:
                    if op.is_dma:
                        continue
                    if op.signal:
                        c += 1
                        op.val = c
            block = st.enter_context(nc.Block())

            def run(ename, eng):
                for op in self.ops[ename]:
                    for p in op.deps:
                        if p.is_dma:
                            eng.wait_ge(dsem[p.dma_key], p.val)
                        else:
                            eng.wait_ge(esem[p.eng], p.val)
                    if op.fn is None:
                        continue
                    ins = op.fn(eng)
                    if op.is_dma:
                        ins.then_inc(dsem[op.dma_key], op.inc)
                    elif op.signal:
                        ins.then_inc(esem[ename], 1)

            @block.tensor
            def _(eng):
                run("pe", eng)

            @block.scalar
            def _(eng):
                run("act", eng)

            @block.vector
            def _(eng):
                run("dve", eng)

            @block.gpsimd
            def _(eng):
                run("pool", eng)

            @block.sync
            def _(eng):
                run("sp", eng)


D = 1024
KC = 8
NH = 16
HD = 64
NQT = 34
NKC = 38
NCH = 40
NT1 = 36
DFF = 2816
NFC = 22
DRNN = 1536
RCH = 16
RB = 96
NE = 8
DFE = 3584
EPS = 1e-6
NEG = -30000.0


def mm(S, ps, out, lhsT, rhs, start, stop, reads):
    S.add("pe", lambda e: e.matmul(out, lhsT=lhsT, rhs=rhs, start=start, stop=stop), reads=reads, writes=[ps])


def tr(S, ps, out, in_, ident, reads):
    S.add("pe", lambda e: e.transpose(out=out, in_=in_, identity=ident), reads=reads, writes=[ps])


def act(S, out, in_, func, reads, writes, bias=None, scale=None, accum_out=None):
    kw = {}
    if bias is not None:
        kw["bias"] = bias
    if scale is not None:
        kw["scale"] = scale
    if accum_out is not None:
        kw["accum_out"] = accum_out
    S.add("act", lambda e: e.activation(out=out, in_=in_, func=func, **kw), reads=reads, writes=writes)


def ts(S, eng, out, in0, s1, s2, op0, op1, reads, writes):
    if op1 is None:
        S.add(eng, lambda e: e.tensor_scalar(out=out, in0=in0, scalar1=s1, scalar2=None, op0=op0), reads=reads, writes=writes)
    else:
        S.add(eng, lambda e: e.tensor_scalar(out=out, in0=in0, scalar1=s1, scalar2=s2, op0=op0, op1=op1), reads=reads, writes=writes)


def stt(S, eng, out, in0, scalar, in1, op0, op1, reads, writes):
    S.add(eng, lambda e: e.scalar_tensor_tensor(out=out, in0=in0, scalar=scalar, in1=in1, op0=op0, op1=op1),
          reads=reads, writes=writes)


def tt(S, eng, out, in0, in1, op, reads, writes):
    S.add(eng, lambda e: e.tensor_tensor(out=out, in0=in0, in1=in1, op=op), reads=reads, writes=writes)


def cp(S, eng, out, in_, reads, writes):
    if eng == "act":
        S.add("act", lambda e: e.copy(out=out, in_=in_), reads=reads, writes=writes)
    else:
        S.add(eng, lambda e: e.tensor_copy(out=out, in_=in_), reads=reads, writes=writes)


def pp_view(vec_ap):
    return vec_ap.rearrange("(c p) -> p c", p=128)


def bc_view(row_ap, n):
    return bass.AP(row_ap.tensor, row_ap.offset, [[0, 128], [1, n]])


class Common:
    def __init__(self, S):
        self.identf = S.sb("identf", [128, 128], F32)
        self.identb = S.sb("identb", [128, 128], BF16)
        self.junk = S.sb("junk", [128, 1024], BF16)
        self.epsb = S.sb("epsb", [128, 1], F32)
        S.add("pool", lambda e: e.memset(self.epsb[:, :], EPS), writes=[self.epsb])
        for b, in (self.identf,), (self.identb,):
            S.add("pool", lambda e, b=b: e.memset(b[:], 1.0), writes=[b])
            S.add("pool", lambda e, b=b: e.affine_select(out=b[:], in_=b[:], pattern=[[-1, 128]], compare_op=ALU.is_equal,
                                                         fill=0.0, base=0, channel_multiplier=1), reads=[b], writes=[b])


def norm_tile(S, C, xt, ss, rstd, xs, pst, hT_dst, G, Sft, reads_extra=(), hT32_dst=None):
    hbuf, hfn = hT_dst
    act(S, C.junk[:, :], xt[:, :], AF.Square, reads=[xt], writes=[C.junk, ss], accum_out=ss[:, 0:1])
    ts(S, "dve", rstd[:, 0:1], ss[:, 0:1], 1.0 / D, EPS, ALU.mult, ALU.add, reads=[ss], writes=[rstd])
    act(S, rstd[:, 0:1], rstd[:, 0:1], AF.Sqrt, reads=[rstd], writes=[rstd])
    S.add("dve", lambda e: e.reciprocal(out=rstd[:, 0:1], in_=rstd[:, 0:1]), reads=[rstd], writes=[rstd])
    act(S, xs[:, :], xt[:, :], AF.Identity, reads=[xt, rstd], writes=[xs], scale=rstd[:, 0:1])
    for half in range(2):
        p = pst[half]
        for q in range(4):
            kc = half * 4 + q
            tr(S, p, p[:, 128 * q:128 * q + 128], xs[:, 128 * kc:128 * kc + 128], C.identf[:, :], reads=[xs, C.identf])
        for q in range(4):
            kc = half * 4 + q
            src = p[:, 128 * q:128 * q + 128]
            if hT32_dst is not None:
                b32, f32fn = hT32_dst
                if kc % 2 == 0:
                    ts(S, "dve", f32fn(kc), src, G[:, kc:kc + 1], Sft[:, kc:kc + 1], ALU.mult, ALU.add,
                       reads=[p, G, Sft], writes=[b32])
                else:
                    act(S, f32fn(kc), src, AF.Identity, reads=[p, G, Sft], writes=[b32],
                        bias=Sft[:, kc:kc + 1], scale=G[:, kc:kc + 1])
                cp(S, "pool", hfn(kc), f32fn(kc), reads=[b32], writes=[hbuf])
            elif kc % 2 == 0:
                ts(S, "dve", hfn(kc), src, G[:, kc:kc + 1], Sft[:, kc:kc + 1], ALU.mult, ALU.add,
                   reads=[p, G, Sft], writes=[hbuf])
            else:
                act(S, hfn(kc), src, AF.Identity, reads=[p, G, Sft], writes=[hbuf],
                    bias=Sft[:, kc:kc + 1], scale=G[:, kc:kc + 1])


def load_mod_pp(S, dst, col, mod_d, layer, stream, which, tmp_ok=True):
    src = pp_view(mod_d[layer, stream, which * D:(which + 1) * D])
    S.dma("sp", dst[:, col * 8:col * 8 + 8], src, dst, reads=[mod_d], writes=[dst], allow_slow_non_contiguous=True)


def phase0_adaln(S, C, I, mod_d):
    m0 = S.off
    cT = S.sb("cT", [128, 8, 2], F32)
    sc = S.sb("sc", [128, 8, 2], F32)
    rep = S.sb("rep", [128, 16, 128], BF16)
    wblk = [S.sb(f"wblk{i}", [128, 8, 512], BF16) for i in range(3)]
    bblk = [S.sb(f"bblk{i}", [128, 512], F32) for i in range(2)]
    res = [S.sb(f"res{i}", [128, 512], F32) for i in range(4)]
    pss = [S.ps(f"p0ps{i}", 512 * i, 512) for i in range(4)]
    for s in range(2):
        S.dma("sp", cT[:, :, s], pp_view(I["cv"][s, :]), cT, writes=[cT], allow_slow_non_contiguous=True)
    act(S, sc[:, :, :], cT[:, :, :], AF.Silu, reads=[cT], writes=[sc])
    for kc in range(8):
        for s in range(2):
            cp(S, "dve", rep[:, kc * 2 + s, :], sc[:, kc, s:s + 1].to_broadcast([128, 128]), reads=[sc], writes=[rep])
    it = 0
    for l in range(2):
        wm = I[f"l{l}_w_mod"]
        bm = I[f"l{l}_b_mod"]
        for j in range(12):
            wb = wblk[it % 3]
            bb = bblk[it % 2]
            S.dma("pool", wb[:, :, :], wm[:, 512 * j:512 * j + 512].rearrange("(kc p) n -> p kc n", p=128), wb, writes=[wb])
            S.dma("sp", bb[:, :], bc_view(bm[512 * j:512 * j + 512], 512), bb, writes=[bb])
            for s in range(2):
                p = pss[(it % 2) * 2 + s]
                r = res[(it % 2) * 2 + s]
                for kc in range(8):
                    mm(S, p, p[:, :], rep[:, kc * 2 + s, :], wb[:, kc, :], kc == 0, kc == 7, reads=[rep, wb])
                tt(S, "dve", r[:, :], p[:, :], bb[:, :], ALU.add, reads=[p, bb], writes=[r])
                S.dma("sp", mod_d[l, s:s + 1, 512 * j:512 * j + 512], r[0:1, :], r, reads=[r], writes=[mod_d])
            it += 1
    S.barrier()
    S.off = m0


def phase1_qkv(S, C, I, mod_d, QT, KT, V):
    m0 = S.off
    wq = S.sb("wqkv", [128, 8, 3072], BF16)
    S.dma("pool", wq[:, :, :], I["l0_w_qkv"].rearrange("(kc p) n -> p kc n", p=128), wq, writes=[wq])
    mods = S.sb("mods1", [128, 32], F32)
    tmpm = S.sb("tmpm1", [128, 24], F32)
    S.dma("sp", tmpm[:, 0:8], pp_view(I["l0_norm1"]), tmpm, writes=[tmpm], allow_slow_non_contiguous=True)
    for s in range(2):
        load_mod_pp(S, tmpm, 1 + s, mod_d, 0, s, 1)
        load_mod_pp(S, mods, 2 * s + 1, mod_d, 0, s, 0)
        stt(S, "dve", mods[:, 16 * s:16 * s + 8], tmpm[:, 8 + 8 * s:16 + 8 * s], 1.0, tmpm[:, 0:8], ALU.add, ALU.mult,
            reads=[tmpm], writes=[mods])
    Gs = [Buf("G", mods[:, 0:8]), Buf("Gc", mods[:, 16:24])]
    Ss = [Buf("S", mods[:, 8:16]), Buf("Sc", mods[:, 24:32])]
    gains = S.sb("gains", [128, 2], F32)
    for half in range(2):
        S.dma("sp", gains[64 * half:64 * half + 64, 0:1], I["l0_q_gain"].rearrange("(p o) -> p o", o=1), gains, writes=[gains])
        S.dma("sp", gains[64 * half:64 * half + 64, 1:2], I["l0_k_gain"].rearrange("(p o) -> p o", o=1), gains, writes=[gains])
    ts(S, "dve", gains[:, 0:1], gains[:, 0:1], HD ** -0.5, None, ALU.mult, None, reads=[gains], writes=[gains])
    bd = S.sb("bd", [128, 128], BF16)
    S.add("pool", lambda e: e.memset(bd[:, :], 0.0), writes=[bd])
    S.add("pool", lambda e: e.memset(bd[0:64, 0:64], 1.0 / 64), reads=[bd], writes=[bd])
    S.add("pool", lambda e: e.memset(bd[64:128, 64:128], 1.0 / 64), reads=[bd], writes=[bd])
    xin = [S.sb(f"xin{i}", [128, 1024], F32) for i in range(3)]
    xs = [S.sb(f"xs{i}", [128, 1024], F32) for i in range(2)]
    ss = [S.sb(f"ss{i}", [128, 1], F32) for i in range(2)]
    rstd = [S.sb(f"rstd{i}", [128, 1], F32) for i in range(2)]
    hT = [S.sb(f"hT{i}", [128, 8, 512], BF16) for i in range(2)]
    sq = [S.sb(f"sq{i}", [128, 512], BF16) for i in range(2)]
    rs = [S.sb(f"rs{i}", [128, 512], F32) for i in range(2)]
    qn = [S.sb(f"qn{i}", [128, 512], BF16) for i in range(3)]
    vt = [S.sb(f"vt{i}", [128, 1024], BF16) for i in range(2)]
    pT = [S.ps(f"pT{i}", 512 * i, 512) for i in range(2)]
    pQ = [S.ps(f"pQ{i}", 1024 + 512 * i, 512) for i in range(2)]
    pR = [S.ps(f"pR{i}", 2048 + 512 * i, 512) for i in range(2)]
    pV = [S.ps(f"pV{i}", 3072 + 512 * i, 512) for i in range(2)]
    nt = 0
    nqk = 0
    for blk in range(NCH // 4):
        hb = hT[blk % 2]
        for t in range(4):
            g = blk * 4 + t
            s = 0 if g >= 2 else 1
            xt = xin[nt % 3]
            S.dma("sp", xt[:, :], I["xk"][128 * g:128 * g + 128, :], xt, writes=[xt])
            norm_tile(S, C, xt, ss[nt % 2], rstd[nt % 2], xs[nt % 2], pT,
                      (hb, lambda kc, hb=hb, t=t: hb[:, kc, 128 * t:128 * t + 128]), Gs[s], Ss[s])
            nt += 1
        for which, dst_d in ((0, QT), (1, KT)):
            for hp in range(8):
                p = pQ[nqk % 2]
                pr = pR[nqk % 2]
                col = which * 1024 + 128 * hp
                for kc in range(8):
                    mm(S, p, p[:, :], wq[:, kc, col:col + 128], hb[:, kc, :], kc == 0, kc == 7, reads=[wq, hb])
                sqb = sq[nqk % 2]
                act(S, sqb[:, :], p[:, :], AF.Square, reads=[p], writes=[sqb])
                mm(S, pr, pr[:, :], bd[:, :], sqb[:, :], True, True, reads=[bd, sqb])
                rsb = rs[nqk % 2]
                act(S, rsb[:, :], pr[:, :], AF.Sqrt, reads=[pr, C.epsb], writes=[rsb], bias=C.epsb[:, 0:1])
                S.add("dve", lambda e, rsb=rsb: e.reciprocal(out=rsb[:, :], in_=rsb[:, :]), reads=[rsb], writes=[rsb])
                qb = qn[nqk % 3]
                stt(S, "dve", qb[:, :], p[:, :], gains[:, which:which + 1], rsb[:, :], ALU.mult, ALU.mult,
                    reads=[p, gains, rsb], writes=[qb])
                S.dma("sp", dst_d[hp, :, 512 * blk:512 * blk + 512], qb[:, :], qb, reads=[qb], writes=[dst_d])
                nqk += 1
        for t in range(4):
            g = blk * 4 + t
            vb = vt[g % 2]
            for cb in range(2):
                p = pV[cb]
                for kc in range(8):
                    mm(S, p, p[:, :], hb[:, kc, 128 * t:128 * t + 128], wq[:, kc, 2048 + 512 * cb:2048 + 512 * cb + 512],
                       kc == 0, kc == 7, reads=[hb, wq])
                cp(S, "act", vb[:, 512 * cb:512 * cb + 512], p[:, :], reads=[p], writes=[vb])
            S.dma("sp", V[128 * g:128 * g + 128, :], vb[:, :], vb, reads=[vb], writes=[V])
    S.barrier()
    S.off = m0


def phase2_attn(S, C, I, mod_d, QT, KT, V, x1a, h2T):
    m0 = S.off
    wo = S.sb("wo", [128, 8, 1024], BF16)
    S.dma("pool", wo[:, :, :], I["l0_w_o"].rearrange("(hp p) n -> p hp n", p=128), wo, writes=[wo])
    bgen = S.sb("bgen", [128, 16, 5, 128], BF16)
    bspec = S.sb("bspec", [128, 16, 5, 128], BF16)
    S.dma("pool", bgen[:, :, :, :], I["bt"][0], bgen, writes=[bgen])
    mods = S.sb("mods2", [128, 32], F32)
    tmpm = S.sb("tmpm2", [128, 24], F32)
    S.dma("sp", tmpm[:, 0:8], pp_view(I["l0_norm2"]), tmpm, writes=[tmpm], allow_slow_non_contiguous=True)
    g1b = []
    for s in range(2):
        load_mod_pp(S, tmpm, 1 + s, mod_d, 0, s, 4)
        load_mod_pp(S, mods, 2 * s + 1, mod_d, 0, s, 3)
        stt(S, "dve", mods[:, 16 * s:16 * s + 8], tmpm[:, 8 + 8 * s:16 + 8 * s], 1.0, tmpm[:, 0:8], ALU.add, ALU.mult,
            reads=[tmpm], writes=[mods])
        gb = S.sb(f"g1b{s}", [128, 1024], F32)
        S.dma("sp", gb[:, :], bc_view(mod_d[0, s, 2 * D:3 * D], D), gb, reads=[mod_d], writes=[gb])
        g1b.append(gb)
    Gs = [Buf("G2", mods[:, 0:8]), Buf("G2c", mods[:, 16:24])]
    Ss = [Buf("S2", mods[:, 8:16]), Buf("S2c", mods[:, 24:32])]
    RING = 6
    ktr = S.sb("ktr", [128, 8, RING, 128], BF16)
    vr = S.sb("vr", [128, RING, 16, 65], BF16)
    ktc = S.sb("ktc", [128, 8, 256], BF16)
    vc = S.sb("vc", [128, 2, 16, 65], BF16)
    kslots = [Buf(f"ks{i}", None) for i in range(RING)]
    vslots = [Buf(f"vs{i}", None) for i in range(RING)]
    S.add("pool", lambda e: e.memset(vr[:, :, :, 64:65], 1.0), writes=vslots)
    S.add("pool", lambda e: e.memset(vc[:, :, :, 64:65], 1.0), writes=[vc])
    S.dma("sp", ktc[:, :, :], KT[:, :, 0:256].rearrange("hp p t -> p hp t"), ktc, reads=[KT], writes=[ktc])
    for cc in range(2):
        S.dma("sp", vc[:, cc, :, 0:64], V[128 * cc:128 * cc + 128, :].rearrange("p (h d) -> p h d", h=16), vc,
              reads=[V], writes=[vc])
    qt = [S.sb(f"qt{i}", [128, 8, 128], BF16) for i in range(2)]
    xt_ = [S.sb(f"x2in{i}", [128, 1024], F32) for i in range(2)]
    tb = [S.sb(f"tb{i}", [128, 5, 128], F32) for i in range(2)]
    pt = [S.sb(f"pt{i}", [128, 7, 128], BF16) for i in range(2)]
    rec = [S.sb(f"rec{i}", [128, 1], F32) for i in range(4)]
    on = [S.sb(f"on{i}", [128, 16, 64], BF16) for i in range(2)]
    oT = [S.sb(f"oT{i}", [128, 8, 128], BF16) for i in range(2)]
    x1t = [S.sb(f"x1t{i}", [128, 1024], F32) for i in range(2)]
    xs = [S.sb(f"xs2{i}", [128, 1024], F32) for i in range(2)]
    ss = [S.sb(f"ss2{i}", [128, 1], F32) for i in range(2)]
    rstd = [S.sb(f"rstd2{i}", [128, 1], F32) for i in range(2)]
    h2 = [S.sb(f"h2t{i}", [128, 8, 128], BF16) for i in range(2)]
    pS = [S.ps(f"pS{i}", 1024 * i, 896) for i in range(2)]
    pO = [S.ps(f"pO{i}", 2048 + 128 * i, 65) for i in range(4)]
    pY = [S.ps(f"pY{i}", 2560 + 512 * i, 512) for i in range(2)]
    pOT = S.ps("pOT", 3584, 512, BF16)

    loaded = set()

    def ensure_chunk(g):
        if g in loaded:
            return
        loaded.add(g)
        sl = g % RING
        S.dma("sp", ktr[:, :, sl, :], KT[:, :, 128 * g:128 * g + 128].rearrange("hp p t -> p hp t"), kslots[sl],
              reads=[KT], writes=[kslots[sl]])
        S.dma("sp", vr[:, sl, :, 0:64], V[128 * g:128 * g + 128, :].rearrange("p (h d) -> p h d", h=16), vslots[sl],
              reads=[V], writes=[vslots[sl]])

    spec_lts = {1: 1, 2: 2, 31: 3, 32: 4}
    nh = 0
    for ti in range(NT1):
        is_ctx = ti < 2
        lt = ti - 2
        g = ti if is_ctx else lt + 4
        s = 1 if is_ctx else 0
        q = qt[ti % 2]
        S.dma("sp", q[:, :, :], QT[:, :, 128 * g:128 * g + 128].rearrange("hp p t -> p hp t"), q, reads=[QT], writes=[q])
        xt = xt_[ti % 2]
        S.dma("sp", xt[:, :], I["xk"][128 * g:128 * g + 128, :], xt, writes=[xt])
        nloc = 0 if is_ctx else 5
        if not is_ctx:
            for j in range(5):
                ensure_chunk(lt + 2 + j)
            if lt in spec_lts:
                S.dma("pool", bspec[:, :, :, :], I["bt"][spec_lts[lt]], bspec, writes=[bspec])
                bias = bspec
            else:
                bias = bgen
        onb = on[ti % 2]
        for h in range(NH):
            hp, half = h // 2, h % 2
            lo = 64 * half
            p = pS[nh % 2]
            ptb = pt[nh % 2]
            for j in range(nloc):
                sl = (lt + 2 + j) % RING
                mm(S, p, p[:, 128 * j:128 * j + 128], ktr[lo:lo + 64, hp, sl, :], q[lo:lo + 64, hp, :], True, True,
                   reads=[kslots[sl], q])
            for cc in range(2):
                jj = nloc + cc
                mm(S, p, p[:, 128 * jj:128 * jj + 128], ktc[lo:lo + 64, hp, 128 * cc:128 * cc + 128], q[lo:lo + 64, hp, :],
                   True, True, reads=[ktc, q])
            if nloc:
                t_ = tb[nh % 2]
                tt(S, "dve", t_[:, :, :], p[:, 0:640].rearrange("p (j q) -> p j q", j=5), bias[:, h, :, :], ALU.add,
                   reads=[p, bias], writes=[t_])
                act(S, ptb[:, 0:5, :], t_[:, :, :], AF.Exp, reads=[t_], writes=[ptb])
            act(S, ptb[:, nloc:nloc + 2, :], p[:, 128 * nloc:128 * nloc + 256].rearrange("p (j q) -> p j q", j=2), AF.Exp,
                reads=[p], writes=[ptb])
            po = pO[nh % 4]
            n = nloc + 2
            for j in range(nloc):
                sl = (lt + 2 + j) % RING
                mm(S, po, po[:, :], ptb[:, j, :], vr[:, sl, h, :], j == 0, False, reads=[ptb, vslots[sl]])
            for cc in range(2):
                mm(S, po, po[:, :], ptb[:, nloc + cc, :], vc[:, cc, h, :], (nloc + cc) == 0, cc == 1, reads=[ptb, vc])
            r_ = rec[nh % 4]
            S.add("dve", lambda e, r_=r_, po=po: e.reciprocal(out=r_[:, 0:1], in_=po[:, 64:65]), reads=[po], writes=[r_])
            ts(S, "dve", onb[:, h, :], po[:, 0:64], r_[:, 0:1], None, ALU.mult, None, reads=[po, r_], writes=[onb])
            nh += 1
        otb = oT[ti % 2]
        for hp in range(8):
            tr(S, pOT, pOT[:, 128 * hp:128 * hp + 128], onb[:, 2 * hp:2 * hp + 2, :].rearrange("p a b -> p (a b)"),
               C.identb[:, :], reads=[onb, C.identb])
        cp(S, "act", otb[:, 0:4, :], pOT[:, 0:512].rearrange("p (a b) -> p a b", a=4), reads=[pOT], writes=[otb])
        cp(S, "dve", otb[:, 4:8, :], pOT[:, 512:1024].rearrange("p (a b) -> p a b", a=4), reads=[pOT], writes=[otb])
        x1 = x1t[ti % 2]
        for cb in range(2):
            py = pY[cb]
            for hp in range(8):
                mm(S, py, py[:, :], otb[:, hp, :], wo[:, hp, 512 * cb:512 * cb + 512], hp == 0, hp == 7, reads=[otb, wo])
            tt(S, "dve", x1[:, 512 * cb:512 * cb + 512], py[:, :], g1b[s][:, 512 * cb:512 * cb + 512], ALU.mult,
               reads=[py, g1b[s]], writes=[x1])
        tt(S, "pool", x1[:, :], x1[:, :], xt[:, :], ALU.add, reads=[x1, xt], writes=[x1])
        S.dma("sp", x1a[128 * ti:128 * ti + 128, :], x1[:, :], x1, reads=[x1], writes=[x1a])
        hb = h2[ti % 2]
        norm_tile(S, C, x1, ss[ti % 2], rstd[ti % 2], xs[ti % 2], pY,
                  (hb, lambda kc, hb=hb: hb[:, kc, :]), Gs[s], Ss[s])
        S.dma("sp", h2T[:, :, 128 * ti:128 * ti + 128].rearrange("kc p t -> p kc t"), hb[:, :, :], hb, reads=[hb], writes=[h2T])
    S.barrier()
    S.off = m0


def phase3_ffn(S, C, I, mod_d, x1a, h2T, x1):
    m0 = S.off
    w1 = S.sb("w1", [128, 8, DFF], BF16)
    w3 = S.sb("w3", [128, 8, DFF], BF16)
    w2 = S.sb("w2", [128, NFC, 1024], BF16)
    for kc in range(8):
        S.dma("pool", w1[:, kc, :], I["l0_ffn_w1"][128 * kc:128 * kc + 128, :], w1, writes=[w1])
        S.dma("pool", w3[:, kc, :], I["l0_ffn_w3"][128 * kc:128 * kc + 128, :], w3, writes=[w3])
    for fc in range(NFC):
        S.dma("pool", w2[:, fc, :], I["l0_ffn_w2"][128 * fc:128 * fc + 128, :], w2, writes=[w2])
    g2b = []
    for s in range(2):
        gb = S.sb(f"g2b{s}", [128, 1024], F32)
        S.dma("sp", gb[:, :], bc_view(mod_d[0, s, 5 * D:6 * D], D), gb, reads=[mod_d], writes=[gb])
        g2b.append(gb)
    hb_ = [S.sb(f"h3b{i}", [128, 8, 512], BF16) for i in range(2)]
    hid = S.sb("hid", [128, NFC, 512], BF16)
    sa = [S.sb(f"sa{i}", [128, 512], F32) for i in range(2)]
    xa = [S.sb(f"xa{i}", [128, 1024], F32) for i in range(2)]
    xo = [S.sb(f"xo{i}", [128, 1024], F32) for i in range(2)]
    pA = [S.ps(f"pA{i}", 512 * i, 512) for i in range(2)]
    pB = [S.ps(f"pB{i}", 1024 + 512 * i, 512) for i in range(2)]
    pY = [S.ps(f"pY3{i}", 2048 + 512 * i, 512) for i in range(4)]
    nf = 0
    nt = 0
    for blk in range(NT1 // 4):
        hb = hb_[blk % 2]
        S.dma("sp", hb[:, :, :], h2T[:, :, 512 * blk:512 * blk + 512].rearrange("kc p t -> p kc t"), hb, reads=[h2T], writes=[hb])
        for fc in range(NFC):
            pa, pb = pA[nf % 2], pB[nf % 2]
            for kc in range(8):
                mm(S, pa, pa[:, :], w1[:, kc, 128 * fc:128 * fc + 128], hb[:, kc, :], kc == 0, kc == 7, reads=[w1, hb])
            for kc in range(8):
                mm(S, pb, pb[:, :], w3[:, kc, 128 * fc:128 * fc + 128], hb[:, kc, :], kc == 0, kc == 7, reads=[w3, hb])
            sb_ = sa[nf % 2]
            act(S, sb_[:, :], pa[:, :], AF.Silu, reads=[pa], writes=[sb_])
            tt(S, "dve", hid[:, fc, :], sb_[:, :], pb[:, :], ALU.mult, reads=[sb_, pb], writes=[hid])
            nf += 1
        for t in range(4):
            ti = blk * 4 + t
            s = 1 if ti < 2 else 0
            xab = xa[nt % 2]
            S.dma("sp", xab[:, :], x1a[128 * ti:128 * ti + 128, :], xab, reads=[x1a], writes=[xab])
            xob = xo[nt % 2]
            for cb in range(2):
                py = pY[(nt % 2) * 2 + cb]
                for fc in range(NFC):
                    mm(S, py, py[:, :], hid[:, fc, 128 * t:128 * t + 128], w2[:, fc, 512 * cb:512 * cb + 512],
                       fc == 0, fc == NFC - 1, reads=[hid, w2])
                tt(S, "dve", xob[:, 512 * cb:512 * cb + 512], py[:, :], g2b[s][:, 512 * cb:512 * cb + 512], ALU.mult,
                   reads=[py, g2b[s]], writes=[xob])
            tt(S, "pool", xob[:, :], xob[:, :], xab[:, :], ALU.add, reads=[xob, xab], writes=[xob])
            S.dma("sp", x1[128 * ti:128 * ti + 128, :], xob[:, :], xob, reads=[xob], writes=[x1])
            nt += 1
    S.barrier()
    S.off = m0


class RnnConsts:
    pass


def rnn_setup(S, C, I, mod_d, pass2):
    R = RnnConsts()
    R.win_x = S.sb("win_x", [128, 8, DRNN], BF16)
    S.dma("pool", R.win_x[:, :, :], I["l1_w_in"][:, DRNN:2 * DRNN].rearrange("(kc p) n -> p kc n", p=128), R.win_x,
          writes=[R.win_x])
    if pass2:
        R.win_g = S.sb("win_g", [128, 8, DRNN], BF16)
        S.dma("pool", R.win_g[:, :, :], I["l1_w_in"][:, 0:DRNN].rearrange("(kc p) n -> p kc n", p=128), R.win_g,
              writes=[R.win_g])
        R.wout = S.sb("wout", [RB, RCH, D], BF16)
        S.dma("pool", R.wout[:, :, :], I["l1_w_out"].rearrange("(ch p) n -> p ch n", p=RB), R.wout, writes=[R.wout])
    R.ga = S.sb("ga", [RB, 2, RCH, RB], BF16)
    R.gx = S.sb("gx", [RB, 2, RCH, RB], BF16)
    for d in range(2):
        S.dma("pool", R.ga[:, d, :, :], I["l1_gate_a_w"][d].rearrange("k c o -> c k o"), R.ga, writes=[R.ga])
        S.dma("pool", R.gx[:, d, :, :], I["l1_gate_x_w"][d].rearrange("k c o -> c k o"), R.gx, writes=[R.gx])
    R.cw = S.sb("cw", [RB, 5, RCH], F32)
    for j in range(4):
        S.dma("sp", R.cw[:, j, :], I["l1_conv_w"][j].rearrange("(ch p) -> p ch", p=RB), R.cw, writes=[R.cw],
              allow_slow_non_contiguous=True)
    S.dma("sp", R.cw[:, 4, :], I["l1_conv_b"].rearrange("(ch p) -> p ch", p=RB), R.cw, writes=[R.cw],
          allow_slow_non_contiguous=True)
    R.gb = S.sb("gb", [RB, 4, RCH], F32)
    R.c1 = S.sb("c1", [RB, 2, RCH], F32)
    lam = S.sb("lamt", [RB, 2 * RCH], F32)
    for d in range(2):
        S.dma("sp", R.gb[:, d, :], I["l1_gate_a_b"][d].rearrange("(ch p) -> p ch", p=RB), R.gb, writes=[R.gb],
              allow_slow_non_contiguous=True)
        S.dma("sp", R.gb[:, 2 + d, :], I["l1_gate_x_b"][d].rearrange("(ch p) -> p ch", p=RB), R.gb, writes=[R.gb],
              allow_slow_non_contiguous=True)
        S.dma("sp", lam[:, RCH * d:RCH * d + RCH], I["l1_lam"][d].rearrange("(ch p) -> p ch", p=RB), lam, writes=[lam],
              allow_slow_non_contiguous=True)
    n = 2 * RCH
    t = S.sb("sp_t", [RB, n], F32)
    w = S.sb("sp_w", [RB, n], F32)
    w2 = S.sb("sp_w2", [RB, n], F32)
    pl = S.sb("sp_pl", [RB, n], F32)
    s2 = S.sb("sp_s2", [RB, n], F32)
    mk = S.sb("sp_mk", [RB, n], F32)
    act(S, t[:, :], lam[:, :], AF.Exp, reads=[lam], writes=[t], scale=-1.0)
    ts(S, "dve", w[:, :], t[:, :], 2.0, None, ALU.add, None, reads=[t], writes=[w])
    S.add("dve", lambda e: e.reciprocal(out=w[:, :], in_=w[:, :]), reads=[w], writes=[w])
    tt(S, "dve", w[:, :], w[:, :], t[:, :], ALU.mult, reads=[w, t], writes=[w])
    tt(S, "dve", w2[:, :], w[:, :], w[:, :], ALU.mult, reads=[w], writes=[w2])
    ts(S, "dve", pl[:, :], w2[:, :], 1.0 / 11, 1.0 / 9, ALU.mult, ALU.add, reads=[w2], writes=[pl])
    for cf in (1.0 / 7, 1.0 / 5, 1.0 / 3, 1.0):
        tt(S, "dve", pl[:, :], pl[:, :], w2[:, :], ALU.mult, reads=[pl, w2], writes=[pl])
        ts(S, "dve", pl[:, :], pl[:, :], cf, None, ALU.add, None, reads=[pl], writes=[pl])
    tt(S, "dve", pl[:, :], pl[:, :], w[:, :], ALU.mult, reads=[pl, w], writes=[pl])
    ts(S, "dve", s2[:, :], t[:, :], 1.0, None, ALU.add, None, reads=[t], writes=[s2])
    act(S, s2[:, :], s2[:, :], AF.Ln, reads=[s2], writes=[s2])
    ts(S, "dve", mk[:, :], t[:, :], 0.5, None, ALU.is_lt, None, reads=[t], writes=[mk])
    stt(S, "dve", pl[:, :], pl[:, :], 2.0, s2[:, :], ALU.mult, ALU.subtract, reads=[pl, s2], writes=[pl])
    tt(S, "dve", pl[:, :], pl[:, :], mk[:, :], ALU.mult, reads=[pl, mk], writes=[pl])
    tt(S, "dve", pl[:, :], pl[:, :], s2[:, :], ALU.add, reads=[pl, s2], writes=[pl])
    ts(S, "dve", R.c1[:, :, :].rearrange("p a b -> p (a b)"), pl[:, :], -8.0, None, ALU.mult, None, reads=[pl], writes=[R.c1])
    R.ones = S.sb("ones_r", [RB, 1], F32)
    S.add("pool", lambda e: e.memset(R.ones[:, :], 1.0 + 2.0 ** -23), writes=[R.ones])
    R.ngb = S.sb("ngb", [RB, 4, RCH], F32)
    ts(S, "dve", R.ngb[:, :, :], R.gb[:, :, :], -1.0, None, ALU.mult, None, reads=[R.gb], writes=[R.ngb])
    R.msk = S.sb("msk", [128, 2], F32)
    S.dma("sp", R.msk[:, :], I["masks"], R.msk, writes=[R.msk])
    R.X = [S.sb(f"X{i}", [RB, 515], F32) for i in range(2)]
    R.xc = [S.sb(f"xc{i}", [RB, 512], F32) for i in range(2)]
    R.xcb = [S.sb(f"xcb{i}", [RB, 512], BF16) for i in range(2)]
    R.r = [S.sb(f"r{i}", [RB, 512], F32) for i in range(2)]
    R.iu = [S.sb(f"iu{i}", [RB, 512], F32) for i in range(2)]
    R.a = [S.sb(f"a{i}", [RB, 512], F32) for i in range(2)]
    R.m = [S.sb(f"m{i}", [RB, 512], F32) for i in range(2)]
    R.hs = [S.sb(f"hs{i}", [RB, 512], F32) for i in range(2)]
    R.win = [S.sb(f"hwin{i}", [128, 8, 516], BF16) for i in range(2)]
    R.pb = [S.ps(f"rb{i}", 512 * i, 512) for i in range(8)]
    R.pxB = [S.ps(f"pxB{i}", 3584 + 4 * i, 3, parts=RB) for i in range(2)]
    return R


def rnn_front(S, R, hwin, L, ch, nchunk, is_ctx, mask_before, mask_after):
    X = R.X[nchunk % 2]
    xc = R.xc[nchunk % 2]
    xcb = R.xcb[nchunk % 2]
    px = R.pb[nchunk % 2]
    col = 96 * ch
    if is_ctx:
        for kc in range(8):
            mm(S, px, px[0:RB, 0:L], R.win_x[:, kc, col:col + RB], hwin[:, kc, 0:L], kc == 0, kc == 7, reads=[R.win_x, hwin])
        S.add("pool", lambda e: e.memset(X[:, 0:2], 0.0), writes=[X])
        S.add("pool", lambda e: e.memset(X[:, L + 2:L + 3], 0.0), writes=[X])
        cp(S, "act", X[:, 2:L + 2], px[0:RB, 0:L], reads=[px], writes=[X])
    else:
        pxb = R.pxB[nchunk % 2]
        for kc in range(8):
            mm(S, px, px[0:RB, 0:512], R.win_x[:, kc, col:col + RB], hwin[:, kc, 0:512], kc == 0, kc == 7, reads=[R.win_x, hwin])
        for kc in range(8):
            mm(S, pxb, pxb[:, 0:3], R.win_x[:, kc, col:col + RB], hwin[:, kc, 512:515], kc == 0, kc == 7, reads=[R.win_x, hwin])
        cp(S, "act", X[:, 0:512], px[0:RB, 0:512], reads=[px], writes=[X])
        cp(S, "dve", X[:, 512:515], pxb[:, 0:3], reads=[pxb], writes=[X])
        if mask_before:
            ts(S, "dve", X[:, 0:2], X[:, 0:2], R.msk[0:RB, 0:1], None, ALU.mult, None, reads=[X, R.msk], writes=[X])
        if mask_after:
            ts(S, "dve", X[:, 514:515], X[:, 514:515], R.msk[0:RB, 1:2], None, ALU.mult, None, reads=[X, R.msk], writes=[X])
    ts(S, "dve", xc[:, 0:L], X[:, 0:L], R.cw[:, 0, ch:ch + 1], R.cw[:, 4, ch:ch + 1], ALU.mult, ALU.add,
       reads=[X, R.cw], writes=[xc])
    for j in range(1, 4):
        stt(S, "dve", xc[:, 0:L], X[:, j:j + L], R.cw[:, j, ch:ch + 1], xc[:, 0:L], ALU.mult, ALU.add,
            reads=[X, R.cw, xc], writes=[xc])
    cp(S, "pool", xcb[:, 0:L], xc[:, 0:L], reads=[xc], writes=[xcb])


def rnn_back(S, C, R, L, ch, nchunk, init, sumr, hs_out, extra_sig=None):
    xc = R.xc[nchunk % 2]
    xcb = R.xcb[nchunk % 2]
    for d in range(2):
        pr = R.pb[2 + d]
        pi = R.pb[4 + d]
        mm(S, pr, pr[0:RB, 0:L], R.ga[:, d, ch, :], xcb[:, 0:L], True, True, reads=[R.ga, xcb])
        mm(S, pi, pi[0:RB, 0:L], R.gx[:, d, ch, :], xcb[:, 0:L], True, True, reads=[R.gx, xcb])
    for d in range(2):
        pr = R.pb[2 + d]
        pi = R.pb[4 + d]
        r, iu = R.r[d], R.iu[d]
        if sumr is not None:
            act(S, r[:, 0:L], pr[0:RB, 0:L], AF.Sigmoid, reads=[pr, R.gb], writes=[r, sumr[d][0]],
                bias=R.gb[:, d, ch:ch + 1], accum_out=sumr[d][1])
        else:
            act(S, r[:, 0:L], pr[0:RB, 0:L], AF.Sigmoid, reads=[pr, R.gb], writes=[r], bias=R.gb[:, d, ch:ch + 1])
        act(S, iu[:, 0:L], pi[0:RB, 0:L], AF.Sigmoid, reads=[pi, R.gb], writes=[iu], bias=R.gb[:, 2 + d, ch:ch + 1])
    if extra_sig is not None:
        extra_sig()
    for d in range(2):
        r, a = R.r[d], R.a[d]
        act(S, a[:, 0:L], r[:, 0:L], AF.Exp, reads=[r, R.c1], writes=[a], scale=R.c1[:, d, ch:ch + 1])
    for d in range(2):
        a, m = R.a[d], R.m[d]
        act(S, m[:, 0:L], a[:, 0:L], AF.Square, reads=[a], writes=[m])
    for d in range(2):
        m = R.m[d]
        act(S, m[:, 0:L], m[:, 0:L], AF.Ln, reads=[m, R.ones], writes=[m], bias=R.ones[:, 0:1], scale=-1.0)
    for d in range(2):
        m = R.m[d]
        act(S, m[:, 0:L], m[:, 0:L], AF.Exp, reads=[m], writes=[m], scale=0.5)
    for d in range(2):
        r, iu, a, m, hs = R.r[d], R.iu[d], R.a[d], R.m[d], hs_out[d]
        tt(S, "dve", iu[:, 0:L], iu[:, 0:L], xc[:, 0:L], ALU.mult, reads=[iu, xc], writes=[iu])
        tt(S, "dve", iu[:, 0:L], iu[:, 0:L], m[:, 0:L], ALU.mult, reads=[iu, m], writes=[iu])
        ini = init[d]
        ini_reads = [] if isinstance(ini, float) else [ini[0]]
        ini_ap = ini if isinstance(ini, float) else ini[1]
        if d == 0:
            S.add("dve", lambda e, hs=hs, a=a, iu=iu, ini_ap=ini_ap: e.tensor_tensor_scan(
                out=hs[:, 0:L], data0=a[:, 0:L], data1=iu[:, 0:L], initial=ini_ap, op0=ALU.mult, op1=ALU.add),
                reads=[a, iu] + ini_reads, writes=[hs])
        else:
            def rv(buf):
                ap = buf[:, 0:L]
                return bass.AP(ap.tensor, ap.offset + (L - 1), [list(ap.ap[0]), [-1, L]])
            S.add("dve", lambda e, hs=hs, a=a, iu=iu, ini_ap=ini_ap: e.tensor_tensor_scan(
                out=rv(hs), data0=rv(a), data1=rv(iu), initial=ini_ap, op0=ALU.mult, op1=ALU.add),
                reads=[a, iu] + ini_reads, writes=[hs])


def phase4a_h1(S, C, I, mod_d, x1, hT1):
    m0 = S.off
    mods = S.sb("mods4", [128, 32], F32)
    tmpm = S.sb("tmpm4", [128, 24], F32)
    S.dma("sp", tmpm[:, 0:8], pp_view(I["l1_norm1"]), tmpm, writes=[tmpm], allow_slow_non_contiguous=True)
    for s in range(2):
        load_mod_pp(S, tmpm, 1 + s, mod_d, 1, s, 1)
        load_mod_pp(S, mods, 2 * s + 1, mod_d, 1, s, 0)
        stt(S, "dve", mods[:, 16 * s:16 * s + 8], tmpm[:, 8 + 8 * s:16 + 8 * s], 1.0, tmpm[:, 0:8], ALU.add, ALU.mult,
            reads=[tmpm], writes=[mods])
    Gs = [Buf("G4", mods[:, 0:8]), Buf("G4c", mods[:, 16:24])]
    Ss = [Buf("S4", mods[:, 8:16]), Buf("S4c", mods[:, 24:32])]
    xin = [S.sb(f"x4in{i}", [128, 1024], F32) for i in range(3)]
    xs = [S.sb(f"xs4{i}", [128, 1024], F32) for i in range(2)]
    ss = [S.sb(f"ss4{i}", [128, 1], F32) for i in range(2)]
    rstd = [S.sb(f"rstd4{i}", [128, 1], F32) for i in range(2)]
    hb_ = [S.sb(f"h4{i}", [128, 8, 128], BF16) for i in range(2)]
    pT = [S.ps(f"pT4{i}", 512 * i, 512) for i in range(2)]
    for ti in range(NT1):
        s = 1 if ti < 2 else 0
        xt = xin[ti % 3]
        S.dma("sp", xt[:, :], x1[128 * ti:128 * ti + 128, :], xt, reads=[x1], writes=[xt])
        hb = hb_[ti % 2]
        norm_tile(S, C, xt, ss[ti % 2], rstd[ti % 2], xs[ti % 2], pT, (hb, lambda kc, hb=hb: hb[:, kc, :]), Gs[s], Ss[s])
        S.dma("sp", hT1[:, :, 128 * ti:128 * ti + 128].rearrange("kc p t -> p kc t"), hb[:, :, :], hb, reads=[hb], writes=[hT1])
    S.barrier()
    S.off = m0


def seg_info(seg):
    if seg == 0:
        return True, 256, 0, False, False
    b = seg - 1
    s = 256 + 128 + 512 * b
    return False, 512, s - 2, b == 0, b == 7


def load_win(S, R, hT1, seg, n):
    is_ctx, L, w0, _, _ = seg_info(seg)
    hw = R.win[n % 2]
    wl = 256 if is_ctx else 515
    S.dma("sp", hw[:, :, 0:wl], hT1[:, :, w0:w0 + wl].rearrange("kc p t -> p kc t"), hw, reads=[hT1], writes=[hw])
    return hw


def phase4b_pass1(S, C, I, mod_d, hT1, SAB):
    m0 = S.off
    R = rnn_setup(S, C, I, mod_d, pass2=False)
    sab = S.sb("sab", [RB, 2, 9, 2, RCH], F32)
    S.add("pool", lambda e: e.memset(sab[:, 0, :, :, :], 0.0), writes=[sab])
    work = []
    for seg in range(9):
        for ch in range(RCH):
            work.append((seg, ch))
    wins = {0: load_win(S, R, hT1, 0, 0)}

    def front(n):
        seg, ch = work[n]
        is_ctx, L, w0, mb, ma = seg_info(seg)
        if ch == 0 and seg + 1 < 9:
            wins[seg + 1] = load_win(S, R, hT1, seg + 1, seg + 1)
        rnn_front(S, R, wins[seg], L, ch, n, is_ctx, mb, ma)

    front(0)
    for n, (seg, ch) in enumerate(work):
        is_ctx, L, w0, mb, ma = seg_info(seg)
        if n + 1 < len(work):
            front(n + 1)
        sumr = [(sab, sab[:, 0, seg, d, ch:ch + 1]) for d in range(2)]
        rnn_back(S, C, R, L, ch, n, [0.0, 0.0], sumr, R.hs)
        cp(S, "pool", sab[:, 1, seg, 0, ch:ch + 1], R.hs[0][:, L - 1:L], reads=[R.hs[0]], writes=[sab])
        cp(S, "pool", sab[:, 1, seg, 1, ch:ch + 1], R.hs[1][:, 0:1], reads=[R.hs[1]], writes=[sab])
    for seg in range(9):
        tt(S, "dve", sab[:, 0, seg, :, :], sab[:, 0, seg, :, :], R.c1[:, :, :], ALU.mult, reads=[sab, R.c1], writes=[sab])
    act(S, sab[:, 0, :, :, :], sab[:, 0, :, :, :], AF.Exp, reads=[sab], writes=[sab])
    S.dma("sp", SAB[:, :], sab[:, :, :, :, :].rearrange("p a s d c -> p (a s d c)"), sab, reads=[sab], writes=[SAB])
    S.barrier()
    S.off = m0


def phase5_fold(S, C, I, SALL, SOWN, hin, NR=4):
    m0 = S.off
    sall = S.sb("sall", [RB, NR, 2 * 9 * 2 * RCH], F32)
    sown = S.sb("sown", [RB, 2, 9, 2, RCH], F32)
    sel = S.sb("sel", [RB, 2 * NR], F32)
    S.dma("sp", sall[:, :, :], SALL.t.rearrange("(j p) f -> p j f", p=RB), sall, reads=[SALL], writes=[sall])
    S.dma("sp", sown[:, :, :, :, :].rearrange("p a s d c -> p (a s d c)"), SOWN[:, :], sown, reads=[SOWN], writes=[sown])
    S.dma("sp", sel[:, :], I["sel"], sel, writes=[sel])
    sv = sall[:, :, :].rearrange("p j (a s d c) -> p j a s d c", a=2, s=9, d=2)
    h = S.sb("hfold", [RB, RCH], F32)
    ae = S.sb("aeff", [RB, 8, RCH], F32)
    be = S.sb("beff", [RB, 8, RCH], F32)
    for d in range(2):
        cp(S, "dve", h[:, :], sown[:, 1, 0, d, :], reads=[sown], writes=[h])
        order = range(NR) if d == 0 else range(NR - 1, -1, -1)
        for j in order:
            mj = sel[:, d * NR + j:d * NR + j + 1]
            ts(S, "dve", ae[:, :, :], sv[:, j, 0, 1:9, d, :], -1.0, None, ALU.add, None, reads=[sall], writes=[ae])
            ts(S, "dve", ae[:, :, :], ae[:, :, :], mj, None, ALU.mult, None, reads=[ae, sel], writes=[ae])
            ts(S, "dve", ae[:, :, :], ae[:, :, :], 1.0, None, ALU.add, None, reads=[ae], writes=[ae])
            ts(S, "dve", be[:, :, :], sv[:, j, 1, 1:9, d, :], mj, None, ALU.mult, None, reads=[sall, sel], writes=[be])
            border = range(8) if d == 0 else range(7, -1, -1)
            for b in border:
                tt(S, "dve", h[:, :], h[:, :], ae[:, b, :], ALU.mult, reads=[h, ae], writes=[h])
                tt(S, "dve", h[:, :], h[:, :], be[:, b, :], ALU.add, reads=[h, be], writes=[h])
        border = list(range(8)) if d == 0 else list(range(7, -1, -1))
        for n, b in enumerate(border):
            cp(S, "dve", hin[:, d, b, :], h[:, :], reads=[h], writes=[hin])
            if n < 7:
                tt(S, "dve", h[:, :], h[:, :], sown[:, 0, 1 + b, d, :], ALU.mult, reads=[h, sown], writes=[h])
                tt(S, "dve", h[:, :], h[:, :], sown[:, 1, 1 + b, d, :], ALU.add, reads=[h, sown], writes=[h])
    S.barrier()
    S.off = m0


def phase6_pass2(S, C, I, mod_d, x1, hT1, hin, x2, h2T1, comb, XS=None, maskall=None):
    R = rnn_setup(S, C, I, mod_d, pass2=True)
    mods = S.sb("mods6", [128, 16], F32)
    tmpm = S.sb("tmpm6", [128, 16], F32)
    S.dma("sp", tmpm[:, 0:8], pp_view(I["l1_norm2"]), tmpm, writes=[tmpm], allow_slow_non_contiguous=True)
    load_mod_pp(S, tmpm, 1, mod_d, 1, 0, 4)
    load_mod_pp(S, mods, 1, mod_d, 1, 0, 3)
    stt(S, "dve", mods[:, 0:8], tmpm[:, 8:16], 1.0, tmpm[:, 0:8], ALU.add, ALU.mult, reads=[tmpm], writes=[mods])
    G2 = Buf("G6", mods[:, 0:8])
    S2 = Buf("S6", mods[:, 8:16])
    g1b = S.sb("g1b6", [128, 1024], F32)
    S.dma("sp", g1b[:, :], bc_view(mod_d[1, 0, 2 * D:3 * D], D), g1b, reads=[mod_d], writes=[g1b])
    wr = S.sb("wr", [128, 8, NE], F32)
    S.dma("sp", wr[:, :, :], I["l1_router_w"].rearrange("(kc p) e -> p kc e", p=128), wr, writes=[wr])
    brb = S.sb("brb", [128, NE], F32)
    S.dma("sp", brb[:, :], bc_view(I["l1_router_b"], NE), brb, writes=[brb])
    yin = S.sb("yin", [RB, RCH, 512], BF16)
    gg = [S.sb(f"gg{i}", [RB, 512], F32) for i in range(2)]
    xt_ = [S.sb(f"x6in{i}", [128, 1024], F32) for i in range(1)] * 2
    ytmp = [S.sb(f"y6{i}", [128, 1024], F32) for i in range(2)]
    xs = [S.sb(f"xs6{i}", [128, 1024], F32) for i in range(1)] * 2
    ss = [S.sb(f"ss6{i}", [128, 1], F32) for i in range(2)]
    rstd = [S.sb(f"rstd6{i}", [128, 1], F32) for i in range(2)]
    h2 = [S.sb(f"h6{i}", [128, 8, 128], BF16) for i in range(2)]
    h32 = [S.sb(f"h32{i}", [128, 8, 128], F32) for i in range(1)] * 2
    rt = [S.sb(f"rt{i}", [128, 48], F32) for i in range(2)]
    xsb = [S.sb(f"xsb{i}", [128, 1024], BF16) for i in range(2)] if XS is not None else None
    pg = R.pb[6]
    plog = S.ps("plog", 3584 + 16, 8)
    ntile = 0
    xg = [S.sb(f"xg{i}", [RB, 512], F32) for i in range(2)]
    work = [(seg, ch) for seg in range(1, 9) for ch in range(RCH)]
    wins = {1: load_win(S, R, hT1, 1, 0)}

    def front(n):
        seg, ch = work[n]
        is_ctx, L, w0, mb, ma = seg_info(seg)
        if ch == 0 and seg + 1 < 9:
            wins[seg + 1] = load_win(S, R, hT1, seg + 1, seg)
        rnn_front(S, R, wins[seg], L, ch, n, False, mb, ma)
        hw = wins[seg]
        for kc in range(8):
            mm(S, pg, pg[0:RB, :], R.win_g[:, kc, 96 * ch:96 * ch + RB], hw[:, kc, 2:514], kc == 0, kc == 7, reads=[R.win_g, hw])
        x_ = xg[n % 2]
        cp(S, "act", x_[:, :], pg[0:RB, :], reads=[pg], writes=[x_])

    front(0)
    for n, (seg, ch) in enumerate(work):
        b = seg - 1
        if True:
            init = [(hin, hin[:, d, b, ch:ch + 1]) for d in range(2)]
            x_ = xg[n % 2]
            g_ = gg[n % 2]
            act(S, g_[:, :], x_[:, :], AF.Square, reads=[x_], writes=[g_])
            ts(S, "dve", g_[:, :], g_[:, :], 0.044715, 1.0, ALU.mult, ALU.add, reads=[g_], writes=[g_])
            tt(S, "dve", g_[:, :], g_[:, :], x_[:, :], ALU.mult, reads=[g_, x_], writes=[g_])
            if n + 1 < len(work):
                front(n + 1)

            def gsig(g_=g_):
                act(S, g_[:, :], g_[:, :], AF.Sigmoid, reads=[g_], writes=[g_], scale=1.5957691216057308)
            rnn_back(S, C, R, 512, ch, n, init, None, R.hs, extra_sig=gsig)
            tt(S, "pool", g_[:, :], g_[:, :], x_[:, :], ALU.mult, reads=[g_, x_], writes=[g_])
            tt(S, "pool", R.hs[0][:, :], R.hs[0][:, :], R.hs[1][:, :], ALU.add, reads=[R.hs[0], R.hs[1]], writes=[R.hs[0]])
            tt(S, "pool", yin[:, ch, :], g_[:, :], R.hs[0][:, :], ALU.mult, reads=[g_, R.hs[0]], writes=[yin])
        if ch != RCH - 1:
            continue
        for t in range(4):
            mt = 4 * b + t
            ti = 2 + 1 + mt
            xt = xt_[ntile % 2]
            S.dma("sp", xt[:, :], x1[128 * ti:128 * ti + 128, :], xt, reads=[x1], writes=[xt])
            yt = ytmp[ntile % 2]
            for cb in range(2):
                py = R.pb[cb]
                for ch in range(RCH):
                    mm(S, py, py[:, :], yin[:, ch, 128 * t:128 * t + 128], R.wout[:, ch, 512 * cb:512 * cb + 512],
                       ch == 0, ch == RCH - 1, reads=[yin, R.wout])
                tt(S, "dve", yt[:, 512 * cb:512 * cb + 512], py[:, :], g1b[:, 512 * cb:512 * cb + 512], ALU.mult,
                   reads=[py, g1b], writes=[yt])
            tt(S, "pool", yt[:, :], yt[:, :], xt[:, :], ALU.add, reads=[yt, xt], writes=[yt])
            S.dma("sp", x2[128 * mt:128 * mt + 128, :], yt[:, :], yt, reads=[yt], writes=[x2])
            hb = h2[ntile % 2]
            hf = h32[ntile % 2]
            norm_tile(S, C, yt, ss[ntile % 2], rstd[ntile % 2], xs[ntile % 2], [R.pb[0], R.pb[1]],
                      (hb, lambda kc, hb=hb: hb[:, kc, :]), G2, S2,
                      hT32_dst=(hf, lambda kc, hf=hf: hf[:, kc, :]))
            if XS is None:
                S.dma("sp", h2T1[:, :, 128 * mt:128 * mt + 128].rearrange("kc p t -> p kc t"), hb[:, :, :], hb, reads=[hb], writes=[h2T1])
            else:
                xb_ = xsb[ntile % 2]
                cp(S, "pool", xb_[:, :], xs[ntile % 2][:, :], reads=[xs[ntile % 2]], writes=[xb_])
                S.dma("sp", XS[128 * mt:128 * mt + 128, :], xb_[:, :], xb_, reads=[xb_], writes=[XS])
            for kc in range(8):
                mm(S, plog, plog[:, :], hf[:, kc, :], wr[:, kc, :], kc == 0, kc == 7, reads=[hf, wr])
            r_ = rt[ntile % 2]
            lg, m8, ex, em, den, nv1 = r_[:, 0:8], r_[:, 8:16], r_[:, 16:24], r_[:, 24:32], r_[:, 32:33], r_[:, 33:34]
            tt(S, "dve", lg, plog[:, :], brb[:, :], ALU.add, reads=[plog, brb], writes=[r_])
            S.add("dve", lambda e, m8=m8, lg=lg: e.max(out=m8, in_=lg), reads=[r_], writes=[r_])
            ts(S, "dve", nv1, r_[:, 8:9], -1.0, None, ALU.mult, None, reads=[r_], writes=[r_])
            act(S, ex, lg, AF.Exp, reads=[r_], writes=[r_], bias=nv1)
            ts(S, "dve", em, lg, r_[:, 9:10], None, ALU.is_ge, None, reads=[r_], writes=[r_])
            if maskall is not None:
                cp(S, "dve", maskall[:, mt, :], em, reads=[r_], writes=[maskall])
            tt(S, "dve", em, em, ex, ALU.mult, reads=[r_], writes=[r_])
            S.add("dve", lambda e, den=den, em=em: e.reduce_sum(out=den, in_=em, axis=AX.X), reads=[r_], writes=[r_])
            S.add("dve", lambda e, den=den: e.reciprocal(out=den, in_=den), reads=[r_], writes=[r_])
            ts(S, "dve", comb[:, mt, :], em, den, None, ALU.mult, None, reads=[r_], writes=[comb])
            ntile += 1
    S.barrier()


def phase7_moe(S, C, I, mod_d, x2, h2T1, comb, out_d):
    m0 = S.off
    m5b = S.sb("m5b", [128, 1024], F32)
    S.dma("sp", m5b[:, :], bc_view(mod_d[1, 0, 5 * D:6 * D], D), m5b, reads=[mod_d], writes=[m5b])
    hT = S.sb("hTg", [128, 8, 2048], BF16)
    acc = [S.sb(f"acc{i}", [128, 1024], F32) for i in range(16)]
    NWB = 2
    w1g = [S.sb(f"w1g{i}", [128, 8, 512], BF16) for i in range(NWB)]
    w3g = [S.sb(f"w3g{i}", [128, 8, 512], BF16) for i in range(NWB)]
    w2g = [S.sb(f"w2g{i}", [128, 4, 1024], BF16) for i in range(NWB)]
    hid = [S.sb(f"hidm{i}", [128, 4, 512], BF16) for i in range(2)]
    sa = [S.sb(f"sam{i}", [128, 512], F32) for i in range(2)]
    xin = [S.sb(f"x7in{i}", [128, 1024], F32) for i in range(2)]
    pA = [S.ps(f"pA7{i}", 512 * i, 512) for i in range(2)]
    pB = [S.ps(f"pB7{i}", 1024 + 512 * i, 512) for i in range(2)]
    pY = [S.ps(f"pY7{i}", 2048 + 512 * i, 512) for i in range(4)]
    nw = 0
    nf = 0
    nh = 0
    nt = 0

    def load_w(e, fg, n):
        S.dma("pool", w1g[n % NWB][:, :, :], I["l1_moe_w1"][e, :, 512 * fg:512 * fg + 512].rearrange("(kc p) n -> p kc n", p=128),
              w1g[n % NWB], writes=[w1g[n % NWB]])
        S.dma("pool", w3g[n % NWB][:, :, :], I["l1_moe_w3"][e, :, 512 * fg:512 * fg + 512].rearrange("(kc p) n -> p kc n", p=128),
              w3g[n % NWB], writes=[w3g[n % NWB]])
        S.dma("pool", w2g[n % NWB][:, :, :], I["l1_moe_w2"][e, 512 * fg:512 * fg + 512, :].rearrange("(fc p) n -> p fc n", p=128),
              w2g[n % NWB], writes=[w2g[n % NWB]])

    steps = [(G, e, fg) for G in range(2) for e in range(NE) for fg in range(7)]
    load_w(steps[0][1], steps[0][2], 0)
    for si, (G, e, fg) in enumerate(steps):
        if e == 0 and fg == 0:
            S.dma("sp", hT[:, :, :], h2T1[:, :, 2048 * G:2048 * G + 2048].rearrange("kc p t -> p kc t"), hT, reads=[h2T1], writes=[hT])
        if si + 1 < len(steps):
            load_w(steps[si + 1][1], steps[si + 1][2], si + 1)
        a1, a3, a2 = w1g[si % NWB], w3g[si % NWB], w2g[si % NWB]
        first = (e == 0 and fg == 0)
        for tb in range(4):
            hd = hid[nh % 2]
            for fc in range(4):
                pa, pb = pA[nf % 2], pB[nf % 2]
                for kc in range(8):
                    mm(S, pa, pa[:, :], a1[:, kc, 128 * fc:128 * fc + 128], hT[:, kc, 512 * tb:512 * tb + 512], kc == 0, kc == 7,
                       reads=[a1, hT])
                for kc in range(8):
                    mm(S, pb, pb[:, :], a3[:, kc, 128 * fc:128 * fc + 128], hT[:, kc, 512 * tb:512 * tb + 512], kc == 0, kc == 7,
                       reads=[a3, hT])
                sb_ = sa[nf % 2]
                act(S, sb_[:, :], pa[:, :], AF.Silu, reads=[pa], writes=[sb_])
                tt(S, "dve", hd[:, fc, :], sb_[:, :], pb[:, :], ALU.mult, reads=[sb_, pb], writes=[hd])
                nf += 1
            for t in range(4):
                tl = 4 * tb + t
                mt = 16 * G + tl
                ac = acc[tl]
                for cb in range(2):
                    py = pY[(nt % 2) * 2 + cb]
                    for fc in range(4):
                        mm(S, py, py[:, :], hd[:, fc, 128 * t:128 * t + 128], a2[:, fc, 512 * cb:512 * cb + 512], fc == 0, fc == 3,
                           reads=[hd, a2])
                    sl = slice(512 * cb, 512 * cb + 512)
                    if first:
                        ts(S, "dve", ac[:, sl], py[:, :], comb[:, mt, e:e + 1], None, ALU.mult, None, reads=[py, comb], writes=[ac])
                    else:
                        stt(S, "dve", ac[:, sl], py[:, :], comb[:, mt, e:e + 1], ac[:, sl], ALU.mult, ALU.add,
                            reads=[py, comb, ac], writes=[ac])
                nt += 1
            nh += 1
        if e == NE - 1 and fg == 6:
            for tl in range(16):
                mt = 16 * G + tl
                xt = xin[tl % 2]
                S.dma("sp", xt[:, :], x2[128 * mt:128 * mt + 128, :], xt, reads=[x2], writes=[xt])
                ac = acc[tl]
                tt(S, "pool", ac[:, :], ac[:, :], m5b[:, :], ALU.mult, reads=[ac, m5b], writes=[ac])
                tt(S, "pool", xt[:, :], xt[:, :], ac[:, :], ALU.add, reads=[xt, ac], writes=[xt])
                S.dma("sp", out_d[128 * mt:128 * mt + 128, :], xt[:, :], xt, reads=[xt], writes=[out_d])
    S.barrier()
    S.off = m0


I32 = mybir.dt.int32
GSZ = 512
NGRP = 24
GT = GSZ // 128
NTB = 1
TBW = GSZ // NTB
TPB = TBW // 128


class Route:
    pass


def phase_route(S, C, I, comb, maskall):
    Rt = Route()
    Rt.slotA_i = S.sb("slotA_i", [128, 32], I32)
    Rt.slotB_i = S.sb("slotB_i", [128, 32], I32)
    Rt.gA = S.sb("gA", [128, 32], F32)
    Rt.gB = S.sb("gB", [128, 32], F32)
    Rt.Eg_i = S.sb("Eg_i", [128, NGRP], I32)
    Rt.tokA = S.sb("tokA", [128, 32, 8], I32)
    Rt.tokB = S.sb("tokB", [128, 32, 8], I32)
    m0 = S.off
    rc = S.sb("rc", [128, 216], F32)
    S.dma("sp", rc[:, :], I["rconst"], rc, writes=[rc])
    Lb = S.sb("Lb", [128, 128], BF16)
    ob = S.sb("ob", [128, 128], BF16)
    mb = S.sb("mb", [128, 256], BF16)
    cp(S, "dve", Lb[:, :], rc[:, 0:128], reads=[rc], writes=[Lb])
    S.add("pool", lambda e: e.memset(ob[:, :], 1.0), writes=[ob])
    cp(S, "dve", mb[:, :], maskall[:, :, :].rearrange("p t e -> p (t e)"), reads=[maskall], writes=[mb])
    p_r = S.ps("p_rin", 0, 256)
    p_c = S.ps("p_cnt", 512, 256)
    mm(S, p_r, p_r[:, :], Lb[:, :], mb[:, :], True, True, reads=[Lb, mb])
    mm(S, p_c, p_c[:, :], ob[:, :], mb[:, :], True, True, reads=[ob, mb])
    rin = S.sb("rin", [128, 32, NE], F32)
    cnt = S.sb("cnt", [128, 32, NE], F32)
    cp(S, "dve", rin[:, :, :].rearrange("p t e -> p (t e)"), p_r[:, :], reads=[p_r], writes=[rin])
    cp(S, "dve", cnt[:, :, :].rearrange("p t e -> p (t e)"), p_c[:, :], reads=[p_c], writes=[cnt])
    ones32 = S.sb("ones32", [128, 32], F32)
    S.add("pool", lambda e: e.memset(ones32[:, :], 1.0), writes=[ones32])
    inc = S.sb("inc", [128, NE, 32], F32)
    for e_ in range(NE):
        S.add("dve", lambda e, e_=e_: e.tensor_tensor_scan(out=inc[:, e_, :], data0=ones32[:, :], data1=cnt[:, :, e_],
                                                           initial=0.0, op0=ALU.mult, op1=ALU.add),
              reads=[ones32, cnt], writes=[inc])
    pre = S.sb("pre", [128, NE, 32], F32)
    tt(S, "dve", pre[:, :, :], inc[:, :, :], cnt[:, :, :].rearrange("p t e -> p e t"), ALU.subtract, reads=[inc, cnt], writes=[pre])
    sm = S.sb("route_sm", [128, 64], F32)
    n_e, G_, gend, gstart, sbase, tmp8 = (sm[:, 0:8], sm[:, 8:16], sm[:, 16:24], sm[:, 24:32], sm[:, 32:40], sm[:, 40:48])
    cp(S, "dve", n_e, inc[:, :, 31], reads=[inc], writes=[sm])
    ts(S, "dve", G_, n_e, 0.0, None, ALU.is_gt, None, reads=[sm], writes=[sm])
    for k in range(1, (4096 + GSZ - 1) // GSZ):
        ts(S, "dve", tmp8, n_e, float(GSZ * k), None, ALU.is_gt, None, reads=[sm], writes=[sm])
        tt(S, "dve", G_, G_, tmp8, ALU.add, reads=[sm], writes=[sm])
    S.add("dve", lambda e: e.tensor_tensor_scan(out=gend, data0=ones32[:, 0:8], data1=G_, initial=0.0, op0=ALU.mult, op1=ALU.add),
          reads=[sm, ones32], writes=[sm])
    tt(S, "dve", gstart, gend, G_, ALU.subtract, reads=[sm], writes=[sm])
    ts(S, "dve", sbase, gstart, float(GSZ), None, ALU.mult, None, reads=[sm], writes=[sm])
    v = S.sb("route_v", [128, 32, NE], F32)
    tt(S, "dve", v[:, :, :], rin[:, :, :], pre[:, :, :].rearrange("p e t -> p t e"), ALU.add, reads=[rin, pre], writes=[v])
    sb_b = bass.AP(sbase.tensor, sbase.offset, [list(sbase.ap[0]), [0, 32], [1, NE]])
    tt(S, "dve", v[:, :, :], v[:, :, :], sb_b, ALU.add, reads=[v, sm], writes=[v])
    stt(S, "dve", v[:, :, :], v[:, :, :], 1.0, maskall[:, :, :], ALU.add, ALU.mult, reads=[v, maskall], writes=[v])
    m8 = S.sb("route_m8", [128, 32, NE], F32)
    for t_ in range(32):
        S.add("dve", lambda e, t_=t_: e.max(out=m8[:, t_, :], in_=v[:, t_, :]), reads=[v], writes=[m8])
    sf = S.sb("route_sf", [128, 2, 32], F32)
    oh = S.sb("route_oh", [128, 32, NE], F32)
    for which, (sl_i, g_) in enumerate(((Rt.slotA_i, Rt.gA), (Rt.slotB_i, Rt.gB))):
        top = m8[:, :, which]
        ts(S, "dve", sf[:, which, :], top, -1.0, None, ALU.add, None, reads=[m8], writes=[sf])
        cp(S, "dve", sl_i[:, :], sf[:, which, :], reads=[sf], writes=[sl_i])
        top_b = bass.AP(top.tensor, top.offset, [list(top.ap[0]), list(top.ap[1]), [0, NE]])
        tt(S, "dve", oh[:, :, :], v[:, :, :], top_b, ALU.is_equal, reads=[v, m8], writes=[oh])
        tt(S, "dve", oh[:, :, :], oh[:, :, :], comb[:, :, :], ALU.mult, reads=[oh, comb], writes=[oh])
        S.add("dve", lambda e, g_=g_: e.reduce_sum(out=g_[:, :], in_=oh[:, :, :], axis=AX.X), reads=[oh], writes=[g_])
    Eg = S.sb("Eg_f", [128, NGRP], F32)
    ind = S.sb("route_ind", [128, 2, NGRP], F32)
    gio = rc[:, 184:184 + NGRP]
    S.add("pool", lambda e: e.memset(Eg[:, :], 0.0), writes=[Eg])
    for e_ in range(1, NE):
        ts(S, "dve", ind[:, 0, :], gio, gstart[:, e_:e_ + 1], None, ALU.is_ge, None, reads=[rc, sm], writes=[ind])
        ts(S, "dve", ind[:, 1, :], gio, gend[:, e_:e_ + 1], None, ALU.is_lt, None, reads=[rc, sm], writes=[ind])
        tt(S, "dve", ind[:, 0, :], ind[:, 0, :], ind[:, 1, :], ALU.mult, reads=[ind], writes=[ind])
        stt(S, "dve", Eg[:, :], ind[:, 0, :], float(e_), Eg[:, :], ALU.mult, ALU.add, reads=[ind, Eg], writes=[Eg])
    cp(S, "dve", Rt.Eg_i[:, :], Eg[:, :], reads=[Eg], writes=[Rt.Eg_i])
    tk = rc[:, 152:184]
    tk_b = bass.AP(tk.tensor, tk.offset, [list(tk.ap[0]), list(tk.ap[1]), [0, 8]])
    tkf = S.sb("tkf", [128, 32, 8], F32)
    cp(S, "dve", tkf[:, :, :], tk_b, reads=[rc], writes=[tkf])
    cp(S, "dve", Rt.tokA[:, :, :], tkf[:, :, :], reads=[tkf], writes=[Rt.tokA])
    ts(S, "dve", tkf[:, :, :], tkf[:, :, :], 4096.0, None, ALU.add, None, reads=[tkf], writes=[tkf])
    cp(S, "dve", Rt.tokB[:, :, :], tkf[:, :, :], reads=[tkf], writes=[Rt.tokB])
    S.barrier()
    S.off = m0
    return Rt


def phase_permute(S, C, I, Rt, XS, Hslot, Tslot):
    m0 = S.off
    zer = S.sb("zer", [128, GT * 1024], BF16)
    S.add("pool", lambda e: e.memset(zer[:, :], 0.0), writes=[zer])
    for i in range(NGRP):
        S.dma("sp", Hslot[GSZ * i:GSZ * i + GSZ, :].rearrange("(a p) n -> p a n", p=128),
              zer[:, :].rearrange("p (a n) -> p a n", a=GT), zer, reads=[zer], writes=[Hslot])
    dump = S.sb("dumpi", [128, 1024], I32)
    S.add("pool", lambda e: e.memset(dump[:, :], 8192), writes=[dump])
    S.dma("sp", Tslot.t.rearrange("(p a) o -> p (a o)", p=128), dump[:, 0:NGRP * GSZ // 128 * 8], dump, reads=[dump], writes=[Tslot])
    for mt in range(32):
        for sl, tk_ in ((Rt.slotA_i, Rt.tokA), (Rt.slotB_i, Rt.tokB)):
            S.add("pool", lambda e, sl=sl, tk_=tk_, mt=mt: e.indirect_dma_start(
                out=Tslot[:, :], out_offset=bass.IndirectOffsetOnAxis(ap=sl[:, mt:mt + 1], axis=0),
                in_=tk_[:, mt, :], in_offset=None, bounds_check=None),
                reads=[tk_, sl, Tslot], writes=[Tslot], dma_buf=tk_)
    xt = [S.sb(f"xperm{i}", [128, 1024], BF16) for i in range(3)]
    for mt in range(32):
        x_ = xt[mt % 3]
        S.dma("sp", x_[:, :], XS[128 * mt:128 * mt + 128, :], x_, reads=[XS], writes=[x_])
        for sl in (Rt.slotA_i, Rt.slotB_i):
            S.add("pool", lambda e, x_=x_, sl=sl, mt=mt: e.indirect_dma_start(
                out=Hslot[:, :], out_offset=bass.IndirectOffsetOnAxis(ap=sl[:, mt:mt + 1], axis=0),
                in_=x_[:, :], in_offset=None, bounds_check=None),
                reads=[x_, sl, Hslot], writes=[Hslot], dma_buf=x_)
    S.barrier()
    S.off = m0


def phase7s_moe(S, C, I, mod_d, Rt, Hslot, Tslot, Yab):
    m0 = S.off
    mods = S.sb("mods7", [128, 16], F32)
    tmpm = S.sb("tmpm7", [128, 16], F32)
    S.dma("sp", tmpm[:, 0:8], pp_view(I["l1_norm2"]), tmpm, writes=[tmpm], allow_slow_non_contiguous=True)
    load_mod_pp(S, tmpm, 1, mod_d, 1, 0, 4)
    load_mod_pp(S, mods, 1, mod_d, 1, 0, 3)
    stt(S, "dve", mods[:, 0:8], tmpm[:, 8:16], 1.0, tmpm[:, 0:8], ALU.add, ALU.mult, reads=[tmpm], writes=[mods])
    hT = [S.sb(f"hTs{i}", [128, 8, GSZ], BF16) for i in range(2)]
    acc = [S.sb(f"accs{i}", [128, 1024], F32) for i in range(GT)]
    NWB = 3
    w1g = [S.sb(f"w1s{i}", [128, 8, 512], BF16) for i in range(NWB)]
    w3g = [S.sb(f"w3s{i}", [128, 8, 512], BF16) for i in range(NWB)]
    w2g = [S.sb(f"w2s{i}", [128, 4, 1024], BF16) for i in range(NWB)]
    hid = [S.sb(f"hids{i}", [128, 4, 512], BF16) for i in range(2)]
    sa = [S.sb(f"sas{i}", [128, 512], F32) for i in range(2)]
    st_ = [S.sb(f"slt{i}", [128, 1024], BF16) for i in range(4)]
    tix = [S.sb(f"tix{i}", [128, 8], I32) for i in range(4)]
    pA = [S.ps(f"pA8{i}", 512 * i, 512) for i in range(2)]
    pB = [S.ps(f"pB8{i}", 1024 + 512 * i, 512) for i in range(2)]
    pY = [S.ps(f"pY8{i}", 2048 + 512 * i, 512) for i in range(4)]
    w1t, w3t, w2t = I["l1_moe_w1"], I["l1_moe_w3"], I["l1_moe_w2"]

    nreg = [0]

    def dyn_load(dst, static_ap, estride, g):
        off0 = static_ap.offset
        pat = [list(x) for x in static_ap.ap]
        tens = static_ap.tensor

        nreg[0] += 1
        rname = f"er{nreg[0]}"

        def allregs(e):
            hs = []
            try:
                while True:
                    nreg[0] += 1
                    hs.append(e.alloc_register(f"gc{nreg[0]}"))
            except ValueError:
                pass
            for h in hs:
                e.free_register(h)
            return hs

        def fn(e):
            before = allregs(e)
            with e.register(rname) as er:
                e.reg_load(er, Rt.Eg_i[0:1, g:g + 1])
                e.reg_mul(er, er, estride)
                e.reg_add(er, er, off0)
                ins = e.dma_start(out=dst[:, :, :], in_=bass.AP(tens, er, pat))
            after = {h.regnum for h in allregs(e)}
            for h in before:
                if h.regnum not in after:
                    e.free_register(h)
            return ins
        S.add("pool", fn, reads=[Rt.Eg_i], writes=[dst], dma_buf=dst)

    def load_w(g, fg, n):
        dyn_load(w1g[n % NWB], w1t[0, :, 512 * fg:512 * fg + 512].rearrange("(kc p) n -> p kc n", p=128), D * DFE, g)
        dyn_load(w3g[n % NWB], w3t[0, :, 512 * fg:512 * fg + 512].rearrange("(kc p) n -> p kc n", p=128), D * DFE, g)
        dyn_load(w2g[n % NWB], w2t[0, 512 * fg:512 * fg + 512, :].rearrange("(fc p) n -> p fc n", p=128), DFE * D, g)

    nst = [0]
    ntr = [0]

    def prologue(g):
        hb = hT[g % 2]
        for t in range(GT):
            x_ = st_[nst[0] % 4]
            nst[0] += 1
            S.dma("sp", x_[:, :], Hslot[GSZ * g + 128 * t:GSZ * g + 128 * t + 128, :], x_, reads=[Hslot], writes=[x_])
            p = pA[ntr[0] % 2]
            ntr[0] += 1
            pv = p.t.bitcast(BF16)
            for kc in range(8):
                tr(S, p, pv[:, 128 * kc:128 * kc + 128], x_[:, 128 * kc:128 * kc + 128], C.identb[:, :], reads=[x_, C.identb])
            for kc in range(8):
                src = pv[:, 128 * kc:128 * kc + 128]
                if kc % 2 == 0:
                    ts(S, "dve", hb[:, kc, 128 * t:128 * t + 128], src, mods[:, kc:kc + 1], mods[:, 8 + kc:9 + kc], ALU.mult, ALU.add,
                       reads=[p, mods], writes=[hb])
                else:
                    act(S, hb[:, kc, 128 * t:128 * t + 128], src, AF.Identity, reads=[p, mods], writes=[hb],
                        bias=mods[:, 8 + kc:9 + kc], scale=mods[:, kc:kc + 1])

    steps = [(g, fg) for g in range(NGRP) for fg in range(7)]
    load_w(0, 0, 0)
    load_w(0, 1, 1)
    prologue(0)
    nf = 0
    nh = 0
    nt = 0
    for si, (g, fg) in enumerate(steps):
        if si + 2 < len(steps):
            load_w(steps[si + 2][0], steps[si + 2][1], si + 2)
        if fg == 6 and g + 1 < NGRP:
            prologue(g + 1)
        hb = hT[g % 2]
        a1, a3, a2 = w1g[si % NWB], w3g[si % NWB], w2g[si % NWB]
        for tb in range(NTB):
            hd = hid[nh % 2]
            for fc in range(4):
                pa, pb = pA[nf % 2], pB[nf % 2]
                for kc in range(8):
                    mm(S, pa, pa[:, 0:TBW], a1[:, kc, 128 * fc:128 * fc + 128], hb[:, kc, TBW * tb:TBW * tb + TBW], kc == 0, kc == 7,
                       reads=[a1, hb])
                for kc in range(8):
                    mm(S, pb, pb[:, 0:TBW], a3[:, kc, 128 * fc:128 * fc + 128], hb[:, kc, TBW * tb:TBW * tb + TBW], kc == 0, kc == 7,
                       reads=[a3, hb])
                sb_ = sa[nf % 2]
                act(S, sb_[:, 0:TBW], pa[:, 0:TBW], AF.Silu, reads=[pa], writes=[sb_])
                tt(S, "dve", hd[:, fc, 0:TBW], sb_[:, 0:TBW], pb[:, 0:TBW], ALU.mult, reads=[sb_, pb], writes=[hd])
                nf += 1
            for t in range(TPB):
                tl = TPB * tb + t
                ac = acc[tl]
                for cb in range(2):
                    py = pY[(nt % 2) * 2 + cb]
                    for fc in range(4):
                        mm(S, py, py[:, :], hd[:, fc, 128 * t:128 * t + 128], a2[:, fc, 512 * cb:512 * cb + 512], fc == 0, fc == 3,
                           reads=[hd, a2])
                    sl = slice(512 * cb, 512 * cb + 512)
                    if fg == 0:
                        cp(S, "dve", ac[:, sl], py[:, :], reads=[py], writes=[ac])
                    else:
                        tt(S, "dve", ac[:, sl], py[:, :], ac[:, sl], ALU.add, reads=[py, ac], writes=[ac])
                if fg == 6:
                    tx = tix[(GT * g + tl) % 4]
                    S.dma("sp", tx[:, :], Tslot[GSZ * g + 128 * tl:GSZ * g + 128 * tl + 128, :], tx, reads=[Tslot], writes=[tx])
                    S.add("pool", lambda e, ac=ac, tx=tx: e.indirect_dma_start(
                        out=Yab[:, :], out_offset=bass.IndirectOffsetOnAxis(ap=tx[:, 0:1], axis=0),
                        in_=ac[:, :], in_offset=None, bounds_check=None),
                        reads=[ac, tx, Yab], writes=[Yab], dma_buf=ac)
                nt += 1
            nh += 1
    S.barrier()
    S.off = m0


def phase8_combine(S, C, I, mod_d, Rt, Yab, x2, out_d):
    m0 = S.off
    m5b = S.sb("m5bs", [128, 1024], F32)
    S.dma("sp", m5b[:, :], bc_view(mod_d[1, 0, 5 * D:6 * D], D), m5b, reads=[mod_d], writes=[m5b])
    ya = [S.sb(f"ya{i}", [128, 1024], F32) for i in range(2)]
    yb = [S.sb(f"yb{i}", [128, 1024], F32) for i in range(2)]
    xi = [S.sb(f"xc8{i}", [128, 1024], F32) for i in range(2)]
    for mt in range(32):
        a_, b_, x_ = ya[mt % 2], yb[mt % 2], xi[mt % 2]
        S.dma("sp", x_[:, :], x2[128 * mt:128 * mt + 128, :], x_, reads=[x2], writes=[x_])
        S.dma("sp", a_[:, :], Yab[128 * mt:128 * mt + 128, :], a_, reads=[Yab], writes=[a_])
        S.dma("sp", b_[:, :], Yab[4096 + 128 * mt:4096 + 128 * mt + 128, :], b_, reads=[Yab], writes=[b_])
        ts(S, "dve", a_[:, :], a_[:, :], Rt.gA[:, mt:mt + 1], None, ALU.mult, None, reads=[a_, Rt.gA], writes=[a_])
        stt(S, "dve", a_[:, :], b_[:, :], Rt.gB[:, mt:mt + 1], a_[:, :], ALU.mult, ALU.add, reads=[b_, Rt.gB, a_], writes=[a_])
        tt(S, "dve", a_[:, :], a_[:, :], m5b[:, :], ALU.mult, reads=[a_, m5b], writes=[a_])
        tt(S, "pool", x_[:, :], x_[:, :], a_[:, :], ALU.add, reads=[x_, a_], writes=[x_])
        S.dma("sp", out_d[128 * mt:128 * mt + 128, :], x_[:, :], x_, reads=[x_], writes=[out_d])
    S.barrier()
    S.off = m0


def declare_inputs(nc, names_shapes):
    I = {}
    for name, shape in names_shapes:
        I[name] = nc.dram_tensor(name, list(shape), F32, kind="ExternalInput").ap()
    return I


A_INPUTS = [
    ("xk", (NCH * 128, D)), ("cv", (2, D)), ("bt", (5, 128, 16, 5, 128)),
    ("l0_w_mod", (D, 6 * D)), ("l0_b_mod", (6 * D,)), ("l0_norm1", (D,)), ("l0_norm2", (D,)),
    ("l0_w_qkv", (D, 3 * D)), ("l0_q_gain", (HD,)), ("l0_k_gain", (HD,)), ("l0_w_o", (D, D)),
    ("l0_ffn_w1", (D, DFF)), ("l0_ffn_w3", (D, DFF)), ("l0_ffn_w2", (DFF, D)),
    ("l1_w_mod", (D, 6 * D)), ("l1_b_mod", (6 * D,)),
]


A_INPUTS2 = [
    ("l1_norm1", (D,)), ("l1_w_in", (D, 2 * DRNN)), ("l1_conv_w", (4, DRNN)), ("l1_conv_b", (DRNN,)),
    ("l1_gate_a_w", (2, RCH, RB, RB)), ("l1_gate_a_b", (2, DRNN)), ("l1_gate_x_w", (2, RCH, RB, RB)),
    ("l1_gate_x_b", (2, DRNN)), ("l1_lam", (2, DRNN)), ("masks", (128, 2)),
]
B_INPUTS = A_INPUTS2 + [
    ("x1", (NT1 * 128, D)), ("mod_in", (2, 2, 6 * D)), ("sall", (4 * RB, 576)), ("sown", (RB, 576)),
    ("sel", (RB, 8)), ("l1_norm2", (D,)), ("l1_w_out", (DRNN, D)), ("l1_router_w", (D, NE)), ("l1_router_b", (NE,)),
    ("l1_moe_w1", (NE, D, DFE)), ("l1_moe_w3", (NE, D, DFE)), ("l1_moe_w2", (NE, DFE, D)),
]


def build_A(debug=False):
    nc = bass.Bass("TRN2", target_bir_lowering=False)
    I = declare_inputs(nc, A_INPUTS + A_INPUTS2)
    S = Sched(nc)
    C = Common(S)
    mod_d = S.dram("mod_d", [2, 2, 6 * D], F32, kind="ExternalOutput")
    QT = S.dram("QT", [8, 128, NCH * 128], BF16)
    KT = S.dram("KT", [8, 128, NCH * 128], BF16)
    V = S.dram("V", [NCH * 128, D], BF16)
    x1a = S.dram("x1a", [NT1 * 128, D], F32, kind="ExternalOutput" if debug else "Internal")
    h2T = S.dram("h2T", [8, 128, NT1 * 128], BF16)
    x1 = S.dram("x1", [NT1 * 128, D], F32, kind="ExternalOutput")
    hT1 = S.dram("hT1", [8, 128, NT1 * 128], BF16)
    SAB = S.dram("sab_out", [RB, 576], F32, kind="ExternalOutput")
    phase0_adaln(S, C, I, mod_d)
    phase1_qkv(S, C, I, mod_d, QT, KT, V)
    phase2_attn(S, C, I, mod_d, QT, KT, V, x1a, h2T)
    phase3_ffn(S, C, I, mod_d, x1a, h2T, x1)
    phase4a_h1(S, C, I, mod_d, x1, hT1)
    phase4b_pass1(S, C, I, mod_d, hT1, SAB)
    S.emit()
    return nc


def build_B(debug=False):
    nc = bass.Bass("TRN2", target_bir_lowering=False)
    I = declare_inputs(nc, B_INPUTS)
    S = Sched(nc)
    C = Common(S)
    mod_d = Buf("mod_in", I["mod_in"])
    x1 = Buf("x1", I["x1"])
    SALL = Buf("sall", I["sall"])
    SOWN = Buf("sown", I["sown"])
    hT1 = S.dram("hT1", [8, 128, NT1 * 128], BF16)
    x2 = S.dram("x2", [32 * 128, D], F32, kind="ExternalOutput" if debug else "Internal")
    h2T1 = S.dram("h2T1", [8, 128, 32 * 128], BF16)
    out_d = S.dram("out", [32 * 128, D], F32, kind="ExternalOutput")
    hin = S.sb("hin", [RB, 2, 8, RCH], F32)
    comb = S.sb("comb", [128, 32, NE], F32)
    phase4a_h1(S, C, I, mod_d, x1, hT1)
    phase5_fold(S, C, I, SALL, SOWN, hin)
    m = S.off
    phase6_pass2(S, C, I, mod_d, x1, hT1, hin, x2, h2T1, comb)
    S.off = m
    phase7_moe(S, C, I, mod_d, x2, h2T1, comb, out_d)
    S.emit()
    return nc


F_INPUTS = A_INPUTS + A_INPUTS2 + [
    ("sel", (RB, 8)), ("l1_norm2", (D,)), ("l1_w_out", (DRNN, D)), ("l1_router_w", (D, NE)), ("l1_router_b", (NE,)),
    ("l1_moe_w1", (NE, D, DFE)), ("l1_moe_w3", (NE, D, DFE)), ("l1_moe_w2", (NE, DFE, D)), ("rconst", (128, 216)),
]


SPARSE = True


def build_fused():
    nc = bass.Bass("TRN2", target_bir_lowering=False)
    I = declare_inputs(nc, F_INPUTS)
    S = Sched(nc)
    C = Common(S)
    mod_d = S.dram("mod_d", [2, 2, 6 * D], F32)
    QT = S.dram("QT", [8, 128, NCH * 128], BF16)
    KT = S.dram("KT", [8, 128, NCH * 128], BF16)
    V = S.dram("V", [NCH * 128, D], BF16)
    x1a = S.dram("x1a", [NT1 * 128, D], F32)
    h2T = S.dram("h2T", [8, 128, NT1 * 128], BF16)
    x1 = S.dram("x1", [NT1 * 128, D], F32)
    hT1 = S.dram("hT1", [8, 128, NT1 * 128], BF16)
    SAB = S.dram("sab_b", [RB, 576], F32)
    SALL = S.dram("sall_g", [4 * RB, 576], F32)
    x2 = S.dram("x2", [32 * 128, D], F32)
    h2T1 = S.dram("h2T1", [8, 128, 32 * 128], BF16)
    out_d = S.dram("out", [32 * 128, D], F32, kind="ExternalOutput")
    phase0_adaln(S, C, I, mod_d)
    phase1_qkv(S, C, I, mod_d, QT, KT, V)
    phase2_attn(S, C, I, mod_d, QT, KT, V, x1a, h2T)
    phase3_ffn(S, C, I, mod_d, x1a, h2T, x1)
    phase4a_h1(S, C, I, mod_d, x1, hT1)
    phase4b_pass1(S, C, I, mod_d, hT1, SAB)
    cc = Buf("cc")
    S.add("pool", lambda e: e.collective_compute("AllGather", ALU.bypass, replica_groups=[[0, 1, 2, 3], [4, 5, 6, 7]],
                                                  ins=[SAB.t.opt()], outs=[SALL.t.opt()]),
          reads=[SAB], writes=[SALL], dma_buf=cc, inc=1)
    hin = S.sb("hin", [RB, 2, 8, RCH], F32)
    comb = S.sb("comb", [128, 32, NE], F32)
    phase5_fold(S, C, I, SALL, SAB, hin)
    if not SPARSE:
        m = S.off
        phase6_pass2(S, C, I, mod_d, x1, hT1, hin, x2, h2T1, comb)
        S.off = m
        phase7_moe(S, C, I, mod_d, x2, h2T1, comb, out_d)
    else:
        maskall = S.sb("maskall", [128, 32, NE], F32)
        XS = S.dram("XS", [32 * 128, D], BF16)
        Hslot = S.dram("Hslot", [NGRP * GSZ, D], BF16)
        Tslot = S.dram("Tslot", [NGRP * GSZ, 8], I32)
        Yab = S.dram("Yab", [8192 + 128, D], F32)
        m = S.off
        phase6_pass2(S, C, I, mod_d, x1, hT1, hin, x2, h2T1, comb, XS=XS, maskall=maskall)
        S.off = m
        Rt = phase_route(S, C, I, comb, maskall)
        phase_permute(S, C, I, Rt, XS, Hslot, Tslot)
        phase7s_moe(S, C, I, mod_d, Rt, Hslot, Tslot, Yab)
        phase8_combine(S, C, I, mod_d, Rt, Yab, x2, out_d)
    S.emit()
    return nc


def make_bias_tables(rpb, k):
    T0 = 32 * k
    out = np.empty((5, 128, 16, 5, 128), np.float32)
    p = np.arange(128)

    def table(gt, kts):
        qr = 2 * gt + p // 64
        qc = p % 64
        rs_ = np.clip(qr - 4, 0, 248)
        cs_ = np.clip(qc - 8, 0, 48)
        tab = np.full((128, 16, 5, 128), NEG, np.float32)
        for j, kt in enumerate(kts):
            if kt < 0 or kt > 127:
                continue
            kr = (2 * kt + p // 64)[:, None]
            kcol = (p % 64)[:, None]
            inwin = (kr >= rs_[None]) & (kr < rs_[None] + 8) & (kcol >= cs_[None]) & (kcol < cs_[None] + 16)
            dr = np.clip(kr - qr[None] + 7, 0, 14)
            dc = np.clip(kcol - qc[None] + 15, 0, 30)
            vals = rpb[:, dr, dc]
            tab[:, :, j, :] = np.where(inwin[:, None, :], vals.transpose(1, 0, 2), NEG)
        return tab

    def kts_for(lt):
        gt = T0 - 1 + lt
        kts = [gt - 2 + j for j in range(5)]
        if gt == 0:
            kts[0] = 3
        if gt == 127:
            kts[4] = 124
        return gt, kts

    out[0] = table(10, [8, 9, 10, 11, 12])
    for i, lt in enumerate((1, 2, 31, 32)):
        gt, kts = kts_for(lt)
        if gt < 0 or gt > 127:
            out[1 + i] = out[0]
        else:
            out[1 + i] = table(gt, kts)
    return out


def make_xk(x, ctx, b, k):
    T0 = 32 * k
    xk = np.zeros((NCH * 128, D), np.float32)
    xk[0:256] = ctx[b]
    for j in range(NKC):
        gt = T0 - 3 + j
        if k == 0 and j == 1:
            gt = 3
        if k == 3 and j == 36:
            gt = 124
        if 0 <= gt < 128:
            xk[256 + 128 * j:256 + 128 * j + 128] = x[b, 128 * gt:128 * gt + 128]
    return xk


_CACHE = {}


def _make_rconst():
    rc = np.zeros((128, 216), np.float32)
    p = np.arange(128)
    rc[:, 0:128] = (p[:, None] < p[None, :]).astype(np.float32)
    rc[:, 128:144] = np.arange(16, dtype=np.float32)[None, :]
    rc[:, 144:152] = np.arange(8, dtype=np.float32)[None, :]
    rc[:, 152:184] = (np.arange(32)[None, :] * 128 + p[:, None]).astype(np.float32)
    rc[:, 184:216] = np.arange(32, dtype=np.float32)[None, :]
    return rc


RCONST = _make_rconst()


def kernel(**inputs):
    inp = {k: np.ascontiguousarray(np.asarray(v, dtype=np.float32)) for k, v in inputs.items()}
    if "F" not in _CACHE:
        _CACHE["F"] = build_fused()
    nc = _CACHE["F"]
    n = 8
    maps = []
    for i in range(n):
        b, k = i // 4, i % 4
        sel = np.zeros((RB, 8), np.float32)
        for j in range(4):
            if j < k:
                sel[:, j] = 1.0
            if j > k:
                sel[:, 4 + j] = 1.0
        m = {"xk": make_xk(inp["x"], inp["ctx"], b, k),
             "cv": np.stack([inp["c"][b], inp["c_ctx"]]).astype(np.float32),
             "bt": make_bias_tables(inp["l0_rpb"], k),
             "masks": np.tile(np.array([[0.0 if k == 0 else 1.0, 0.0 if k == 3 else 1.0]], np.float32), (128, 1)),
             "sel": sel, "rconst": RCONST}
        for name, _ in F_INPUTS:
            if name not in m:
                m[name] = inp[name]
        maps.append(m)
    res = run_bass_kernel_spmd(nc, maps, core_ids=list(range(n)))
    out = np.empty((2, 16384, D), np.float32)
    for i in range(n):
        b, k = i // 4, i % 4
        out[b, 4096 * k:4096 * k + 4096] = np.asarray(res.results[i]["out"])
    return out
```

```python
import numpy as np
from contextlib import ExitStack
import concourse.bass as bass
import concourse.mybir as mybir
from concourse.bass_utils import run_bass_kernel_spmd

F32 = mybir.dt.float32
BF16 = mybir.dt.bfloat16
AF = mybir.ActivationFunctionType
ALU = mybir.AluOpType
AX = mybir.AxisListType

ENGS = ("pe", "act", "dve", "pool", "sp")


class Buf:
    __slots__ = ("name", "t", "last_w", "reads")

    def __init__(self, name, t=None):
        self.name = name
        self.t = t
        self.last_w = None
        self.reads = []

    def __getitem__(self, k):
        return self.t[k]


class Op:
    __slots__ = ("eng", "fn", "deps", "signal", "pos", "dma_key", "val", "is_dma", "inc")

    def __init__(self, eng, fn):
        self.eng = eng
        self.fn = fn
        self.deps = []
        self.signal = False
        self.pos = 0
        self.is_dma = False
        self.dma_key = None
        self.val = 0
        self.inc = 16


class Sched:
    ARENA_F32 = 53000

    def __init__(self, nc, same_engine_sync=True):
        self.nc = nc
        self.ops = {e: [] for e in ENGS}
        self.same = same_engine_sync
        self.waited = {e: {} for e in ENGS}
        self.dma_cnt = {}
        self.dma_keys = []
        self.slot_of = {}
        self.bar_pos = {}
        self.off = 0
        self.arena = None
        self.peak = 0
        self.psum = None
        self.ndram = 0

    def sb(self, name, shape, dtype, off=None):
        if self.arena is None:
            self.arena = self.nc.alloc_sbuf_tensor("arena", [128, self.ARENA_F32], F32)
        esz = 2 if dtype == BF16 else 4
        nel = int(np.prod(shape[1:]))
        nbytes = (nel * esz + 63) // 64 * 64
        if off is None:
            off = self.off
            self.off += nbytes
        assert off + nbytes <= self.ARENA_F32 * 4, (name, off, nbytes)
        self.peak = max(self.peak, off + nbytes)
        a = self.arena[0:shape[0], off // 4: off // 4 + nbytes // 4]
        if dtype != F32:
            a = a.bitcast(dtype)
        a = a[:, 0:nel]
        if len(shape) > 2:
            names = "abcdefg"[:len(shape) - 1]
            pat = "p (" + " ".join(names) + ") -> p " + " ".join(names)
            a = a.rearrange(pat, **{nm: shape[1 + i] for i, nm in enumerate(names[:-1])})
        return Buf(name, a)

    def ps(self, name, col, ncols, dtype=F32, parts=128):
        if self.psum is None:
            self.psum = self.nc.alloc_psum_tensor("psum_all", [128, 4096], F32).ap()
        a = self.psum[0:parts, col:col + ncols]
        if dtype != F32:
            a = a.bitcast(dtype)
        return Buf(name, a)

    def dram(self, name, shape, dtype, kind="Internal"):
        return Buf(name, self.nc.dram_tensor(name, list(shape), dtype, kind=kind).ap())

    def _need(self, op, prod):
        if prod is None or prod is op:
            return
        e = op.eng
        w = self.waited[e]
        if prod.is_dma:
            k = ("d", prod.dma_key)
            if w.get(k, 0) >= prod.val:
                return
            w[k] = prod.val
            op.deps.append(prod)
            return
        if prod.eng == e and (not self.same or e == "pe"):
            return
        if w.get(prod.eng, -1) >= prod.pos:
            return
        w[prod.eng] = prod.pos
        prod.signal = True
        op.deps.append(prod)

    def add(self, eng, fn, reads=(), writes=(), dma_buf=None, inc=16):
        op = Op(eng, fn)
        op.inc = inc
        op.pos = len(self.ops[eng])
        if dma_buf is not None:
            op.is_dma = True
            bid = id(dma_buf)
            if bid not in self.slot_of:
                slot = len(self.slot_of)
                self.slot_of[bid] = slot
                if slot >= len(self.dma_keys):
                    self.dma_keys.append(slot)
                    self.dma_cnt[slot] = 0
            key = self.slot_of[bid]
            self.dma_cnt[key] += inc
            op.dma_key = key
            op.val = self.dma_cnt[key]
        for b in reads:
            self._need(op, b.last_w)
        for b in writes:
            self._need(op, b.last_w)
            for r in b.reads:
                self._need(op, r)
        for b in reads:
            b.reads.append(op)
        for b in writes:
            b.last_w = op
            b.reads = []
        self.ops[eng].append(op)
        return op

    def dma(self, eng, out, in_, sbuf, reads=(), writes=(), **kw):
        return self.add(eng, lambda e: e.dma_start(out=out, in_=in_, **kw),
                        reads=reads, writes=writes, dma_buf=sbuf)

    def barrier(self):
        lasts = []
        for e in ENGS:
            for op in reversed(self.ops[e]):
                if not op.is_dma and op.fn is not None:
                    lasts.append(op)
                    break
        last_dma = {}
        for e in ENGS:
            for op in self.ops[e][self.bar_pos.get(e, 0):]:
                if op.is_dma:
                    last_dma[op.dma_key] = op
        for e in ENGS:
            op = Op(e, None)
            op.pos = len(self.ops[e])
            for p in lasts:
                if p.eng != e:
                    self._need(op, p)
            for p in last_dma.values():
                self._need(op, p)
            self.ops[e].append(op)
            self.bar_pos[e] = len(self.ops[e])
        self.slot_of = {}

    def emit(self):
        nc = self.nc
        with ExitStack() as st:
            esem = {e: st.enter_context(nc.semaphore(f"s_{e}")) for e in ENGS}
            dsem = {k: st.enter_context(nc.semaphore(f"d_{i}")) for i, k in enumerate(self.dma_keys)}
            for e in ENGS:
                c = 0
                for op in self.ops[e]:
                    if op.is_dma:
                        continue
                    if op.signal:
                        c += 1
                        op.val = c
            block = st.enter_context(nc.Block())

            def run(ename, eng):
                for op in self.ops[ename]:
                    for p in op.deps:
                        if p.is_dma:
                            eng.wait_ge(dsem[p.dma_key], p.val)
                        else:
                            eng.wait_ge(esem[p.eng], p.val)
                    if op.fn is None:
                        continue
                    ins = op.fn(eng)
                    if op.is_dma:
                        ins.then_inc(dsem[op.dma_key], op.inc)
                    elif op.signal:
                        ins.then_inc(esem[ename], 1)

            @block.tensor
            def _(eng):
                run("pe", eng)

            @block.scalar
            def _(eng):
                run("act", eng)

            @block.vector
            def _(eng):
                run("dve", eng)

            @block.gpsimd
            def _(eng):
                run("pool", eng)

            @block.sync
            def _(eng):
                run("sp", eng)


D = 1024
KC = 8
NH = 16
HD = 64
NQT = 34
NKC = 38
NCH = 40
NT1 = 36
DFF = 2816
NFC = 22
DRNN = 1536
RCH = 16
RB = 96
NE = 8
DFE = 3584
EPS = 1e-6
NEG = -30000.0


def mm(S, ps, out, lhsT, rhs, start, stop, reads):
    S.add("pe", lambda e: e.matmul(out, lhsT=lhsT, rhs=rhs, start=start, stop=stop), reads=reads, writes=[ps])


def tr(S, ps, out, in_, ident, reads):
    S.add("pe", lambda e: e.transpose(out=out, in_=in_, identity=ident), reads=reads, writes=[ps])


def act(S, out, in_, func, reads, writes, bias=None, scale=None, accum_out=None):
    kw = {}
    if bias is not None:
        kw["bias"] = bias
    if scale is not None:
        kw["scale"] = scale
    if accum_out is not None:
        kw["accum_out"] = accum_out
    S.add("act", lambda e: e.activation(out=out, in_=in_, func=func, **kw), reads=reads, writes=writes)


def ts(S, eng, out, in0, s1, s2, op0, op1, reads, writes):
    if op1 is None:
        S.add(eng, lambda e: e.tensor_scalar(out=out, in0=in0, scalar1=s1, scalar2=None, op0=op0), reads=reads, writes=writes)
    else:
        S.add(eng, lambda e: e.tensor_scalar(out=out, in0=in0, scalar1=s1, scalar2=s2, op0=op0, op1=op1), reads=reads, writes=writes)


def stt(S, eng, out, in0, scalar, in1, op0, op1, reads, writes):
    S.add(eng, lambda e: e.scalar_tensor_tensor(out=out, in0=in0, scalar=scalar, in1=in1, op0=op0, op1=op1),
          reads=reads, writes=writes)


def tt(S, eng, out, in0, in1, op, reads, writes):
    S.add(eng, lambda e: e.tensor_tensor(out=out, in0=in0, in1=in1, op=op), reads=reads, writes=writes)


def cp(S, eng, out, in_, reads, writes):
    if eng == "act":
        S.add("act", lambda e: e.copy(out=out, in_=in_), reads=reads, writes=writes)
    else:
        S.add(eng, lambda e: e.tensor_copy(out=out, in_=in_), reads=reads, writes=writes)


def pp_view(vec_ap):
    return vec_ap.rearrange("(c p) -> p c", p=128)


def bc_view(row_ap, n):
    return bass.AP(row_ap.tensor, row_ap.offset, [[0, 128], [1, n]])


class Common:
    def __init__(self, S):
        self.identf = S.sb("identf", [128, 128], F32)
        self.identb = S.sb("identb", [128, 128], BF16)
        self.junk = S.sb("junk", [128, 1024], BF16)
        self.epsb = S.sb("epsb", [128, 1], F32)
        S.add("pool", lambda e: e.memset(self.epsb[:, :], EPS), writes=[self.epsb])
        for b, in (self.identf,), (self.identb,):
            S.add("pool", lambda e, b=b: e.memset(b[:], 1.0), writes=[b])
            S.add("pool", lambda e, b=b: e.affine_select(out=b[:], in_=b[:], pattern=[[-1, 128]], compare_op=ALU.is_equal,
                                                         fill=0.0, base=0, channel_multiplier=1), reads=[b], writes=[b])


def norm_tile(S, C, xt, ss, rstd, xs, pst, hT_dst, G, Sft, reads_extra=(), hT32_dst=None):
    hbuf, hfn = hT_dst
    act(S, C.junk[:, :], xt[:, :], AF.Square, reads=[xt], writes=[C.junk, ss], accum_out=ss[:, 0:1])
    ts(S, "dve", rstd[:, 0:1], ss[:, 0:1], 1.0 / D, EPS, ALU.mult, ALU.add, reads=[ss], writes=[rstd])
    act(S, rstd[:, 0:1], rstd[:, 0:1], AF.Sqrt, reads=[rstd], writes=[rstd])
    S.add("dve", lambda e: e.reciprocal(out=rstd[:, 0:1], in_=rstd[:, 0:1]), reads=[rstd], writes=[rstd])
    act(S, xs[:, :], xt[:, :], AF.Identity, reads=[xt, rstd], writes=[xs], scale=rstd[:, 0:1])
    for half in range(2):
        p = pst[half]
        for q in range(4):
            kc = half * 4 + q
            tr(S, p, p[:, 128 * q:128 * q + 128], xs[:, 128 * kc:128 * kc + 128], C.identf[:, :], reads=[xs, C.identf])
        for q in range(4):
            kc = half * 4 + q
            src = p[:, 128 * q:128 * q + 128]
            if hT32_dst is not None:
                b32, f32fn = hT32_dst
                if kc % 2 == 0:
                    ts(S, "dve", f32fn(kc), src, G[:, kc:kc + 1], Sft[:, kc:kc + 1], ALU.mult, ALU.add,
                       reads=[p, G, Sft], writes=[b32])
                else:
                    act(S, f32fn(kc), src, AF.Identity, reads=[p, G, Sft], writes=[b32],
                        bias=Sft[:, kc:kc + 1], scale=G[:, kc:kc + 1])
                cp(S, "pool", hfn(kc), f32fn(kc), reads=[b32], writes=[hbuf])
            elif kc % 2 == 0:
                ts(S, "dve", hfn(kc), src, G[:, kc:kc + 1], Sft[:, kc:kc + 1], ALU.mult, ALU.add,
                   reads=[p, G, Sft], writes=[hbuf])
            else:
                act(S, hfn(kc), src, AF.Identity, reads=[p, G, Sft], writes=[hbuf],
                    bias=Sft[:, kc:kc + 1], scale=G[:, kc:kc + 1])


def load_mod_pp(S, dst, col, mod_d, layer, stream, which, tmp_ok=True):
    src = pp_view(mod_d[layer, stream, which * D:(which + 1) * D])
    S.dma("sp", dst[:, col * 8:col * 8 + 8], src, dst, reads=[mod_d], writes=[dst], allow_slow_non_contiguous=True)


def phase0_adaln(S, C, I, mod_d):
    m0 = S.off
    cT = S.sb("cT", [128, 8, 2], F32)
    sc = S.sb("sc", [128, 8, 2], F32)
    rep = S.sb("rep", [128, 16, 128], BF16)
    wblk = [S.sb(f"wblk{i}", [128, 8, 512], BF16) for i in range(3)]
    bblk = [S.sb(f"bblk{i}", [128, 512], F32) for i in range(2)]
    res = [S.sb(f"res{i}", [128, 512], F32) for i in range(4)]
    pss = [S.ps(f"p0ps{i}", 512 * i, 512) for i in range(4)]
    for s in range(2):
        S.dma("sp", cT[:, :, s], pp_view(I["cv"][s, :]), cT, writes=[cT], allow_slow_non_contiguous=True)
    act(S, sc[:, :, :], cT[:, :, :], AF.Silu, reads=[cT], writes=[sc])
    for kc in range(8):
        for s in range(2):
            cp(S, "dve", rep[:, kc * 2 + s, :], sc[:, kc, s:s + 1].to_broadcast([128, 128]), reads=[sc], writes=[rep])
    it = 0
    for l in range(2):
        wm = I[f"l{l}_w_mod"]
        bm = I[f"l{l}_b_mod"]
        for j in range(12):
            wb = wblk[it % 3]
            bb = bblk[it % 2]
            S.dma("pool", wb[:, :, :], wm[:, 512 * j:512 * j + 512].rearrange("(kc p) n -> p kc n", p=128), wb, writes=[wb])
            S.dma("sp", bb[:, :], bc_view(bm[512 * j:512 * j + 512], 512), bb, writes=[bb])
            for s in range(2):
                p = pss[(it % 2) * 2 + s]
                r = res[(it % 2) * 2 + s]
                for kc in range(8):
                    mm(S, p, p[:, :], rep[:, kc * 2 + s, :], wb[:, kc, :], kc == 0, kc == 7, reads=[rep, wb])
                tt(S, "dve", r[:, :], p[:, :], bb[:, :], ALU.add, reads=[p, bb], writes=[r])
                S.dma("sp", mod_d[l, s:s + 1, 512 * j:512 * j + 512], r[0:1, :], r, reads=[r], writes=[mod_d])
            it += 1
    S.barrier()
    S.off = m0


def phase1_qkv(S, C, I, mod_d, QT, KT, V):
    m0 = S.off
    wq = S.sb("wqkv", [128, 8, 3072], BF16)
    S.dma("pool", wq[:, :, :], I["l0_w_qkv"].rearrange("(kc p) n -> p kc n", p=128), wq, writes=[wq])
    mods = S.sb("mods1", [128, 32], F32)
    tmpm = S.sb("tmpm1", [128, 24], F32)
    S.dma("sp", tmpm[:, 0:8], pp_view(I["l0_norm1"]), tmpm, writes=[tmpm], allow_slow_non_contiguous=True)
    for s in range(2):
        load_mod_pp(S, tmpm, 1 + s, mod_d, 0, s, 1)
        load_mod_pp(S, mods, 2 * s + 1, mod_d, 0, s, 0)
        stt(S, "dve", mods[:, 16 * s:16 * s + 8], tmpm[:, 8 + 8 * s:16 + 8 * s], 1.0, tmpm[:, 0:8], ALU.add, ALU.mult,
            reads=[tmpm], writes=[mods])
    Gs = [Buf("G", mods[:, 0:8]), Buf("Gc", mods[:, 16:24])]
    Ss = [Buf("S", mods[:, 8:16]), Buf("Sc", mods[:, 24:32])]
    gains = S.sb("gains", [128, 2], F32)
    for half in range(2):
        S.dma("sp", gains[64 * half:64 * half + 64, 0:1], I["l0_q_gain"].rearrange("(p o) -> p o", o=1), gains, writes=[gains])
        S.dma("sp", gains[64 * half:64 * half + 64, 1:2], I["l0_k_gain"].rearrange("(p o) -> p o", o=1), gains, writes=[gains])
    ts(S, "dve", gains[:, 0:1], gains[:, 0:1], HD ** -0.5, None, ALU.mult, None, reads=[gains], writes=[gains])
    bd = S.sb("bd", [128, 128], BF16)
    S.add("pool", lambda e: e.memset(bd[:, :], 0.0), writes=[bd])
    S.add("pool", lambda e: e.memset(bd[0:64, 0:64], 1.0 / 64), reads=[bd], writes=[bd])
    S.add("pool", lambda e: e.memset(bd[64:128, 64:128], 1.0 / 64), reads=[bd], writes=[bd])
    xin = [S.sb(f"xin{i}", [128, 1024], F32) for i in range(3)]
    xs = [S.sb(f"xs{i}", [128, 1024], F32) for i in range(2)]
    ss = [S.sb(f"ss{i}", [128, 1], F32) for i in range(2)]
    rstd = [S.sb(f"rstd{i}", [128, 1], F32) for i in range(2)]
    hT = [S.sb(f"hT{i}", [128, 8, 512], BF16) for i in range(2)]
    sq = [S.sb(f"sq{i}", [128, 512], BF16) for i in range(2)]
    rs = [S.sb(f"rs{i}", [128, 512], F32) for i in range(2)]
    qn = [S.sb(f"qn{i}", [128, 512], BF16) for i in range(3)]
    vt = [S.sb(f"vt{i}", [128, 1024], BF16) for i in range(2)]
    pT = [S.ps(f"pT{i}", 512 * i, 512) for i in range(2)]
    pQ = [S.ps(f"pQ{i}", 1024 + 512 * i, 512) for i in range(2)]
    pR = [S.ps(f"pR{i}", 2048 + 512 * i, 512) for i in range(2)]
    pV = [S.ps(f"pV{i}", 3072 + 512 * i, 512) for i in range(2)]
    nt = 0
    nqk = 0
    for blk in range(NCH // 4):
        hb = hT[blk % 2]
        for t in range(4):
            g = blk * 4 + t
            s = 0 if g >= 2 else 1
            xt = xin[nt % 3]
            S.dma("sp", xt[:, :], I["xk"][128 * g:128 * g + 128, :], xt, writes=[xt])
            norm_tile(S, C, xt, ss[nt % 2], rstd[nt % 2], xs[nt % 2], pT,
                      (hb, lambda kc, hb=hb, t=t: hb[:, kc, 128 * t:128 * t + 128]), Gs[s], Ss[s])
            nt += 1
        for which, dst_d in ((0, QT), (1, KT)):
            for hp in range(8):
                p = pQ[nqk % 2]
                pr = pR[nqk % 2]
                col = which * 1024 + 128 * hp
                for kc in range(8):
                    mm(S, p, p[:, :], wq[:, kc, col:col + 128], hb[:, kc, :], kc == 0, kc == 7, reads=[wq, hb])
                sqb = sq[nqk % 2]
                act(S, sqb[:, :], p[:, :], AF.Square, reads=[p], writes=[sqb])
                mm(S, pr, pr[:, :], bd[:, :], sqb[:, :], True, True, reads=[bd, sqb])
                rsb = rs[nqk % 2]
                act(S, rsb[:, :], pr[:, :], AF.Sqrt, reads=[pr, C.epsb], writes=[rsb], bias=C.epsb[:, 0:1])
                S.add("dve", lambda e, rsb=rsb: e.reciprocal(out=rsb[:, :], in_=rsb[:, :]), reads=[rsb], writes=[rsb])
                qb = qn[nqk % 3]
                stt(S, "dve", qb[:, :], p[:, :], gains[:, which:which + 1], rsb[:, :], ALU.mult, ALU.mult,
                    reads=[p, gains, rsb], writes=[qb])
                S.dma("sp", dst_d[hp, :, 512 * blk:512 * blk + 512], qb[:, :], qb, reads=[qb], writes=[dst_d])
                nqk += 1
        for t in range(4):
            g = blk * 4 + t
            vb = vt[g % 2]
            for cb in range(2):
                p = pV[cb]
                for kc in range(8):
                    mm(S, p, p[:, :], hb[:, kc, 128 * t:128 * t + 128], wq[:, kc, 2048 + 512 * cb:2048 + 512 * cb + 512],
                       kc == 0, kc == 7, reads=[hb, wq])
                cp(S, "act", vb[:, 512 * cb:512 * cb + 512], p[:, :], reads=[p], writes=[vb])
            S.dma("sp", V[128 * g:128 * g + 128, :], vb[:, :], vb, reads=[vb], writes=[V])
    S.barrier()
    S.off = m0


def phase2_attn(S, C, I, mod_d, QT, KT, V, x1a, h2T):
    m0 = S.off
    wo = S.sb("wo", [128, 8, 1024], BF16)
    S.dma("pool", wo[:, :, :], I["l0_w_o"].rearrange("(hp p) n -> p hp n", p=128), wo, writes=[wo])
    bgen = S.sb("bgen", [128, 16, 5, 128], BF16)
    bspec = S.sb("bspec", [128, 16, 5, 128], BF16)
    S.dma("pool", bgen[:, :, :, :], I["bt"][0], bgen, writes=[bgen])
    mods = S.sb("mods2", [128, 32], F32)
    tmpm = S.sb("tmpm2", [128, 24], F32)
    S.dma("sp", tmpm[:, 0:8], pp_view(I["l0_norm2"]), tmpm, writes=[tmpm], allow_slow_non_contiguous=True)
    g1b = []
    for s in range(2):
        load_mod_pp(S, tmpm, 1 + s, mod_d, 0, s, 4)
        load_mod_pp(S, mods, 2 * s + 1, mod_d, 0, s, 3)
        stt(S, "dve", mods[:, 16 * s:16 * s + 8], tmpm[:, 8 + 8 * s:16 + 8 * s], 1.0, tmpm[:, 0:8], ALU.add, ALU.mult,
            reads=[tmpm], writes=[mods])
        gb = S.sb(f"g1b{s}", [128, 1024], F32)
        S.dma("sp", gb[:, :], bc_view(mod_d[0, s, 2 * D:3 * D], D), gb, reads=[mod_d], writes=[gb])
        g1b.append(gb)
    Gs = [Buf("G2", mods[:, 0:8]), Buf("G2c", mods[:, 16:24])]
    Ss = [Buf("S2", mods[:, 8:16]), Buf("S2c", mods[:, 24:32])]
    RING = 6
    ktr = S.sb("ktr", [128, 8, RING, 128], BF16)
    vr = S.sb("vr", [128, RING, 16, 65], BF16)
    ktc = S.sb("ktc", [128, 8, 256], BF16)
    vc = S.sb("vc", [128, 2, 16, 65], BF16)
    kslots = [Buf(f"ks{i}", None) for i in range(RING)]
    vslots = [Buf(f"vs{i}", None) for i in range(RING)]
    S.add("pool", lambda e: e.memset(vr[:, :, :, 64:65], 1.0), writes=vslots)
    S.add("pool", lambda e: e.memset(vc[:, :, :, 64:65], 1.0), writes=[vc])
    S.dma("sp", ktc[:, :, :], KT[:, :, 0:256].rearrange("hp p t -> p hp t"), ktc, reads=[KT], writes=[ktc])
    for cc in range(2):
        S.dma("sp", vc[:, cc, :, 0:64], V[128 * cc:128 * cc + 128, :].rearrange("p (h d) -> p h d", h=16), vc,
              reads=[V], writes=[vc])
    qt = [S.sb(f"qt{i}", [128, 8, 128], BF16) for i in range(2)]
    xt_ = [S.sb(f"x2in{i}", [128, 1024], F32) for i in range(2)]
    tb = [S.sb(f"tb{i}", [128, 5, 128], F32) for i in range(2)]
    pt = [S.sb(f"pt{i}", [128, 7, 128], BF16) for i in range(2)]
    rec = [S.sb(f"rec{i}", [128, 1], F32) for i in range(4)]
    on = [S.sb(f"on{i}", [128, 16, 64], BF16) for i in range(2)]
    oT = [S.sb(f"oT{i}", [128, 8, 128], BF16) for i in range(2)]
    x1t = [S.sb(f"x1t{i}", [128, 1024], F32) for i in range(2)]
    xs = [S.sb(f"xs2{i}", [128, 1024], F32) for i in range(2)]
    ss = [S.sb(f"ss2{i}", [128, 1], F32) for i in range(2)]
    rstd = [S.sb(f"rstd2{i}", [128, 1], F32) for i in range(2)]
    h2 = [S.sb(f"h2t{i}", [128, 8, 128], BF16) for i in range(2)]
    pS = [S.ps(f"pS{i}", 1024 * i, 896) for i in range(2)]
    pO = [S.ps(f"pO{i}", 2048 + 128 * i, 65) for i in range(4)]
    pY = [S.ps(f"pY{i}", 2560 + 512 * i, 512) for i in range(2)]
    pOT = S.ps("pOT", 3584, 512, BF16)

    loaded = set()

    def ensure_chunk(g):
        if g in loaded:
            return
        loaded.add(g)
        sl = g % RING
        S.dma("sp", ktr[:, :, sl, :], KT[:, :, 128 * g:128 * g + 128].rearrange("hp p t -> p hp t"), kslots[sl],
              reads=[KT], writes=[kslots[sl]])
        S.dma("sp", vr[:, sl, :, 0:64], V[128 * g:128 * g + 128, :].rearrange("p (h d) -> p h d", h=16), vslots[sl],
              reads=[V], writes=[vslots[sl]])

    spec_lts = {1: 1, 2: 2, 31: 3, 32: 4}
    nh = 0
    for ti in range(NT1):
        is_ctx = ti < 2
        lt = ti - 2
        g = ti if is_ctx else lt + 4
        s = 1 if is_ctx else 0
        q = qt[ti % 2]
        S.dma("sp", q[:, :, :], QT[:, :, 128 * g:128 * g + 128].rearrange("hp p t -> p hp t"), q, reads=[QT], writes=[q])
        xt = xt_[ti % 2]
        S.dma("sp", xt[:, :], I["xk"][128 * g:128 * g + 128, :], xt, writes=[xt])
        nloc = 0 if is_ctx else 5
        if not is_ctx:
            for j in range(5):
                ensure_chunk(lt + 2 + j)
            if lt in spec_lts:
                S.dma("pool", bspec[:, :, :, :], I["bt"][spec_lts[lt]], bspec, writes=[bspec])
                bias = bspec
            else:
                bias = bgen
        onb = on[ti % 2]
        for h in range(NH):
            hp, half = h // 2, h % 2
            lo = 64 * half
            p = pS[nh % 2]
            ptb = pt[nh % 2]
            for j in range(nloc):
                sl = (lt + 2 + j) % RING
                mm(S, p, p[:, 128 * j:128 * j + 128], ktr[lo:lo + 64, hp, sl, :], q[lo:lo + 64, hp, :], True, True,
                   reads=[kslots[sl], q])
            for cc in range(2):
                jj = nloc + cc
                mm(S, p, p[:, 128 * jj:128 * jj + 128], ktc[lo:lo + 64, hp, 128 * cc:128 * cc + 128], q[lo:lo + 64, hp, :],
                   True, True, reads=[ktc, q])
            if nloc:
                t_ = tb[nh % 2]
                tt(S, "dve", t_[:, :, :], p[:, 0:640].rearrange("p (j q) -> p j q", j=5), bias[:, h, :, :], ALU.add,
                   reads=[p, bias], writes=[t_])
                act(S, ptb[:, 0:5, :], t_[:, :, :], AF.Exp, reads=[t_], writes=[ptb])
            act(S, ptb[:, nloc:nloc + 2, :], p[:, 128 * nloc:128 * nloc + 256].rearrange("p (j q) -> p j q", j=2), AF.Exp,
                reads=[p], writes=[ptb])
            po = pO[nh % 4]
            n = nloc + 2
            for j in range(nloc):
                sl = (lt + 2 + j) % RING
                mm(S, po, po[:, :], ptb[:, j, :], vr[:, sl, h, :], j == 0, False, reads=[ptb, vslots[sl]])
            for cc in range(2):
                mm(S, po, po[:, :], ptb[:, nloc + cc, :], vc[:, cc, h, :], (nloc + cc) == 0, cc == 1, reads=[ptb, vc])
            r_ = rec[nh % 4]
            S.add("dve", lambda e, r_=r_, po=po: e.reciprocal(out=r_[:, 0:1], in_=po[:, 64:65]), reads=[po], writes=[r_])
            ts(S, "dve", onb[:, h, :], po[:, 0:64], r_[:, 0:1], None, ALU.mult, None, reads=[po, r_], writes=[onb])
            nh += 1
        otb = oT[ti % 2]
        for hp in range(8):
            tr(S, pOT, pOT[:, 128 * hp:128 * hp + 128], onb[:, 2 * hp:2 * hp + 2, :].rearrange("p a b -> p (a b)"),
               C.identb[:, :], reads=[onb, C.identb])
        cp(S, "act", otb[:, 0:4, :], pOT[:, 0:512].rearrange("p (a b) -> p a b", a=4), reads=[pOT], writes=[otb])
        cp(S, "dve", otb[:, 4:8, :], pOT[:, 512:1024].rearrange("p (a b) -> p a b", a=4), reads=[pOT], writes=[otb])
        x1 = x1t[ti % 2]
        for cb in range(2):
            py = pY[cb]
            for hp in range(8):
                mm(S, py, py[:, :], otb[:, hp, :], wo[:, hp, 512 * cb:512 * cb + 512], hp == 0, hp == 7, reads=[otb, wo])
            tt(S, "dve", x1[:, 512 * cb:512 * cb + 512], py[:, :], g1b[s][:, 512 * cb:512 * cb + 512], ALU.mult,
               reads=[py, g1b[s]], writes=[x1])
        tt(S, "pool", x1[:, :], x1[:, :], xt[:, :], ALU.add, reads=[x1, xt], writes=[x1])
        S.dma("sp", x1a[128 * ti:128 * ti + 128, :], x1[:, :], x1, reads=[x1], writes=[x1a])
        hb = h2[ti % 2]
        norm_tile(S, C, x1, ss[ti % 2], rstd[ti % 2], xs[ti % 2], pY,
                  (hb, lambda kc, hb=hb: hb[:, kc, :]), Gs[s], Ss[s])
        S.dma("sp", h2T[:, :, 128 * ti:128 * ti + 128].rearrange("kc p t -> p kc t"), hb[:, :, :], hb, reads=[hb], writes=[h2T])
    S.barrier()
    S.off = m0


def phase3_ffn(S, C, I, mod_d, x1a, h2T, x1):
    m0 = S.off
    w1 = S.sb("w1", [128, 8, DFF], BF16)
    w3 = S.sb("w3", [128, 8, DFF], BF16)
    w2 = S.sb("w2", [128, NFC, 1024], BF16)
    for kc in range(8):
        S.dma("pool", w1[:, kc, :], I["l0_ffn_w1"][128 * kc:128 * kc + 128, :], w1, writes=[w1])
        S.dma("pool", w3[:, kc, :], I["l0_ffn_w3"][128 * kc:128 * kc + 128, :], w3, writes=[w3])
    for fc in range(NFC):
        S.dma("pool", w2[:, fc, :], I["l0_ffn_w2"][128 * fc:128 * fc + 128, :], w2, writes=[w2])
    g2b = []
    for s in range(2):
        gb = S.sb(f"g2b{s}", [128, 1024], F32)
        S.dma("sp", gb[:, :], bc_view(mod_d[0, s, 5 * D:6 * D], D), gb, reads=[mod_d], writes=[gb])
        g2b.append(gb)
    hb_ = [S.sb(f"h3b{i}", [128, 8, 512], BF16) for i in range(2)]
    hid = S.sb("hid", [128, NFC, 512], BF16)
    sa = [S.sb(f"sa{i}", [128, 512], F32) for i in range(2)]
    xa = [S.sb(f"xa{i}", [128, 1024], F32) for i in range(2)]
    xo = [S.sb(f"xo{i}", [128, 1024], F32) for i in range(2)]
    pA = [S.ps(f"pA{i}", 512 * i, 512) for i in range(2)]
    pB = [S.ps(f"pB{i}", 1024 + 512 * i, 512) for i in range(2)]
    pY = [S.ps(f"pY3{i}", 2048 + 512 * i, 512) for i in range(4)]
    nf = 0
    nt = 0
    for blk in range(NT1 // 4):
        hb = hb_[blk % 2]
        S.dma("sp", hb[:, :, :], h2T[:, :, 512 * blk:512 * blk + 512].rearrange("kc p t -> p kc t"), hb, reads=[h2T], writes=[hb])
        for fc in range(NFC):
            pa, pb = pA[nf % 2], pB[nf % 2]
            for kc in range(8):
                mm(S, pa, pa[:, :], w1[:, kc, 128 * fc:128 * fc + 128], hb[:, kc, :], kc == 0, kc == 7, reads=[w1, hb])
            for kc in range(8):
                mm(S, pb, pb[:, :], w3[:, kc, 128 * fc:128 * fc + 128], hb[:, kc, :], kc == 0, kc == 7, reads=[w3, hb])
            sb_ = sa[nf % 2]
            act(S, sb_[:, :], pa[:, :], AF.Silu, reads=[pa], writes=[sb_])
            tt(S, "dve", hid[:, fc, :], sb_[:, :], pb[:, :], ALU.mult, reads=[sb_, pb], writes=[hid])
            nf += 1
        for t in range(4):
            ti = blk * 4 + t
            s = 1 if ti < 2 else 0
            xab = xa[nt % 2]
            S.dma("sp", xab[:, :], x1a[128 * ti:128 * ti + 128, :], xab, reads=[x1a], writes=[xab])
            xob = xo[nt % 2]
            for cb in range(2):
                py = pY[(nt % 2) * 2 + cb]
                for fc in range(NFC):
                    mm(S, py, py[:, :], hid[:, fc, 128 * t:128 * t + 128], w2[:, fc, 512 * cb:512 * cb + 512],
                       fc == 0, fc == NFC - 1, reads=[hid, w2])
                tt(S, "dve", xob[:, 512 * cb:512 * cb + 512], py[:, :], g2b[s][:, 512 * cb:512 * cb + 512], ALU.mult,
                   reads=[py, g2b[s]], writes=[xob])
            tt(S, "pool", xob[:, :], xob[:, :], xab[:, :], ALU.add, reads=[xob, xab], writes=[xob])
            S.dma("sp", x1[128 * ti:128 * ti + 128, :], xob[:, :], xob, reads=[xob], writes=[x1])
            nt += 1
    S.barrier()
    S.off = m0


class RnnConsts:
    pass


def rnn_setup(S, C, I, mod_d, pass2):
    R = RnnConsts()
    R.win_x = S.sb("win_x", [128, 8, DRNN], BF16)
    S.dma("pool", R.win_x[:, :, :], I["l1_w_in"][:, DRNN:2 * DRNN].rearrange("(kc p) n -> p kc n", p=128), R.win_x,
          writes=[R.win_x])
    if pass2:
        R.win_g = S.sb("win_g", [128, 8, DRNN], BF16)
        S.dma("pool", R.win_g[:, :, :], I["l1_w_in"][:, 0:DRNN].rearrange("(kc p) n -> p kc n", p=128), R.win_g,
              writes=[R.win_g])
        R.wout = S.sb("wout", [RB, RCH, D], BF16)
        S.dma("pool", R.wout[:, :, :], I["l1_w_out"].rearrange("(ch p) n -> p ch n", p=RB), R.wout, writes=[R.wout])
    R.ga = S.sb("ga", [RB, 2, RCH, RB], BF16)
    R.gx = S.sb("gx", [RB, 2, RCH, RB], BF16)
    for d in range(2):
        S.dma("pool", R.ga[:, d, :, :], I["l1_gate_a_w"][d].rearrange("k c o -> c k o"), R.ga, writes=[R.ga])
        S.dma("pool", R.gx[:, d, :, :], I["l1_gate_x_w"][d].rearrange("k c o -> c k o"), R.gx, writes=[R.gx])
    R.cw = S.sb("cw", [RB, 5, RCH], F32)
    for j in range(4):
        S.dma("sp", R.cw[:, j, :], I["l1_conv_w"][j].rearrange("(ch p) -> p ch", p=RB), R.cw, writes=[R.cw],
              allow_slow_non_contiguous=True)
    S.dma("sp", R.cw[:, 4, :], I["l1_conv_b"].rearrange("(ch p) -> p ch", p=RB), R.cw, writes=[R.cw],
          allow_slow_non_contiguous=True)
    R.gb = S.sb("gb", [RB, 4, RCH], F32)
    R.c1 = S.sb("c1", [RB, 2, RCH], F32)
    lam = S.sb("lamt", [RB, 2 * RCH], F32)
    for d in range(2):
        S.dma("sp", R.gb[:, d, :], I["l1_gate_a_b"][d].rearrange("(ch p) -> p ch", p=RB), R.gb, writes=[R.gb],
              allow_slow_non_contiguous=True)
        S.dma("sp", R.gb[:, 2 + d, :], I["l1_gate_x_b"][d].rearrange("(ch p) -> p ch", p=RB), R.gb, writes=[R.gb],
              allow_slow_non_contiguous=True)
        S.dma("sp", lam[:, RCH * d:RCH * d + RCH], I["l1_lam"][d].rearrange("(ch p) -> p ch", p=RB), lam, writes=[lam],
              allow_slow_non_contiguous=True)
    n = 2 * RCH
    t = S.sb("sp_t", [RB, n], F32)
    w = S.sb("sp_w", [RB, n], F32)
    w2 = S.sb("sp_w2", [RB, n], F32)
    pl = S.sb("sp_pl", [RB, n], F32)
    s2 = S.sb("sp_s2", [RB, n], F32)
    mk = S.sb("sp_mk", [RB, n], F32)
    act(S, t[:, :], lam[:, :], AF.Exp, reads=[lam], writes=[t], scale=-1.0)
    ts(S, "dve", w[:, :], t[:, :], 2.0, None, ALU.add, None, reads=[t], writes=[w])
    S.add("dve", lambda e: e.reciprocal(out=w[:, :], in_=w[:, :]), reads=[w], writes=[w])
    tt(S, "dve", w[:, :], w[:, :], t[:, :], ALU.mult, reads=[w, t], writes=[w])
    tt(S, "dve", w2[:, :], w[:, :], w[:, :], ALU.mult, reads=[w], writes=[w2])
    ts(S, "dve", pl[:, :], w2[:, :], 1.0 / 11, 1.0 / 9, ALU.mult, ALU.add, reads=[w2], writes=[pl])
    for cf in (1.0 / 7, 1.0 / 5, 1.0 / 3, 1.0):
        tt(S, "dve", pl[:, :], pl[:, :], w2[:, :], ALU.mult, reads=[pl, w2], writes=[pl])
        ts(S, "dve", pl[:, :], pl[:, :], cf, None, ALU.add, None, reads=[pl], writes=[pl])
    tt(S, "dve", pl[:, :], pl[:, :], w[:, :], ALU.mult, reads=[pl, w], writes=[pl])
    ts(S, "dve", s2[:, :], t[:, :], 1.0, None, ALU.add, None, reads=[t], writes=[s2])
    act(S, s2[:, :], s2[:, :], AF.Ln, reads=[s2], writes=[s2])
    ts(S, "dve", mk[:, :], t[:, :], 0.5, None, ALU.is_lt, None, reads=[t], writes=[mk])
    stt(S, "dve", pl[:, :], pl[:, :], 2.0, s2[:, :], ALU.mult, ALU.subtract, reads=[pl, s2], writes=[pl])
    tt(S, "dve", pl[:, :], pl[:, :], mk[:, :], ALU.mult, reads=[pl, mk], writes=[pl])
    tt(S, "dve", pl[:, :], pl[:, :], s2[:, :], ALU.add, reads=[pl, s2], writes=[pl])
    ts(S, "dve", R.c1[:, :, :].rearrange("p a b -> p (a b)"), pl[:, :], -8.0, None, ALU.mult, None, reads=[pl], writes=[R.c1])
    R.ones = S.sb("ones_r", [RB, 1], F32)
    S.add("pool", lambda e: e.memset(R.ones[:, :], 1.0 + 2.0 ** -23), writes=[R.ones])
    R.ngb = S.sb("ngb", [RB, 4, RCH], F32)
    ts(S, "dve", R.ngb[:, :, :], R.gb[:, :, :], -1.0, None, ALU.mult, None, reads=[R.gb], writes=[R.ngb])
    R.msk = S.sb("msk", [128, 2], F32)
    S.dma("sp", R.msk[:, :], I["masks"], R.msk, writes=[R.msk])
    R.X = [S.sb(f"X{i}", [RB, 515], F32) for i in range(2)]
    R.xc = [S.sb(f"xc{i}", [RB, 512], F32) for i in range(2)]
    R.xcb = [S.sb(f"xcb{i}", [RB, 512], BF16) for i in range(2)]
    R.r = [S.sb(f"r{i}", [RB, 512], F32) for i in range(2)]
    R.iu = [S.sb(f"iu{i}", [RB, 512], F32) for i in range(2)]
    R.a = [S.sb(f"a{i}", [RB, 512], F32) for i in range(2)]
    R.m = [S.sb(f"m{i}", [RB, 512], F32) for i in range(2)]
    R.hs = [S.sb(f"hs{i}", [RB, 512], F32) for i in range(2)]
    R.win = [S.sb(f"hwin{i}", [128, 8, 516], BF16) for i in range(2)]
    R.pb = [S.ps(f"rb{i}", 512 * i, 512) for i in range(8)]
    R.pxB = [S.ps(f"pxB{i}", 3584 + 4 * i, 3, parts=RB) for i in range(2)]
    return R


def rnn_front(S, R, hwin, L, ch, nchunk, is_ctx, mask_before, mask_after):
    X = R.X[nchunk % 2]
    xc = R.xc[nchunk % 2]
    xcb = R.xcb[nchunk % 2]
    px = R.pb[nchunk % 2]
    col = 96 * ch
    if is_ctx:
        for kc in range(8):
            mm(S, px, px[0:RB, 0:L], R.win_x[:, kc, col:col + RB], hwin[:, kc, 0:L], kc == 0, kc == 7, reads=[R.win_x, hwin])
        S.add("pool", lambda e: e.memset(X[:, 0:2], 0.0), writes=[X])
        S.add("pool", lambda e: e.memset(X[:, L + 2:L + 3], 0.0), writes=[X])
        cp(S, "act", X[:, 2:L + 2], px[0:RB, 0:L], reads=[px], writes=[X])
    else:
        pxb = R.pxB[nchunk % 2]
        for kc in range(8):
            mm(S, px, px[0:RB, 0:512], R.win_x[:, kc, col:col + RB], hwin[:, kc, 0:512], kc == 0, kc == 7, reads=[R.win_x, hwin])
        for kc in range(8):
            mm(S, pxb, pxb[:, 0:3], R.win_x[:, kc, col:col + RB], hwin[:, kc, 512:515], kc == 0, kc == 7, reads=[R.win_x, hwin])
        cp(S, "act", X[:, 0:512], px[0:RB, 0:512], reads=[px], writes=[X])
        cp(S, "dve", X[:, 512:515], pxb[:, 0:3], reads=[pxb], writes=[X])
        if mask_before:
            ts(S, "dve", X[:, 0:2], X[:, 0:2], R.msk[0:RB, 0:1], None, ALU.mult, None, reads=[X, R.msk], writes=[X])
        if mask_after:
            ts(S, "dve", X[:, 514:515], X[:, 514:515], R.msk[0:RB, 1:2], None, ALU.mult, None, reads=[X, R.msk], writes=[X])
    ts(S, "dve", xc[:, 0:L], X[:, 0:L], R.cw[:, 0, ch:ch + 1], R.cw[:, 4, ch:ch + 1], ALU.mult, ALU.add,
       reads=[X, R.cw], writes=[xc])
    for j in range(1, 4):
        stt(S, "dve", xc[:, 0:L], X[:, j:j + L], R.cw[:, j, ch:ch + 1], xc[:, 0:L], ALU.mult, ALU.add,
            reads=[X, R.cw, xc], writes=[xc])
    cp(S, "pool", xcb[:, 0:L], xc[:, 0:L], reads=[xc], writes=[xcb])


def rnn_back(S, C, R, L, ch, nchunk, init, sumr, hs_out, extra_sig=None):
    xc = R.xc[nchunk % 2]
    xcb = R.xcb[nchunk % 2]
    for d in range(2):
        pr = R.pb[2 + d]
        pi = R.pb[4 + d]
        mm(S, pr, pr[0:RB, 0:L], R.ga[:, d, ch, :], xcb[:, 0:L], True, True, reads=[R.ga, xcb])
        mm(S, pi, pi[0:RB, 0:L], R.gx[:, d, ch, :], xcb[:, 0:L], True, True, reads=[R.gx, xcb])
    for d in range(2):
        pr = R.pb[2 + d]
        pi = R.pb[4 + d]
        r, iu = R.r[d], R.iu[d]
        if sumr is not None:
            act(S, r[:, 0:L], pr[0:RB, 0:L], AF.Sigmoid, reads=[pr, R.gb], writes=[r, sumr[d][0]],
                bias=R.gb[:, d, ch:ch + 1], accum_out=sumr[d][1])
        else:
            act(S, r[:, 0:L], pr[0:RB, 0:L], AF.Sigmoid, reads=[pr, R.gb], writes=[r], bias=R.gb[:, d, ch:ch + 1])
        act(S, iu[:, 0:L], pi[0:RB, 0:L], AF.Sigmoid, reads=[pi, R.gb], writes=[iu], bias=R.gb[:, 2 + d, ch:ch + 1])
    if extra_sig is not None:
        extra_sig()
    for d in range(2):
        r, a = R.r[d], R.a[d]
        act(S, a[:, 0:L], r[:, 0:L], AF.Exp, reads=[r, R.c1], writes=[a], scale=R.c1[:, d, ch:ch + 1])
    for d in range(2):
        a, m = R.a[d], R.m[d]
        act(S, m[:, 0:L], a[:, 0:L], AF.Square, reads=[a], writes=[m])
    for d in range(2):
        m = R.m[d]
        act(S, m[:, 0:L], m[:, 0:L], AF.Ln, reads=[m, R.ones], writes=[m], bias=R.ones[:, 0:1], scale=-1.0)
    for d in range(2):
        m = R.m[d]
        act(S, m[:, 0:L], m[:, 0:L], AF.Exp, reads=[m], writes=[m], scale=0.5)
    for d in range(2):
        r, iu, a, m, hs = R.r[d], R.iu[d], R.a[d], R.m[d], hs_out[d]
        tt(S, "dve", iu[:, 0:L], iu[:, 0:L], xc[:, 0:L], ALU.mult, reads=[iu, xc], writes=[iu])
        tt(S, "dve", iu[:, 0:L], iu[:, 0:L], m[:, 0:L], ALU.mult, reads=[iu, m], writes=[iu])
        ini = init[d]
        ini_reads = [] if isinstance(ini, float) else [ini[0]]
        ini_ap = ini if isinstance(ini, float) else ini[1]
        if d == 0:
            S.add("dve", lambda e, hs=hs, a=a, iu=iu, ini_ap=ini_ap: e.tensor_tensor_scan(
                out=hs[:, 0:L], data0=a[:, 0:L], data1=iu[:, 0:L], initial=ini_ap, op0=ALU.mult, op1=ALU.add),
                reads=[a, iu] + ini_reads, writes=[hs])
        else:
            def rv(buf):
                ap = buf[:, 0:L]
                return bass.AP(ap.tensor, ap.offset + (L - 1), [list(ap.ap[0]), [-1, L]])
            S.add("dve", lambda e, hs=hs, a=a, iu=iu, ini_ap=ini_ap: e.tensor_tensor_scan(
                out=rv(hs), data0=rv(a), data1=rv(iu), initial=ini_ap, op0=ALU.mult, op1=ALU.add),
                reads=[a, iu] + ini_reads, writes=[hs])


def phase4a_h1(S, C, I, mod_d, x1, hT1):
    m0 = S.off
    mods = S.sb("mods4", [128, 32], F32)
    tmpm = S.sb("tmpm4", [128, 24], F32)
    S.dma("sp", tmpm[:, 0:8], pp_view(I["l1_norm1"]), tmpm, writes=[tmpm], allow_slow_non_contiguous=True)
    for s in range(2):
        load_mod_pp(S, tmpm, 1 + s, mod_d, 1, s, 1)
        load_mod_pp(S, mods, 2 * s + 1, mod_d, 1, s, 0)
        stt(S, "dve", mods[:, 16 * s:16 * s + 8], tmpm[:, 8 + 8 * s:16 + 8 * s], 1.0, tmpm[:, 0:8], ALU.add, ALU.mult,
            reads=[tmpm], writes=[mods])
    Gs = [Buf("G4", mods[:, 0:8]), Buf("G4c", mods[:, 16:24])]
    Ss = [Buf("S4", mods[:, 8:16]), Buf("S4c", mods[:, 24:32])]
    xin = [S.sb(f"x4in{i}", [128, 1024], F32) for i in range(3)]
    xs = [S.sb(f"xs4{i}", [128, 1024], F32) for i in range(2)]
    ss = [S.sb(f"ss4{i}", [128, 1], F32) for i in range(2)]
    rstd = [S.sb(f"rstd4{i}", [128, 1], F32) for i in range(2)]
    hb_ = [S.sb(f"h4{i}", [128, 8, 128], BF16) for i in range(2)]
    pT = [S.ps(f"pT4{i}", 512 * i, 512) for i in range(2)]
    for ti in range(NT1):
        s = 1 if ti < 2 else 0
        xt = xin[ti % 3]
        S.dma("sp", xt[:, :], x1[128 * ti:128 * ti + 128, :], xt, reads=[x1], writes=[xt])
        hb = hb_[ti % 2]
        norm_tile(S, C, xt, ss[ti % 2], rstd[ti % 2], xs[ti % 2], pT, (hb, lambda kc, hb=hb: hb[:, kc, :]), Gs[s], Ss[s])
        S.dma("sp", hT1[:, :, 128 * ti:128 * ti + 128].rearrange("kc p t -> p kc t"), hb[:, :, :], hb, reads=[hb], writes=[hT1])
    S.barrier()
    S.off = m0


def seg_info(seg):
    if seg == 0:
        return True, 256, 0, False, False
    b = seg - 1
    s = 256 + 128 + 512 * b
    return False, 512, s - 2, b == 0, b == 7


def load_win(S, R, hT1, seg, n):
    is_ctx, L, w0, _, _ = seg_info(seg)
    hw = R.win[n % 2]
    wl = 256 if is_ctx else 515
    S.dma("sp", hw[:, :, 0:wl], hT1[:, :, w0:w0 + wl].rearrange("kc p t -> p kc t"), hw, reads=[hT1], writes=[hw])
    return hw


def phase4b_pass1(S, C, I, mod_d, hT1, SAB):
    m0 = S.off
    R = rnn_setup(S, C, I, mod_d, pass2=False)
    sab = S.sb("sab", [RB, 2, 9, 2, RCH], F32)
    S.add("pool", lambda e: e.memset(sab[:, 0, :, :, :], 0.0), writes=[sab])
    work = []
    for seg in range(9):
        for ch in range(RCH):
            work.append((seg, ch))
    wins = {0: load_win(S, R, hT1, 0, 0)}

    def front(n):
        seg, ch = work[n]
        is_ctx, L, w0, mb, ma = seg_info(seg)
        if ch == 0 and seg + 1 < 9:
            wins[seg + 1] = load_win(S, R, hT1, seg + 1, seg + 1)
        rnn_front(S, R, wins[seg], L, ch, n, is_ctx, mb, ma)

    front(0)
    for n, (seg, ch) in enumerate(work):
        is_ctx, L, w0, mb, ma = seg_info(seg)
        if n + 1 < len(work):
            front(n + 1)
        sumr = [(sab, sab[:, 0, seg, d, ch:ch + 1]) for d in range(2)]
        rnn_back(S, C, R, L, ch, n, [0.0, 0.0], sumr, R.hs)
        cp(S, "pool", sab[:, 1, seg, 0, ch:ch + 1], R.hs[0][:, L - 1:L], reads=[R.hs[0]], writes=[sab])
        cp(S, "pool", sab[:, 1, seg, 1, ch:ch + 1], R.hs[1][:, 0:1], reads=[R.hs[1]], writes=[sab])
    for seg in range(9):
        tt(S, "dve", sab[:, 0, seg, :, :], sab[:, 0, seg, :, :], R.c1[:, :, :], ALU.mult, reads=[sab, R.c1], writes=[sab])
    act(S, sab[:, 0, :, :, :], sab[:, 0, :, :, :], AF.Exp, reads=[sab], writes=[sab])
    S.dma("sp", SAB[:, :], sab[:, :, :, :, :].rearrange("p a s d c -> p (a s d c)"), sab, reads=[sab], writes=[SAB])
    S.barrier()
    S.off = m0


def phase5_fold(S, C, I, SALL, SOWN, hin, NR=4):
    m0 = S.off
    sall = S.sb("sall", [RB, NR, 2 * 9 * 2 * RCH], F32)
    sown = S.sb("sown", [RB, 2, 9, 2, RCH], F32)
    sel = S.sb("sel", [RB, 2 * NR], F32)
    S.dma("sp", sall[:, :, :], SALL.t.rearrange("(j p) f -> p j f", p=RB), sall, reads=[SALL], writes=[sall])
    S.dma("sp", sown[:, :, :, :, :].rearrange("p a s d c -> p (a s d c)"), SOWN[:, :], sown, reads=[SOWN], writes=[sown])
    S.dma("sp", sel[:, :], I["sel"], sel, writes=[sel])
    sv = sall[:, :, :].rearrange("p j (a s d c) -> p j a s d c", a=2, s=9, d=2)
    h = S.sb("hfold", [RB, RCH], F32)
    ae = S.sb("aeff", [RB, 8, RCH], F32)
    be = S.sb("beff", [RB, 8, RCH], F32)
    for d in range(2):
        cp(S, "dve", h[:, :], sown[:, 1, 0, d, :], reads=[sown], writes=[h])
        order = range(NR) if d == 0 else range(NR - 1, -1, -1)
        for j in order:
            mj = sel[:, d * NR + j:d * NR + j + 1]
            ts(S, "dve", ae[:, :, :], sv[:, j, 0, 1:9, d, :], -1.0, None, ALU.add, None, reads=[sall], writes=[ae])
            ts(S, "dve", ae[:, :, :], ae[:, :, :], mj, None, ALU.mult, None, reads=[ae, sel], writes=[ae])
            ts(S, "dve", ae[:, :, :], ae[:, :, :], 1.0, None, ALU.add, None, reads=[ae], writes=[ae])
            ts(S, "dve", be[:, :, :], sv[:, j, 1, 1:9, d, :], mj, None, ALU.mult, None, reads=[sall, sel], writes=[be])
            border = range(8) if d == 0 else range(7, -1, -1)
            for b in border:
                tt(S, "dve", h[:, :], h[:, :], ae[:, b, :], ALU.mult, reads=[h, ae], writes=[h])
                tt(S, "dve", h[:, :], h[:, :], be[:, b, :], ALU.add, reads=[h, be], writes=[h])
        border = list(range(8)) if d == 0 else list(range(7, -1, -1))
        for n, b in enumerate(border):
            cp(S, "dve", hin[:, d, b, :], h[:, :], reads=[h], writes=[hin])
            if n < 7:
                tt(S, "dve", h[:, :], h[:, :], sown[:, 0, 1 + b, d, :], ALU.mult, reads=[h, sown], writes=[h])
                tt(S, "dve", h[:, :], h[:, :], sown[:, 1, 1 + b, d, :], ALU.add, reads=[h, sown], writes=[h])
    S.barrier()
    S.off = m0


def phase6_pass2(S, C, I, mod_d, x1, hT1, hin, x2, h2T1, comb, XS=None, maskall=None):
    R = rnn_setup(S, C, I, mod_d, pass2=True)
    mods = S.sb("mods6", [128, 16], F32)
    tmpm = S.sb("tmpm6", [128, 16], F32)
    S.dma("sp", tmpm[:, 0:8], pp_view(I["l1_norm2"]), tmpm, writes=[tmpm], allow_slow_non_contiguous=True)
    load_mod_pp(S, tmpm, 1, mod_d, 1, 0, 4)
    load_mod_pp(S, mods, 1, mod_d, 1, 0, 3)
    stt(S, "dve", mods[:, 0:8], tmpm[:, 8:16], 1.0, tmpm[:, 0:8], ALU.add, ALU.mult, reads=[tmpm], writes=[mods])
    G2 = Buf("G6", mods[:, 0:8])
    S2 = Buf("S6", mods[:, 8:16])
    g1b = S.sb("g1b6", [128, 1024], F32)
    S.dma("sp", g1b[:, :], bc_view(mod_d[1, 0, 2 * D:3 * D], D), g1b, reads=[mod_d], writes=[g1b])
    wr = S.sb("wr", [128, 8, NE], F32)
    S.dma("sp", wr[:, :, :], I["l1_router_w"].rearrange("(kc p) e -> p kc e", p=128), wr, writes=[wr])
    brb = S.sb("brb", [128, NE], F32)
    S.dma("sp", brb[:, :], bc_view(I["l1_router_b"], NE), brb, writes=[brb])
    yin = S.sb("yin", [RB, RCH, 512], BF16)
    gg = [S.sb(f"gg{i}", [RB, 512], F32) for i in range(2)]
    xt_ = [S.sb(f"x6in{i}", [128, 1024], F32) for i in range(1)] * 2
    ytmp = [S.sb(f"y6{i}", [128, 1024], F32) for i in range(2)]
    xs = [S.sb(f"xs6{i}", [128, 1024], F32) for i in range(1)] * 2
    ss = [S.sb(f"ss6{i}", [128, 1], F32) for i in range(2)]
    rstd = [S.sb(f"rstd6{i}", [128, 1], F32) for i in range(2)]
    h2 = [S.sb(f"h6{i}", [128, 8, 128], BF16) for i in range(2)]
    h32 = [S.sb(f"h32{i}", [128, 8, 128], F32) for i in range(1)] * 2
    rt = [S.sb(f"rt{i}", [128, 48], F32) for i in range(2)]
    xsb = [S.sb(f"xsb{i}", [128, 1024], BF16) for i in range(2)] if XS is not None else None
    pg = R.pb[6]
    plog = S.ps("plog", 3584 + 16, 8)
    ntile = 0
    xg = [S.sb(f"xg{i}", [RB, 512], F32) for i in range(2)]
    work = [(seg, ch) for seg in range(1, 9) for ch in range(RCH)]
    wins = {1: load_win(S, R, hT1, 1, 0)}

    def front(n):
        seg, ch = work[n]
        is_ctx, L, w0, mb, ma = seg_info(seg)
        if ch == 0 and seg + 1 < 9:
            wins[seg + 1] = load_win(S, R, hT1, seg + 1, seg)
        rnn_front(S, R, wins[seg], L, ch, n, False, mb, ma)
        hw = wins[seg]
        for kc in range(8):
            mm(S, pg, pg[0:RB, :], R.win_g[:, kc, 96 * ch:96 * ch + RB], hw[:, kc, 2:514], kc == 0, kc == 7, reads=[R.win_g, hw])
        x_ = xg[n % 2]
        cp(S, "act", x_[:, :], pg[0:RB, :], reads=[pg], writes=[x_])

    front(0)
    for n, (seg, ch) in enumerate(work):
        b = seg - 1
        if True:
            init = [(hin, hin[:, d, b, ch:ch + 1]) for d in range(2)]
            x_ = xg[n % 2]
            g_ = gg[n % 2]
            act(S, g_[:, :], x_[:, :], AF.Square, reads=[x_], writes=[g_])
            ts(S, "dve", g_[:, :], g_[:, :], 0.044715, 1.0, ALU.mult, ALU.add, reads=[g_], writes=[g_])
            tt(S, "dve", g_[:, :], g_[:, :], x_[:, :], ALU.mult, reads=[g_, x_], writes=[g_])
            if n + 1 < len(work):
                front(n + 1)

            def gsig(g_=g_):
                act(S, g_[:, :], g_[:, :], AF.Sigmoid, reads=[g_], writes=[g_], scale=1.5957691216057308)
            rnn_back(S, C, R, 512, ch, n, init, None, R.hs, extra_sig=gsig)
            tt(S, "pool", g_[:, :], g_[:, :], x_[:, :], ALU.mult, reads=[g_, x_], writes=[g_])
            tt(S, "pool", R.hs[0][:, :], R.hs[0][:, :], R.hs[1][:, :], ALU.add, reads=[R.hs[0], R.hs[1]], writes=[R.hs[0]])
            tt(S, "pool", yin[:, ch, :], g_[:, :], R.hs[0][:, :], ALU.mult, reads=[g_, R.hs[0]], writes=[yin])
        if ch != RCH - 1:
            continue
        for t in range(4):
            mt = 4 * b + t
            ti = 2 + 1 + mt
            xt = xt_[ntile % 2]
            S.dma("sp", xt[:, :], x1[128 * ti:128 * ti + 128, :], xt, reads=[x1], writes=[xt])
            yt = ytmp[ntile % 2]
            for cb in range(2):
                py = R.pb[cb]
                for ch in range(RCH):
                    mm(S, py, py[:, :], yin[:, ch, 128 * t:128 * t + 128], R.wout[:, ch, 512 * cb:512 * cb + 512],
                       ch == 0, ch == RCH - 1, reads=[yin, R.wout])
                tt(S, "dve", yt[:, 512 * cb:512 * cb + 512], py[:, :], g1b[:, 512 * cb:512 * cb + 512], ALU.mult,
                   reads=[py, g1b], writes=[yt])
            tt(S, "pool", yt[:, :], yt[:, :], xt[:, :], ALU.add, reads=[yt, xt], writes=[yt])
            S.dma("sp", x2[128 * mt:128 * mt + 128, :], yt[:, :], yt, reads=[yt], writes=[x2])
            hb = h2[ntile % 2]
            hf = h32[ntile % 2]
            norm_tile(S, C, yt, ss[ntile % 2], rstd[ntile % 2], xs[ntile % 2], [R.pb[0], R.pb[1]],
                      (hb, lambda kc, hb=hb: hb[:, kc, :]), G2, S2,
                      hT32_dst=(hf, lambda kc, hf=hf: hf[:, kc, :]))
            if XS is None:
                S.dma("sp", h2T1[:, :, 128 * mt:128 * mt + 128].rearrange("kc p t -> p kc t"), hb[:, :, :], hb, reads=[hb], writes=[h2T1])
            else:
                xb_ = xsb[ntile % 2]
                cp(S, "pool", xb_[:, :], xs[ntile % 2][:, :], reads=[xs[ntile % 2]], writes=[xb_])
                S.dma("sp", XS[128 * mt:128 * mt + 128, :], xb_[:, :], xb_, reads=[xb_], writes=[XS])
            for kc in range(8):
                mm(S, plog, plog[:, :], hf[:, kc, :], wr[:, kc, :], kc == 0, kc == 7, reads=[hf, wr])
            r_ = rt[ntile % 2]
            lg, m8, ex, em, den, nv1 = r_[:, 0:8], r_[:, 8:16], r_[:, 16:24], r_[:, 24:32], r_[:, 32:33], r_[:, 33:34]
            tt(S, "dve", lg, plog[:, :], brb[:, :], ALU.add, reads=[plog, brb], writes=[r_])
            S.add("dve", lambda e, m8=m8, lg=lg: e.max(out=m8, in_=lg), reads=[r_], writes=[r_])
            ts(S, "dve", nv1, r_[:, 8:9], -1.0, None, ALU.mult, None, reads=[r_], writes=[r_])
            act(S, ex, lg, AF.Exp, reads=[r_], writes=[r_], bias=nv1)
            ts(S, "dve", em, lg, r_[:, 9:10], None, ALU.is_ge, None, reads=[r_], writes=[r_])
            if maskall is not None:
                cp(S, "dve", maskall[:, mt, :], em, reads=[r_], writes=[maskall])
            tt(S, "dve", em, em, ex, ALU.mult, reads=[r_], writes=[r_])
            S.add("dve", lambda e, den=den, em=em: e.reduce_sum(out=den, in_=em, axis=AX.X), reads=[r_], writes=[r_])
            S.add("dve", lambda e, den=den: e.reciprocal(out=den, in_=den), reads=[r_], writes=[r_])
            ts(S, "dve", comb[:, mt, :], em, den, None, ALU.mult, None, reads=[r_], writes=[comb])
            ntile += 1
    S.barrier()


def phase7_moe(S, C, I, mod_d, x2, h2T1, comb, out_d):
    m0 = S.off
    m5b = S.sb("m5b", [128, 1024], F32)
    S.dma("sp", m5b[:, :], bc_view(mod_d[1, 0, 5 * D:6 * D], D), m5b, reads=[mod_d], writes=[m5b])
    hT = S.sb("hTg", [128, 8, 2048], BF16)
    acc = [S.sb(f"acc{i}", [128, 1024], F32) for i in range(16)]
    NWB = 2
    w1g = [S.sb(f"w1g{i}", [128, 8, 512], BF16) for i in range(NWB)]
    w3g = [S.sb(f"w3g{i}", [128, 8, 512], BF16) for i in range(NWB)]
    w2g = [S.sb(f"w2g{i}", [128, 4, 1024], BF16) for i in range(NWB)]
    hid = [S.sb(f"hidm{i}", [128, 4, 512], BF16) for i in range(2)]
    sa = [S.sb(f"sam{i}", [128, 512], F32) for i in range(2)]
    xin = [S.sb(f"x7in{i}", [128, 1024], F32) for i in range(2)]
    pA = [S.ps(f"pA7{i}", 512 * i, 512) for i in range(2)]
    pB = [S.ps(f"pB7{i}", 1024 + 512 * i, 512) for i in range(2)]
    pY = [S.ps(f"pY7{i}", 2048 + 512 * i, 512) for i in range(4)]
    nw = 0
    nf = 0
    nh = 0
    nt = 0

    def load_w(e, fg, n):
        S.dma("pool", w1g[n % NWB][:, :, :], I["l1_moe_w1"][e, :, 512 * fg:512 * fg + 512].rearrange("(kc p) n -> p kc n", p=128),
              w1g[n % NWB], writes=[w1g[n % NWB]])
        S.dma("pool", w3g[n % NWB][:, :, :], I["l1_moe_w3"][e, :, 512 * fg:512 * fg + 512].rearrange("(kc p) n -> p kc n", p=128),
              w3g[n % NWB], writes=[w3g[n % NWB]])
        S.dma("pool", w2g[n % NWB][:, :, :], I["l1_moe_w2"][e, 512 * fg:512 * fg + 512, :].rearrange("(fc p) n -> p fc n", p=128),
              w2g[n % NWB], writes=[w2g[n % NWB]])

    steps = [(G, e, fg) for G in range(2) for e in range(NE) for fg in range(7)]
    load_w(steps[0][1], steps[0][2], 0)
    for si, (G, e, fg) in enumerate(steps):
        if e == 0 and fg == 0:
            S.dma("sp", hT[:, :, :], h2T1[:, :, 2048 * G:2048 * G + 2048].rearrange("kc p t -> p kc t"), hT, reads=[h2T1], writes=[hT])
        if si + 1 < len(steps):
            load_w(steps[si + 1][1], steps[si + 1][2], si + 1)
        a1, a3, a2 = w1g[si % NWB], w3g[si % NWB], w2g[si % NWB]
        first = (e == 0 and fg == 0)
        for tb in range(4):
            hd = hid[nh % 2]
            for fc in range(4):
                pa, pb = pA[nf % 2], pB[nf % 2]
                for kc in range(8):
                    mm(S, pa, pa[:, :], a1[:, kc, 128 * fc:128 * fc + 128], hT[:, kc, 512 * tb:512 * tb + 512], kc == 0, kc == 7,
                       reads=[a1, hT])
                for kc in range(8):
                    mm(S, pb, pb[:, :], a3[:, kc, 128 * fc:128 * fc + 128], hT[:, kc, 512 * tb:512 * tb + 512], kc == 0, kc == 7,
                       reads=[a3, hT])
                sb_ = sa[nf % 2]
                act(S, sb_[:, :], pa[:, :], AF.Silu, reads=[pa], writes=[sb_])
                tt(S, "dve", hd[:, fc, :], sb_[:, :], pb[:, :], ALU.mult, reads=[sb_, pb], writes=[hd])
                nf += 1
            for t in range(4):
                tl = 4 * tb + t
                mt = 16 * G + tl
                ac = acc[tl]
                for cb in range(2):
                    py = pY[(nt % 2) * 2 + cb]
                    for fc in range(4):
                        mm(S, py, py[:, :], hd[:, fc, 128 * t:128 * t + 128], a2[:, fc, 512 * cb:512 * cb + 512], fc == 0, fc == 3,
                           reads=[hd, a2])
                    sl = slice(512 * cb, 512 * cb + 512)
                    if first:
                        ts(S, "dve", ac[:, sl], py[:, :], comb[:, mt, e:e + 1], None, ALU.mult, None, reads=[py, comb], writes=[ac])
                    else:
                        stt(S, "dve", ac[:, sl], py[:, :], comb[:, mt, e:e + 1], ac[:, sl], ALU.mult, ALU.add,
                            reads=[py, comb, ac], writes=[ac])
                nt += 1
            nh += 1
        if e == NE - 1 and fg == 6:
            for tl in range(16):
                mt = 16 * G + tl
                xt = xin[tl % 2]
                S.dma("sp", xt[:, :], x2[128 * mt:128 * mt + 128, :], xt, reads=[x2], writes=[xt])
                ac = acc[tl]
                tt(S, "pool", ac[:, :], ac[:, :], m5b[:, :], ALU.mult, reads=[ac, m5b], writes=[ac])
                tt(S, "pool", xt[:, :], xt[:, :], ac[:, :], ALU.add, reads=[xt, ac], writes=[xt])
                S.dma("sp", out_d[128 * mt:128 * mt + 128, :], xt[:, :], xt, reads=[xt], writes=[out_d])
    S.barrier()
    S.off = m0


I32 = mybir.dt.int32
GSZ = 512
NGRP = 24
GT = GSZ // 128
NTB = 1
TBW = GSZ // NTB
TPB = TBW // 128


class Route:
    pass


def phase_route(S, C, I, comb, maskall):
    Rt = Route()
    Rt.slotA_i = S.sb("slotA_i", [128, 32], I32)
    Rt.slotB_i = S.sb("slotB_i", [128, 32], I32)
    Rt.gA = S.sb("gA", [128, 32], F32)
    Rt.gB = S.sb("gB", [128, 32], F32)
    Rt.Eg_i = S.sb("Eg_i", [128, NGRP], I32)
    Rt.tokA = S.sb("tokA", [128, 32, 8], I32)
    Rt.tokB = S.sb("tokB", [128, 32, 8], I32)
    m0 = S.off
    rc = S.sb("rc", [128, 216], F32)
    S.dma("sp", rc[:, :], I["rconst"], rc, writes=[rc])
    Lb = S.sb("Lb", [128, 128], BF16)
    ob = S.sb("ob", [128, 128], BF16)
    mb = S.sb("mb", [128, 256], BF16)
    cp(S, "dve", Lb[:, :], rc[:, 0:128], reads=[rc], writes=[Lb])
    S.add("pool", lambda e: e.memset(ob[:, :], 1.0), writes=[ob])
    cp(S, "dve", mb[:, :], maskall[:, :, :].rearrange("p t e -> p (t e)"), reads=[maskall], writes=[mb])
    p_r = S.ps("p_rin", 0, 256)
    p_c = S.ps("p_cnt", 512, 256)
    mm(S, p_r, p_r[:, :], Lb[:, :], mb[:, :], True, True, reads=[Lb, mb])
    mm(S, p_c, p_c[:, :], ob[:, :], mb[:, :], True, True, reads=[ob, mb])
    rin = S.sb("rin", [128, 32, NE], F32)
    cnt = S.sb("cnt", [128, 32, NE], F32)
    cp(S, "dve", rin[:, :, :].rearrange("p t e -> p (t e)"), p_r[:, :], reads=[p_r], writes=[rin])
    cp(S, "dve", cnt[:, :, :].rearrange("p t e -> p (t e)"), p_c[:, :], reads=[p_c], writes=[cnt])
    ones32 = S.sb("ones32", [128, 32], F32)
    S.add("pool", lambda e: e.memset(ones32[:, :], 1.0), writes=[ones32])
    inc = S.sb("inc", [128, NE, 32], F32)
    for e_ in range(NE):
        S.add("dve", lambda e, e_=e_: e.tensor_tensor_scan(out=inc[:, e_, :], data0=ones32[:, :], data1=cnt[:, :, e_],
                                                           initial=0.0, op0=ALU.mult, op1=ALU.add),
              reads=[ones32, cnt], writes=[inc])
    pre = S.sb("pre", [128, NE, 32], F32)
    tt(S, "dve", pre[:, :, :], inc[:, :, :], cnt[:, :, :].rearrange("p t e -> p e t"), ALU.subtract, reads=[inc, cnt], writes=[pre])
    sm = S.sb("route_sm", [128, 64], F32)
    n_e, G_, gend, gstart, sbase, tmp8 = (sm[:, 0:8], sm[:, 8:16], sm[:, 16:24], sm[:, 24:32], sm[:, 32:40], sm[:, 40:48])
    cp(S, "dve", n_e, inc[:, :, 31], reads=[inc], writes=[sm])
    ts(S, "dve", G_, n_e, 0.0, None, ALU.is_gt, None, reads=[sm], writes=[sm])
    for k in range(1, (4096 + GSZ - 1) // GSZ):
        ts(S, "dve", tmp8, n_e, float(GSZ * k), None, ALU.is_gt, None, reads=[sm], writes=[sm])
        tt(S, "dve", G_, G_, tmp8, ALU.add, reads=[sm], writes=[sm])
    S.add("dve", lambda e: e.tensor_tensor_scan(out=gend, data0=ones32[:, 0:8], data1=G_, initial=0.0, op0=ALU.mult, op1=ALU.add),
          reads=[sm, ones32], writes=[sm])
    tt(S, "dve", gstart, gend, G_, ALU.subtract, reads=[sm], writes=[sm])
    ts(S, "dve", sbase, gstart, float(GSZ), None, ALU.mult, None, reads=[sm], writes=[sm])
    v = S.sb("route_v", [128, 32, NE], F32)
    tt(S, "dve", v[:, :, :], rin[:, :, :], pre[:, :, :].rearrange("p e t -> p t e"), ALU.add, reads=[rin, pre], writes=[v])
    sb_b = bass.AP(sbase.tensor, sbase.offset, [list(sbase.ap[0]), [0, 32], [1, NE]])
    tt(S, "dve", v[:, :, :], v[:, :, :], sb_b, ALU.add, reads=[v, sm], writes=[v])
    stt(S, "dve", v[:, :, :], v[:, :, :], 1.0, maskall[:, :, :], ALU.add, ALU.mult, reads=[v, maskall], writes=[v])
    m8 = S.sb("route_m8", [128, 32, NE], F32)
    for t_ in range(32):
        S.add("dve", lambda e, t_=t_: e.max(out=m8[:, t_, :], in_=v[:, t_, :]), reads=[v], writes=[m8])
    sf = S.sb("route_sf", [128, 2, 32], F32)
    oh = S.sb("route_oh", [128, 32, NE], F32)
    for which, (sl_i, g_) in enumerate(((Rt.slotA_i, Rt.gA), (Rt.slotB_i, Rt.gB))):
        top = m8[:, :, which]
        ts(S, "dve", sf[:, which, :], top, -1.0, None, ALU.add, None, reads=[m8], writes=[sf])
        cp(S, "dve", sl_i[:, :], sf[:, which, :], reads=[sf], writes=[sl_i])
        top_b = bass.AP(top.tensor, top.offset, [list(top.ap[0]), list(top.ap[1]), [0, NE]])
        tt(S, "dve", oh[:, :, :], v[:, :, :], top_b, ALU.is_equal, reads=[v, m8], writes=[oh])
        tt(S, "dve", oh[:, :, :], oh[:, :, :], comb[:, :, :], ALU.mult, reads=[oh, comb], writes=[oh])
        S.add("dve", lambda e, g_=g_: e.reduce_sum(out=g_[:, :], in_=oh[:, :, :], axis=AX.X), reads=[oh], writes=[g_])
    Eg = S.sb("Eg_f", [128, NGRP], F32)
    ind = S.sb("route_ind", [128, 2, NGRP], F32)
    gio = rc[:, 184:184 + NGRP]
    S.add("pool", lambda e: e.memset(Eg[:, :], 0.0), writes=[Eg])
    for e_ in range(1, NE):
        ts(S, "dve", ind[:, 0, :], gio, gstart[:, e_:e_ + 1], None, ALU.is_ge, None, reads=[rc, sm], writes=[ind])
        ts(S, "dve", ind[:, 1, :], gio, gend[:, e_:e_ + 1], None, ALU.is_lt, None, reads=[rc, sm], writes=[ind])
        tt(S, "dve", ind[:, 0, :], ind[:, 0, :], ind[:, 1, :], ALU.mult, reads=[ind], writes=[ind])
        stt(S, "dve", Eg[:, :], ind[:, 0, :], float(e_), Eg[:, :], ALU.mult, ALU.add, reads=[ind, Eg], writes=[Eg])
    cp(S, "dve", Rt.Eg_i[:, :], Eg[:, :], reads=[Eg], writes=[Rt.Eg_i])
    tk = rc[:, 152:184]
    tk_b = bass.AP(tk.tensor, tk.offset, [list(tk.ap[0]), list(tk.ap[1]), [0, 8]])
    tkf = S.sb("tkf", [128, 32, 8], F32)
    cp(S, "dve", tkf[:, :, :], tk_b, reads=[rc], writes=[tkf])
    cp(S, "dve", Rt.tokA[:, :, :], tkf[:, :, :], reads=[tkf], writes=[Rt.tokA])
    ts(S, "dve", tkf[:, :, :], tkf[:, :, :], 4096.0, None, ALU.add, None, reads=[tkf], writes=[tkf])
    cp(S, "dve", Rt.tokB[:, :, :], tkf[:, :, :], reads=[tkf], writes=[Rt.tokB])
    S.barrier()
    S.off = m0
    return Rt


def phase_permute(S, C, I, Rt, XS, Hslot, Tslot):
    m0 = S.off
    zer = S.sb("zer", [128, GT * 1024], BF16)
    S.add("pool", lambda e: e.memset(zer[:, :], 0.0), writes=[zer])
    for i in range(NGRP):
        S.dma("sp", Hslot[GSZ * i:GSZ * i + GSZ, :].rearrange("(a p) n -> p a n", p=128),
              zer[:, :].rearrange("p (a n) -> p a n", a=GT), zer, reads=[zer], writes=[Hslot])
    dump = S.sb("dumpi", [128, 1024], I32)
    S.add("pool", lambda e: e.memset(dump[:, :], 8192), writes=[dump])
    S.dma("sp", Tslot.t.rearrange("(p a) o -> p (a o)", p=128), dump[:, 0:NGRP * GSZ // 128 * 8], dump, reads=[dump], writes=[Tslot])
    for mt in range(32):
        for sl, tk_ in ((Rt.slotA_i, Rt.tokA), (Rt.slotB_i, Rt.tokB)):
            S.add("pool", lambda e, sl=sl, tk_=tk_, mt=mt: e.indirect_dma_start(
                out=Tslot[:, :], out_offset=bass.IndirectOffsetOnAxis(ap=sl[:, mt:mt + 1], axis=0),
                in_=tk_[:, mt, :], in_offset=None, bounds_check=None),
                reads=[tk_, sl, Tslot], writes=[Tslot], dma_buf=tk_)
    xt = [S.sb(f"xperm{i}", [128, 1024], BF16) for i in range(3)]
    for mt in range(32):
        x_ = xt[mt % 3]
        S.dma("sp", x_[:, :], XS[128 * mt:128 * mt + 128, :], x_, reads=[XS], writes=[x_])
        for sl in (Rt.slotA_i, Rt.slotB_i):
            S.add("pool", lambda e, x_=x_, sl=sl, mt=mt: e.indirect_dma_start(
                out=Hslot[:, :], out_offset=bass.IndirectOffsetOnAxis(ap=sl[:, mt:mt + 1], axis=0),
                in_=x_[:, :], in_offset=None, bounds_check=None),
                reads=[x_, sl, Hslot], writes=[Hslot], dma_buf=x_)
    S.barrier()
    S.off = m0


def phase7s_moe(S, C, I, mod_d, Rt, Hslot, Tslot, Yab):
    m0 = S.off
    mods = S.sb("mods7", [128, 16], F32)
    tmpm = S.sb("tmpm7", [128, 16], F32)
    S.dma("sp", tmpm[:, 0:8], pp_view(I["l1_norm2"]), tmpm, writes=[tmpm], allow_slow_non_contiguous=True)
    load_mod_pp(S, tmpm, 1, mod_d, 1, 0, 4)
    load_mod_pp(S, mods, 1, mod_d, 1, 0, 3)
    stt(S, "dve", mods[:, 0:8], tmpm[:, 8:16], 1.0, tmpm[:, 0:8], ALU.add, ALU.mult, reads=[tmpm], writes=[mods])
    hT = [S.sb(f"hTs{i}", [128, 8, GSZ], BF16) for i in range(2)]
    acc = [S.sb(f"accs{i}", [128, 1024], F32) for i in range(GT)]
    NWB = 4
    w1g = [S.sb(f"w1s{i}", [128, 8, 512], BF16) for i in range(NWB)]
    w3g = [S.sb(f"w3s{i}", [128, 8, 512], BF16) for i in range(NWB)]
    w2g = [S.sb(f"w2s{i}", [128, 4, 1024], BF16) for i in range(NWB)]
    hid = [S.sb(f"hids{i}", [128, 4, 512], BF16) for i in range(2)]
    sa = [S.sb(f"sas{i}", [128, 512], F32) for i in range(2)]
    st_ = [S.sb(f"slt{i}", [128, 1024], BF16) for i in range(4)]
    tix = [S.sb(f"tix{i}", [128, 8], I32) for i in range(4)]
    pA = [S.ps(f"pA8{i}", 512 * i, 512) for i in range(2)]
    pB = [S.ps(f"pB8{i}", 1024 + 512 * i, 512) for i in range(2)]
    pY = [S.ps(f"pY8{i}", 2048 + 512 * i, 512) for i in range(4)]
    w1t, w3t, w2t = I["l1_moe_w1"], I["l1_moe_w3"], I["l1_moe_w2"]

    nreg = [0]

    def dyn_load(dst, static_ap, estride, g):
        off0 = static_ap.offset
        pat = [list(x) for x in static_ap.ap]
        tens = static_ap.tensor

        nreg[0] += 1
        rname = f"er{nreg[0]}"

        def allregs(e):
            hs = []
            try:
                while True:
                    nreg[0] += 1
                    hs.append(e.alloc_register(f"gc{nreg[0]}"))
            except ValueError:
                pass
            for h in hs:
                e.free_register(h)
            return hs

        def fn(e):
            before = allregs(e)
            with e.register(rname) as er:
                e.reg_load(er, Rt.Eg_i[0:1, g:g + 1])
                e.reg_mul(er, er, estride)
                e.reg_add(er, er, off0)
                ins = e.dma_start(out=dst[:, :, :], in_=bass.AP(tens, er, pat))
            after = {h.regnum for h in allregs(e)}
            for h in before:
                if h.regnum not in after:
                    e.free_register(h)
            return ins
        S.add("pool", fn, reads=[Rt.Eg_i], writes=[dst], dma_buf=dst)

    def load_w(g, fg, n):
        dyn_load(w1g[n % NWB], w1t[0, :, 512 * fg:512 * fg + 512].rearrange("(kc p) n -> p kc n", p=128), D * DFE, g)
        dyn_load(w3g[n % NWB], w3t[0, :, 512 * fg:512 * fg + 512].rearrange("(kc p) n -> p kc n", p=128), D * DFE, g)
        dyn_load(w2g[n % NWB], w2t[0, 512 * fg:512 * fg + 512, :].rearrange("(fc p) n -> p fc n", p=128), DFE * D, g)

    nst = [0]
    ntr = [0]

    def prologue(g):
        hb = hT[g % 2]
        for t in range(GT):
            x_ = st_[nst[0] % 4]
            nst[0] += 1
            S.dma("sp", x_[:, :], Hslot[GSZ * g + 128 * t:GSZ * g + 128 * t + 128, :], x_, reads=[Hslot], writes=[x_])
            p = pA[ntr[0] % 2]
            ntr[0] += 1
            pv = p.t.bitcast(BF16)
            for kc in range(8):
                tr(S, p, pv[:, 128 * kc:128 * kc + 128], x_[:, 128 * kc:128 * kc + 128], C.identb[:, :], reads=[x_, C.identb])
            for kc in range(8):
                src = pv[:, 128 * kc:128 * kc + 128]
                if kc % 2 == 0:
                    ts(S, "dve", hb[:, kc, 128 * t:128 * t + 128], src, mods[:, kc:kc + 1], mods[:, 8 + kc:9 + kc], ALU.mult, ALU.add,
                       reads=[p, mods], writes=[hb])
                else:
                    act(S, hb[:, kc, 128 * t:128 * t + 128], src, AF.Identity, reads=[p, mods], writes=[hb],
                        bias=mods[:, 8 + kc:9 + kc], scale=mods[:, kc:kc + 1])

    steps = [(g, fg) for g in range(NGRP) for fg in range(7)]
    load_w(0, 0, 0)
    load_w(0, 1, 1)
    load_w(0, 2, 2)
    prologue(0)
    nf = 0
    nh = 0
    nt = 0
    for si, (g, fg) in enumerate(steps):
        if si + 3 < len(steps):
            load_w(steps[si + 3][0], steps[si + 3][1], si + 3)
        if fg == 6 and g + 1 < NGRP:
            prologue(g + 1)
        hb = hT[g % 2]
        a1, a3, a2 = w1g[si % NWB], w3g[si % NWB], w2g[si % NWB]
        for tb in range(NTB):
            hd = hid[nh % 2]
            for fc in range(4):
                pa, pb = pA[nf % 2], pB[nf % 2]
                for kc in range(8):
                    mm(S, pa, pa[:, 0:TBW], a1[:, kc, 128 * fc:128 * fc + 128], hb[:, kc, TBW * tb:TBW * tb + TBW], kc == 0, kc == 7,
                       reads=[a1, hb])
                for kc in range(8):
                    mm(S, pb, pb[:, 0:TBW], a3[:, kc, 128 * fc:128 * fc + 128], hb[:, kc, TBW * tb:TBW * tb + TBW], kc == 0, kc == 7,
                       reads=[a3, hb])
                sb_ = sa[nf % 2]
                act(S, sb_[:, 0:TBW], pa[:, 0:TBW], AF.Silu, reads=[pa], writes=[sb_])
                tt(S, "dve", hd[:, fc, 0:TBW], sb_[:, 0:TBW], pb[:, 0:TBW], ALU.mult, reads=[sb_, pb], writes=[hd])
                nf += 1
            for t in range(TPB):
                tl = TPB * tb + t
                ac = acc[tl]
                for cb in range(2):
                    py = pY[(nt % 2) * 2 + cb]
                    for fc in range(4):
                        mm(S, py, py[:, :], hd[:, fc, 128 * t:128 * t + 128], a2[:, fc, 512 * cb:512 * cb + 512], fc == 0, fc == 3,
                           reads=[hd, a2])
                    sl = slice(512 * cb, 512 * cb + 512)
                    if fg == 0:
                        cp(S, "dve", ac[:, sl], py[:, :], reads=[py], writes=[ac])
                    else:
                        tt(S, "dve", ac[:, sl], py[:, :], ac[:, sl], ALU.add, reads=[py, ac], writes=[ac])
                if fg == 6:
                    tx = tix[(GT * g + tl) % 4]
                    S.dma("sp", tx[:, :], Tslot[GSZ * g + 128 * tl:GSZ * g + 128 * tl + 128, :], tx, reads=[Tslot], writes=[tx])
                    S.add("pool", lambda e, ac=ac, tx=tx: e.indirect_dma_start(
                        out=Yab[:, :], out_offset=bass.IndirectOffsetOnAxis(ap=tx[:, 0:1], axis=0),
                        in_=ac[:, :], in_offset=None, bounds_check=None),
                        reads=[ac, tx, Yab], writes=[Yab], dma_buf=ac)
                nt += 1
            nh += 1
    S.barrier()
    S.off = m0


def phase8_combine(S, C, I, mod_d, Rt, Yab, x2, out_d):
    m0 = S.off
    m5b = S.sb("m5bs", [128, 1024], F32)
    S.dma("sp", m5b[:, :], bc_view(mod_d[1, 0, 5 * D:6 * D], D), m5b, reads=[mod_d], writes=[m5b])
    ya = [S.sb(f"ya{i}", [128, 1024], F32) for i in range(2)]
    yb = [S.sb(f"yb{i}", [128, 1024], F32) for i in range(2)]
    xi = [S.sb(f"xc8{i}", [128, 1024], F32) for i in range(2)]
    for mt in range(32):
        a_, b_, x_ = ya[mt % 2], yb[mt % 2], xi[mt % 2]
        S.dma("sp", x_[:, :], x2[128 * mt:128 * mt + 128, :], x_, reads=[x2], writes=[x_])
        S.dma("sp", a_[:, :], Yab[128 * mt:128 * mt + 128, :], a_, reads=[Yab], writes=[a_])
        S.dma("sp", b_[:, :], Yab[4096 + 128 * mt:4096 + 128 * mt + 128, :], b_, reads=[Yab], writes=[b_])
        ts(S, "dve", a_[:, :], a_[:, :], Rt.gA[:, mt:mt + 1], None, ALU.mult, None, reads=[a_, Rt.gA], writes=[a_])
        stt(S, "dve", a_[:, :], b_[:, :], Rt.gB[:, mt:mt + 1], a_[:, :], ALU.mult, ALU.add, reads=[b_, Rt.gB, a_], writes=[a_])
        tt(S, "dve", a_[:, :], a_[:, :], m5b[:, :], ALU.mult, reads=[a_, m5b], writes=[a_])
        tt(S, "pool", x_[:, :], x_[:, :], a_[:, :], ALU.add, reads=[x_, a_], writes=[x_])
        S.dma("sp", out_d[128 * mt:128 * mt + 128, :], x_[:, :], x_, reads=[x_], writes=[out_d])
    S.barrier()
    S.off = m0


def declare_inputs(nc, names_shapes):
    I = {}
    for name, shape in names_shapes:
        I[name] = nc.dram_tensor(name, list(shape), F32, kind="ExternalInput").ap()
    return I


A_INPUTS = [
    ("xk", (NCH * 128, D)), ("cv", (2, D)), ("bt", (5, 128, 16, 5, 128)),
    ("l0_w_mod", (D, 6 * D)), ("l0_b_mod", (6 * D,)), ("l0_norm1", (D,)), ("l0_norm2", (D,)),
    ("l0_w_qkv", (D, 3 * D)), ("l0_q_gain", (HD,)), ("l0_k_gain", (HD,)), ("l0_w_o", (D, D)),
    ("l0_ffn_w1", (D, DFF)), ("l0_ffn_w3", (D, DFF)), ("l0_ffn_w2", (DFF, D)),
    ("l1_w_mod", (D, 6 * D)), ("l1_b_mod", (6 * D,)),
]


A_INPUTS2 = [
    ("l1_norm1", (D,)), ("l1_w_in", (D, 2 * DRNN)), ("l1_conv_w", (4, DRNN)), ("l1_conv_b", (DRNN,)),
    ("l1_gate_a_w", (2, RCH, RB, RB)), ("l1_gate_a_b", (2, DRNN)), ("l1_gate_x_w", (2, RCH, RB, RB)),
    ("l1_gate_x_b", (2, DRNN)), ("l1_lam", (2, DRNN)), ("masks", (128, 2)),
]
B_INPUTS = A_INPUTS2 + [
    ("x1", (NT1 * 128, D)), ("mod_in", (2, 2, 6 * D)), ("sall", (4 * RB, 576)), ("sown", (RB, 576)),
    ("sel", (RB, 8)), ("l1_norm2", (D,)), ("l1_w_out", (DRNN, D)), ("l1_router_w", (D, NE)), ("l1_router_b", (NE,)),
    ("l1_moe_w1", (NE, D, DFE)), ("l1_moe_w3", (NE, D, DFE)), ("l1_moe_w2", (NE, DFE, D)),
]


def build_A(debug=False):
    nc = bass.Bass("TRN2", target_bir_lowering=False)
    I = declare_inputs(nc, A_INPUTS + A_INPUTS2)
    S = Sched(nc)
    C = Common(S)
    mod_d = S.dram("mod_d", [2, 2, 6 * D], F32, kind="ExternalOutput")
    QT = S.dram("QT", [8, 128, NCH * 128], BF16)
    KT = S.dram("KT", [8, 128, NCH * 128], BF16)
    V = S.dram("V", [NCH * 128, D], BF16)
    x1a = S.dram("x1a", [NT1 * 128, D], F32, kind="ExternalOutput" if debug else "Internal")
    h2T = S.dram("h2T", [8, 128, NT1 * 128], BF16)
    x1 = S.dram("x1", [NT1 * 128, D], F32, kind="ExternalOutput")
    hT1 = S.dram("hT1", [8, 128, NT1 * 128], BF16)
    SAB = S.dram("sab_out", [RB, 576], F32, kind="ExternalOutput")
    phase0_adaln(S, C, I, mod_d)
    phase1_qkv(S, C, I, mod_d, QT, KT, V)
    phase2_attn(S, C, I, mod_d, QT, KT, V, x1a, h2T)
    phase3_ffn(S, C, I, mod_d, x1a, h2T, x1)
    phase4a_h1(S, C, I, mod_d, x1, hT1)
    phase4b_pass1(S, C, I, mod_d, hT1, SAB)
    S.emit()
    return nc


def build_B(debug=False):
    nc = bass.Bass("TRN2", target_bir_lowering=False)
    I = declare_inputs(nc, B_INPUTS)
    S = Sched(nc)
    C = Common(S)
    mod_d = Buf("mod_in", I["mod_in"])
    x1 = Buf("x1", I["x1"])
    SALL = Buf("sall", I["sall"])
    SOWN = Buf("sown", I["sown"])
    hT1 = S.dram("hT1", [8, 128, NT1 * 128], BF16)
    x2 = S.dram("x2", [32 * 128, D], F32, kind="ExternalOutput" if debug else "Internal")
    h2T1 = S.dram("h2T1", [8, 128, 32 * 128], BF16)
    out_d = S.dram("out", [32 * 128, D], F32, kind="ExternalOutput")
    hin = S.sb("hin", [RB, 2, 8, RCH], F32)
    comb = S.sb("comb", [128, 32, NE], F32)
    phase4a_h1(S, C, I, mod_d, x1, hT1)
    phase5_fold(S, C, I, SALL, SOWN, hin)
    m = S.off
    phase6_pass2(S, C, I, mod_d, x1, hT1, hin, x2, h2T1, comb)
    S.off = m
    phase7_moe(S, C, I, mod_d, x2, h2T1, comb, out_d)
    S.emit()
    return nc


F_INPUTS = A_INPUTS + A_INPUTS2 + [
    ("sel", (RB, 8)), ("l1_norm2", (D,)), ("l1_w_out", (DRNN, D)), ("l1_router_w", (D, NE)), ("l1_router_b", (NE,)),
    ("l1_moe_w1", (NE, D, DFE)), ("l1_moe_w3", (NE, D, DFE)), ("l1_moe_w2", (NE, DFE, D)), ("rconst", (128, 216)),
]


SPARSE = True


def build_fused():
    nc = bass.Bass("TRN2", target_bir_lowering=False)
    I = declare_inputs(nc, F_INPUTS)
    S = Sched(nc)
    C = Common(S)
    mod_d = S.dram("mod_d", [2, 2, 6 * D], F32)
    QT = S.dram("QT", [8, 128, NCH * 128], BF16)
    KT = S.dram("KT", [8, 128, NCH * 128], BF16)
    V = S.dram("V", [NCH * 128, D], BF16)
    x1a = S.dram("x1a", [NT1 * 128, D], F32)
    h2T = S.dram("h2T", [8, 128, NT1 * 128], BF16)
    x1 = S.dram("x1", [NT1 * 128, D], F32)
    hT1 = S.dram("hT1", [8, 128, NT1 * 128], BF16)
    SAB = S.dram("sab_b", [RB, 576], F32)
    SALL = S.dram("sall_g", [4 * RB, 576], F32)
    x2 = S.dram("x2", [32 * 128, D], F32)
    h2T1 = S.dram("h2T1", [8, 128, 32 * 128], BF16)
    out_d = S.dram("out", [32 * 128, D], F32, kind="ExternalOutput")
    phase0_adaln(S, C, I, mod_d)
    phase1_qkv(S, C, I, mod_d, QT, KT, V)
    phase2_attn(S, C, I, mod_d, QT, KT, V, x1a, h2T)
    phase3_ffn(S, C, I, mod_d, x1a, h2T, x1)
    phase4a_h1(S, C, I, mod_d, x1, hT1)
    phase4b_pass1(S, C, I, mod_d, hT1, SAB)
    cc = Buf("cc")
    S.add("pool", lambda e: e.collective_compute("AllGather", ALU.bypass, replica_groups=[[0, 1, 2, 3], [4, 5, 6, 7]],
                                                  ins=[SAB.t.opt()], outs=[SALL.t.opt()]),
          reads=[SAB], writes=[SALL], dma_buf=cc, inc=1)
    hin = S.sb("hin", [RB, 2, 8, RCH], F32)
    comb = S.sb("comb", [128, 32, NE], F32)
    phase5_fold(S, C, I, SALL, SAB, hin)
    if not SPARSE:
        m = S.off
        phase6_pass2(S, C, I, mod_d, x1, hT1, hin, x2, h2T1, comb)
        S.off = m
        phase7_moe(S, C, I, mod_d, x2, h2T1, comb, out_d)
    else:
        maskall = S.sb("maskall", [128, 32, NE], F32)
        XS = S.dram("XS", [32 * 128, D], BF16)
        Hslot = S.dram("Hslot", [NGRP * GSZ, D], BF16)
        Tslot = S.dram("Tslot", [NGRP * GSZ, 8], I32)
        Yab = S.dram("Yab", [8192 + 128, D], F32)
        m = S.off
        phase6_pass2(S, C, I, mod_d, x1, hT1, hin, x2, h2T1, comb, XS=XS, maskall=maskall)
        S.off = m
        Rt = phase_route(S, C, I, comb, maskall)
        phase_permute(S, C, I, Rt, XS, Hslot, Tslot)
        phase7s_moe(S, C, I, mod_d, Rt, Hslot, Tslot, Yab)
        phase8_combine(S, C, I, mod_d, Rt, Yab, x2, out_d)
    S.emit()
    return nc


def make_bias_tables(rpb, k):
    T0 = 32 * k
    out = np.empty((5, 128, 16, 5, 128), np.float32)
    p = np.arange(128)

    def table(gt, kts):
        qr = 2 * gt + p // 64
        qc = p % 64
        rs_ = np.clip(qr - 4, 0, 248)
        cs_ = np.clip(qc - 8, 0, 48)
        tab = np.full((128, 16, 5, 128), NEG, np.float32)
        for j, kt in enumerate(kts):
            if kt < 0 or kt > 127:
                continue
            kr = (2 * kt + p // 64)[:, None]
            kcol = (p % 64)[:, None]
            inwin = (kr >= rs_[None]) & (kr < rs_[None] + 8) & (kcol >= cs_[None]) & (kcol < cs_[None] + 16)
            dr = np.clip(kr - qr[None] + 7, 0, 14)
            dc = np.clip(kcol - qc[None] + 15, 0, 30)
            vals = rpb[:, dr, dc]
            tab[:, :, j, :] = np.where(inwin[:, None, :], vals.transpose(1, 0, 2), NEG)
        return tab

    def kts_for(lt):
        gt = T0 - 1 + lt
        kts = [gt - 2 + j for j in range(5)]
        if gt == 0:
            kts[0] = 3
        if gt == 127:
            kts[4] = 124
        return gt, kts

    out[0] = table(10, [8, 9, 10, 11, 12])
    for i, lt in enumerate((1, 2, 31, 32)):
        gt, kts = kts_for(lt)
        if gt < 0 or gt > 127:
            out[1 + i] = out[0]
        else:
            out[1 + i] = table(gt, kts)
    return out


def make_xk(x, ctx, b, k):
    T0 = 32 * k
    xk = np.zeros((NCH * 128, D), np.float32)
    xk[0:256] = ctx[b]
    for j in range(NKC):
        gt = T0 - 3 + j
        if k == 0 and j == 1:
            gt = 3
        if k == 3 and j == 36:
            gt = 124
        if 0 <= gt < 128:
            xk[256 + 128 * j:256 + 128 * j + 128] = x[b, 128 * gt:128 * gt + 128]
    return xk


_CACHE = {}


def _make_rconst():
    rc = np.zeros((128, 216), np.float32)
    p = np.arange(128)
    rc[:, 0:128] = (p[:, None] < p[None, :]).astype(np.float32)
    rc[:, 128:144] = np.arange(16, dtype=np.float32)[None, :]
    rc[:, 144:152] = np.arange(8, dtype=np.float32)[None, :]
    rc[:, 152:184] = (np.arange(32)[None, :] * 128 + p[:, None]).astype(np.float32)
    rc[:, 184:216] = np.arange(32, dtype=np.float32)[None, :]
    return rc


RCONST = _make_rconst()


def kernel(**inputs):
    inp = {k: np.ascontiguousarray(np.asarray(v, dtype=np.float32)) for k, v in inputs.items()}
    if "F" not in _CACHE:
        _CACHE["F"] = build_fused()
    nc = _CACHE["F"]
    n = 8
    maps = []
    for i in range(n):
        b, k = i // 4, i % 4
        sel = np.zeros((RB, 8), np.float32)
        for j in range(4):
            if j < k:
                sel[:, j] = 1.0
            if j > k:
                sel[:, 4 + j] = 1.0
        m = {"xk": make_xk(inp["x"], inp["ctx"], b, k),
             "cv": np.stack([inp["c"][b], inp["c_ctx"]]).astype(np.float32),
             "bt": make_bias_tables(inp["l0_rpb"], k),
             "masks": np.tile(np.array([[0.0 if k == 0 else 1.0, 0.0 if k == 3 else 1.0]], np.float32), (128, 1)),
             "sel": sel, "rconst": RCONST}
        for name, _ in F_INPUTS:
            if name not in m:
                m[name] = inp[name]
        maps.append(m)
    res = run_bass_kernel_spmd(nc, maps, core_ids=list(range(n)))
    out = np.empty((2, 16384, D), np.float32)
    for i in range(n):
        b, k = i // 4, i % 4
        out[b, 4096 * k:4096 * k + 4096] = np.asarray(res.results[i]["out"])
    return out
```

```python
import numpy as np
from contextlib import ExitStack
import concourse.bass as bass
import concourse.mybir as mybir
from concourse.bass_utils import run_bass_kernel_spmd

F32 = mybir.dt.float32
BF16 = mybir.dt.bfloat16
AF = mybir.ActivationFunctionType
ALU = mybir.AluOpType
AX = mybir.AxisListType

ENGS = ("pe", "act", "dve", "pool", "sp")


class Buf:
    __slots__ = ("name", "t", "last_w", "reads")

    def __init__(self, name, t=None):
        self.name = name
        self.t = t
        self.last_w = None
        self.reads = []

    def __getitem__(self, k):
        return self.t[k]


class Op:
    __slots__ = ("eng", "fn", "deps", "signal", "pos", "dma_key", "val", "is_dma", "inc")

    def __init__(self, eng, fn):
        self.eng = eng
        self.fn = fn
        self.deps = []
        self.signal = False
        self.pos = 0
        self.is_dma = False
        self.dma_key = None
        self.val = 0
        self.inc = 16


class Sched:
    ARENA_F32 = 53000

    def __init__(self, nc, same_engine_sync=True):
        self.nc = nc
        self.ops = {e: [] for e in ENGS}
        self.same = same_engine_sync
        self.waited = {e: {} for e in ENGS}
        self.dma_cnt = {}
        self.dma_keys = []
        self.slot_of = {}
        self.bar_pos = {}
        self.off = 0
        self.arena = None
        self.peak = 0
        self.psum = None
        self.ndram = 0

    def sb(self, name, shape, dtype, off=None):
        if self.arena is None:
            self.arena = self.nc.alloc_sbuf_tensor("arena", [128, self.ARENA_F32], F32)
        esz = 2 if dtype == BF16 else 4
        nel = int(np.prod(shape[1:]))
        nbytes = (nel * esz + 63) // 64 * 64
        if off is None:
            off = self.off
            self.off += nbytes
        assert off + nbytes <= self.ARENA_F32 * 4, (name, off, nbytes)
        self.peak = max(self.peak, off + nbytes)
        a = self.arena[0:shape[0], off // 4: off // 4 + nbytes // 4]
        if dtype != F32:
            a = a.bitcast(dtype)
        a = a[:, 0:nel]
        if len(shape) > 2:
            names = "abcdefg"[:len(shape) - 1]
            pat = "p (" + " ".join(names) + ") -> p " + " ".join(names)
            a = a.rearrange(pat, **{nm: shape[1 + i] for i, nm in enumerate(names[:-1])})
        return Buf(name, a)

    def ps(self, name, col, ncols, dtype=F32, parts=128):
        if self.psum is None:
            self.psum = self.nc.alloc_psum_tensor("psum_all", [128, 4096], F32).ap()
        a = self.psum[0:parts, col:col + ncols]
        if dtype != F32:
            a = a.bitcast(dtype)
        return Buf(name, a)

    def dram(self, name, shape, dtype, kind="Internal"):
        return Buf(name, self.nc.dram_tensor(name, list(shape), dtype, kind=kind).ap())

    def _need(self, op, prod):
        if prod is None or prod is op:
            return
        e = op.eng
        w = self.waited[e]
        if prod.is_dma:
            k = ("d", prod.dma_key)
            if w.get(k, 0) >= prod.val:
                return
            w[k] = prod.val
            op.deps.append(prod)
            return
        if prod.eng == e and (not self.same or e == "pe"):
            return
        if w.get(prod.eng, -1) >= prod.pos:
            return
        w[prod.eng] = prod.pos
        prod.signal = True
        op.deps.append(prod)

    def add(self, eng, fn, reads=(), writes=(), dma_buf=None, inc=16):
        op = Op(eng, fn)
        op.inc = inc
        op.pos = len(self.ops[eng])
        if dma_buf is not None:
            op.is_dma = True
            bid = id(dma_buf)
            if bid not in self.slot_of:
                slot = len(self.slot_of)
                self.slot_of[bid] = slot
                if slot >= len(self.dma_keys):
                    self.dma_keys.append(slot)
                    self.dma_cnt[slot] = 0
            key = self.slot_of[bid]
            self.dma_cnt[key] += inc
            op.dma_key = key
            op.val = self.dma_cnt[key]
        for b in reads:
            self._need(op, b.last_w)
        for b in writes:
            self._need(op, b.last_w)
            for r in b.reads:
                self._need(op, r)
        for b in reads:
            b.reads.append(op)
        for b in writes:
            b.last_w = op
            b.reads = []
        self.ops[eng].append(op)
        return op

    def dma(self, eng, out, in_, sbuf, reads=(), writes=(), **kw):
        return self.add(eng, lambda e: e.dma_start(out=out, in_=in_, **kw),
                        reads=reads, writes=writes, dma_buf=sbuf)

    def barrier(self):
        lasts = []
        for e in ENGS:
            for op in reversed(self.ops[e]):
                if not op.is_dma and op.fn is not None:
                    lasts.append(op)
                    break
        last_dma = {}
        for e in ENGS:
            for op in self.ops[e][self.bar_pos.get(e, 0):]:
                if op.is_dma:
                    last_dma[op.dma_key] = op
        for e in ENGS:
            op = Op(e, None)
            op.pos = len(self.ops[e])
            for p in lasts:
                if p.eng != e:
                    self._need(op, p)
            for p in last_dma.values():
                self._need(op, p)
            self.ops[e].append(op)
            self.bar_pos[e] = len(self.ops[e])
        self.slot_of = {}

    def emit(self):
        nc = self.nc
        with ExitStack() as st:
            esem = {e: st.enter_context(nc.semaphore(f"s_{e}")) for e in ENGS}
            dsem = {k: st.enter_context(nc.semaphore(f"d_{i}")) for i, k in enumerate(self.dma_keys)}
            for e in ENGS:
                c = 0
                for op in self.ops[e]:
                    if op.is_dma:
                        continue
                    if op.signal:
                        c += 1
                        op.val = c
            block = st.enter_context(nc.Block())

            def run(ename, eng):
                for op in self.ops[ename]:
                    for p in op.deps:
                        if p.is_dma:
                            eng.wait_ge(dsem[p.dma_key], p.val)
                        else:
                            eng.wait_ge(esem[p.eng], p.val)
                    if op.fn is None:
                        continue
                    ins = op.fn(eng)
                    if op.is_dma:
                        ins.then_inc(dsem[op.dma_key], op.inc)
                    elif op.signal:
                        ins.then_inc(esem[ename], 1)

            @block.tensor
            def _(eng):
                run("pe", eng)

            @block.scalar
            def _(eng):
                run("act", eng)

            @block.vector
            def _(eng):
                run("dve", eng)

            @block.gpsimd
            def _(eng):
                run("pool", eng)

            @block.sync
            def _(eng):
                run("sp", eng)


D = 1024
KC = 8
NH = 16
HD = 64
NQT = 34
NKC = 38
NCH = 40
NT1 = 36
DFF = 2816
NFC = 22
DRNN = 1536
RCH = 16
RB = 96
NE = 8
DFE = 3584
EPS = 1e-6
NEG = -30000.0


def mm(S, ps, out, lhsT, rhs, start, stop, reads):
    S.add("pe", lambda e: e.matmul(out, lhsT=lhsT, rhs=rhs, start=start, stop=stop), reads=reads, writes=[ps])


def tr(S, ps, out, in_, ident, reads):
    S.add("pe", lambda e: e.transpose(out=out, in_=in_, identity=ident), reads=reads, writes=[ps])


def act(S, out, in_, func, reads, writes, bias=None, scale=None, accum_out=None):
    kw = {}
    if bias is not None:
        kw["bias"] = bias
    if scale is not None:
        kw["scale"] = scale
    if accum_out is not None:
        kw["accum_out"] = accum_out
    S.add("act", lambda e: e.activation(out=out, in_=in_, func=func, **kw), reads=reads, writes=writes)


def ts(S, eng, out, in0, s1, s2, op0, op1, reads, writes):
    if op1 is None:
        S.add(eng, lambda e: e.tensor_scalar(out=out, in0=in0, scalar1=s1, scalar2=None, op0=op0), reads=reads, writes=writes)
    else:
        S.add(eng, lambda e: e.tensor_scalar(out=out, in0=in0, scalar1=s1, scalar2=s2, op0=op0, op1=op1), reads=reads, writes=writes)


def stt(S, eng, out, in0, scalar, in1, op0, op1, reads, writes):
    S.add(eng, lambda e: e.scalar_tensor_tensor(out=out, in0=in0, scalar=scalar, in1=in1, op0=op0, op1=op1),
          reads=reads, writes=writes)


def tt(S, eng, out, in0, in1, op, reads, writes):
    S.add(eng, lambda e: e.tensor_tensor(out=out, in0=in0, in1=in1, op=op), reads=reads, writes=writes)


def cp(S, eng, out, in_, reads, writes):
    if eng == "act":
        S.add("act", lambda e: e.copy(out=out, in_=in_), reads=reads, writes=writes)
    else:
        S.add(eng, lambda e: e.tensor_copy(out=out, in_=in_), reads=reads, writes=writes)


def pp_view(vec_ap):
    return vec_ap.rearrange("(c p) -> p c", p=128)


def bc_view(row_ap, n):
    return bass.AP(row_ap.tensor, row_ap.offset, [[0, 128], [1, n]])


class Common:
    def __init__(self, S):
        self.identf = S.sb("identf", [128, 128], F32)
        self.identb = S.sb("identb", [128, 128], BF16)
        self.junk = S.sb("junk", [128, 1024], BF16)
        self.epsb = S.sb("epsb", [128, 1], F32)
        S.add("pool", lambda e: e.memset(self.epsb[:, :], EPS), writes=[self.epsb])
        for b, in (self.identf,), (self.identb,):
            S.add("pool", lambda e, b=b: e.memset(b[:], 1.0), writes=[b])
            S.add("pool", lambda e, b=b: e.affine_select(out=b[:], in_=b[:], pattern=[[-1, 128]], compare_op=ALU.is_equal,
                                                         fill=0.0, base=0, channel_multiplier=1), reads=[b], writes=[b])


def norm_tile(S, C, xt, ss, rstd, xs, pst, hT_dst, G, Sft, reads_extra=(), hT32_dst=None):
    hbuf, hfn = hT_dst
    act(S, C.junk[:, :], xt[:, :], AF.Square, reads=[xt], writes=[C.junk, ss], accum_out=ss[:, 0:1])
    ts(S, "dve", rstd[:, 0:1], ss[:, 0:1], 1.0 / D, EPS, ALU.mult, ALU.add, reads=[ss], writes=[rstd])
    act(S, rstd[:, 0:1], rstd[:, 0:1], AF.Sqrt, reads=[rstd], writes=[rstd])
    S.add("dve", lambda e: e.reciprocal(out=rstd[:, 0:1], in_=rstd[:, 0:1]), reads=[rstd], writes=[rstd])
    act(S, xs[:, :], xt[:, :], AF.Identity, reads=[xt, rstd], writes=[xs], scale=rstd[:, 0:1])
    for half in range(2):
        p = pst[half]
        for q in range(4):
            kc = half * 4 + q
            tr(S, p, p[:, 128 * q:128 * q + 128], xs[:, 128 * kc:128 * kc + 128], C.identf[:, :], reads=[xs, C.identf])
        for q in range(4):
            kc = half * 4 + q
            src = p[:, 128 * q:128 * q + 128]
            if hT32_dst is not None:
                b32, f32fn = hT32_dst
                if kc % 2 == 0:
                    ts(S, "dve", f32fn(kc), src, G[:, kc:kc + 1], Sft[:, kc:kc + 1], ALU.mult, ALU.add,
                       reads=[p, G, Sft], writes=[b32])
                else:
                    act(S, f32fn(kc), src, AF.Identity, reads=[p, G, Sft], writes=[b32],
                        bias=Sft[:, kc:kc + 1], scale=G[:, kc:kc + 1])
                cp(S, "pool", hfn(kc), f32fn(kc), reads=[b32], writes=[hbuf])
            elif kc % 2 == 0:
                ts(S, "dve", hfn(kc), src, G[:, kc:kc + 1], Sft[:, kc:kc + 1], ALU.mult, ALU.add,
                   reads=[p, G, Sft], writes=[hbuf])
            else:
                act(S, hfn(kc), src, AF.Identity, reads=[p, G, Sft], writes=[hbuf],
                    bias=Sft[:, kc:kc + 1], scale=G[:, kc:kc + 1])


def load_mod_pp(S, dst, col, mod_d, layer, stream, which, tmp_ok=True):
    src = pp_view(mod_d[layer, stream, which * D:(which + 1) * D])
    S.dma("sp", dst[:, col * 8:col * 8 + 8], src, dst, reads=[mod_d], writes=[dst], allow_slow_non_contiguous=True)


def phase0_adaln(S, C, I, mod_d):
    m0 = S.off
    cT = S.sb("cT", [128, 8, 2], F32)
    sc = S.sb("sc", [128, 8, 2], F32)
    rep = S.sb("rep", [128, 16, 128], BF16)
    wblk = [S.sb(f"wblk{i}", [128, 8, 512], BF16) for i in range(3)]
    bblk = [S.sb(f"bblk{i}", [128, 512], F32) for i in range(2)]
    res = [S.sb(f"res{i}", [128, 512], F32) for i in range(4)]
    pss = [S.ps(f"p0ps{i}", 512 * i, 512) for i in range(4)]
    for s in range(2):
        S.dma("sp", cT[:, :, s], pp_view(I["cv"][s, :]), cT, writes=[cT], allow_slow_non_contiguous=True)
    act(S, sc[:, :, :], cT[:, :, :], AF.Silu, reads=[cT], writes=[sc])
    for kc in range(8):
        for s in range(2):
            cp(S, "dve", rep[:, kc * 2 + s, :], sc[:, kc, s:s + 1].to_broadcast([128, 128]), reads=[sc], writes=[rep])
    it = 0
    for l in range(2):
        wm = I[f"l{l}_w_mod"]
        bm = I[f"l{l}_b_mod"]
        for j in range(12):
            wb = wblk[it % 3]
            bb = bblk[it % 2]
            S.dma("pool", wb[:, :, :], wm[:, 512 * j:512 * j + 512].rearrange("(kc p) n -> p kc n", p=128), wb, writes=[wb])
            S.dma("sp", bb[:, :], bc_view(bm[512 * j:512 * j + 512], 512), bb, writes=[bb])
            for s in range(2):
                p = pss[(it % 2) * 2 + s]
                r = res[(it % 2) * 2 + s]
                for kc in range(8):
                    mm(S, p, p[:, :], rep[:, kc * 2 + s, :], wb[:, kc, :], kc == 0, kc == 7, reads=[rep, wb])
                tt(S, "dve", r[:, :], p[:, :], bb[:, :], ALU.add, reads=[p, bb], writes=[r])
                S.dma("sp", mod_d[l, s:s + 1, 512 * j:512 * j + 512], r[0:1, :], r, reads=[r], writes=[mod_d])
            it += 1
    S.barrier()
    S.off = m0


def phase1_qkv(S, C, I, mod_d, QT, KT, V):
    m0 = S.off
    wq = S.sb("wqkv", [128, 8, 3072], BF16)
    S.dma("pool", wq[:, :, :], I["l0_w_qkv"].rearrange("(kc p) n -> p kc n", p=128), wq, writes=[wq])
    mods = S.sb("mods1", [128, 32], F32)
    tmpm = S.sb("tmpm1", [128, 24], F32)
    S.dma("sp", tmpm[:, 0:8], pp_view(I["l0_norm1"]), tmpm, writes=[tmpm], allow_slow_non_contiguous=True)
    for s in range(2):
        load_mod_pp(S, tmpm, 1 + s, mod_d, 0, s, 1)
        load_mod_pp(S, mods, 2 * s + 1, mod_d, 0, s, 0)
        stt(S, "dve", mods[:, 16 * s:16 * s + 8], tmpm[:, 8 + 8 * s:16 + 8 * s], 1.0, tmpm[:, 0:8], ALU.add, ALU.mult,
            reads=[tmpm], writes=[mods])
    Gs = [Buf("G", mods[:, 0:8]), Buf("Gc", mods[:, 16:24])]
    Ss = [Buf("S", mods[:, 8:16]), Buf("Sc", mods[:, 24:32])]
    gains = S.sb("gains", [128, 2], F32)
    for half in range(2):
        S.dma("sp", gains[64 * half:64 * half + 64, 0:1], I["l0_q_gain"].rearrange("(p o) -> p o", o=1), gains, writes=[gains])
        S.dma("sp", gains[64 * half:64 * half + 64, 1:2], I["l0_k_gain"].rearrange("(p o) -> p o", o=1), gains, writes=[gains])
    ts(S, "dve", gains[:, 0:1], gains[:, 0:1], HD ** -0.5, None, ALU.mult, None, reads=[gains], writes=[gains])
    bd = S.sb("bd", [128, 128], BF16)
    S.add("pool", lambda e: e.memset(bd[:, :], 0.0), writes=[bd])
    S.add("pool", lambda e: e.memset(bd[0:64, 0:64], 1.0 / 64), reads=[bd], writes=[bd])
    S.add("pool", lambda e: e.memset(bd[64:128, 64:128], 1.0 / 64), reads=[bd], writes=[bd])
    xin = [S.sb(f"xin{i}", [128, 1024], F32) for i in range(3)]
    xs = [S.sb(f"xs{i}", [128, 1024], F32) for i in range(2)]
    ss = [S.sb(f"ss{i}", [128, 1], F32) for i in range(2)]
    rstd = [S.sb(f"rstd{i}", [128, 1], F32) for i in range(2)]
    hT = [S.sb(f"hT{i}", [128, 8, 512], BF16) for i in range(2)]
    sq = [S.sb(f"sq{i}", [128, 512], BF16) for i in range(2)]
    rs = [S.sb(f"rs{i}", [128, 512], F32) for i in range(2)]
    qn = [S.sb(f"qn{i}", [128, 512], BF16) for i in range(3)]
    vt = [S.sb(f"vt{i}", [128, 1024], BF16) for i in range(2)]
    pT = [S.ps(f"pT{i}", 512 * i, 512) for i in range(2)]
    pQ = [S.ps(f"pQ{i}", 1024 + 512 * i, 512) for i in range(2)]
    pR = [S.ps(f"pR{i}", 2048 + 512 * i, 512) for i in range(2)]
    pV = [S.ps(f"pV{i}", 3072 + 512 * i, 512) for i in range(2)]
    nt = 0
    nqk = 0
    for blk in range(NCH // 4):
        hb = hT[blk % 2]
        for t in range(4):
            g = blk * 4 + t
            s = 0 if g >= 2 else 1
            xt = xin[nt % 3]
            S.dma("sp", xt[:, :], I["xk"][128 * g:128 * g + 128, :], xt, writes=[xt])
            norm_tile(S, C, xt, ss[nt % 2], rstd[nt % 2], xs[nt % 2], pT,
                      (hb, lambda kc, hb=hb, t=t: hb[:, kc, 128 * t:128 * t + 128]), Gs[s], Ss[s])
            nt += 1
        for which, dst_d in ((0, QT), (1, KT)):
            for hp in range(8):
                p = pQ[nqk % 2]
                pr = pR[nqk % 2]
                col = which * 1024 + 128 * hp
                for kc in range(8):
                    mm(S, p, p[:, :], wq[:, kc, col:col + 128], hb[:, kc, :], kc == 0, kc == 7, reads=[wq, hb])
                sqb = sq[nqk % 2]
                act(S, sqb[:, :], p[:, :], AF.Square, reads=[p], writes=[sqb])
                mm(S, pr, pr[:, :], bd[:, :], sqb[:, :], True, True, reads=[bd, sqb])
                rsb = rs[nqk % 2]
                act(S, rsb[:, :], pr[:, :], AF.Sqrt, reads=[pr, C.epsb], writes=[rsb], bias=C.epsb[:, 0:1])
                S.add("dve", lambda e, rsb=rsb: e.reciprocal(out=rsb[:, :], in_=rsb[:, :]), reads=[rsb], writes=[rsb])
                qb = qn[nqk % 3]
                stt(S, "dve", qb[:, :], p[:, :], gains[:, which:which + 1], rsb[:, :], ALU.mult, ALU.mult,
                    reads=[p, gains, rsb], writes=[qb])
                S.dma("sp", dst_d[hp, :, 512 * blk:512 * blk + 512], qb[:, :], qb, reads=[qb], writes=[dst_d])
                nqk += 1
        for t in range(4):
            g = blk * 4 + t
            vb = vt[g % 2]
            for cb in range(2):
                p = pV[cb]
                for kc in range(8):
                    mm(S, p, p[:, :], hb[:, kc, 128 * t:128 * t + 128], wq[:, kc, 2048 + 512 * cb:2048 + 512 * cb + 512],
                       kc == 0, kc == 7, reads=[hb, wq])
                cp(S, "act", vb[:, 512 * cb:512 * cb + 512], p[:, :], reads=[p], writes=[vb])
            S.dma("sp", V[128 * g:128 * g + 128, :], vb[:, :], vb, reads=[vb], writes=[V])
    S.barrier()
    S.off = m0


def phase2_attn(S, C, I, mod_d, QT, KT, V, x1a, h2T):
    m0 = S.off
    wo = S.sb("wo", [128, 8, 1024], BF16)
    S.dma("pool", wo[:, :, :], I["l0_w_o"].rearrange("(hp p) n -> p hp n", p=128), wo, writes=[wo])
    bgen = S.sb("bgen", [128, 16, 5, 128], BF16)
    bspec = S.sb("bspec", [128, 16, 5, 128], BF16)
    S.dma("pool", bgen[:, :, :, :], I["bt"][0], bgen, writes=[bgen])
    mods = S.sb("mods2", [128, 32], F32)
    tmpm = S.sb("tmpm2", [128, 24], F32)
    S.dma("sp", tmpm[:, 0:8], pp_view(I["l0_norm2"]), tmpm, writes=[tmpm], allow_slow_non_contiguous=True)
    g1b = []
    for s in range(2):
        load_mod_pp(S, tmpm, 1 + s, mod_d, 0, s, 4)
        load_mod_pp(S, mods, 2 * s + 1, mod_d, 0, s, 3)
        stt(S, "dve", mods[:, 16 * s:16 * s + 8], tmpm[:, 8 + 8 * s:16 + 8 * s], 1.0, tmpm[:, 0:8], ALU.add, ALU.mult,
            reads=[tmpm], writes=[mods])
        gb = S.sb(f"g1b{s}", [128, 1024], F32)
        S.dma("sp", gb[:, :], bc_view(mod_d[0, s, 2 * D:3 * D], D), gb, reads=[mod_d], writes=[gb])
        g1b.append(gb)
    Gs = [Buf("G2", mods[:, 0:8]), Buf("G2c", mods[:, 16:24])]
    Ss = [Buf("S2", mods[:, 8:16]), Buf("S2c", mods[:, 24:32])]
    RING = 6
    ktr = S.sb("ktr", [128, 8, RING, 128], BF16)
    vr = S.sb("vr", [128, RING, 16, 65], BF16)
    ktc = S.sb("ktc", [128, 8, 256], BF16)
    vc = S.sb("vc", [128, 2, 16, 65], BF16)
    kslots = [Buf(f"ks{i}", None) for i in range(RING)]
    vslots = [Buf(f"vs{i}", None) for i in range(RING)]
    S.add("pool", lambda e: e.memset(vr[:, :, :, 64:65], 1.0), writes=vslots)
    S.add("pool", lambda e: e.memset(vc[:, :, :, 64:65], 1.0), writes=[vc])
    S.dma("sp", ktc[:, :, :], KT[:, :, 0:256].rearrange("hp p t -> p hp t"), ktc, reads=[KT], writes=[ktc])
    for cc in range(2):
        S.dma("sp", vc[:, cc, :, 0:64], V[128 * cc:128 * cc + 128, :].rearrange("p (h d) -> p h d", h=16), vc,
              reads=[V], writes=[vc])
    qt = [S.sb(f"qt{i}", [128, 8, 128], BF16) for i in range(2)]
    xt_ = [S.sb(f"x2in{i}", [128, 1024], F32) for i in range(2)]
    tb = [S.sb(f"tb{i}", [128, 5, 128], F32) for i in range(2)]
    pt = [S.sb(f"pt{i}", [128, 7, 128], BF16) for i in range(2)]
    rec = [S.sb(f"rec{i}", [128, 1], F32) for i in range(4)]
    on = [S.sb(f"on{i}", [128, 16, 64], BF16) for i in range(2)]
    oT = [S.sb(f"oT{i}", [128, 8, 128], BF16) for i in range(2)]
    x1t = [S.sb(f"x1t{i}", [128, 1024], F32) for i in range(2)]
    xs = [S.sb(f"xs2{i}", [128, 1024], F32) for i in range(2)]
    ss = [S.sb(f"ss2{i}", [128, 1], F32) for i in range(2)]
    rstd = [S.sb(f"rstd2{i}", [128, 1], F32) for i in range(2)]
    h2 = [S.sb(f"h2t{i}", [128, 8, 128], BF16) for i in range(2)]
    pS = [S.ps(f"pS{i}", 1024 * i, 896) for i in range(2)]
    pO = [S.ps(f"pO{i}", 2048 + 128 * i, 65) for i in range(4)]
    pY = [S.ps(f"pY{i}", 2560 + 512 * i, 512) for i in range(2)]
    pOT = S.ps("pOT", 3584, 512, BF16)

    loaded = set()

    def ensure_chunk(g):
        if g in loaded:
            return
        loaded.add(g)
        sl = g % RING
        S.dma("sp", ktr[:, :, sl, :], KT[:, :, 128 * g:128 * g + 128].rearrange("hp p t -> p hp t"), kslots[sl],
              reads=[KT], writes=[kslots[sl]])
        S.dma("sp", vr[:, sl, :, 0:64], V[128 * g:128 * g + 128, :].rearrange("p (h d) -> p h d", h=16), vslots[sl],
              reads=[V], writes=[vslots[sl]])

    spec_lts = {1: 1, 2: 2, 31: 3, 32: 4}
    nh = 0
    for ti in range(NT1):
        is_ctx = ti < 2
        lt = ti - 2
        g = ti if is_ctx else lt + 4
        s = 1 if is_ctx else 0
        q = qt[ti % 2]
        S.dma("sp", q[:, :, :], QT[:, :, 128 * g:128 * g + 128].rearrange("hp p t -> p hp t"), q, reads=[QT], writes=[q])
        xt = xt_[ti % 2]
        S.dma("sp", xt[:, :], I["xk"][128 * g:128 * g + 128, :], xt, writes=[xt])
        nloc = 0 if is_ctx else 5
        if not is_ctx:
            for j in range(5):
                ensure_chunk(lt + 2 + j)
            if lt in spec_lts:
                S.dma("pool", bspec[:, :, :, :], I["bt"][spec_lts[lt]], bspec, writes=[bspec])
                bias = bspec
            else:
                bias = bgen
        onb = on[ti % 2]
        for h in range(NH):
            hp, half = h // 2, h % 2
            lo = 64 * half
            p = pS[nh % 2]
            ptb = pt[nh % 2]
            for j in range(nloc):
                sl = (lt + 2 + j) % RING
                mm(S, p, p[:, 128 * j:128 * j + 128], ktr[lo:lo + 64, hp, sl, :], q[lo:lo + 64, hp, :], True, True,
                   reads=[kslots[sl], q])
            for cc in range(2):
                jj = nloc + cc
                mm(S, p, p[:, 128 * jj:128 * jj + 128], ktc[lo:lo + 64, hp, 128 * cc:128 * cc + 128], q[lo:lo + 64, hp, :],
                   True, True, reads=[ktc, q])
            if nloc:
                t_ = tb[nh % 2]
                tt(S, "dve", t_[:, :, :], p[:, 0:640].rearrange("p (j q) -> p j q", j=5), bias[:, h, :, :], ALU.add,
                   reads=[p, bias], writes=[t_])
                act(S, ptb[:, 0:5, :], t_[:, :, :], AF.Exp, reads=[t_], writes=[ptb])
            act(S, ptb[:, nloc:nloc + 2, :], p[:, 128 * nloc:128 * nloc + 256].rearrange("p (j q) -> p j q", j=2), AF.Exp,
                reads=[p], writes=[ptb])
            po = pO[nh % 4]
            n = nloc + 2
            for j in range(nloc):
                sl = (lt + 2 + j) % RING
                mm(S, po, po[:, :], ptb[:, j, :], vr[:, sl, h, :], j == 0, False, reads=[ptb, vslots[sl]])
            for cc in range(2):
                mm(S, po, po[:, :], ptb[:, nloc + cc, :], vc[:, cc, h, :], (nloc + cc) == 0, cc == 1, reads=[ptb, vc])
            r_ = rec[nh % 4]
            S.add("dve", lambda e, r_=r_, po=po: e.reciprocal(out=r_[:, 0:1], in_=po[:, 64:65]), reads=[po], writes=[r_])
            ts(S, "dve", onb[:, h, :], po[:, 0:64], r_[:, 0:1], None, ALU.mult, None, reads=[po, r_], writes=[onb])
            nh += 1
        otb = oT[ti % 2]
        for hp in range(8):
            tr(S, pOT, pOT[:, 128 * hp:128 * hp + 128], onb[:, 2 * hp:2 * hp + 2, :].rearrange("p a b -> p (a b)"),
               C.identb[:, :], reads=[onb, C.identb])
        cp(S, "act", otb[:, 0:4, :], pOT[:, 0:512].rearrange("p (a b) -> p a b", a=4), reads=[pOT], writes=[otb])
        cp(S, "dve", otb[:, 4:8, :], pOT[:, 512:1024].rearrange("p (a b) -> p a b", a=4), reads=[pOT], writes=[otb])
        x1 = x1t[ti % 2]
        for cb in range(2):
            py = pY[cb]
            for hp in range(8):
                mm(S, py, py[:, :], otb[:, hp, :], wo[:, hp, 512 * cb:512 * cb + 512], hp == 0, hp == 7, reads=[otb, wo])
            tt(S, "dve", x1[:, 512 * cb:512 * cb + 512], py[:, :], g1b[s][:, 512 * cb:512 * cb + 512], ALU.mult,
               reads=[py, g1b[s]], writes=[x1])
        tt(S, "pool", x1[:, :], x1[:, :], xt[:, :], ALU.add, reads=[x1, xt], writes=[x1])
        S.dma("sp", x1a[128 * ti:128 * ti + 128, :], x1[:, :], x1, reads=[x1], writes=[x1a])
        hb = h2[ti % 2]
        norm_tile(S, C, x1, ss[ti % 2], rstd[ti % 2], xs[ti % 2], pY,
                  (hb, lambda kc, hb=hb: hb[:, kc, :]), Gs[s], Ss[s])
        S.dma("sp", h2T[:, :, 128 * ti:128 * ti + 128].rearrange("kc p t -> p kc t"), hb[:, :, :], hb, reads=[hb], writes=[h2T])
    S.barrier()
    S.off = m0


def phase3_ffn(S, C, I, mod_d, x1a, h2T, x1):
    m0 = S.off
    w1 = S.sb("w1", [128, 8, DFF], BF16)
    w3 = S.sb("w3", [128, 8, DFF], BF16)
    w2 = S.sb("w2", [128, NFC, 1024], BF16)
    for kc in range(8):
        S.dma("pool", w1[:, kc, :], I["l0_ffn_w1"][128 * kc:128 * kc + 128, :], w1, writes=[w1])
        S.dma("pool", w3[:, kc, :], I["l0_ffn_w3"][128 * kc:128 * kc + 128, :], w3, writes=[w3])
    for fc in range(NFC):
        S.dma("pool", w2[:, fc, :], I["l0_ffn_w2"][128 * fc:128 * fc + 128, :], w2, writes=[w2])
    g2b = []
    for s in range(2):
        gb = S.sb(f"g2b{s}", [128, 1024], F32)
        S.dma("sp", gb[:, :], bc_view(mod_d[0, s, 5 * D:6 * D], D), gb, reads=[mod_d], writes=[gb])
        g2b.append(gb)
    hb_ = [S.sb(f"h3b{i}", [128, 8, 512], BF16) for i in range(2)]
    hid = S.sb("hid", [128, NFC, 512], BF16)
    sa = [S.sb(f"sa{i}", [128, 512], F32) for i in range(2)]
    xa = [S.sb(f"xa{i}", [128, 1024], F32) for i in range(2)]
    xo = [S.sb(f"xo{i}", [128, 1024], F32) for i in range(2)]
    pA = [S.ps(f"pA{i}", 512 * i, 512) for i in range(2)]
    pB = [S.ps(f"pB{i}", 1024 + 512 * i, 512) for i in range(2)]
    pY = [S.ps(f"pY3{i}", 2048 + 512 * i, 512) for i in range(4)]
    nf = 0
    nt = 0
    for blk in range(NT1 // 4):
        hb = hb_[blk % 2]
        S.dma("sp", hb[:, :, :], h2T[:, :, 512 * blk:512 * blk + 512].rearrange("kc p t -> p kc t"), hb, reads=[h2T], writes=[hb])
        for fc in range(NFC):
            pa, pb = pA[nf % 2], pB[nf % 2]
            for kc in range(8):
                mm(S, pa, pa[:, :], w1[:, kc, 128 * fc:128 * fc + 128], hb[:, kc, :], kc == 0, kc == 7, reads=[w1, hb])
            for kc in range(8):
                mm(S, pb, pb[:, :], w3[:, kc, 128 * fc:128 * fc + 128], hb[:, kc, :], kc == 0, kc == 7, reads=[w3, hb])
            sb_ = sa[nf % 2]
            act(S, sb_[:, :], pa[:, :], AF.Silu, reads=[pa], writes=[sb_])
            tt(S, "dve", hid[:, fc, :], sb_[:, :], pb[:, :], ALU.mult, reads=[sb_, pb], writes=[hid])
            nf += 1
        for t in range(4):
            ti = blk * 4 + t
            s = 1 if ti < 2 else 0
            xab = xa[nt % 2]
            S.dma("sp", xab[:, :], x1a[128 * ti:128 * ti + 128, :], xab, reads=[x1a], writes=[xab])
            xob = xo[nt % 2]
            for cb in range(2):
                py = pY[(nt % 2) * 2 + cb]
                for fc in range(NFC):
                    mm(S, py, py[:, :], hid[:, fc, 128 * t:128 * t + 128], w2[:, fc, 512 * cb:512 * cb + 512],
                       fc == 0, fc == NFC - 1, reads=[hid, w2])
                tt(S, "dve", xob[:, 512 * cb:512 * cb + 512], py[:, :], g2b[s][:, 512 * cb:512 * cb + 512], ALU.mult,
                   reads=[py, g2b[s]], writes=[xob])
            tt(S, "pool", xob[:, :], xob[:, :], xab[:, :], ALU.add, reads=[xob, xab], writes=[xob])
            S.dma("sp", x1[128 * ti:128 * ti + 128, :], xob[:, :], xob, reads=[xob], writes=[x1])
            nt += 1
    S.barrier()
    S.off = m0


class RnnConsts:
    pass


def rnn_setup(S, C, I, mod_d, pass2):
    R = RnnConsts()
    R.win_x = S.sb("win_x", [128, 8, DRNN], BF16)
    S.dma("pool", R.win_x[:, :, :], I["l1_w_in"][:, DRNN:2 * DRNN].rearrange("(kc p) n -> p kc n", p=128), R.win_x,
          writes=[R.win_x])
    if pass2:
        R.win_g = S.sb("win_g", [128, 8, DRNN], BF16)
        S.dma("pool", R.win_g[:, :, :], I["l1_w_in"][:, 0:DRNN].rearrange("(kc p) n -> p kc n", p=128), R.win_g,
              writes=[R.win_g])
        R.wout = S.sb("wout", [RB, RCH, D], BF16)
        S.dma("pool", R.wout[:, :, :], I["l1_w_out"].rearrange("(ch p) n -> p ch n", p=RB), R.wout, writes=[R.wout])
    R.ga = S.sb("ga", [RB, 2, RCH, RB], BF16)
    R.gx = S.sb("gx", [RB, 2, RCH, RB], BF16)
    for d in range(2):
        S.dma("pool", R.ga[:, d, :, :], I["l1_gate_a_w"][d].rearrange("k c o -> c k o"), R.ga, writes=[R.ga])
        S.dma("pool", R.gx[:, d, :, :], I["l1_gate_x_w"][d].rearrange("k c o -> c k o"), R.gx, writes=[R.gx])
    R.cw = S.sb("cw", [RB, 5, RCH], F32)
    for j in range(4):
        S.dma("sp", R.cw[:, j, :], I["l1_conv_w"][j].rearrange("(ch p) -> p ch", p=RB), R.cw, writes=[R.cw],
              allow_slow_non_contiguous=True)
    S.dma("sp", R.cw[:, 4, :], I["l1_conv_b"].rearrange("(ch p) -> p ch", p=RB), R.cw, writes=[R.cw],
          allow_slow_non_contiguous=True)
    R.gb = S.sb("gb", [RB, 4, RCH], F32)
    R.c1 = S.sb("c1", [RB, 2, RCH], F32)
    lam = S.sb("lamt", [RB, 2 * RCH], F32)
    for d in range(2):
        S.dma("sp", R.gb[:, d, :], I["l1_gate_a_b"][d].rearrange("(ch p) -> p ch", p=RB), R.gb, writes=[R.gb],
              allow_slow_non_contiguous=True)
        S.dma("sp", R.gb[:, 2 + d, :], I["l1_gate_x_b"][d].rearrange("(ch p) -> p ch", p=RB), R.gb, writes=[R.gb],
              allow_slow_non_contiguous=True)
        S.dma("sp", lam[:, RCH * d:RCH * d + RCH], I["l1_lam"][d].rearrange("(ch p) -> p ch", p=RB), lam, writes=[lam],
              allow_slow_non_contiguous=True)
    n = 2 * RCH
    t = S.sb("sp_t", [RB, n], F32)
    w = S.sb("sp_w", [RB, n], F32)
    w2 = S.sb("sp_w2", [RB, n], F32)
    pl = S.sb("sp_pl", [RB, n], F32)
    s2 = S.sb("sp_s2", [RB, n], F32)
    mk = S.sb("sp_mk", [RB, n], F32)
    act(S, t[:, :], lam[:, :], AF.Exp, reads=[lam], writes=[t], scale=-1.0)
    ts(S, "dve", w[:, :], t[:, :], 2.0, None, ALU.add, None, reads=[t], writes=[w])
    S.add("dve", lambda e: e.reciprocal(out=w[:, :], in_=w[:, :]), reads=[w], writes=[w])
    tt(S, "dve", w[:, :], w[:, :], t[:, :], ALU.mult, reads=[w, t], writes=[w])
    tt(S, "dve", w2[:, :], w[:, :], w[:, :], ALU.mult, reads=[w], writes=[w2])
    ts(S, "dve", pl[:, :], w2[:, :], 1.0 / 11, 1.0 / 9, ALU.mult, ALU.add, reads=[w2], writes=[pl])
    for cf in (1.0 / 7, 1.0 / 5, 1.0 / 3, 1.0):
        tt(S, "dve", pl[:, :], pl[:, :], w2[:, :], ALU.mult, reads=[pl, w2], writes=[pl])
        ts(S, "dve", pl[:, :], pl[:, :], cf, None, ALU.add, None, reads=[pl], writes=[pl])
    tt(S, "dve", pl[:, :], pl[:, :], w[:, :], ALU.mult, reads=[pl, w], writes=[pl])
    ts(S, "dve", s2[:, :], t[:, :], 1.0, None, ALU.add, None, reads=[t], writes=[s2])
    act(S, s2[:, :], s2[:, :], AF.Ln, reads=[s2], writes=[s2])
    ts(S, "dve", mk[:, :], t[:, :], 0.5, None, ALU.is_lt, None, reads=[t], writes=[mk])
    stt(S, "dve", pl[:, :], pl[:, :], 2.0, s2[:, :], ALU.mult, ALU.subtract, reads=[pl, s2], writes=[pl])
    tt(S, "dve", pl[:, :], pl[:, :], mk[:, :], ALU.mult, reads=[pl, mk], writes=[pl])
    tt(S, "dve", pl[:, :], pl[:, :], s2[:, :], ALU.add, reads=[pl, s2], writes=[pl])
    ts(S, "dve", R.c1[:, :, :].rearrange("p a b -> p (a b)"), pl[:, :], -8.0, None, ALU.mult, None, reads=[pl], writes=[R.c1])
    R.ones = S.sb("ones_r", [RB, 1], F32)
    S.add("pool", lambda e: e.memset(R.ones[:, :], 1.0 + 2.0 ** -23), writes=[R.ones])
    R.ngb = S.sb("ngb", [RB, 4, RCH], F32)
    ts(S, "dve", R.ngb[:, :, :], R.gb[:, :, :], -1.0, None, ALU.mult, None, reads=[R.gb], writes=[R.ngb])
    R.msk = S.sb("msk", [128, 2], F32)
    S.dma("sp", R.msk[:, :], I["masks"], R.msk, writes=[R.msk])
    R.X = [S.sb(f"X{i}", [RB, 515], F32) for i in range(2)]
    R.xc = [S.sb(f"xc{i}", [RB, 512], F32) for i in range(2)]
    R.xcb = [S.sb(f"xcb{i}", [RB, 512], BF16) for i in range(2)]
    R.r = [S.sb(f"r{i}", [RB, 512], F32) for i in range(2)]
    R.iu = [S.sb(f"iu{i}", [RB, 512], F32) for i in range(2)]
    R.a = [S.sb(f"a{i}", [RB, 512], F32) for i in range(2)]
    R.m = [S.sb(f"m{i}", [RB, 512], F32) for i in range(2)]
    R.hs = [S.sb(f"hs{i}", [RB, 512], F32) for i in range(2)]
    R.win = [S.sb(f"hwin{i}", [128, 8, 516], BF16) for i in range(2)]
    R.pb = [S.ps(f"rb{i}", 512 * i, 512) for i in range(8)]
    R.pxB = [S.ps(f"pxB{i}", 3584 + 4 * i, 3, parts=RB) for i in range(2)]
    return R


def rnn_front(S, R, hwin, L, ch, nchunk, is_ctx, mask_before, mask_after):
    X = R.X[nchunk % 2]
    xc = R.xc[nchunk % 2]
    xcb = R.xcb[nchunk % 2]
    px = R.pb[nchunk % 2]
    col = 96 * ch
    if is_ctx:
        for kc in range(8):
            mm(S, px, px[0:RB, 0:L], R.win_x[:, kc, col:col + RB], hwin[:, kc, 0:L], kc == 0, kc == 7, reads=[R.win_x, hwin])
        S.add("pool", lambda e: e.memset(X[:, 0:2], 0.0), writes=[X])
        S.add("pool", lambda e: e.memset(X[:, L + 2:L + 3], 0.0), writes=[X])
        cp(S, "act", X[:, 2:L + 2], px[0:RB, 0:L], reads=[px], writes=[X])
    else:
        pxb = R.pxB[nchunk % 2]
        for kc in range(8):
            mm(S, px, px[0:RB, 0:512], R.win_x[:, kc, col:col + RB], hwin[:, kc, 0:512], kc == 0, kc == 7, reads=[R.win_x, hwin])
        for kc in range(8):
            mm(S, pxb, pxb[:, 0:3], R.win_x[:, kc, col:col + RB], hwin[:, kc, 512:515], kc == 0, kc == 7, reads=[R.win_x, hwin])
        cp(S, "act", X[:, 0:512], px[0:RB, 0:512], reads=[px], writes=[X])
        cp(S, "dve", X[:, 512:515], pxb[:, 0:3], reads=[pxb], writes=[X])
        if mask_before:
            ts(S, "dve", X[:, 0:2], X[:, 0:2], R.msk[0:RB, 0:1], None, ALU.mult, None, reads=[X, R.msk], writes=[X])
        if mask_after:
            ts(S, "dve", X[:, 514:515], X[:, 514:515], R.msk[0:RB, 1:2], None, ALU.mult, None, reads=[X, R.msk], writes=[X])
    ts(S, "dve", xc[:, 0:L], X[:, 0:L], R.cw[:, 0, ch:ch + 1], R.cw[:, 4, ch:ch + 1], ALU.mult, ALU.add,
       reads=[X, R.cw], writes=[xc])
    for j in range(1, 4):
        stt(S, "dve", xc[:, 0:L], X[:, j:j + L], R.cw[:, j, ch:ch + 1], xc[:, 0:L], ALU.mult, ALU.add,
            reads=[X, R.cw, xc], writes=[xc])
    cp(S, "pool", xcb[:, 0:L], xc[:, 0:L], reads=[xc], writes=[xcb])


def rnn_back(S, C, R, L, ch, nchunk, init, sumr, hs_out, extra_sig=None):
    xc = R.xc[nchunk % 2]
    xcb = R.xcb[nchunk % 2]
    for d in range(2):
        pr = R.pb[2 + d]
        pi = R.pb[4 + d]
        mm(S, pr, pr[0:RB, 0:L], R.ga[:, d, ch, :], xcb[:, 0:L], True, True, reads=[R.ga, xcb])
        mm(S, pi, pi[0:RB, 0:L], R.gx[:, d, ch, :], xcb[:, 0:L], True, True, reads=[R.gx, xcb])
    for d in range(2):
        pr = R.pb[2 + d]
        pi = R.pb[4 + d]
        r, iu = R.r[d], R.iu[d]
        if sumr is not None:
            act(S, r[:, 0:L], pr[0:RB, 0:L], AF.Sigmoid, reads=[pr, R.gb], writes=[r, sumr[d][0]],
                bias=R.gb[:, d, ch:ch + 1], accum_out=sumr[d][1])
        else:
            act(S, r[:, 0:L], pr[0:RB, 0:L], AF.Sigmoid, reads=[pr, R.gb], writes=[r], bias=R.gb[:, d, ch:ch + 1])
        act(S, iu[:, 0:L], pi[0:RB, 0:L], AF.Sigmoid, reads=[pi, R.gb], writes=[iu], bias=R.gb[:, 2 + d, ch:ch + 1])
    if extra_sig is not None:
        extra_sig()
    for d in range(2):
        r, a = R.r[d], R.a[d]
        act(S, a[:, 0:L], r[:, 0:L], AF.Exp, reads=[r, R.c1], writes=[a], scale=R.c1[:, d, ch:ch + 1])
    for d in range(2):
        a, m = R.a[d], R.m[d]
        act(S, m[:, 0:L], a[:, 0:L], AF.Square, reads=[a], writes=[m])
    for d in range(2):
        m = R.m[d]
        act(S, m[:, 0:L], m[:, 0:L], AF.Ln, reads=[m, R.ones], writes=[m], bias=R.ones[:, 0:1], scale=-1.0)
    for d in range(2):
        m = R.m[d]
        act(S, m[:, 0:L], m[:, 0:L], AF.Exp, reads=[m], writes=[m], scale=0.5)
    for d in range(2):
        r, iu, a, m, hs = R.r[d], R.iu[d], R.a[d], R.m[d], hs_out[d]
        tt(S, "dve", iu[:, 0:L], iu[:, 0:L], xc[:, 0:L], ALU.mult, reads=[iu, xc], writes=[iu])
        tt(S, "dve", iu[:, 0:L], iu[:, 0:L], m[:, 0:L], ALU.mult, reads=[iu, m], writes=[iu])
        ini = init[d]
        ini_reads = [] if isinstance(ini, float) else [ini[0]]
        ini_ap = ini if isinstance(ini, float) else ini[1]
        if d == 0:
            S.add("dve", lambda e, hs=hs, a=a, iu=iu, ini_ap=ini_ap: e.tensor_tensor_scan(
                out=hs[:, 0:L], data0=a[:, 0:L], data1=iu[:, 0:L], initial=ini_ap, op0=ALU.mult, op1=ALU.add),
                reads=[a, iu] + ini_reads, writes=[hs])
        else:
            def rv(buf):
                ap = buf[:, 0:L]
                return bass.AP(ap.tensor, ap.offset + (L - 1), [list(ap.ap[0]), [-1, L]])
            S.add("dve", lambda e, hs=hs, a=a, iu=iu, ini_ap=ini_ap: e.tensor_tensor_scan(
                out=rv(hs), data0=rv(a), data1=rv(iu), initial=ini_ap, op0=ALU.mult, op1=ALU.add),
                reads=[a, iu] + ini_reads, writes=[hs])


def phase4a_h1(S, C, I, mod_d, x1, hT1):
    m0 = S.off
    mods = S.sb("mods4", [128, 32], F32)
    tmpm = S.sb("tmpm4", [128, 24], F32)
    S.dma("sp", tmpm[:, 0:8], pp_view(I["l1_norm1"]), tmpm, writes=[tmpm], allow_slow_non_contiguous=True)
    for s in range(2):
        load_mod_pp(S, tmpm, 1 + s, mod_d, 1, s, 1)
        load_mod_pp(S, mods, 2 * s + 1, mod_d, 1, s, 0)
        stt(S, "dve", mods[:, 16 * s:16 * s + 8], tmpm[:, 8 + 8 * s:16 + 8 * s], 1.0, tmpm[:, 0:8], ALU.add, ALU.mult,
            reads=[tmpm], writes=[mods])
    Gs = [Buf("G4", mods[:, 0:8]), Buf("G4c", mods[:, 16:24])]
    Ss = [Buf("S4", mods[:, 8:16]), Buf("S4c", mods[:, 24:32])]
    xin = [S.sb(f"x4in{i}", [128, 1024], F32) for i in range(3)]
    xs = [S.sb(f"xs4{i}", [128, 1024], F32) for i in range(2)]
    ss = [S.sb(f"ss4{i}", [128, 1], F32) for i in range(2)]
    rstd = [S.sb(f"rstd4{i}", [128, 1], F32) for i in range(2)]
    hb_ = [S.sb(f"h4{i}", [128, 8, 128], BF16) for i in range(2)]
    pT = [S.ps(f"pT4{i}", 512 * i, 512) for i in range(2)]
    for ti in range(NT1):
        s = 1 if ti < 2 else 0
        xt = xin[ti % 3]
        S.dma("sp", xt[:, :], x1[128 * ti:128 * ti + 128, :], xt, reads=[x1], writes=[xt])
        hb = hb_[ti % 2]
        norm_tile(S, C, xt, ss[ti % 2], rstd[ti % 2], xs[ti % 2], pT, (hb, lambda kc, hb=hb: hb[:, kc, :]), Gs[s], Ss[s])
        S.dma("sp", hT1[:, :, 128 * ti:128 * ti + 128].rearrange("kc p t -> p kc t"), hb[:, :, :], hb, reads=[hb], writes=[hT1])
    S.barrier()
    S.off = m0


def seg_info(seg):
    if seg == 0:
        return True, 256, 0, False, False
    b = seg - 1
    s = 256 + 128 + 512 * b
    return False, 512, s - 2, b == 0, b == 7


def load_win(S, R, hT1, seg, n):
    is_ctx, L, w0, _, _ = seg_info(seg)
    hw = R.win[n % 2]
    wl = 256 if is_ctx else 515
    S.dma("sp", hw[:, :, 0:wl], hT1[:, :, w0:w0 + wl].rearrange("kc p t -> p kc t"), hw, reads=[hT1], writes=[hw])
    return hw


def phase4b_pass1(S, C, I, mod_d, hT1, SAB):
    m0 = S.off
    R = rnn_setup(S, C, I, mod_d, pass2=False)
    sab = S.sb("sab", [RB, 2, 9, 2, RCH], F32)
    S.add("pool", lambda e: e.memset(sab[:, 0, :, :, :], 0.0), writes=[sab])
    work = []
    for seg in range(9):
        for ch in range(RCH):
            work.append((seg, ch))
    wins = {0: load_win(S, R, hT1, 0, 0)}

    def front(n):
        seg, ch = work[n]
        is_ctx, L, w0, mb, ma = seg_info(seg)
        if ch == 0 and seg + 1 < 9:
            wins[seg + 1] = load_win(S, R, hT1, seg + 1, seg + 1)
        rnn_front(S, R, wins[seg], L, ch, n, is_ctx, mb, ma)

    front(0)
    for n, (seg, ch) in enumerate(work):
        is_ctx, L, w0, mb, ma = seg_info(seg)
        if n + 1 < len(work):
            front(n + 1)
        sumr = [(sab, sab[:, 0, seg, d, ch:ch + 1]) for d in range(2)]
        rnn_back(S, C, R, L, ch, n, [0.0, 0.0], sumr, R.hs)
        cp(S, "pool", sab[:, 1, seg, 0, ch:ch + 1], R.hs[0][:, L - 1:L], reads=[R.hs[0]], writes=[sab])
        cp(S, "pool", sab[:, 1, seg, 1, ch:ch + 1], R.hs[1][:, 0:1], reads=[R.hs[1]], writes=[sab])
    for seg in range(9):
        tt(S, "dve", sab[:, 0, seg, :, :], sab[:, 0, seg, :, :], R.c1[:, :, :], ALU.mult, reads=[sab, R.c1], writes=[sab])
    act(S, sab[:, 0, :, :, :], sab[:, 0, :, :, :], AF.Exp, reads=[sab], writes=[sab])
    S.dma("sp", SAB[:, :], sab[:, :, :, :, :].rearrange("p a s d c -> p (a s d c)"), sab, reads=[sab], writes=[SAB])
    S.barrier()
    S.off = m0


def phase5_fold(S, C, I, SALL, SOWN, hin, NR=4):
    m0 = S.off
    sall = S.sb("sall", [RB, NR, 2 * 9 * 2 * RCH], F32)
    sown = S.sb("sown", [RB, 2, 9, 2, RCH], F32)
    sel = S.sb("sel", [RB, 2 * NR], F32)
    S.dma("sp", sall[:, :, :], SALL.t.rearrange("(j p) f -> p j f", p=RB), sall, reads=[SALL], writes=[sall])
    S.dma("sp", sown[:, :, :, :, :].rearrange("p a s d c -> p (a s d c)"), SOWN[:, :], sown, reads=[SOWN], writes=[sown])
    S.dma("sp", sel[:, :], I["sel"], sel, writes=[sel])
    sv = sall[:, :, :].rearrange("p j (a s d c) -> p j a s d c", a=2, s=9, d=2)
    h = S.sb("hfold", [RB, RCH], F32)
    ae = S.sb("aeff", [RB, 8, RCH], F32)
    be = S.sb("beff", [RB, 8, RCH], F32)
    for d in range(2):
        cp(S, "dve", h[:, :], sown[:, 1, 0, d, :], reads=[sown], writes=[h])
        order = range(NR) if d == 0 else range(NR - 1, -1, -1)
        for j in order:
            mj = sel[:, d * NR + j:d * NR + j + 1]
            ts(S, "dve", ae[:, :, :], sv[:, j, 0, 1:9, d, :], -1.0, None, ALU.add, None, reads=[sall], writes=[ae])
            ts(S, "dve", ae[:, :, :], ae[:, :, :], mj, None, ALU.mult, None, reads=[ae, sel], writes=[ae])
            ts(S, "dve", ae[:, :, :], ae[:, :, :], 1.0, None, ALU.add, None, reads=[ae], writes=[ae])
            ts(S, "dve", be[:, :, :], sv[:, j, 1, 1:9, d, :], mj, None, ALU.mult, None, reads=[sall, sel], writes=[be])
            border = range(8) if d == 0 else range(7, -1, -1)
            for b in border:
                tt(S, "dve", h[:, :], h[:, :], ae[:, b, :], ALU.mult, reads=[h, ae], writes=[h])
                tt(S, "dve", h[:, :], h[:, :], be[:, b, :], ALU.add, reads=[h, be], writes=[h])
        border = list(range(8)) if d == 0 else list(range(7, -1, -1))
        for n, b in enumerate(border):
            cp(S, "dve", hin[:, d, b, :], h[:, :], reads=[h], writes=[hin])
            if n < 7:
                tt(S, "dve", h[:, :], h[:, :], sown[:, 0, 1 + b, d, :], ALU.mult, reads=[h, sown], writes=[h])
                tt(S, "dve", h[:, :], h[:, :], sown[:, 1, 1 + b, d, :], ALU.add, reads=[h, sown], writes=[h])
    S.barrier()
    S.off = m0


def phase6_pass2(S, C, I, mod_d, x1, hT1, hin, x2, h2T1, comb, XS=None, maskall=None):
    R = rnn_setup(S, C, I, mod_d, pass2=True)
    mods = S.sb("mods6", [128, 16], F32)
    tmpm = S.sb("tmpm6", [128, 16], F32)
    S.dma("sp", tmpm[:, 0:8], pp_view(I["l1_norm2"]), tmpm, writes=[tmpm], allow_slow_non_contiguous=True)
    load_mod_pp(S, tmpm, 1, mod_d, 1, 0, 4)
    load_mod_pp(S, mods, 1, mod_d, 1, 0, 3)
    stt(S, "dve", mods[:, 0:8], tmpm[:, 8:16], 1.0, tmpm[:, 0:8], ALU.add, ALU.mult, reads=[tmpm], writes=[mods])
    G2 = Buf("G6", mods[:, 0:8])
    S2 = Buf("S6", mods[:, 8:16])
    g1b = S.sb("g1b6", [128, 1024], F32)
    S.dma("sp", g1b[:, :], bc_view(mod_d[1, 0, 2 * D:3 * D], D), g1b, reads=[mod_d], writes=[g1b])
    wr = S.sb("wr", [128, 8, NE], F32)
    S.dma("sp", wr[:, :, :], I["l1_router_w"].rearrange("(kc p) e -> p kc e", p=128), wr, writes=[wr])
    brb = S.sb("brb", [128, NE], F32)
    S.dma("sp", brb[:, :], bc_view(I["l1_router_b"], NE), brb, writes=[brb])
    yin = S.sb("yin", [RB, RCH, 512], BF16)
    gg = [S.sb(f"gg{i}", [RB, 512], F32) for i in range(2)]
    xt_ = [S.sb(f"x6in{i}", [128, 1024], F32) for i in range(1)] * 2
    ytmp = [S.sb(f"y6{i}", [128, 1024], F32) for i in range(2)]
    xs = [S.sb(f"xs6{i}", [128, 1024], F32) for i in range(1)] * 2
    ss = [S.sb(f"ss6{i}", [128, 1], F32) for i in range(2)]
    rstd = [S.sb(f"rstd6{i}", [128, 1], F32) for i in range(2)]
    h2 = [S.sb(f"h6{i}", [128, 8, 128], BF16) for i in range(2)]
    h32 = [S.sb(f"h32{i}", [128, 8, 128], F32) for i in range(1)] * 2
    rt = [S.sb(f"rt{i}", [128, 48], F32) for i in range(2)]
    xsb = [S.sb(f"xsb{i}", [128, 1024], BF16) for i in range(2)] if XS is not None else None
    pg = R.pb[6]
    plog = S.ps("plog", 3584 + 16, 8)
    ntile = 0
    xg = [S.sb(f"xg{i}", [RB, 512], F32) for i in range(2)]
    work = [(seg, ch) for seg in range(1, 9) for ch in range(RCH)]
    wins = {1: load_win(S, R, hT1, 1, 0)}

    def front(n):
        seg, ch = work[n]
        is_ctx, L, w0, mb, ma = seg_info(seg)
        if ch == 0 and seg + 1 < 9:
            wins[seg + 1] = load_win(S, R, hT1, seg + 1, seg)
        rnn_front(S, R, wins[seg], L, ch, n, False, mb, ma)
        hw = wins[seg]
        for kc in range(8):
            mm(S, pg, pg[0:RB, :], R.win_g[:, kc, 96 * ch:96 * ch + RB], hw[:, kc, 2:514], kc == 0, kc == 7, reads=[R.win_g, hw])
        x_ = xg[n % 2]
        cp(S, "act", x_[:, :], pg[0:RB, :], reads=[pg], writes=[x_])

    front(0)
    for n, (seg, ch) in enumerate(work):
        b = seg - 1
        if True:
            init = [(hin, hin[:, d, b, ch:ch + 1]) for d in range(2)]
            x_ = xg[n % 2]
            g_ = gg[n % 2]
            act(S, g_[:, :], x_[:, :], AF.Square, reads=[x_], writes=[g_])
            ts(S, "dve", g_[:, :], g_[:, :], 0.044715, 1.0, ALU.mult, ALU.add, reads=[g_], writes=[g_])
            tt(S, "dve", g_[:, :], g_[:, :], x_[:, :], ALU.mult, reads=[g_, x_], writes=[g_])
            if n + 1 < len(work):
                front(n + 1)

            def gsig(g_=g_):
                act(S, g_[:, :], g_[:, :], AF.Sigmoid, reads=[g_], writes=[g_], scale=1.5957691216057308)
            rnn_back(S, C, R, 512, ch, n, init, None, R.hs, extra_sig=gsig)
            tt(S, "pool", g_[:, :], g_[:, :], x_[:, :], ALU.mult, reads=[g_, x_], writes=[g_])
            tt(S, "pool", R.hs[0][:, :], R.hs[0][:, :], R.hs[1][:, :], ALU.add, reads=[R.hs[0], R.hs[1]], writes=[R.hs[0]])
            tt(S, "pool", yin[:, ch, :], g_[:, :], R.hs[0][:, :], ALU.mult, reads=[g_, R.hs[0]], writes=[yin])
        if ch != RCH - 1:
            continue
        for t in range(4):
            mt = 4 * b + t
            ti = 2 + 1 + mt
            xt = xt_[ntile % 2]
            S.dma("sp", xt[:, :], x1[128 * ti:128 * ti + 128, :], xt, reads=[x1], writes=[xt])
            yt = ytmp[ntile % 2]
            for cb in range(2):
                py = R.pb[cb]
                for ch in range(RCH):
                    mm(S, py, py[:, :], yin[:, ch, 128 * t:128 * t + 128], R.wout[:, ch, 512 * cb:512 * cb + 512],
                       ch == 0, ch == RCH - 1, reads=[yin, R.wout])
                tt(S, "dve", yt[:, 512 * cb:512 * cb + 512], py[:, :], g1b[:, 512 * cb:512 * cb + 512], ALU.mult,
                   reads=[py, g1b], writes=[yt])
            tt(S, "pool", yt[:, :], yt[:, :], xt[:, :], ALU.add, reads=[yt, xt], writes=[yt])
            S.dma("sp", x2[128 * mt:128 * mt + 128, :], yt[:, :], yt, reads=[yt], writes=[x2])
            hb = h2[ntile % 2]
            hf = h32[ntile % 2]
            norm_tile(S, C, yt, ss[ntile % 2], rstd[ntile % 2], xs[ntile % 2], [R.pb[0], R.pb[1]],
                      (hb, lambda kc, hb=hb: hb[:, kc, :]), G2, S2,
                      hT32_dst=(hf, lambda kc, hf=hf: hf[:, kc, :]))
            if XS is None:
                S.dma("sp", h2T1[:, :, 128 * mt:128 * mt + 128].rearrange("kc p t -> p kc t"), hb[:, :, :], hb, reads=[hb], writes=[h2T1])
            else:
                xb_ = xsb[ntile % 2]
                cp(S, "pool", xb_[:, :], xs[ntile % 2][:, :], reads=[xs[ntile % 2]], writes=[xb_])
                S.dma("sp", XS[128 * mt:128 * mt + 128, :], xb_[:, :], xb_, reads=[xb_], writes=[XS])
            for kc in range(8):
                mm(S, plog, plog[:, :], hf[:, kc, :], wr[:, kc, :], kc == 0, kc == 7, reads=[hf, wr])
            r_ = rt[ntile % 2]
            lg, m8, ex, em, den, nv1 = r_[:, 0:8], r_[:, 8:16], r_[:, 16:24], r_[:, 24:32], r_[:, 32:33], r_[:, 33:34]
            tt(S, "dve", lg, plog[:, :], brb[:, :], ALU.add, reads=[plog, brb], writes=[r_])
            S.add("dve", lambda e, m8=m8, lg=lg: e.max(out=m8, in_=lg), reads=[r_], writes=[r_])
            ts(S, "dve", nv1, r_[:, 8:9], -1.0, None, ALU.mult, None, reads=[r_], writes=[r_])
            act(S, ex, lg, AF.Exp, reads=[r_], writes=[r_], bias=nv1)
            ts(S, "dve", em, lg, r_[:, 9:10], None, ALU.is_ge, None, reads=[r_], writes=[r_])
            if maskall is not None:
                cp(S, "dve", maskall[:, mt, :], em, reads=[r_], writes=[maskall])
            tt(S, "dve", em, em, ex, ALU.mult, reads=[r_], writes=[r_])
            S.add("dve", lambda e, den=den, em=em: e.reduce_sum(out=den, in_=em, axis=AX.X), reads=[r_], writes=[r_])
            S.add("dve", lambda e, den=den: e.reciprocal(out=den, in_=den), reads=[r_], writes=[r_])
            ts(S, "dve", comb[:, mt, :], em, den, None, ALU.mult, None, reads=[r_], writes=[comb])
            ntile += 1
    S.barrier()


def phase7_moe(S, C, I, mod_d, x2, h2T1, comb, out_d):
    m0 = S.off
    m5b = S.sb("m5b", [128, 1024], F32)
    S.dma("sp", m5b[:, :], bc_view(mod_d[1, 0, 5 * D:6 * D], D), m5b, reads=[mod_d], writes=[m5b])
    hT = S.sb("hTg", [128, 8, 2048], BF16)
    acc = [S.sb(f"acc{i}", [128, 1024], F32) for i in range(16)]
    NWB = 2
    w1g = [S.sb(f"w1g{i}", [128, 8, 512], BF16) for i in range(NWB)]
    w3g = [S.sb(f"w3g{i}", [128, 8, 512], BF16) for i in range(NWB)]
    w2g = [S.sb(f"w2g{i}", [128, 4, 1024], BF16) for i in range(NWB)]
    hid = [S.sb(f"hidm{i}", [128, 4, 512], BF16) for i in range(2)]
    sa = [S.sb(f"sam{i}", [128, 512], F32) for i in range(2)]
    xin = [S.sb(f"x7in{i}", [128, 1024], F32) for i in range(2)]
    pA = [S.ps(f"pA7{i}", 512 * i, 512) for i in range(2)]
    pB = [S.ps(f"pB7{i}", 1024 + 512 * i, 512) for i in range(2)]
    pY = [S.ps(f"pY7{i}", 2048 + 512 * i, 512) for i in range(4)]
    nw = 0
    nf = 0
    nh = 0
    nt = 0

    def load_w(e, fg, n):
        S.dma("pool", w1g[n % NWB][:, :, :], I["l1_moe_w1"][e, :, 512 * fg:512 * fg + 512].rearrange("(kc p) n -> p kc n", p=128),
              w1g[n % NWB], writes=[w1g[n % NWB]])
        S.dma("pool", w3g[n % NWB][:, :, :], I["l1_moe_w3"][e, :, 512 * fg:512 * fg + 512].rearrange("(kc p) n -> p kc n", p=128),
              w3g[n % NWB], writes=[w3g[n % NWB]])
        S.dma("pool", w2g[n % NWB][:, :, :], I["l1_moe_w2"][e, 512 * fg:512 * fg + 512, :].rearrange("(fc p) n -> p fc n", p=128),
              w2g[n % NWB], writes=[w2g[n % NWB]])

    steps = [(G, e, fg) for G in range(2) for e in range(NE) for fg in range(7)]
    load_w(steps[0][1], steps[0][2], 0)
    for si, (G, e, fg) in enumerate(steps):
        if e == 0 and fg == 0:
            S.dma("sp", hT[:, :, :], h2T1[:, :, 2048 * G:2048 * G + 2048].rearrange("kc p t -> p kc t"), hT, reads=[h2T1], writes=[hT])
        if si + 1 < len(steps):
            load_w(steps[si + 1][1], steps[si + 1][2], si + 1)
        a1, a3, a2 = w1g[si % NWB], w3g[si % NWB], w2g[si % NWB]
        first = (e == 0 and fg == 0)
        for tb in range(4):
            hd = hid[nh % 2]
            for fc in range(4):
                pa, pb = pA[nf % 2], pB[nf % 2]
                for kc in range(8):
                    mm(S, pa, pa[:, :], a1[:, kc, 128 * fc:128 * fc + 128], hT[:, kc, 512 * tb:512 * tb + 512], kc == 0, kc == 7,
                       reads=[a1, hT])
                for kc in range(8):
                    mm(S, pb, pb[:, :], a3[:, kc, 128 * fc:128 * fc + 128], hT[:, kc, 512 * tb:512 * tb + 512], kc == 0, kc == 7,
                       reads=[a3, hT])
                sb_ = sa[nf % 2]
                act(S, sb_[:, :], pa[:, :], AF.Silu, reads=[pa], writes=[sb_])
                tt(S, "dve", hd[:, fc, :], sb_[:, :], pb[:, :], ALU.mult, reads=[sb_, pb], writes=[hd])
                nf += 1
            for t in range(4):
                tl = 4 * tb + t
                mt = 16 * G + tl
                ac = acc[tl]
                for cb in range(2):
                    py = pY[(nt % 2) * 2 + cb]
                    for fc in range(4):
                        mm(S, py, py[:, :], hd[:, fc, 128 * t:128 * t + 128], a2[:, fc, 512 * cb:512 * cb + 512], fc == 0, fc == 3,
                           reads=[hd, a2])
                    sl = slice(512 * cb, 512 * cb + 512)
                    if first:
                        ts(S, "dve", ac[:, sl], py[:, :], comb[:, mt, e:e + 1], None, ALU.mult, None, reads=[py, comb], writes=[ac])
                    else:
                        stt(S, "dve", ac[:, sl], py[:, :], comb[:, mt, e:e + 1], ac[:, sl], ALU.mult, ALU.add,
                            reads=[py, comb, ac], writes=[ac])
                nt += 1
            nh += 1
        if e == NE - 1 and fg == 6:
            for tl in range(16):
                mt = 16 * G + tl
                xt = xin[tl % 2]
                S.dma("sp", xt[:, :], x2[128 * mt:128 * mt + 128, :], xt, reads=[x2], writes=[xt])
                ac = acc[tl]
                tt(S, "pool", ac[:, :], ac[:, :], m5b[:, :], ALU.mult, reads=[ac, m5b], writes=[ac])
                tt(S, "pool", xt[:, :], xt[:, :], ac[:, :], ALU.add, reads=[xt, ac], writes=[xt])
                S.dma("sp", out_d[128 * mt:128 * mt + 128, :], xt[:, :], xt, reads=[xt], writes=[out_d])
    S.barrier()
    S.off = m0


I32 = mybir.dt.int32
GSZ = 512
NGRP = 24
GT = GSZ // 128
NTB = 1
TBW = GSZ // NTB
TPB = TBW // 128


class Route:
    pass


def phase_route(S, C, I, comb, maskall):
    Rt = Route()
    Rt.slotA_i = S.sb("slotA_i", [128, 32], I32)
    Rt.slotB_i = S.sb("slotB_i", [128, 32], I32)
    Rt.gA = S.sb("gA", [128, 32], F32)
    Rt.gB = S.sb("gB", [128, 32], F32)
    Rt.Eg_i = S.sb("Eg_i", [128, NGRP], I32)
    Rt.tokA = S.sb("tokA", [128, 32, 8], I32)
    Rt.tokB = S.sb("tokB", [128, 32, 8], I32)
    m0 = S.off
    rc = S.sb("rc", [128, 216], F32)
    S.dma("sp", rc[:, :], I["rconst"], rc, writes=[rc])
    Lb = S.sb("Lb", [128, 128], BF16)
    ob = S.sb("ob", [128, 128], BF16)
    mb = S.sb("mb", [128, 256], BF16)
    cp(S, "dve", Lb[:, :], rc[:, 0:128], reads=[rc], writes=[Lb])
    S.add("pool", lambda e: e.memset(ob[:, :], 1.0), writes=[ob])
    cp(S, "dve", mb[:, :], maskall[:, :, :].rearrange("p t e -> p (t e)"), reads=[maskall], writes=[mb])
    p_r = S.ps("p_rin", 0, 256)
    p_c = S.ps("p_cnt", 512, 256)
    mm(S, p_r, p_r[:, :], Lb[:, :], mb[:, :], True, True, reads=[Lb, mb])
    mm(S, p_c, p_c[:, :], ob[:, :], mb[:, :], True, True, reads=[ob, mb])
    rin = S.sb("rin", [128, 32, NE], F32)
    cnt = S.sb("cnt", [128, 32, NE], F32)
    cp(S, "dve", rin[:, :, :].rearrange("p t e -> p (t e)"), p_r[:, :], reads=[p_r], writes=[rin])
    cp(S, "dve", cnt[:, :, :].rearrange("p t e -> p (t e)"), p_c[:, :], reads=[p_c], writes=[cnt])
    ones32 = S.sb("ones32", [128, 32], F32)
    S.add("pool", lambda e: e.memset(ones32[:, :], 1.0), writes=[ones32])
    inc = S.sb("inc", [128, NE, 32], F32)
    for e_ in range(NE):
        S.add("dve", lambda e, e_=e_: e.tensor_tensor_scan(out=inc[:, e_, :], data0=ones32[:, :], data1=cnt[:, :, e_],
                                                           initial=0.0, op0=ALU.mult, op1=ALU.add),
              reads=[ones32, cnt], writes=[inc])
    pre = S.sb("pre", [128, NE, 32], F32)
    tt(S, "dve", pre[:, :, :], inc[:, :, :], cnt[:, :, :].rearrange("p t e -> p e t"), ALU.subtract, reads=[inc, cnt], writes=[pre])
    sm = S.sb("route_sm", [128, 64], F32)
    n_e, G_, gend, gstart, sbase, tmp8 = (sm[:, 0:8], sm[:, 8:16], sm[:, 16:24], sm[:, 24:32], sm[:, 32:40], sm[:, 40:48])
    cp(S, "dve", n_e, inc[:, :, 31], reads=[inc], writes=[sm])
    ts(S, "dve", G_, n_e, 0.0, None, ALU.is_gt, None, reads=[sm], writes=[sm])
    for k in range(1, (4096 + GSZ - 1) // GSZ):
        ts(S, "dve", tmp8, n_e, float(GSZ * k), None, ALU.is_gt, None, reads=[sm], writes=[sm])
        tt(S, "dve", G_, G_, tmp8, ALU.add, reads=[sm], writes=[sm])
    S.add("dve", lambda e: e.tensor_tensor_scan(out=gend, data0=ones32[:, 0:8], data1=G_, initial=0.0, op0=ALU.mult, op1=ALU.add),
          reads=[sm, ones32], writes=[sm])
    tt(S, "dve", gstart, gend, G_, ALU.subtract, reads=[sm], writes=[sm])
    ts(S, "dve", sbase, gstart, float(GSZ), None, ALU.mult, None, reads=[sm], writes=[sm])
    v = S.sb("route_v", [128, 32, NE], F32)
    tt(S, "dve", v[:, :, :], rin[:, :, :], pre[:, :, :].rearrange("p e t -> p t e"), ALU.add, reads=[rin, pre], writes=[v])
    sb_b = bass.AP(sbase.tensor, sbase.offset, [list(sbase.ap[0]), [0, 32], [1, NE]])
    tt(S, "dve", v[:, :, :], v[:, :, :], sb_b, ALU.add, reads=[v, sm], writes=[v])
    stt(S, "dve", v[:, :, :], v[:, :, :], 1.0, maskall[:, :, :], ALU.add, ALU.mult, reads=[v, maskall], writes=[v])
    m8 = S.sb("route_m8", [128, 32, NE], F32)
    for t_ in range(32):
        S.add("dve", lambda e, t_=t_: e.max(out=m8[:, t_, :], in_=v[:, t_, :]), reads=[v], writes=[m8])
    sf = S.sb("route_sf", [128, 2, 32], F32)
    oh = S.sb("route_oh", [128, 32, NE], F32)
    for which, (sl_i, g_) in enumerate(((Rt.slotA_i, Rt.gA), (Rt.slotB_i, Rt.gB))):
        top = m8[:, :, which]
        ts(S, "dve", sf[:, which, :], top, -1.0, None, ALU.add, None, reads=[m8], writes=[sf])
        cp(S, "dve", sl_i[:, :], sf[:, which, :], reads=[sf], writes=[sl_i])
        top_b = bass.AP(top.tensor, top.offset, [list(top.ap[0]), list(top.ap[1]), [0, NE]])
        tt(S, "dve", oh[:, :, :], v[:, :, :], top_b, ALU.is_equal, reads=[v, m8], writes=[oh])
        tt(S, "dve", oh[:, :, :], oh[:, :, :], comb[:, :, :], ALU.mult, reads=[oh, comb], writes=[oh])
        S.add("dve", lambda e, g_=g_: e.reduce_sum(out=g_[:, :], in_=oh[:, :, :], axis=AX.X), reads=[oh], writes=[g_])
    Eg = S.sb("Eg_f", [128, NGRP], F32)
    ind = S.sb("route_ind", [128, 2, NGRP], F32)
    gio = rc[:, 184:184 + NGRP]
    S.add("pool", lambda e: e.memset(Eg[:, :], 0.0), writes=[Eg])
    for e_ in range(1, NE):
        ts(S, "dve", ind[:, 0, :], gio, gstart[:, e_:e_ + 1], None, ALU.is_ge, None, reads=[rc, sm], writes=[ind])
        ts(S, "dve", ind[:, 1, :], gio, gend[:, e_:e_ + 1], None, ALU.is_lt, None, reads=[rc, sm], writes=[ind])
        tt(S, "dve", ind[:, 0, :], ind[:, 0, :], ind[:, 1, :], ALU.mult, reads=[ind], writes=[ind])
        stt(S, "dve", Eg[:, :], ind[:, 0, :], float(e_), Eg[:, :], ALU.mult, ALU.add, reads=[ind, Eg], writes=[Eg])
    cp(S, "dve", Rt.Eg_i[:, :], Eg[:, :], reads=[Eg], writes=[Rt.Eg_i])
    tk = rc[:, 152:184]
    tk_b = bass.AP(tk.tensor, tk.offset, [list(tk.ap[0]), list(tk.ap[1]), [0, 8]])
    tkf = S.sb("tkf", [128, 32, 8], F32)
    cp(S, "dve", tkf[:, :, :], tk_b, reads=[rc], writes=[tkf])
    cp(S, "dve", Rt.tokA[:, :, :], tkf[:, :, :], reads=[tkf], writes=[Rt.tokA])
    ts(S, "dve", tkf[:, :, :], tkf[:, :, :], 4096.0, None, ALU.add, None, reads=[tkf], writes=[tkf])
    cp(S, "dve", Rt.tokB[:, :, :], tkf[:, :, :], reads=[tkf], writes=[Rt.tokB])
    S.barrier()
    S.off = m0
    return Rt


def phase_permute(S, C, I, Rt, XS, Hslot, Tslot):
    m0 = S.off
    zer = S.sb("zer", [128, GT * 1024], BF16)
    S.add("pool", lambda e: e.memset(zer[:, :], 0.0), writes=[zer])
    for i in range(NGRP):
        S.dma("sp", Hslot[GSZ * i:GSZ * i + GSZ, :].rearrange("(a p) n -> p a n", p=128),
              zer[:, :].rearrange("p (a n) -> p a n", a=GT), zer, reads=[zer], writes=[Hslot])
    dump = S.sb("dumpi", [128, 1024], I32)
    S.add("pool", lambda e: e.memset(dump[:, :], 8192), writes=[dump])
    S.dma("sp", Tslot.t.rearrange("(p a) o -> p (a o)", p=128), dump[:, 0:NGRP * GSZ // 128 * 8], dump, reads=[dump], writes=[Tslot])
    for mt in range(32):
        for sl, tk_ in ((Rt.slotA_i, Rt.tokA), (Rt.slotB_i, Rt.tokB)):
            S.add("pool", lambda e, sl=sl, tk_=tk_, mt=mt: e.indirect_dma_start(
                out=Tslot[:, :], out_offset=bass.IndirectOffsetOnAxis(ap=sl[:, mt:mt + 1], axis=0),
                in_=tk_[:, mt, :], in_offset=None, bounds_check=None),
                reads=[tk_, sl, Tslot], writes=[Tslot], dma_buf=tk_)
    xt = [S.sb(f"xperm{i}", [128, 1024], BF16) for i in range(3)]
    for mt in range(32):
        x_ = xt[mt % 3]
        S.dma("sp", x_[:, :], XS[128 * mt:128 * mt + 128, :], x_, reads=[XS], writes=[x_])
        for sl in (Rt.slotA_i, Rt.slotB_i):
            S.add("pool", lambda e, x_=x_, sl=sl, mt=mt: e.indirect_dma_start(
                out=Hslot[:, :], out_offset=bass.IndirectOffsetOnAxis(ap=sl[:, mt:mt + 1], axis=0),
                in_=x_[:, :], in_offset=None, bounds_check=None),
                reads=[x_, sl, Hslot], writes=[Hslot], dma_buf=x_)
    S.barrier()
    S.off = m0


def phase7s_moe(S, C, I, mod_d, Rt, Hslot, Tslot, Yab):
    m0 = S.off
    mods = S.sb("mods7", [128, 16], F32)
    tmpm = S.sb("tmpm7", [128, 16], F32)
    S.dma("sp", tmpm[:, 0:8], pp_view(I["l1_norm2"]), tmpm, writes=[tmpm], allow_slow_non_contiguous=True)
    load_mod_pp(S, tmpm, 1, mod_d, 1, 0, 4)
    load_mod_pp(S, mods, 1, mod_d, 1, 0, 3)
    stt(S, "dve", mods[:, 0:8], tmpm[:, 8:16], 1.0, tmpm[:, 0:8], ALU.add, ALU.mult, reads=[tmpm], writes=[mods])
    hT = [S.sb(f"hTs{i}", [128, 8, GSZ], BF16) for i in range(2)]
    acc = [S.sb(f"accs{i}", [128, 1024], F32) for i in range(GT)]
    NWB = 4
    w1g = [S.sb(f"w1s{i}", [128, 8, 512], BF16) for i in range(NWB)]
    w3g = [S.sb(f"w3s{i}", [128, 8, 512], BF16) for i in range(NWB)]
    w2g = [S.sb(f"w2s{i}", [128, 4, 1024], BF16) for i in range(NWB)]
    hid = [S.sb(f"hids{i}", [128, 4, 512], BF16) for i in range(2)]
    sa = [S.sb(f"sas{i}", [128, 512], F32) for i in range(2)]
    st_ = [S.sb(f"slt{i}", [128, 1024], BF16) for i in range(4)]
    tix = [S.sb(f"tix{i}", [128, 8], I32) for i in range(4)]
    pA = [S.ps(f"pA8{i}", 512 * i, 512) for i in range(2)]
    pB = [S.ps(f"pB8{i}", 1024 + 512 * i, 512) for i in range(2)]
    pY = [S.ps(f"pY8{i}", 2048 + 512 * i, 512) for i in range(4)]
    w1t, w3t, w2t = I["l1_moe_w1"], I["l1_moe_w3"], I["l1_moe_w2"]

    nreg = [0]

    def dyn_load(dst, static_ap, estride, g):
        off0 = static_ap.offset
        pat = [list(x) for x in static_ap.ap]
        tens = static_ap.tensor

        nreg[0] += 1
        rname = f"er{nreg[0]}"

        def allregs(e):
            hs = []
            try:
                while True:
                    nreg[0] += 1
                    hs.append(e.alloc_register(f"gc{nreg[0]}"))
            except ValueError:
                pass
            for h in hs:
                e.free_register(h)
            return hs

        def fn(e):
            before = allregs(e)
            with e.register(rname) as er:
                e.reg_load(er, Rt.Eg_i[0:1, g:g + 1])
                e.reg_mul(er, er, estride)
                e.reg_add(er, er, off0)
                ins = e.dma_start(out=dst[:, :, :], in_=bass.AP(tens, er, pat))
            after = {h.regnum for h in allregs(e)}
            for h in before:
                if h.regnum not in after:
                    e.free_register(h)
            return ins
        S.add("pool", fn, reads=[Rt.Eg_i], writes=[dst], dma_buf=dst)

    def load_w(g, fg, n):
        dyn_load(w1g[n % NWB], w1t[0, :, 512 * fg:512 * fg + 512].rearrange("(kc p) n -> p kc n", p=128), D * DFE, g)
        dyn_load(w3g[n % NWB], w3t[0, :, 512 * fg:512 * fg + 512].rearrange("(kc p) n -> p kc n", p=128), D * DFE, g)
        dyn_load(w2g[n % NWB], w2t[0, 512 * fg:512 * fg + 512, :].rearrange("(fc p) n -> p fc n", p=128), DFE * D, g)

    nst = [0]
    ntr = [0]

    def prologue(g):
        hb = hT[g % 2]
        for t in range(GT):
            x_ = st_[nst[0] % 4]
            nst[0] += 1
            S.dma("sp", x_[:, :], Hslot[GSZ * g + 128 * t:GSZ * g + 128 * t + 128, :], x_, reads=[Hslot], writes=[x_])
            p = pA[ntr[0] % 2]
            ntr[0] += 1
            pv = p.t.bitcast(BF16)
            for kc in range(8):
                tr(S, p, pv[:, 128 * kc:128 * kc + 128], x_[:, 128 * kc:128 * kc + 128], C.identb[:, :], reads=[x_, C.identb])
            for kc in range(8):
                src = pv[:, 128 * kc:128 * kc + 128]
                if kc % 2 == 0:
                    ts(S, "dve", hb[:, kc, 128 * t:128 * t + 128], src, mods[:, kc:kc + 1], mods[:, 8 + kc:9 + kc], ALU.mult, ALU.add,
                       reads=[p, mods], writes=[hb])
                else:
                    act(S, hb[:, kc, 128 * t:128 * t + 128], src, AF.Identity, reads=[p, mods], writes=[hb],
                        bias=mods[:, 8 + kc:9 + kc], scale=mods[:, kc:kc + 1])

    steps = [(g, fg) for g in range(NGRP) for fg in range(7)]
    load_w(0, 0, 0)
    load_w(0, 1, 1)
    load_w(0, 2, 2)
    prologue(0)
    nf = 0
    nh = 0
    nt = 0
    for si, (g, fg) in enumerate(steps):
        if si + 3 < len(steps):
            load_w(steps[si + 3][0], steps[si + 3][1], si + 3)
        if fg == 6 and g + 1 < NGRP:
            prologue(g + 1)
        hb = hT[g % 2]
        a1, a3, a2 = w1g[si % NWB], w3g[si % NWB], w2g[si % NWB]
        for tb in range(NTB):
            hd = hid[nh % 2]
            for fc in range(4):
                pa, pb = pA[nf % 2], pB[nf % 2]
                for kc in range(8):
                    mm(S, pa, pa[:, 0:TBW], a1[:, kc, 128 * fc:128 * fc + 128], hb[:, kc, TBW * tb:TBW * tb + TBW], kc == 0, kc == 7,
                       reads=[a1, hb])
                for kc in range(8):
                    mm(S, pb, pb[:, 0:TBW], a3[:, kc, 128 * fc:128 * fc + 128], hb[:, kc, TBW * tb:TBW * tb + TBW], kc == 0, kc == 7,
                       reads=[a3, hb])
                sb_ = sa[nf % 2]
                act(S, sb_[:, 0:TBW], pa[:, 0:TBW], AF.Silu, reads=[pa], writes=[sb_])
                tt(S, "dve", hd[:, fc, 0:TBW], sb_[:, 0:TBW], pb[:, 0:TBW], ALU.mult, reads=[sb_, pb], writes=[hd])
                nf += 1
            for t in range(TPB):
                tl = TPB * tb + t
                ac = acc[tl]
                for cb in range(2):
                    py = pY[(nt % 2) * 2 + cb]
                    for fc in range(4):
                        mm(S, py, py[:, :], hd[:, fc, 128 * t:128 * t + 128], a2[:, fc, 512 * cb:512 * cb + 512], fc == 0, fc == 3,
                           reads=[hd, a2])
                    sl = slice(512 * cb, 512 * cb + 512)
                    if fg == 0:
                        cp(S, "dve", ac[:, sl], py[:, :], reads=[py], writes=[ac])
                    else:
                        tt(S, "dve", ac[:, sl], py[:, :], ac[:, sl], ALU.add, reads=[py, ac], writes=[ac])
                if fg == 6:
                    tx = tix[(GT * g + tl) % 4]
                    S.dma("sp", tx[:, :], Tslot[GSZ * g + 128 * tl:GSZ * g + 128 * tl + 128, :], tx, reads=[Tslot], writes=[tx])
                    S.add("pool", lambda e, ac=ac, tx=tx: e.indirect_dma_start(
                        out=Yab[:, :], out_offset=bass.IndirectOffsetOnAxis(ap=tx[:, 0:1], axis=0),
                        in_=ac[:, :], in_offset=None, bounds_check=None),
                        reads=[ac, tx, Yab], writes=[Yab], dma_buf=ac)
                nt += 1
            nh += 1
    S.barrier()
    S.off = m0


def phase8_combine(S, C, I, mod_d, Rt, Yab, x2, out_d):
    m0 = S.off
    m5b = S.sb("m5bs", [128, 1024], F32)
    S.dma("sp", m5b[:, :], bc_view(mod_d[1, 0, 5 * D:6 * D], D), m5b, reads=[mod_d], writes=[m5b])
    NB8 = 3
    ya = [S.sb(f"ya{i}", [128, 1024], F32) for i in range(NB8)]
    yb = [S.sb(f"yb{i}", [128, 1024], F32) for i in range(NB8)]
    xi = [S.sb(f"xc8{i}", [128, 1024], F32) for i in range(NB8)]

    def loads(mt):
        a_, b_, x_ = ya[mt % NB8], yb[mt % NB8], xi[mt % NB8]
        S.dma("sp", x_[:, :], x2[128 * mt:128 * mt + 128, :], x_, reads=[x2], writes=[x_])
        S.dma("sp", a_[:, :], Yab[128 * mt:128 * mt + 128, :], a_, reads=[Yab], writes=[a_])
        S.dma("sp", b_[:, :], Yab[4096 + 128 * mt:4096 + 128 * mt + 128, :], b_, reads=[Yab], writes=[b_])

    loads(0)
    for mt in range(32):
        if mt + 1 < 32:
            loads(mt + 1)
        a_, b_, x_ = ya[mt % NB8], yb[mt % NB8], xi[mt % NB8]
        ts(S, "dve", a_[:, :], a_[:, :], Rt.gA[:, mt:mt + 1], None, ALU.mult, None, reads=[a_, Rt.gA], writes=[a_])
        stt(S, "dve", a_[:, :], b_[:, :], Rt.gB[:, mt:mt + 1], a_[:, :], ALU.mult, ALU.add, reads=[b_, Rt.gB, a_], writes=[a_])
        tt(S, "dve", a_[:, :], a_[:, :], m5b[:, :], ALU.mult, reads=[a_, m5b], writes=[a_])
        tt(S, "pool", x_[:, :], x_[:, :], a_[:, :], ALU.add, reads=[x_, a_], writes=[x_])
        S.dma("sp", out_d[128 * mt:128 * mt + 128, :], x_[:, :], x_, reads=[x_], writes=[out_d])
    S.barrier()
    S.off = m0


def declare_inputs(nc, names_shapes):
    I = {}
    for name, shape in names_shapes:
        I[name] = nc.dram_tensor(name, list(shape), F32, kind="ExternalInput").ap()
    return I


A_INPUTS = [
    ("xk", (NCH * 128, D)), ("cv", (2, D)), ("bt", (5, 128, 16, 5, 128)),
    ("l0_w_mod", (D, 6 * D)), ("l0_b_mod", (6 * D,)), ("l0_norm1", (D,)), ("l0_norm2", (D,)),
    ("l0_w_qkv", (D, 3 * D)), ("l0_q_gain", (HD,)), ("l0_k_gain", (HD,)), ("l0_w_o", (D, D)),
    ("l0_ffn_w1", (D, DFF)), ("l0_ffn_w3", (D, DFF)), ("l0_ffn_w2", (DFF, D)),
    ("l1_w_mod", (D, 6 * D)), ("l1_b_mod", (6 * D,)),
]


A_INPUTS2 = [
    ("l1_norm1", (D,)), ("l1_w_in", (D, 2 * DRNN)), ("l1_conv_w", (4, DRNN)), ("l1_conv_b", (DRNN,)),
    ("l1_gate_a_w", (2, RCH, RB, RB)), ("l1_gate_a_b", (2, DRNN)), ("l1_gate_x_w", (2, RCH, RB, RB)),
    ("l1_gate_x_b", (2, DRNN)), ("l1_lam", (2, DRNN)), ("masks", (128, 2)),
]
B_INPUTS = A_INPUTS2 + [
    ("x1", (NT1 * 128, D)), ("mod_in", (2, 2, 6 * D)), ("sall", (4 * RB, 576)), ("sown", (RB, 576)),
    ("sel", (RB, 8)), ("l1_norm2", (D,)), ("l1_w_out", (DRNN, D)), ("l1_router_w", (D, NE)), ("l1_router_b", (NE,)),
    ("l1_moe_w1", (NE, D, DFE)), ("l1_moe_w3", (NE, D, DFE)), ("l1_moe_w2", (NE, DFE, D)),
]


def build_A(debug=False):
    nc = bass.Bass("TRN2", target_bir_lowering=False)
    I = declare_inputs(nc, A_INPUTS + A_INPUTS2)
    S = Sched(nc)
    C = Common(S)
    mod_d = S.dram("mod_d", [2, 2, 6 * D], F32, kind="ExternalOutput")
    QT = S.dram("QT", [8, 128, NCH * 128], BF16)
    KT = S.dram("KT", [8, 128, NCH * 128], BF16)
    V = S.dram("V", [NCH * 128, D], BF16)
    x1a = S.dram("x1a", [NT1 * 128, D], F32, kind="ExternalOutput" if debug else "Internal")
    h2T = S.dram("h2T", [8, 128, NT1 * 128], BF16)
    x1 = S.dram("x1", [NT1 * 128, D], F32, kind="ExternalOutput")
    hT1 = S.dram("hT1", [8, 128, NT1 * 128], BF16)
    SAB = S.dram("sab_out", [RB, 576], F32, kind="ExternalOutput")
    phase0_adaln(S, C, I, mod_d)
    phase1_qkv(S, C, I, mod_d, QT, KT, V)
    phase2_attn(S, C, I, mod_d, QT, KT, V, x1a, h2T)
    phase3_ffn(S, C, I, mod_d, x1a, h2T, x1)
    phase4a_h1(S, C, I, mod_d, x1, hT1)
    phase4b_pass1(S, C, I, mod_d, hT1, SAB)
    S.emit()
    return nc


def build_B(debug=False):
    nc = bass.Bass("TRN2", target_bir_lowering=False)
    I = declare_inputs(nc, B_INPUTS)
    S = Sched(nc)
    C = Common(S)
    mod_d = Buf("mod_in", I["mod_in"])
    x1 = Buf("x1", I["x1"])
    SALL = Buf("sall", I["sall"])
    SOWN = Buf("sown", I["sown"])
    hT1 = S.dram("hT1", [8, 128, NT1 * 128], BF16)
    x2 = S.dram("x2", [32 * 128, D], F32, kind="ExternalOutput" if debug else "Internal")
    h2T1 = S.dram("h2T1", [8, 128, 32 * 128], BF16)
    out_d = S.dram("out", [32 * 128, D], F32, kind="ExternalOutput")
    hin = S.sb("hin", [RB, 2, 8, RCH], F32)
    comb = S.sb("comb", [128, 32, NE], F32)
    phase4a_h1(S, C, I, mod_d, x1, hT1)
    phase5_fold(S, C, I, SALL, SOWN, hin)
    m = S.off
    phase6_pass2(S, C, I, mod_d, x1, hT1, hin, x2, h2T1, comb)
    S.off = m
    phase7_moe(S, C, I, mod_d, x2, h2T1, comb, out_d)
    S.emit()
    return nc


F_INPUTS = A_INPUTS + A_INPUTS2 + [
    ("sel", (RB, 8)), ("l1_norm2", (D,)), ("l1_w_out", (DRNN, D)), ("l1_router_w", (D, NE)), ("l1_router_b", (NE,)),
    ("l1_moe_w1", (NE, D, DFE)), ("l1_moe_w3", (NE, D, DFE)), ("l1_moe_w2", (NE, DFE, D)), ("rconst", (128, 216)),
]


SPARSE = True


def build_fused():
    nc = bass.Bass("TRN2", target_bir_lowering=False)
    I = declare_inputs(nc, F_INPUTS)
    S = Sched(nc)
    C = Common(S)
    mod_d = S.dram("mod_d", [2, 2, 6 * D], F32)
    QT = S.dram("QT", [8, 128, NCH * 128], BF16)
    KT = S.dram("KT", [8, 128, NCH * 128], BF16)
    V = S.dram("V", [NCH * 128, D], BF16)
    x1a = S.dram("x1a", [NT1 * 128, D], F32)
    h2T = S.dram("h2T", [8, 128, NT1 * 128], BF16)
    x1 = S.dram("x1", [NT1 * 128, D], F32)
    hT1 = S.dram("hT1", [8, 128, NT1 * 128], BF16)
    SAB = S.dram("sab_b", [RB, 576], F32)
    SALL = S.dram("sall_g", [4 * RB, 576], F32)
    x2 = S.dram("x2", [32 * 128, D], F32)
    h2T1 = S.dram("h2T1", [8, 128, 32 * 128], BF16)
    out_d = S.dram("out", [32 * 128, D], F32, kind="ExternalOutput")
    phase0_adaln(S, C, I, mod_d)
    phase1_qkv(S, C, I, mod_d, QT, KT, V)
    phase2_attn(S, C, I, mod_d, QT, KT, V, x1a, h2T)
    phase3_ffn(S, C, I, mod_d, x1a, h2T, x1)
    phase4a_h1(S, C, I, mod_d, x1, hT1)
    phase4b_pass1(S, C, I, mod_d, hT1, SAB)
    cc = Buf("cc")
    S.add("pool", lambda e: e.collective_compute("AllGather", ALU.bypass, replica_groups=[[0, 1, 2, 3], [4, 5, 6, 7]],
                                                  ins=[SAB.t.opt()], outs=[SALL.t.opt()]),
          reads=[SAB], writes=[SALL], dma_buf=cc, inc=1)
    hin = S.sb("hin", [RB, 2, 8, RCH], F32)
    comb = S.sb("comb", [128, 32, NE], F32)
    phase5_fold(S, C, I, SALL, SAB, hin)
    if not SPARSE:
        m = S.off
        phase6_pass2(S, C, I, mod_d, x1, hT1, hin, x2, h2T1, comb)
        S.off = m
        phase7_moe(S, C, I, mod_d, x2, h2T1, comb, out_d)
    else:
        maskall = S.sb("maskall", [128, 32, NE], F32)
        XS = S.dram("XS", [32 * 128, D], BF16)
        Hslot = S.dram("Hslot", [NGRP * GSZ, D], BF16)
        Tslot = S.dram("Tslot", [NGRP * GSZ, 8], I32)
        Yab = S.dram("Yab", [8192 + 128, D], F32)
        m = S.off
        phase6_pass2(S, C, I, mod_d, x1, hT1, hin, x2, h2T1, comb, XS=XS, maskall=maskall)
        S.off = m
        Rt = phase_route(S, C, I, comb, maskall)
        phase_permute(S, C, I, Rt, XS, Hslot, Tslot)
        phase7s_moe(S, C, I, mod_d, Rt, Hslot, Tslot, Yab)
        phase8_combine(S, C, I, mod_d, Rt, Yab, x2, out_d)
    S.emit()
    return nc


def make_bias_tables(rpb, k):
    T0 = 32 * k
    out = np.empty((5, 128, 16, 5, 128), np.float32)
    p = np.arange(128)

    def table(gt, kts):
        qr = 2 * gt + p // 64
        qc = p % 64
        rs_ = np.clip(qr - 4, 0, 248)
        cs_ = np.clip(qc - 8, 0, 48)
        tab = np.full((128, 16, 5, 128), NEG, np.float32)
        for j, kt in enumerate(kts):
            if kt < 0 or kt > 127:
                continue
            kr = (2 * kt + p // 64)[:, None]
            kcol = (p % 64)[:, None]
            inwin = (kr >= rs_[None]) & (kr < rs_[None] + 8) & (kcol >= cs_[None]) & (kcol < cs_[None] + 16)
            dr = np.clip(kr - qr[None] + 7, 0, 14)
            dc = np.clip(kcol - qc[None] + 15, 0, 30)
            vals = rpb[:, dr, dc]
            tab[:, :, j, :] = np.where(inwin[:, None, :], vals.transpose(1, 0, 2), NEG)
        return tab

    def kts_for(lt):
        gt = T0 - 1 + lt
        kts = [gt - 2 + j for j in range(5)]
        if gt == 0:
            kts[0] = 3
        if gt == 127:
            kts[4] = 124
        return gt, kts

    out[0] = table(10, [8, 9, 10, 11, 12])
    for i, lt in enumerate((1, 2, 31, 32)):
        gt, kts = kts_for(lt)
        if gt < 0 or gt > 127:
            out[1 + i] = out[0]
        else:
            out[1 + i] = table(gt, kts)
    return out


def make_xk(x, ctx, b, k):
    T0 = 32 * k
    xk = np.zeros((NCH * 128, D), np.float32)
    xk[0:256] = ctx[b]
    for j in range(NKC):
        gt = T0 - 3 + j
        if k == 0 and j == 1:
            gt = 3
        if k == 3 and j == 36:
            gt = 124
        if 0 <= gt < 128:
            xk[256 + 128 * j:256 + 128 * j + 128] = x[b, 128 * gt:128 * gt + 128]
    return xk


_CACHE = {}


def _make_rconst():
    rc = np.zeros((128, 216), np.float32)
    p = np.arange(128)
    rc[:, 0:128] = (p[:, None] < p[None, :]).astype(np.float32)
    rc[:, 128:144] = np.arange(16, dtype=np.float32)[None, :]
    rc[:, 144:152] = np.arange(8, dtype=np.float32)[None, :]
    rc[:, 152:184] = (np.arange(32)[None, :] * 128 + p[:, None]).astype(np.float32)
    rc[:, 184:216] = np.arange(32, dtype=np.float32)[None, :]
    return rc


RCONST = _make_rconst()


def kernel(**inputs):
    inp = {k: np.ascontiguousarray(np.asarray(v, dtype=np.float32)) for k, v in inputs.items()}
    if "F" not in _CACHE:
        _CACHE["F"] = build_fused()
    nc = _CACHE["F"]
    n = 8
    maps = []
    for i in range(n):
        b, k = i // 4, i % 4
        sel = np.zeros((RB, 8), np.float32)
        for j in range(4):
            if j < k:
                sel[:, j] = 1.0
            if j > k:
                sel[:, 4 + j] = 1.0
        m = {"xk": make_xk(inp["x"], inp["ctx"], b, k),
             "cv": np.stack([inp["c"][b], inp["c_ctx"]]).astype(np.float32),
             "bt": make_bias_tables(inp["l0_rpb"], k),
             "masks": np.tile(np.array([[0.0 if k == 0 else 1.0, 0.0 if k == 3 else 1.0]], np.float32), (128, 1)),
             "sel": sel, "rconst": RCONST}
        for name, _ in F_INPUTS:
            if name not in m:
                m[name] = inp[name]
        maps.append(m)
    res = run_bass_kernel_spmd(nc, maps, core_ids=list(range(n)))
    out = np.empty((2, 16384, D), np.float32)
    for i in range(n):
        b, k = i // 4, i % 4
        out[b, 4096 * k:4096 * k + 4096] = np.asarray(res.results[i]["out"])
    return out
```
